# Optimizing a Trainium2 kernel written in Bass

```python
import math
import jax, jax.numpy as jnp
from jax import lax
import numpy as np

D_MODEL = 2048
BATCH = 4
SEQ = 4096
DEPTH = 4

D_MIX = D_MODEL
W_GROUP = D_MIX // 4
ROPE_THETA = 500000.0
NORM_EPS = 1e-6
Q_BLOCK = 128

MLA_HEADS = 4
MLA_NOPE = 128
MLA_ROPE = 64
MLA_V = W_GROUP // MLA_HEADS
MLA_Q_LORA = 512
MLA_KV_LORA = 256

S5_GROUP = 16
S5_GROUPS = W_GROUP // S5_GROUP
S5_STATE = 64
S5_DT_MIN = 0.001
S5_DT_MAX = 0.1

RWKV_HEAD = 64
RWKV_HEADS = W_GROUP // RWKV_HEAD
RWKV_DECAY_LORA = 64
RWKV_ICL_LORA = 64
RWKV_GN_EPS = 64e-5

DIFF_HEADS = 4
DIFF_QK = W_GROUP // (2 * DIFF_HEADS)
DIFF_V = 2 * DIFF_QK
DIFF_ROT = DIFF_QK // 4
DIFF_SUBLN_EPS = 1e-5

N_RWKV = 3 * W_GROUP + RWKV_DECAY_LORA + RWKV_ICL_LORA
PROJ_SIZES = (MLA_Q_LORA, MLA_KV_LORA, MLA_ROPE,
              W_GROUP,
              N_RWKV,
              W_GROUP, W_GROUP, W_GROUP,
              D_MIX)
RWKV_SIZES = (W_GROUP, W_GROUP, W_GROUP, RWKV_DECAY_LORA, RWKV_ICL_LORA)
N_IN = sum(PROJ_SIZES)

kernel_name = 'hymba_style_mla_s5_rwkv7_diffattn'


def _split_points(sizes):
    return [int(v) for v in np.cumsum(sizes)[:-1]]


def rms_norm(x, g, eps=NORM_EPS):
    xf = x.astype(jnp.float32)
    y = xf * lax.rsqrt(jnp.mean(xf * xf, axis=-1, keepdims=True) + eps)
    return (y * g.astype(jnp.float32)).astype(x.dtype)


def rope_tables(positions, rot):
    inv = ROPE_THETA ** (-jnp.arange(0, rot, 2, dtype=jnp.float32) / rot)
    ang = positions.astype(jnp.float32)[..., None] * inv
    return jnp.cos(ang)[:, :, None, :], jnp.sin(ang)[:, :, None, :]


def apply_rope(x, cos, sin):
    rot = 2 * cos.shape[-1]
    xr = x[..., :rot].astype(jnp.float32)
    x1, x2 = xr[..., :rot // 2], xr[..., rot // 2:]
    y = jnp.concatenate([x1 * cos - x2 * sin, x2 * cos + x1 * sin], axis=-1).astype(x.dtype)
    return jnp.concatenate([y, x[..., rot:]], axis=-1)


def _to_blocks(t):
    b, s = t.shape[:2]
    t = t.reshape((b, s // Q_BLOCK, Q_BLOCK) + t.shape[2:])
    return jnp.moveaxis(t, 1, 0)


def _from_blocks(t):
    t = jnp.moveaxis(t, 0, 1)
    return t.reshape((t.shape[0], -1) + t.shape[3:])


def _causal_mask(blk, seq):
    qpos = blk * Q_BLOCK + jnp.arange(Q_BLOCK)
    return qpos[:, None] >= jnp.arange(seq)[None, :]


def causal_attention(q, k, v, scale):
    seq = k.shape[1]
    nb = q.shape[1] // Q_BLOCK

    def one_block(args):
        qb, blk = args
        s = jnp.einsum('bqhd,bkhd->bhqk', qb, k, preferred_element_type=jnp.float32) * scale
        s = jnp.where(_causal_mask(blk, seq), s, -jnp.inf)
        p = jax.nn.softmax(s, axis=-1)
        return jnp.einsum('bhqk,bkhd->bqhd', p.astype(v.dtype), v)

    return _from_blocks(lax.map(one_block, (_to_blocks(q), jnp.arange(nb))))


def causal_diff_attention(q, k, v, lam, scale):
    seq = k.shape[1]
    nb = q.shape[1] // Q_BLOCK

    def one_block(args):
        qb, blk = args
        s = jnp.einsum('bqhmd,bkhmd->bhmqk', qb, k, preferred_element_type=jnp.float32) * scale
        s = jnp.where(_causal_mask(blk, seq), s, -jnp.inf)
        p = jax.nn.softmax(s, axis=-1)
        pd = p[:, :, 0] - lam * p[:, :, 1]
        return jnp.einsum('bhqk,bkhd->bqhd', pd.astype(v.dtype), v)

    return _from_blocks(lax.map(one_block, (_to_blocks(q), jnp.arange(nb))))


def mla_branch(c_q, c_kv, k_rope, q_norm_g, kv_norm_g, w_uq, w_ukv, cos, sin):
    b, s, _ = c_q.shape
    q = (rms_norm(c_q, q_norm_g) @ w_uq).reshape(b, s, MLA_HEADS, MLA_NOPE + MLA_ROPE)
    kv = (rms_norm(c_kv, kv_norm_g) @ w_ukv).reshape(b, s, MLA_HEADS, MLA_NOPE + MLA_V)
    q = jnp.concatenate([q[..., :MLA_NOPE], apply_rope(q[..., MLA_NOPE:], cos, sin)], axis=-1)
    k_pe = apply_rope(k_rope[:, :, None, :], cos, sin)
    k = jnp.concatenate([kv[..., :MLA_NOPE],
                         jnp.broadcast_to(k_pe, (b, s, MLA_HEADS, MLA_ROPE))], axis=-1)
    v = kv[..., MLA_NOPE:]
    o = causal_attention(q, k, v, (MLA_NOPE + MLA_ROPE) ** -0.5)
    return o.reshape(b, s, MLA_HEADS * MLA_V)


def s5_branch(u, a_re, a_im, log_dt, b_re, b_im, c_re, c_im, d, w_glu, b_glu):
    f32 = jnp.float32
    bsz, s, _ = u.shape
    uf = u.astype(f32)
    ug = uf.reshape(bsz, s, S5_GROUPS, S5_GROUP)
    lr = jnp.minimum(a_re.astype(f32), -1e-4)
    li = a_im.astype(f32)
    dt = jnp.exp(log_dt.astype(f32))[:, None]
    mag = jnp.exp(dt * lr)
    ab_re, ab_im = mag * jnp.cos(dt * li), mag * jnp.sin(dt * li)
    den = lr * lr + li * li
    nr, ni = ab_re - 1.0, ab_im
    f_re = (nr * lr + ni * li) / den
    f_im = (ni * lr - nr * li) / den
    br, bi = b_re.astype(f32), b_im.astype(f32)
    bb_re = f_re[..., None] * br - f_im[..., None] * bi
    bb_im = f_re[..., None] * bi + f_im[..., None] * br
    bu_re = jnp.einsum('bsgc,gpc->bsgp', ug, bb_re)
    bu_im = jnp.einsum('bsgc,gpc->bsgp', ug, bb_im)
    shape = bu_re.shape
    elems = (jnp.broadcast_to(ab_re, shape), jnp.broadcast_to(ab_im, shape), bu_re, bu_im)

    def combine(e1, e2):
        a1r, a1i, b1r, b1i = e1
        a2r, a2i, b2r, b2i = e2
        return (a2r * a1r - a2i * a1i, a2r * a1i + a2i * a1r,
                a2r * b1r - a2i * b1i + b2r, a2r * b1i + a2i * b1r + b2i)

    _, _, h_re, h_im = lax.associative_scan(combine, elems, axis=1)
    y = (jnp.einsum('bsgp,gcp->bsgc', h_re, c_re.astype(f32))
         - jnp.einsum('bsgp,gcp->bsgc', h_im, c_im.astype(f32)))
    y = y.reshape(bsz, s, W_GROUP) + d.astype(f32) * uf
    g = jax.nn.gelu(y)
    out = g * jax.nn.sigmoid(g @ w_glu.astype(f32) + b_glu.astype(f32))
    return out.astype(u.dtype)


def rwkv7_branch(z, mu, w0, w2, a0, a2, k_k, k_a, r_k, ln_g, ln_b):
    f32 = jnp.float32
    bsz, s, _ = z.shape
    z_prev = jnp.pad(z, ((0, 0), (1, 0), (0, 0)))[:, :-1]
    z = z + (z_prev - z) * mu
    r, k, v, w_lo, a_lo = jnp.split(z, _split_points(RWKV_SIZES), axis=-1)
    w_log = -jax.nn.softplus(-(w0 + jnp.tanh(w_lo) @ w2)) - 0.5
    decay = jnp.exp(-jnp.exp(w_log.astype(f32)))
    a = jax.nn.sigmoid(a0 + a_lo @ a2)

    def heads(t):
        return t.reshape(bsz, s, RWKV_HEADS, RWKV_HEAD).astype(f32)

    kk = heads(k * k_k)
    kk = kk / jnp.maximum(jnp.sqrt(jnp.sum(kk * kk, axis=-1, keepdims=True)), 1e-12)
    k = k * (1.0 + (a - 1.0) * k_a)
    r_h, k_h, v_h, w_h, a_h = heads(r), heads(k), heads(v), heads(decay), heads(a)
    seq_in = tuple(jnp.moveaxis(t, 1, 0) for t in (r_h, w_h, k_h, v_h, -kk, kk * a_h))

    def step(state, inp):
        r_t, w_t, k_t, v_t, a_t, b_t = inp
        sa = jnp.einsum('bhvk,bhk->bhv', state, a_t)
        state = (state * w_t[:, :, None, :] + sa[..., None] * b_t[:, :, None, :]
                 + v_t[..., None] * k_t[:, :, None, :])
        return state, jnp.einsum('bhvk,bhk->bhv', state, r_t)

    state0 = jnp.zeros((bsz, RWKV_HEADS, RWKV_HEAD, RWKV_HEAD), f32)
    _, o = lax.scan(step, state0, seq_in)
    o = jnp.moveaxis(o, 0, 1)
    mean = jnp.mean(o, axis=-1, keepdims=True)
    var = jnp.mean(jnp.square(o - mean), axis=-1, keepdims=True)
    o = ((o - mean) * lax.rsqrt(var + RWKV_GN_EPS)).reshape(bsz, s, W_GROUP)
    o = o * ln_g.astype(f32) + ln_b.astype(f32)
    bonus = jnp.sum(r_h * k_h * r_k.astype(f32), axis=-1, keepdims=True) * v_h
    return (o + bonus.reshape(bsz, s, W_GROUP)).astype(z.dtype)


def diff_branch(q, k, v, lq1, lk1, lq2, lk2, subln_g, lam_init, cos, sin):
    f32 = jnp.float32
    bsz, s, _ = q.shape
    q = apply_rope(q.reshape(bsz, s, 2 * DIFF_HEADS, DIFF_QK), cos, sin)
    k = apply_rope(k.reshape(bsz, s, 2 * DIFF_HEADS, DIFF_QK), cos, sin)
    q = q.reshape(bsz, s, DIFF_HEADS, 2, DIFF_QK)
    k = k.reshape(bsz, s, DIFF_HEADS, 2, DIFF_QK)
    v = v.reshape(bsz, s, DIFF_HEADS, DIFF_V)
    lam = (jnp.exp(jnp.sum(lq1.astype(f32) * lk1.astype(f32)))
           - jnp.exp(jnp.sum(lq2.astype(f32) * lk2.astype(f32))) + lam_init)
    o = causal_diff_attention(q, k, v, lam, DIFF_QK ** -0.5)
    o = rms_norm(o, subln_g, eps=DIFF_SUBLN_EPS) * (1.0 - lam_init)
    return o.reshape(bsz, s, W_GROUP)


def setup_inputs(seed: int = 0) -> dict:
    key = jax.random.key(seed)
    ks = jax.random.split(key, 40)
    L = DEPTH

    def nrm(k, shape, scale):
        return jax.random.normal(k, shape, jnp.float32) * scale

    x = jax.random.normal(ks[0], (BATCH, SEQ, D_MODEL), jnp.float32)
    offset = jax.random.randint(ks[1], (BATCH, 1), 0, 1024, dtype=jnp.int32)
    positions = offset + jnp.arange(SEQ, dtype=jnp.int32)[None, :]
    s5_im0 = math.pi * jnp.arange(S5_STATE, dtype=jnp.float32)
    return {
        'x': x,
        'positions': positions,
        'norm_g': 1.0 + nrm(ks[2], (L, D_MODEL), 0.02),
        'w_in': nrm(ks[3], (L, D_MODEL, N_IN), D_MODEL ** -0.5),
        'w_out': nrm(ks[4], (L, D_MIX, D_MODEL), D_MIX ** -0.5),
        'mla_q_norm_g': 1.0 + nrm(ks[5], (L, MLA_Q_LORA), 0.02),
        'mla_kv_norm_g': 1.0 + nrm(ks[6], (L, MLA_KV_LORA), 0.02),
        'mla_w_uq': nrm(ks[7], (L, MLA_Q_LORA, MLA_HEADS * (MLA_NOPE + MLA_ROPE)), MLA_Q_LORA ** -0.5),
        'mla_w_ukv': nrm(ks[8], (L, MLA_KV_LORA, MLA_HEADS * (MLA_NOPE + MLA_V)), MLA_KV_LORA ** -0.5),
        's5_a_re': -0.5 + nrm(ks[9], (L, S5_GROUPS, S5_STATE), 0.01),
        's5_a_im': s5_im0 + nrm(ks[10], (L, S5_GROUPS, S5_STATE), 0.01),
        's5_log_dt': jax.random.uniform(ks[11], (L, S5_GROUPS), jnp.float32,
                                        math.log(S5_DT_MIN), math.log(S5_DT_MAX)),
        's5_b_re': nrm(ks[12], (L, S5_GROUPS, S5_STATE, S5_GROUP), (2 * S5_GROUP) ** -0.5),
        's5_b_im': nrm(ks[13], (L, S5_GROUPS, S5_STATE, S5_GROUP), (2 * S5_GROUP) ** -0.5),
        's5_c_re': nrm(ks[14], (L, S5_GROUPS, S5_GROUP, S5_STATE), (2 * S5_STATE) ** -0.5),
        's5_c_im': nrm(ks[15], (L, S5_GROUPS, S5_GROUP, S5_STATE), (2 * S5_STATE) ** -0.5),
        's5_d': nrm(ks[16], (L, W_GROUP), 1.0),
        's5_w_glu': nrm(ks[17], (L, W_GROUP, W_GROUP), W_GROUP ** -0.5),
        's5_b_glu': nrm(ks[18], (L, W_GROUP), 0.01),
        'rwkv_mu': jax.random.uniform(ks[19], (L, N_RWKV), jnp.float32),
        'rwkv_w0': jnp.linspace(-6.0, -1.0, W_GROUP, dtype=jnp.float32)[None, :] + nrm(ks[20], (L, W_GROUP), 0.1),
        'rwkv_w2': nrm(ks[21], (L, RWKV_DECAY_LORA, W_GROUP), 0.5 * RWKV_DECAY_LORA ** -0.5),
        'rwkv_a0': nrm(ks[22], (L, W_GROUP), 0.1),
        'rwkv_a2': nrm(ks[23], (L, RWKV_ICL_LORA, W_GROUP), 0.5 * RWKV_ICL_LORA ** -0.5),
        'rwkv_k_k': 0.85 + nrm(ks[24], (L, W_GROUP), 0.05),
        'rwkv_k_a': 1.0 + nrm(ks[25], (L, W_GROUP), 0.05),
        'rwkv_r_k': nrm(ks[26], (L, RWKV_HEADS, RWKV_HEAD), 0.1),
        'rwkv_ln_g': 1.0 + nrm(ks[27], (L, W_GROUP), 0.02),
        'rwkv_ln_b': nrm(ks[28], (L, W_GROUP), 0.01),
        'diff_lq1': nrm(ks[29], (L, DIFF_QK), 0.1),
        'diff_lk1': nrm(ks[30], (L, DIFF_QK), 0.1),
        'diff_lq2': nrm(ks[31], (L, DIFF_QK), 0.1),
        'diff_lk2': nrm(ks[32], (L, DIFF_QK), 0.1),
        'diff_subln_g': 1.0 + nrm(ks[33], (L, DIFF_V), 0.02),
        'final_norm_g': 1.0 + nrm(ks[34], (D_MODEL,), 0.02),
    }


def reference(x, positions, norm_g, w_in, w_out, mla_q_norm_g, mla_kv_norm_g, mla_w_uq, mla_w_ukv,
              s5_a_re, s5_a_im, s5_log_dt, s5_b_re, s5_b_im, s5_c_re, s5_c_im, s5_d, s5_w_glu, s5_b_glu,
              rwkv_mu, rwkv_w0, rwkv_w2, rwkv_a0, rwkv_a2, rwkv_k_k, rwkv_k_a, rwkv_r_k, rwkv_ln_g, rwkv_ln_b,
              diff_lq1, diff_lk1, diff_lq2, diff_lk2, diff_subln_g, final_norm_g):
    cos_a, sin_a = rope_tables(positions, MLA_ROPE)
    cos_d, sin_d = rope_tables(positions, DIFF_ROT)
    points = _split_points(PROJ_SIZES)
    for l in range(DEPTH):
        h = rms_norm(x, norm_g[l])
        proj = h @ w_in[l]
        c_q, c_kv, k_rope, u_s5, z_rwkv, q_d, k_d, v_d, gate = jnp.split(proj, points, axis=-1)
        y_a = mla_branch(c_q, c_kv, k_rope, mla_q_norm_g[l], mla_kv_norm_g[l],
                         mla_w_uq[l], mla_w_ukv[l], cos_a, sin_a)
        y_b = s5_branch(u_s5, s5_a_re[l], s5_a_im[l], s5_log_dt[l], s5_b_re[l], s5_b_im[l],
                        s5_c_re[l], s5_c_im[l], s5_d[l], s5_w_glu[l], s5_b_glu[l])
        y_c = rwkv7_branch(z_rwkv, rwkv_mu[l], rwkv_w0[l], rwkv_w2[l], rwkv_a0[l], rwkv_a2[l],
                           rwkv_k_k[l], rwkv_k_a[l], rwkv_r_k[l], rwkv_ln_g[l], rwkv_ln_b[l])
        lam_init = 0.8 - 0.6 * math.exp(-0.3 * l)
        y_d = diff_branch(q_d, k_d, v_d, diff_lq1[l], diff_lk1[l], diff_lq2[l], diff_lk2[l],
                          diff_subln_g[l], lam_init, cos_d, sin_d)
        mixed = jnp.concatenate([y_a, y_b, y_c, y_d], axis=-1) * jax.nn.silu(gate)
        x = x + (mixed @ w_out[l]).astype(x.dtype)
    return rms_norm(x, final_norm_g)
```

```python
from contextlib import ExitStack
import math
import numpy as np
import concourse.bass as bass
import concourse.mybir as mybir

F32 = mybir.dt.float32
BF16 = mybir.dt.bfloat16
I32 = mybir.dt.int32
AF = mybir.ActivationFunctionType
ALU = mybir.AluOpType
AX = mybir.AxisListType

NDS = 44
NDS_HW = 24


class Dep:
    __slots__ = ("w", "r", "excl")

    def __init__(self):
        self.excl = False
        self.w = {}
        self.r = {}


class T:
    def __init__(self, t, dep=None):
        self.t = t
        self.dep = dep or Dep()

    def __getitem__(self, k):
        return self.t[k]

    def ap(self):
        return self.t.ap() if hasattr(self.t, "ap") else self.t[:]


def _deps(x):
    return x.dep if isinstance(x, T) else x


class KB:
    def __init__(self, nc):
        self.nc = nc
        self.es = ExitStack()
        self.eng = {"pe": nc.tensor, "act": nc.scalar, "dve": nc.vector,
                    "pool": nc.gpsimd, "sp": nc.sync}
        self.sem = {}
        for e in ("pe", "act", "dve", "pool"):
            self.sem[e] = self.es.enter_context(nc.semaphore("s_" + e))
        self.cnt = {e: 0 for e in self.sem}
        self.dsem = [self.es.enter_context(nc.semaphore(f"sd{i}")) for i in range(NDS)]
        self.dcnt = [0] * NDS
        self.dnext = 0
        self.dnext_sw = 0
        self.seen = {e: {} for e in self.eng}
        self.ccsem = self.es.enter_context(nc.semaphore("s_cc"))
        self.cccnt = 0
        self.ninst = 0
        self.scopes = []

    def sb(self, name, shape, dtype, stack=None):
        self.uid = getattr(self, "uid", 0) + 1
        name = f"{name}_u{self.uid}"
        t = (stack or self.es).enter_context(self.nc.sbuf_tensor(name, list(shape), dtype))
        return T(t)

    def ps(self, name, shape, dtype, stack=None):
        t = (stack or self.es).enter_context(self.nc.psum_tensor(name, list(shape), dtype))
        r = T(t)
        r.dep.excl = True
        return r

    def dram(self, name, shape, dtype, kind="Internal"):
        t = self.nc.dram_tensor(name, list(shape), dtype, kind=kind)
        return T(t)

    def _wait(self, E, tok):
        if tok is None:
            return
        kind, key, val = tok
        if kind == "e" and key == E and E in ("pe", "sp"):
            return
        sk = (kind, key)
        if self.seen[E].get(sk, 0) >= val:
            return
        sem = self.sem[key] if kind == "e" else (self.dsem[key] if kind == "d" else self.ccsem)
        self.eng[E].wait_ge(sem, val)
        self.ninst += 1
        self.seen[E][sk] = val

    def _collect(self, E, reads, writes, waw=True):
        for d in reads:
            d = _deps(d)
            for (kd, ky), v in list(d.w.items()):
                self._wait(E, (kd, ky, v))
            if d.excl:
                for (kd, ky), v in list(d.r.items()):
                    if ky != E:
                        self._wait(E, (kd, ky, v))
        for d in writes:
            d = _deps(d)
            if waw:
                for (kd, ky), v in list(d.w.items()):
                    self._wait(E, (kd, ky, v))
            for (kd, ky), v in list(d.r.items()):
                self._wait(E, (kd, ky, v))

    def _update(self, tok, reads, writes, waw=True):
        kk = (tok[0], tok[1])
        for d in writes:
            d = _deps(d)
            if waw:
                d.w = {kk: tok[2]}
                d.r = {}
            else:
                d.w[kk] = max(d.w.get(kk, 0), tok[2])
        for d in reads:
            d = _deps(d)
            d.r[kk] = max(d.r.get(kk, 0), tok[2])

    def op(self, E, fn, reads=(), writes=(), inc=True):
        self._collect(E, reads, writes)
        inst = fn(self.eng[E])
        self.ninst += 1
        if inc:
            self.cnt[E] += 1
            inst.then_inc(self.sem[E], 1)
            tok = ("e", E, self.cnt[E])
        else:
            tok = ("e", E, self.cnt[E] + 1)
        self._update(tok, reads, writes)
        return inst

    def dma(self, Q, out, in_, reads=(), writes=(), waw=True, **kw):
        self._collect(Q, reads, writes, waw)
        if Q == "pool":
            i = NDS_HW + self.dnext_sw
            self.dnext_sw = (self.dnext_sw + 1) % (NDS - NDS_HW)
        else:
            i = self.dnext
            self.dnext = (i + 1) % NDS_HW
        if self.dcnt[i] > 0:
            self._wait(Q, ("d", i, 16 * self.dcnt[i]))
        inst = self.eng[Q].dma_start(out=out, in_=in_, **kw)
        inst.then_inc(self.dsem[i], 16)
        self.ninst += 1
        self.dcnt[i] += 1
        tok = ("d", i, 16 * self.dcnt[i])
        self._update(tok, reads, writes, waw)
        return tok

    def collective(self, in_ap, out_ap, reads=(), writes=(), groups=None):
        self._collect("pool", reads, writes, True)
        inst = self.nc.gpsimd.collective_compute("AllReduce", ALU.add, replica_groups=groups,
                                                 ins=[in_ap], outs=[out_ap])
        self.cccnt += 1
        inst.then_inc(self.ccsem, 1)
        self.ninst += 1
        tok = ("c", 0, self.cccnt)
        self._update(tok, reads, writes, True)
        return tok

    def barrier(self, engines=("pe", "act", "dve", "pool", "sp")):
        for E in engines:
            if self.cccnt > 0:
                self._wait(E, ("c", 0, self.cccnt))
            for p in self.sem:
                if p != E and self.cnt[p] > 0:
                    self._wait(E, ("e", p, self.cnt[p]))
            for i in range(NDS):
                if self.dcnt[i] > 0:
                    self._wait(E, ("d", i, 16 * self.dcnt[i]))
        for E in ("act", "dve", "pool"):
            if E in engines and self.cnt[E] > 0:
                self._wait(E, ("e", E, self.cnt[E]))

    def finish(self):
        self.barrier(engines=("sp",))


S = 4096
D = 2048
TCH = 512
NTC = S // TCH
EPS = 1e-6


class Ctx:
    def __init__(self, k):
        self.k = k
        self.pf = [k.ps(f"pf{i}", [128, 512], F32) for i in range(6)]
        self.pb = [k.ps(f"pb{i}", [128, 1024], BF16) for i in range(2)]
        self.pfi = 0
        self.pbi = 0
        self.rr = 0
        self.us5 = None

    def next_pf(self, lo=0, hi=6):
        n = hi - lo
        p = self.pf[lo + (self.pfi % n)]
        self.pfi += 1
        return p

    def next_pb(self):
        p = self.pb[self.pbi % 2]
        self.pbi += 1
        return p

    def evac_eng(self):
        self.rr += 1
        return "act" if self.rr % 2 else "dve"


def copy_op(k, E, out, in_, reads, writes):
    if E == "act":
        k.op("act", lambda e: e.copy(out=out, in_=in_), reads=reads, writes=writes)
    else:
        k.op(E, lambda e: e.tensor_copy(out=out, in_=in_), reads=reads, writes=writes)


def load_consts(k, c, ident_d, stack):
    idf = k.sb("idf", [128, 128], F32, stack)
    c.ident = k.sb("c_ident", [128, 128], BF16)
    c.ones = k.sb("c_ones", [128, 128], BF16)
    k.dma("sp", idf[:], ident_d[:, :], writes=[idf])
    k.op("dve", lambda e: e.tensor_copy(out=c.ident[:], in_=idf[:]), reads=[idf], writes=[c.ident])
    k.op("dve", lambda e: e.memset(c.ones[:], 1.0), writes=[c.ones])


def stage_norm(k, c, xa, xb, g_bc_d, hT, xa_deps=None):
    with ExitStack() as st:
        gbc = k.sb("n_gbc", [128, D], F32, st)
        k.dma("sp", gbc[:], g_bc_d.partition_broadcast(128), writes=[gbc])
        xt = [k.sb(f"n_xt{i}", [128, D], F32, st) for i in range(2)]
        xt2 = [k.sb(f"n_xu{i}", [128, D], F32, st) for i in range(2)]
        junk = k.sb("n_junk", [128, D], BF16, st)
        xn = [k.sb(f"n_xn{i}", [128, D], BF16, st) for i in range(2)]
        ss = [k.sb(f"n_ss{i}", [128, 4], F32, st) for i in range(2)]
        hst = [k.sb(f"n_hst{i}", [128, 16, TCH], BF16, st) for i in range(2)]
        for tt in range(S // 128):
            b = tt % 2
            tcn, tl = divmod(tt, 4)
            hs = hst[tcn % 2]
            rows = slice(tt * 128, (tt + 1) * 128)
            k.dma("sp", xt[b][:], xa[rows, :], reads=[xa_deps[tt // 4] if xa_deps else xa], writes=[xt[b]])
            if xb is not None:
                k.dma("sp", xt2[b][:], xb[rows, :], reads=[xb], writes=[xt2[b]])
                k.op("pool", lambda e: e.tensor_tensor(out=xt[b][:], in0=xt[b][:], in1=xt2[b][:], op=ALU.add),
                     reads=[xt[b], xt2[b]], writes=[xt[b]])
            k.op("act", lambda e: e.activation(out=junk[:], in_=xt[b][:], func=AF.Square,
                                               accum_out=ss[b][:, 0:1]),
                 reads=[xt[b]], writes=[junk, ss[b]])
            k.op("dve", lambda e: e.tensor_scalar(out=ss[b][:, 1:2], in0=ss[b][:, 0:1], scalar1=1.0 / D,
                                                  scalar2=EPS, op0=ALU.mult, op1=ALU.add),
                 reads=[ss[b]], writes=[ss[b]])
            k.op("act", lambda e: e.activation(out=ss[b][:, 2:3], in_=ss[b][:, 1:2], func=AF.Sqrt),
                 reads=[ss[b]], writes=[ss[b]])
            k.op("dve", lambda e: e.reciprocal(out=ss[b][:, 3:4], in_=ss[b][:, 2:3]),
                 reads=[ss[b]], writes=[ss[b]])
            k.op("dve", lambda e: e.scalar_tensor_tensor(out=xn[b][:], in0=xt[b][:], scalar=ss[b][:, 3:4],
                                                         in1=gbc[:], op0=ALU.mult, op1=ALU.mult),
                 reads=[xt[b], ss[b], gbc], writes=[xn[b]])
            for half in range(2):
                pb = c.next_pb()
                for j in range(8):
                    kc = half * 8 + j
                    k.op("pe", lambda e: e.transpose(pb[:, j * 128:(j + 1) * 128],
                                                     xn[b][:, kc * 128:(kc + 1) * 128], c.ident[:]),
                         reads=[xn[b], c.ident], writes=[pb], inc=(j == 7))
                k.op("act", lambda e: e.copy(out=hs[:, half * 8:(half + 1) * 8, tl * 128:(tl + 1) * 128],
                                             in_=pb[:].rearrange("p (a b) -> p a b", a=8)),
                     reads=[pb], writes=[hs])
            if tl == 3:
                k.dma("pool", hT[:, tcn * TCH:(tcn + 1) * TCH].rearrange("(kc p) t -> p kc t", p=128),
                      hs[:], reads=[hs], writes=[hT], waw=False)


def load_w_bf16(k, st, w_ap_fn, KC, N, name):
    wbf = k.sb(name, [128, KC, N], BF16, st)
    stg = [k.sb(f"{name}_s{i}", [128, N], F32, st) for i in range(2)]
    deps = [Dep() for _ in range(KC)]
    for kc in range(KC):
        s = stg[kc % 2]
        k.dma("sp", s[:], w_ap_fn(kc), writes=[s])
        E = "pool" if kc % 2 == 0 else "dve"
        k.op(E, lambda e: e.tensor_copy(out=wbf[:, kc, :], in_=s[:]), reads=[s], writes=[deps[kc]])
    return wbf, deps


def linear_fm(k, c, st, inT, KC, wbf, wdeps, n_tiles, epilogue, name, in_cast=False):
    xin = [k.sb(f"{name}_x{i}", [128, KC, TCH], BF16, st) for i in range(2)]
    for tc in range(NTC):
        xi = xin[tc % 2]
        k.dma("sp", xi[:], inT[:, tc * TCH:(tc + 1) * TCH].rearrange("(kc p) t -> p kc t", p=128),
              reads=[inT], writes=[xi])
        for ni, (c0, ncol) in enumerate(n_tiles):
            ps = c.next_pf()
            for kc in range(KC):
                k.op("pe", lambda e: e.matmul(ps[:ncol, :], lhsT=wbf[:, kc, c0:c0 + ncol], rhs=xi[:, kc, :],
                                              start=(kc == 0), stop=(kc == KC - 1)),
                     reads=[wdeps[kc], xi], writes=[ps], inc=(kc == KC - 1))
            epilogue(tc, ni, ps, ncol)


def linear_tm(k, c, st, inT, KC, wbf, wdeps, c0, N, epilogue, name):
    xin = [k.sb(f"{name}_x{i}", [128, KC, TCH], BF16, st) for i in range(2)]
    for tc in range(NTC):
        xi = xin[tc % 2]
        k.dma("sp", xi[:], inT[:, tc * TCH:(tc + 1) * TCH].rearrange("(kc p) t -> p kc t", p=128),
              reads=[inT], writes=[xi])
        for sub in range(4):
            ps = c.next_pf()
            for kc in range(KC):
                k.op("pe", lambda e: e.matmul(ps[:, :N], lhsT=xi[:, kc, sub * 128:(sub + 1) * 128],
                                              rhs=wbf[:, kc, c0:c0 + N],
                                              start=(kc == 0), stop=(kc == KC - 1)),
                     reads=[wdeps[kc], xi], writes=[ps], inc=(kc == KC - 1))
            epilogue(tc * 4 + sub, ps)


class Stager:
    def __init__(self, k, st, name, shape, dtype, n=4):
        self.k = k
        self.bufs = [k.sb(f"{name}{i}", shape, dtype, st) for i in range(n)]
        self.i = 0

    def next(self):
        b = self.bufs[self.i % len(self.bufs)]
        self.i += 1
        return b


def epi_store_fm(k, c, stg, outT, row0_of):
    def epi(tc, ni, ps, ncol):
        s = stg.next()
        copy_op(k, c.evac_eng(), s[:ncol, :], ps[:ncol, :], [ps], [s])
        r0 = row0_of(ni)
        k.dma("pool", outT[r0:r0 + ncol, tc * TCH:(tc + 1) * TCH], s[:ncol, :], reads=[s], writes=[outT], waw=False)
        return s
    return epi


def stage_inproj(k, c, hT, w_in_d, NF, NV, pT, pV):
    KC = D // 128
    tiles = []
    c0 = 0
    while c0 < NF:
        tiles.append((c0, min(128, NF - c0)))
        c0 += 128
    half = (len(tiles) + 1) // 2
    groups = [tiles[:half], tiles[half:]]
    for gi, grp in enumerate(groups):
        with ExitStack() as st:
            g0 = grp[0][0]
            gN = grp[-1][0] + grp[-1][1] - g0
            wbf, wd = load_w_bf16(k, st, lambda kc: w_in_d[kc * 128:(kc + 1) * 128, g0:g0 + gN], KC, gN, f"ip_w{gi}")
            stg = Stager(k, st, f"ip_o{gi}_", [128, TCH], F32, 4)
            rel = [(a - g0, b) for a, b in grp]
            base_epi = epi_store_fm(k, c, stg, pT, lambda ni: grp[ni][0])
            stu = Stager(k, st, f"ip_u{gi}_", [128, 8, 64], BF16, 3)

            def epi(tc, ni, ps, ncol, grp=grp, base_epi=base_epi, stu=stu):
                sst = base_epi(tc, ni, ps, ncol)
                r0 = grp[ni][0]
                import os as _os
                if R_U <= r0 < R_U + 512 and c.us5 is not None and _os.environ.get('NOHOOK') != '1':
                    su = stu.next()
                    k.op("pool", lambda e: e.tensor_copy(out=su[:],
                                                         in_=sst[:, :].rearrange("p (j t) -> p t j", t=8)),
                         reads=[sst], writes=[su])
                    g0 = (r0 - R_U) // 16
                    hq = _os.environ.get("HOOKDMA", "pool")
                    for gl in range(8 if hq != "none" else 0):
                        k.dma(hq, c.us5[g0 + gl, :, tc * 64:(tc + 1) * 64].rearrange("(t c) j -> c t j", c=16),
                              su[gl * 16:(gl + 1) * 16, :, :], reads=[su], writes=[c.us5], waw=False)
            linear_fm(k, c, st, hT, KC, wbf, wd, rel, epi, f"ip{gi}")
        k.barrier()
    with ExitStack() as st:
        wbf, wd = load_w_bf16(k, st, lambda kc: w_in_d[kc * 128:(kc + 1) * 128, NF:NF + NV], KC, NV, "ip_wv")
        stg = Stager(k, st, "ip_ov_", [128, NV], F32, 4)

        def epi(tt, ps):
            s = stg.next()
            copy_op(k, c.evac_eng(), s[:, :], ps[:, :NV], [ps], [s])
            k.dma("pool", pV[tt * 128:(tt + 1) * 128, :], s[:, :], reads=[s], writes=[pV], waw=False)
        linear_tm(k, c, st, hT, KC, wbf, wd, 0, NV, epi, "ipv")
    k.barrier()


TWO_PI = 2.0 * np.pi
CW1 = 6.28125
CW2 = float(np.float32(TWO_PI - 6.28125))
CW3 = float(TWO_PI - 6.28125 - float(np.float32(TWO_PI - 6.28125)))


def make_rope_tables(k, c, pos_d, cst, col_inv, col_sgn, cosT, sinT, name):
    HS = S // 2
    with ExitStack() as st:
        posi = k.sb(name + "_pi", [128, HS], I32, st)
        ang = k.sb(name + "_ang", [128, HS], F32, st)
        a2 = k.sb(name + "_a2", [128, HS], F32, st)
        ni = k.sb(name + "_ni", [128, HS], I32, st)
        nf = k.sb(name + "_nf", [128, HS], F32, st)
        r = k.sb(name + "_r", [128, HS], F32, st)
        m = k.sb(name + "_m", [128, HS], F32, st)
        o = k.sb(name + "_o", [128, HS], F32, st)
        for hh in range(2):
            sl = slice(hh * HS, (hh + 1) * HS)
            k.dma("sp", posi[:], pos_d[0:1, sl].partition_broadcast(128), writes=[posi])
            k.op("dve", lambda e: e.tensor_copy(out=ang[:], in_=posi[:]), reads=[posi], writes=[ang])
            k.op("dve", lambda e: e.tensor_scalar(out=ang[:], in0=ang[:], scalar1=cst[:, col_inv:col_inv + 1],
                                                  scalar2=None, op0=ALU.mult), reads=[ang, cst], writes=[ang])
            for which, shift, dst in (("s", 0.0, sinT), ("c", np.pi / 2, cosT)):
                if which == "s":
                    k.op("dve", lambda e: e.tensor_scalar(out=ni[:], in0=ang[:], scalar1=float(1.0 / TWO_PI),
                                                          scalar2=None, op0=ALU.mult), reads=[ang], writes=[ni])
                    k.op("dve", lambda e: e.tensor_copy(out=nf[:], in_=ni[:]), reads=[ni], writes=[nf])
                    k.op("dve", lambda e: e.scalar_tensor_tensor(out=a2[:], in0=nf[:], scalar=-CW1, in1=ang[:],
                                                                 op0=ALU.mult, op1=ALU.add), reads=[nf, ang], writes=[a2])
                    k.op("dve", lambda e: e.scalar_tensor_tensor(out=a2[:], in0=nf[:], scalar=-CW2, in1=a2[:],
                                                                 op0=ALU.mult, op1=ALU.add), reads=[nf, a2], writes=[a2])
                    k.op("dve", lambda e: e.scalar_tensor_tensor(out=a2[:], in0=nf[:], scalar=-CW3, in1=a2[:],
                                                                 op0=ALU.mult, op1=ALU.add), reads=[nf, a2], writes=[a2])
                k.op("dve", lambda e: e.tensor_scalar(out=r[:], in0=a2[:], scalar1=float(shift), scalar2=None,
                                                      op0=ALU.add), reads=[a2], writes=[r])
                k.op("dve", lambda e: e.tensor_single_scalar(out=m[:], in_=r[:], scalar=float(np.pi), op=ALU.is_gt),
                     reads=[r], writes=[m])
                k.op("dve", lambda e: e.scalar_tensor_tensor(out=r[:], in0=m[:], scalar=-TWO_PI, in1=r[:],
                                                             op0=ALU.mult, op1=ALU.add), reads=[m, r], writes=[r])
                k.op("dve", lambda e: e.tensor_single_scalar(out=m[:], in_=r[:], scalar=float(-np.pi), op=ALU.is_lt),
                     reads=[r], writes=[m])
                k.op("dve", lambda e: e.scalar_tensor_tensor(out=r[:], in0=m[:], scalar=TWO_PI, in1=r[:],
                                                             op0=ALU.mult, op1=ALU.add), reads=[m, r], writes=[r])
                k.op("dve", lambda e: e.tensor_scalar(out=r[:], in0=r[:], scalar1=float(np.pi), scalar2=float(-np.pi),
                                                      op0=ALU.min, op1=ALU.max), reads=[r], writes=[r])
                k.op("act", lambda e: e.activation(out=o[:], in_=r[:], func=AF.Sin), reads=[r], writes=[o])
                if which == "s":
                    k.op("dve", lambda e: e.tensor_scalar(out=o[:], in0=o[:], scalar1=cst[:, col_sgn:col_sgn + 1],
                                                          scalar2=None, op0=ALU.mult), reads=[o, cst], writes=[o])
                k.dma("sp", dst[:, sl], o[:], reads=[o], writes=[dst], waw=False)
    k.barrier()


def rmsnorm_fm(k, c, srcT, r0, nt, g_sb, gcol0, dstT, eps, name):
    n = nt * 128
    with ExitStack() as st:
        xin = [k.sb(f"{name}_x{i}", [128, nt, TCH], F32, st) for i in range(2)]
        sq = [k.sb(f"{name}_q{i}", [128, nt, TCH], BF16, st) for i in range(2)]
        rs = [k.sb(f"{name}_r{i}", [128, TCH], F32, st) for i in range(2)]
        ob = [k.sb(f"{name}_o{i}", [128, nt, TCH], BF16, st) for i in range(2)]
        for tc in range(NTC):
            b = tc % 2
            tsl = slice(tc * TCH, (tc + 1) * TCH)
            k.dma("sp", xin[b][:], srcT[r0:r0 + n, tsl].rearrange("(t p) s -> p t s", p=128),
                  reads=[srcT], writes=[xin[b]])
            k.op("act", lambda e: e.activation(out=sq[b][:], in_=xin[b][:], func=AF.Square),
                 reads=[xin[b]], writes=[sq[b]])
            ps = c.next_pf()
            for t in range(nt):
                k.op("pe", lambda e: e.matmul(ps[:, :], lhsT=c.ones[:], rhs=sq[b][:, t, :],
                                              start=(t == 0), stop=(t == nt - 1)),
                     reads=[c.ones, sq[b]], writes=[ps], inc=(t == nt - 1))
            k.op("dve", lambda e: e.tensor_scalar(out=rs[b][:], in0=ps[:, :], scalar1=1.0 / n, scalar2=float(eps),
                                                  op0=ALU.mult, op1=ALU.add), reads=[ps], writes=[rs[b]])
            k.op("act", lambda e: e.activation(out=rs[b][:], in_=rs[b][:], func=AF.Sqrt),
                 reads=[rs[b]], writes=[rs[b]])
            k.op("dve", lambda e: e.reciprocal(out=rs[b][:], in_=rs[b][:]), reads=[rs[b]], writes=[rs[b]])
            for t in range(nt):
                k.op("dve", lambda e: e.scalar_tensor_tensor(out=ob[b][:, t, :], in0=xin[b][:, t, :],
                                                             scalar=g_sb[:, gcol0 + t:gcol0 + t + 1], in1=rs[b][:],
                                                             op0=ALU.mult, op1=ALU.mult),
                     reads=[xin[b], g_sb, rs[b]], writes=[ob[b]])
            k.dma("pool", dstT[0:n, tsl].rearrange("(t p) s -> p t s", p=128), ob[b][:],
                  reads=[ob[b]], writes=[dstT], waw=False)
    k.barrier()


def rope_tile(k, c, st_bufs, src_ap, src_dep, perm, cos_sb, sin_sb, out_ap, out_dep):
    xb, xf, t1 = st_bufs["xb"], st_bufs["xf"], st_bufs["t1"]
    k.op("act", lambda e: e.copy(out=xf[:], in_=src_ap), reads=[src_dep], writes=[xf])
    k.op("dve", lambda e: e.tensor_copy(out=xb[:], in_=xf[:]), reads=[xf], writes=[xb])
    ps = c.next_pf()
    k.op("pe", lambda e: e.matmul(ps[:, :], lhsT=perm[:], rhs=xb[:], start=True, stop=True),
         reads=[perm, xb], writes=[ps])
    k.op("dve", lambda e: e.tensor_tensor(out=t1[:], in0=ps[:, :], in1=sin_sb, op=ALU.mult),
         reads=[ps, st_bufs["tab"]], writes=[t1])
    k.op("pool", lambda e: e.tensor_tensor(out=xf[:], in0=xf[:], in1=cos_sb, op=ALU.mult),
         reads=[xf, st_bufs["tab"]], writes=[xf])
    k.op("dve", lambda e: e.tensor_tensor(out=out_ap, in0=xf[:], in1=t1[:], op=ALU.add),
         reads=[xf, t1], writes=[out_dep])


def attention_head(k, c, st, name, maps, Vsb, vdep, dv, scale, masks, post):
    raise NotImplementedError


def attn_qchunk(k, c, j, maps, Vfn, vdep, dv, scale, masks, ptbufs, acc_banks):
    nkt = 4 * j + 4
    for mi, parts in enumerate(maps):
        oacc, sacc = acc_banks[mi]
        for kt in range(nkt):
            ps = c.pf[(c.pfi) % 2]
            c.pfi += 1
            for pi, p in enumerate(parts):
                k.op("pe", lambda e: e.matmul(ps[:, :], lhsT=p["K"](kt), rhs=p["Q"](),
                                              start=(pi == 0), stop=(pi == len(parts) - 1)),
                     reads=[p["kd"], p["qd"]], writes=[ps], inc=(pi == len(parts) - 1))
            pt = ptbufs[c.rr % len(ptbufs)]
            c.rr += 1
            k.op("act", lambda e: e.activation(out=pt[:], in_=ps[:, :], func=AF.Exp, scale=float(scale)),
                 reads=[ps], writes=[pt])
            if kt >= 4 * j:
                mk = masks[kt - 4 * j]
                k.op("pool", lambda e: e.tensor_tensor(out=pt[:], in0=pt[:], in1=mk[:], op=ALU.mult),
                     reads=[pt, mk], writes=[pt])
            k.op("pe", lambda e: e.matmul(oacc[:dv, :], lhsT=Vfn(kt), rhs=pt[:],
                                          start=(kt == 0), stop=(kt == nkt - 1)),
                 reads=[vdep, pt], writes=[oacc], inc=False)
            k.op("pe", lambda e: e.matmul(sacc[:, :], lhsT=c.ones[:], rhs=pt[:],
                                          start=(kt == 0), stop=(kt == nkt - 1)),
                 reads=[c.ones, pt], writes=[sacc], inc=True)


R_CQ, R_CKV, R_U, R_R, R_K, R_QD, R_KD, R_GATE, R_KROPE, R_LORA = 0, 512, 768, 1280, 1536, 1792, 2048, 2304, 3328, 3456
NF = 3840
NV = 256


class RopeCtx:
    def __init__(self, k, c, st, name, cosT, sinT, perm):
        self.k, self.c = k, c
        self.cosT, self.sinT, self.perm = cosT, sinT, perm
        self.tab = [k.sb(f"{name}_tab{i}", [128, 2, TCH], F32, st) for i in range(2)]
        self.xb = [k.sb(f"{name}_xb{i}", [128, TCH], BF16, st) for i in range(2)]
        self.xf = [k.sb(f"{name}_xf{i}", [128, TCH], F32, st) for i in range(2)]
        self.t1 = [k.sb(f"{name}_t1{i}", [128, TCH], F32, st) for i in range(2)]
        self.cur = None
        self.n = 0

    def load(self, tc):
        k = self.k
        tb = self.tab[tc % 2]
        tsl = slice(tc * TCH, (tc + 1) * TCH)
        k.dma("sp", tb[:, 0, :], self.cosT[:, tsl], reads=[self.cosT], writes=[tb])
        k.dma("sp", tb[:, 1, :], self.sinT[:, tsl], reads=[self.cosT], writes=[tb], waw=False)
        self.cur = tb

    def apply(self, src_ap, src_dep, out_ap, out_dep):
        k, c = self.k, self.c
        i = self.n % 2
        self.n += 1
        xb, xf, t1, tb = self.xb[i], self.xf[i], self.t1[i], self.cur
        k.op("act", lambda e: e.copy(out=xf[:], in_=src_ap), reads=[src_dep], writes=[xf])
        k.op("dve", lambda e: e.tensor_copy(out=xb[:], in_=xf[:]), reads=[xf], writes=[xb])
        ps = c.next_pf(0, 2)
        k.op("pe", lambda e: e.matmul(ps[:, :], lhsT=self.perm[:], rhs=xb[:], start=True, stop=True),
             reads=[self.perm, xb], writes=[ps])
        k.op("dve", lambda e: e.tensor_tensor(out=t1[:], in0=ps[:, :], in1=tb[:, 1, :], op=ALU.mult),
             reads=[ps, tb], writes=[t1])
        k.op("pool", lambda e: e.tensor_tensor(out=xf[:], in0=xf[:], in1=tb[:, 0, :], op=ALU.mult),
             reads=[xf, tb], writes=[xf])
        k.op("dve", lambda e: e.tensor_tensor(out=out_ap, in0=xf[:], in1=t1[:], op=ALU.add),
             reads=[xf, t1], writes=[out_dep])


def stage_mla(k, c, L):
    pT = c.pT
    rmsnorm_fm(k, c, pT, R_CQ, 4, L.prm, L.col["gq"], c.cqnT, EPS, "nq")
    rmsnorm_fm(k, c, pT, R_CKV, 2, L.prm, L.col["gkv"], c.ckvnT, EPS, "nkv")
    with ExitStack() as st:
        wbf, wd = load_w_bf16(k, st, lambda kc: L.wuq[kc * 128:(kc + 1) * 128, :], 4, 384, "wuq")
        stg = Stager(k, st, "mq_o", [128, TCH], BF16, 4)
        rp = RopeCtx(k, c, st, "mqr", c.ropeA_cos, c.ropeA_sin, c.permA)

        def epi(tc, ni, ps, ncol):
            s = stg.next()
            if ni < 2:
                copy_op(k, c.evac_eng(), s[:, :], ps[:, :], [ps], [s])
            else:
                rp.load(tc)
                rp.apply(ps[:, :], ps, s[:, :], s)
            k.dma("pool", c.qT[ni * 128:(ni + 1) * 128, tc * TCH:(tc + 1) * TCH], s[:, :], reads=[s],
                  writes=[c.qT], waw=False)
        linear_fm(k, c, st, c.cqnT, 4, wbf, wd, [(0, 128), (128, 128), (256, 128)], epi, "mq")
    k.barrier()
    with ExitStack() as st:
        wbf, wd = load_w_bf16(k, st, lambda kc: L.wukv[kc * 128:(kc + 1) * 128, :], 2, 512, "wukv")
        stg = Stager(k, st, "mk_o", [128, TCH], BF16, 4)
        epi = epi_store_fm(k, c, stg, c.knT, lambda ni: ni * 128)
        linear_fm(k, c, st, c.ckvnT, 2, wbf, wd, [(0, 128), (128, 128)], epi, "mk")
        stgv = Stager(k, st, "mv_o", [128, 256], BF16, 4)

        def epiv(tt, ps):
            s = stgv.next()
            copy_op(k, c.evac_eng(), s[:, :], ps[:, :256], [ps], [s])
            k.dma("pool", c.vA[tt * 128:(tt + 1) * 128, :], s[:, :], reads=[s], writes=[c.vA], waw=False)
        linear_tm(k, c, st, c.ckvnT, 2, wbf, wd, 256, 256, epiv, "mv")
        rp = RopeCtx(k, c, st, "mkr", c.ropeA_cos, c.ropeA_sin, c.permA)
        kin = [k.sb(f"mkr_in{i}", [128, TCH], F32, st) for i in range(2)]
        for tc in range(NTC):
            tsl = slice(tc * TCH, (tc + 1) * TCH)
            ki = kin[tc % 2]
            k.dma("sp", ki[:], pT[R_KROPE:R_KROPE + 128, tsl], reads=[pT], writes=[ki])
            rp.load(tc)
            s = stg.next()
            rp.apply(ki[:], ki, s[:, :], s)
            k.dma("pool", c.krT[:, tsl], s[:, :], reads=[s], writes=[c.krT], waw=False)
    k.barrier()
    scale = (128 + 64) ** -0.5
    for h in range(2):
        with ExitStack() as st:
            Kn = k.sb("ma_kn", [128, S], BF16, st)
            Kr = k.sb("ma_kr", [128, S], BF16, st)
            Vs = k.sb("ma_v", [128, S // 128, 128], BF16, st)
            k.dma("sp", Kn[:], c.knT[h * 128:(h + 1) * 128, :], reads=[c.knT], writes=[Kn])
            k.dma("sp", Kr[:], c.krT[:, :], reads=[c.krT], writes=[Kr])
            k.dma("sp", Vs[:], c.vA[:, h * 128:(h + 1) * 128].rearrange("(kt p) d -> p kt d", p=128),
                  reads=[c.vA], writes=[Vs])
            Qn = [k.sb(f"ma_qn{i}", [128, TCH], BF16, st) for i in range(2)]
            Qr = [k.sb(f"ma_qr{i}", [128, TCH], BF16, st) for i in range(2)]
            ptb = [k.sb(f"ma_pt{i}", [128, TCH], BF16, st) for i in range(3)]
            rec = [k.sb(f"ma_rec{i}", [128, TCH], F32, st) for i in range(2)]
            ost = [k.sb(f"ma_o{i}", [128, TCH], F32, st) for i in range(2)]
            hs = slice(h * 64, (h + 1) * 64)
            for j in range(NTC):
                b = j % 2
                tsl = slice(j * TCH, (j + 1) * TCH)
                k.dma("sp", Qn[b][:], c.qT[h * 128:(h + 1) * 128, tsl], reads=[c.qT], writes=[Qn[b]])
                k.dma("sp", Qr[b][:], c.qT[256:384, tsl], reads=[c.qT], writes=[Qr[b]])
                parts = [dict(K=lambda kt: Kn[:, kt * 128:(kt + 1) * 128], Q=lambda: Qn[b][:], kd=Kn, qd=Qn[b]),
                         dict(K=lambda kt: Kr[hs, kt * 128:(kt + 1) * 128], Q=lambda: Qr[b][hs, :], kd=Kr, qd=Qr[b])]
                oacc, sacc = c.pf[2 + 2 * b], c.pf[3 + 2 * b]
                attn_qchunk(k, c, j, [parts], lambda kt: Vs[:, kt, :], Vs, 128, scale, c.masks, ptb, [(oacc, sacc)])
                k.op("dve", lambda e: e.reciprocal(out=rec[b][:], in_=sacc[:, :]), reads=[sacc], writes=[rec[b]])
                k.op("dve", lambda e: e.tensor_tensor(out=ost[b][:], in0=oacc[:, :], in1=rec[b][:], op=ALU.mult),
                     reads=[oacc, rec[b]], writes=[ost[b]])
                k.dma("pool", c.mixT[h * 128:(h + 1) * 128, tsl], ost[b][:], reads=[ost[b]], writes=[c.mixT],
                      waw=False)
        k.barrier()


PRM_COLS = {}
_pc = 0


def _reg(name, n):
    global _pc
    PRM_COLS[name] = _pc
    _pc += n


_reg("gq", 4)
_reg("gkv", 2)


def const_mats():
    ident = np.eye(128, dtype=np.float32)
    permA = np.zeros((128, 128), np.float32)
    for r in range(128):
        d = r % 64
        permA[r, r + 32 if d < 32 else r - 32] = 1.0
    permD = np.zeros((128, 128), np.float32)
    for r in range(128):
        d = r % 64
        if d < 8:
            permD[r, r + 8] = 1.0
        elif d < 16:
            permD[r, r - 8] = 1.0
    masks = np.zeros((128, 4, 512), np.float32)
    kk = np.arange(128)[:, None]
    qq = np.arange(512)[None, :]
    for r in range(4):
        masks[:, r, :] = (qq >= 128 * r + kk)
    tmask = np.zeros((128, 128), np.float32)
    ti = np.arange(128) // 16
    tmask[:, :] = (ti[None, :] >= ti[:, None])
    hidx = np.arange(128) // 64
    same = (hidx[:, None] == hidx[None, :]).astype(np.float32)
    blk1 = same.copy()
    blk64 = same / 64.0
    cmat = np.concatenate([ident, permA, permD, tmask, blk1, blk64], axis=1)
    cst = np.zeros((128, 8 + 32 + 512), np.float32)
    invA = (500000.0 ** (-np.arange(0, 64, 2, dtype=np.float32) / np.float32(64))).astype(np.float32)
    invD = (500000.0 ** (-np.arange(0, 16, 2, dtype=np.float32) / np.float32(16))).astype(np.float32)
    for r in range(128):
        d = r % 64
        cst[r, 0] = invA[d % 32]
        cst[r, 1] = -1.0 if d < 32 else 1.0
        cst[r, 2] = invD[d % 8] if d < 16 else 0.0
        cst[r, 3] = (-1.0 if d < 8 else 1.0) if d < 16 else 0.0
    tau = np.zeros(32, np.float32)
    tau[0:8] = -np.arange(8)
    tau[8:17] = np.arange(9)
    tau[17:25] = 7 - np.arange(8)
    cst[:, 8:40] = tau[None, :]
    cst[:, 40:552] = (8.0 * (np.arange(512) + 1))[None, :]
    return cmat, masks.reshape(128, 2048), cst


def const_mats2():
    hidx = np.arange(128) // 64
    idx = np.arange(128) % 64
    same = (hidx[:, None] == hidx[None, :])
    mSL = (same & (idx[:, None] > idx[None, :])).astype(np.float32)
    mSU = (same & (idx[:, None] < idx[None, :])).astype(np.float32)
    mUI = (same & (idx[:, None] <= idx[None, :])).astype(np.float32)
    ident = np.eye(128, dtype=np.float32)
    return np.concatenate([np.tile(mSL, (1, 4)), np.tile(mSU, (1, 4)), np.tile(mUI, (1, 4)), np.tile(ident, (1, 8))], axis=1)


NCST = 552


def load_all_consts(k, c, cmat_d, cmask_d, cst_d, cm2_d=None):
    c.ident = k.sb("c_ident", [128, 128], BF16)
    c.permA = k.sb("c_permA", [128, 128], BF16)
    c.permD = k.sb("c_permD", [128, 128], BF16)
    c.ones = k.sb("c_ones", [128, 128], BF16)
    c.cst = k.sb("c_cst", [128, NCST], F32)
    c.tmask = k.sb("c_tmask", [128, 128], F32)
    c.blk1 = k.sb("c_blk1", [128, 128], BF16)
    c.blk64 = k.sb("c_blk64", [128, 128], BF16)
    c.mSL = k.sb("c_mSL", [128, 512], BF16)
    c.mSU = k.sb("c_mSU", [128, 512], BF16)
    c.mUI = k.sb("c_mUI", [128, 512], BF16)
    c.ident8 = k.sb("c_ident8", [128, 1024], BF16)
    c.masks = [k.sb(f"c_mask{r}", [128, 512], BF16) for r in range(4)]
    with ExitStack() as st:
        f = k.sb("lc_f", [128, 768], F32, st)
        m2 = k.sb("lc_m2", [128, 2560], F32, st)
        m = k.sb("lc_m", [128, 2048], F32, st)
        k.dma("sp", f[:], cmat_d[:, :], writes=[f])
        k.dma("sp", m[:], cmask_d[:, :], writes=[m])
        k.dma("sp", c.cst[:], cst_d[:, :], writes=[c.cst])
        k.op("dve", lambda e: e.tensor_copy(out=c.ident[:], in_=f[:, 0:128]), reads=[f], writes=[c.ident])
        k.op("dve", lambda e: e.tensor_copy(out=c.permA[:], in_=f[:, 128:256]), reads=[f], writes=[c.permA])
        k.op("dve", lambda e: e.tensor_copy(out=c.permD[:], in_=f[:, 256:384]), reads=[f], writes=[c.permD])
        k.op("dve", lambda e: e.tensor_copy(out=c.tmask[:], in_=f[:, 384:512]), reads=[f], writes=[c.tmask])
        k.op("dve", lambda e: e.tensor_copy(out=c.blk1[:], in_=f[:, 512:640]), reads=[f], writes=[c.blk1])
        k.op("dve", lambda e: e.tensor_copy(out=c.blk64[:], in_=f[:, 640:768]), reads=[f], writes=[c.blk64])
        if cm2_d is not None:
            k.dma("sp", m2[:], cm2_d[:, :], writes=[m2])
            k.op("dve", lambda e: e.tensor_copy(out=c.mSL[:], in_=m2[:, 0:512]), reads=[m2], writes=[c.mSL])
            k.op("dve", lambda e: e.tensor_copy(out=c.mSU[:], in_=m2[:, 512:1024]), reads=[m2], writes=[c.mSU])
            k.op("dve", lambda e: e.tensor_copy(out=c.mUI[:], in_=m2[:, 1024:1536]), reads=[m2], writes=[c.mUI])
            k.op("dve", lambda e: e.tensor_copy(out=c.ident8[:], in_=m2[:, 1536:2560]), reads=[m2], writes=[c.ident8])
        k.op("dve", lambda e: e.memset(c.ones[:], 1.0), writes=[c.ones])
        for r in range(4):
            k.op("dve", lambda e: e.tensor_copy(out=c.masks[r][:], in_=m[:, r * 512:(r + 1) * 512]),
                 reads=[m], writes=[c.masks[r]])
        k.barrier()


O_CQ, O_CKV, O_KR, O_U, O_Z, O_QD, O_KD, O_VD, O_GATE = 0, 512, 768, 832, 1344, 3008, 3520, 4032, 4544


def s5_gperm(half):
    return np.concatenate([half * 16 + np.arange(16), (1 - half) * 16 + np.arange(16)])


def s5_chperm(half):
    return (s5_gperm(half)[:, None] * 16 + np.arange(16)[None, :]).reshape(-1)


def fm_cols(half):
    a = np.arange
    cols = [O_CQ + a(512), O_CKV + a(256), O_U + s5_chperm(half),
            O_Z + half * 256 + a(256), O_Z + 512 + half * 256 + a(256),
            O_QD + half * 256 + a(256), O_KD + half * 256 + a(256)]
    for b in range(4):
        cols.append(O_GATE + b * 512 + half * 256 + a(256))
    cols += [O_KR + a(64), O_KR + a(64), O_Z + 1536 + a(64), O_Z + 1600 + a(64), O_Z + 1024 + half * 256 + a(256)]
    return np.concatenate(cols)


def tm_cols(half):
    a = np.arange
    return O_VD + half * 256 + a(256)


def pt_layout(v, nt):
    return np.ascontiguousarray(np.asarray(v).reshape(nt, 128).T)


def core_layer_arrays(inp, l, half):
    out = {}
    w_in = inp["w_in"][l]
    out["w_in"] = np.ascontiguousarray(w_in[:, np.concatenate([fm_cols(half), tm_cols(half)])])
    hs = [2 * half, 2 * half + 1]
    wuq = inp["mla_w_uq"][l]
    out["wuq"] = np.ascontiguousarray(np.concatenate(
        [wuq[:, h * 192:h * 192 + 128] for h in hs] + [wuq[:, h * 192 + 128:(h + 1) * 192] for h in hs], axis=1))
    wukv = inp["mla_w_ukv"][l]
    out["wukv"] = np.ascontiguousarray(np.concatenate(
        [wukv[:, h * 256:h * 256 + 128] for h in hs] + [wukv[:, h * 256 + 128:(h + 1) * 256] for h in hs], axis=1))
    prm = np.zeros((128, _pc), np.float32)

    def put(name, arr):
        arr = np.asarray(arr, np.float32)
        if arr.ndim == 1:
            arr = arr[:, None]
        prm[:arr.shape[0], PRM_COLS[name]:PRM_COLS[name] + arr.shape[1]] = arr
    put("gq", pt_layout(inp["mla_q_norm_g"][l], 4))
    put("gkv", pt_layout(inp["mla_kv_norm_g"][l], 2))
    for nm in ("lq1", "lk1", "lq2", "lk2"):
        put(nm, np.broadcast_to(inp["diff_" + nm][l][None, :], (128, 64)))
    put("gsub", inp["diff_subln_g"][l])
    lam_init = 0.8 - 0.6 * math.exp(-0.3 * l)
    put("lam_init", np.full((128,), lam_init, np.float32))
    put("omlam", np.full((128,), 1.0 - lam_init, np.float32))
    fill_more(inp, l, half, put, out)
    out["prm"] = prm
    return out


def st_layout(a):
    a = np.asarray(a)
    rest = a.shape[2:]
    a = a.reshape((16, 128) + rest)
    return np.ascontiguousarray(np.moveaxis(a, 0, 1))


def fill_more(inp, l, half, put, out=None):
    gp = s5_gperm(half)
    chp = s5_chperm(half)
    put("s5_are", st_layout(inp["s5_a_re"][l][gp]))
    put("s5_aim", st_layout(inp["s5_a_im"][l][gp]))
    put("s5_ldt", st_layout(np.broadcast_to(inp["s5_log_dt"][l][gp][:, None], (32, 64))))
    put("s5_d", pt_layout(inp["s5_d"][l][chp], 4))
    put("s5_bg", pt_layout(inp["s5_b_glu"][l][half * 256:(half + 1) * 256], 2))
    mu = inp["rwkv_mu"][l]
    my = slice(half * 256, (half + 1) * 256)
    put("rw_mur", pt_layout(mu[0:512][my], 2))
    put("rw_muk", pt_layout(mu[512:1024][my], 2))
    put("rw_muv", pt_layout(mu[1024:1536][my], 2))
    put("rw_mul", mu[1536:1664])
    put("rw_w0", pt_layout(inp["rwkv_w0"][l][my], 2))
    put("rw_a0", pt_layout(inp["rwkv_a0"][l][my], 2))
    put("rw_kk", pt_layout(inp["rwkv_k_k"][l][my], 2))
    put("rw_ka", pt_layout(inp["rwkv_k_a"][l][my], 2))
    put("rw_rk", pt_layout(inp["rwkv_r_k"][l].reshape(512)[my], 2))
    put("rw_lng", pt_layout(inp["rwkv_ln_g"][l][my], 2))
    put("rw_lnb", pt_layout(inp["rwkv_ln_b"][l][my], 2))
    if out is not None:
        out["w2a2"] = np.ascontiguousarray(np.concatenate([inp["rwkv_w2"][l][:, my], inp["rwkv_a2"][l][:, my]], axis=0))
        b = np.stack([inp["s5_b_re"][l][gp], inp["s5_b_im"][l][gp]], axis=2)
        out["s5b"] = st_layout(b).reshape(128, 512).astype(np.float32)
        cc = np.stack([np.swapaxes(inp["s5_c_re"][l][gp], 1, 2), np.swapaxes(inp["s5_c_im"][l][gp], 1, 2)], axis=2)
        out["s5c"] = st_layout(cc).reshape(128, 512).astype(np.float32)
        out["wglu"] = np.ascontiguousarray(inp["s5_w_glu"][l][chp][:, half * 256:(half + 1) * 256])


_reg("lq1", 64)
_reg("lk1", 64)
_reg("lq2", 64)
_reg("lk2", 64)
_reg("gsub", 1)
_reg("lam_init", 1)
_reg("omlam", 1)
DIFF_EPS = 1e-5


def stage_diff(k, c, L):
    pT, pV = c.pT, c.pV
    with ExitStack() as st:
        rp = RopeCtx(k, c, st, "dr", c.ropeD_cos, c.ropeD_sin, c.permD)
        xin = [k.sb(f"dr_in{i}", [128, TCH], F32, st) for i in range(3)]
        stg = Stager(k, st, "dr_o", [128, TCH], BF16, 4)
        n = 0
        for tc in range(NTC):
            tsl = slice(tc * TCH, (tc + 1) * TCH)
            rp.load(tc)
            for (r0, dst) in ((R_QD, c.qdT), (R_KD, c.kdT)):
                for hd in range(2):
                    xi = xin[n % 3]
                    n += 1
                    k.dma("sp", xi[:], pT[r0 + hd * 128:r0 + (hd + 1) * 128, tsl], reads=[pT], writes=[xi])
                    s = stg.next()
                    rp.apply(xi[:], xi, s[:, :], s)
                    k.dma("pool", dst[hd * 128:(hd + 1) * 128, tsl], s[:, :], reads=[s], writes=[dst], waw=False)
    k.barrier()
    with ExitStack() as st0:
        sm = k.sb("df_sm", [128, 8], F32, st0)
        tmp = k.sb("df_tmp", [128, 64], F32, st0)
        cq1, ck1, cq2, ck2, cg = (L.col[n_] for n_ in ("lq1", "lk1", "lq2", "lk2", "gsub"))
        for i, (a, b_) in enumerate(((cq1, ck1), (cq2, ck2))):
            k.op("dve", lambda e: e.tensor_tensor(out=tmp[:], in0=L.prm[:, a:a + 64], in1=L.prm[:, b_:b_ + 64],
                                                  op=ALU.mult), reads=[L.prm], writes=[tmp])
            k.op("dve", lambda e: e.reduce_sum(out=sm[:, i:i + 1], in_=tmp[:], axis=AX.X), reads=[tmp], writes=[sm])
            k.op("act", lambda e: e.activation(out=sm[:, 2 + i:3 + i], in_=sm[:, i:i + 1], func=AF.Exp),
                 reads=[sm], writes=[sm])
        k.op("dve", lambda e: e.tensor_tensor(out=sm[:, 4:5], in0=sm[:, 3:4], in1=sm[:, 2:3], op=ALU.subtract),
             reads=[sm], writes=[sm])
        cli, col_ = L.col["lam_init"], L.col["omlam"]
        k.op("dve", lambda e: e.tensor_tensor(out=sm[:, 5:6], in0=sm[:, 4:5], in1=L.prm[:, cli:cli + 1], op=ALU.subtract),
             reads=[sm, L.prm], writes=[sm])
        k.op("dve", lambda e: e.tensor_tensor(out=sm[:, 6:7], in0=L.prm[:, cg:cg + 1], in1=L.prm[:, col_:col_ + 1], op=ALU.mult),
             reads=[L.prm], writes=[sm])
        nlam = sm[:, 5:6]
        gs = sm[:, 6:7]
        scale = 64 ** -0.5
        for hd in range(2):
            with ExitStack() as st:
                Kd = k.sb("da_k", [128, S], BF16, st)
                Vf = k.sb("da_vf", [128, S // 128, 128], F32, st)
                Vs = k.sb("da_v", [128, S // 128, 128], BF16, st)
                k.dma("sp", Kd[:], c.kdT[hd * 128:(hd + 1) * 128, :], reads=[c.kdT], writes=[Kd])
                k.dma("sp", Vf[:], pV[:, hd * 128:(hd + 1) * 128].rearrange("(kt p) d -> p kt d", p=128),
                      reads=[pV], writes=[Vf])
                k.op("pool", lambda e: e.tensor_copy(out=Vs[:], in_=Vf[:]), reads=[Vf], writes=[Vs])
                Qd = [k.sb(f"da_q{i}", [128, TCH], BF16, st) for i in range(2)]
                ptb = [k.sb(f"da_pt{i}", [128, TCH], BF16, st) for i in range(3)]
                rec = k.sb("da_rec", [128, TCH], F32, st)
                o1 = k.sb("da_o1", [128, TCH], F32, st)
                o2 = k.sb("da_o2", [128, TCH], F32, st)
                sq = k.sb("da_sq", [128, TCH], BF16, st)
                rs = k.sb("da_rs", [128, TCH], F32, st)
                ost = [k.sb(f"da_o{i}", [128, TCH], F32, st) for i in range(2)]
                for j in range(NTC):
                    b = j % 2
                    tsl = slice(j * TCH, (j + 1) * TCH)
                    k.dma("sp", Qd[b][:], c.qdT[hd * 128:(hd + 1) * 128, tsl], reads=[c.qdT], writes=[Qd[b]])
                    maps = []
                    for m in range(2):
                        ms = slice(m * 64, (m + 1) * 64)
                        maps.append([dict(K=(lambda kt, ms=ms: Kd[ms, kt * 128:(kt + 1) * 128]),
                                          Q=(lambda ms=ms: Qd[b][ms, :]), kd=Kd, qd=Qd[b])])
                    accs = [(c.pf[2], c.pf[3]), (c.pf[4], c.pf[5])]
                    attn_qchunk(k, c, j, maps, lambda kt: Vs[:, kt, :], Vs, 128, scale, c.masks, ptb, accs)
                    (oa1, sa1), (oa2, sa2) = accs
                    k.op("dve", lambda e: e.reciprocal(out=rec[:], in_=sa1[:, :]), reads=[sa1], writes=[rec])
                    k.op("dve", lambda e: e.tensor_tensor(out=o1[:], in0=oa1[:, :], in1=rec[:], op=ALU.mult),
                         reads=[oa1, rec], writes=[o1])
                    k.op("dve", lambda e: e.reciprocal(out=rec[:], in_=sa2[:, :]), reads=[sa2], writes=[rec])
                    k.op("dve", lambda e: e.tensor_tensor(out=o2[:], in0=oa2[:, :], in1=rec[:], op=ALU.mult),
                         reads=[oa2, rec], writes=[o2])
                    k.op("dve", lambda e: e.scalar_tensor_tensor(out=o1[:], in0=o2[:], scalar=nlam, in1=o1[:],
                                                                 op0=ALU.mult, op1=ALU.add),
                         reads=[o2, o1, sm], writes=[o1])
                    k.op("act", lambda e: e.activation(out=sq[:], in_=o1[:], func=AF.Square), reads=[o1], writes=[sq])
                    ps = c.next_pf(0, 2)
                    k.op("pe", lambda e: e.matmul(ps[:, :], lhsT=c.ones[:], rhs=sq[:], start=True, stop=True),
                         reads=[c.ones, sq], writes=[ps])
                    k.op("dve", lambda e: e.tensor_scalar(out=rs[:], in0=ps[:, :], scalar1=1.0 / 128, scalar2=DIFF_EPS,
                                                          op0=ALU.mult, op1=ALU.add), reads=[ps], writes=[rs])
                    k.op("act", lambda e: e.activation(out=rs[:], in_=rs[:], func=AF.Sqrt), reads=[rs], writes=[rs])
                    k.op("dve", lambda e: e.reciprocal(out=rs[:], in_=rs[:]), reads=[rs], writes=[rs])
                    k.op("dve", lambda e: e.scalar_tensor_tensor(out=ost[b][:], in0=o1[:], scalar=gs, in1=rs[:],
                                                                 op0=ALU.mult, op1=ALU.mult),
                         reads=[o1, rs, sm], writes=[ost[b]])
                    k.dma("pool", c.mixT[768 + hd * 128:768 + (hd + 1) * 128, tsl], ost[b][:], reads=[ost[b]],
                          writes=[c.mixT], waw=False)
            k.barrier()


_reg("s5_are", 16)
_reg("s5_aim", 16)
_reg("s5_ldt", 16)
_reg("s5_d", 4)
_reg("s5_bg", 2)
Z_NEG0, Z_POS0, Z_REV0 = 0, 8, 17
GELU_C = 1.5957691216057308


def sincos_alloc(k, st, name, shape):
    return dict(ni=k.sb(name + "_ni", shape, I32, st), nf=k.sb(name + "_nf", shape, F32, st),
                a2=k.sb(name + "_a2", shape, F32, st), r=k.sb(name + "_r", shape, F32, st),
                m=k.sb(name + "_m", shape, F32, st))


def sincos_tile(k, tmp, ang, sin_out, cos_out):
    ni, nf, a2, r, m = tmp["ni"], tmp["nf"], tmp["a2"], tmp["r"], tmp["m"]
    k.op("dve", lambda e: e.tensor_scalar(out=ni[:], in0=ang[:], scalar1=float(1.0 / TWO_PI), scalar2=None,
                                          op0=ALU.mult), reads=[ang], writes=[ni])
    k.op("dve", lambda e: e.tensor_copy(out=nf[:], in_=ni[:]), reads=[ni], writes=[nf])
    k.op("dve", lambda e: e.scalar_tensor_tensor(out=a2[:], in0=nf[:], scalar=-CW1, in1=ang[:], op0=ALU.mult,
                                                 op1=ALU.add), reads=[nf, ang], writes=[a2])
    k.op("dve", lambda e: e.scalar_tensor_tensor(out=a2[:], in0=nf[:], scalar=-CW2, in1=a2[:], op0=ALU.mult,
                                                 op1=ALU.add), reads=[nf, a2], writes=[a2])
    k.op("dve", lambda e: e.scalar_tensor_tensor(out=a2[:], in0=nf[:], scalar=-CW3, in1=a2[:], op0=ALU.mult,
                                                 op1=ALU.add), reads=[nf, a2], writes=[a2])
    for shift, dst in ((0.0, sin_out), (np.pi / 2, cos_out)):
        k.op("dve", lambda e: e.tensor_scalar(out=r[:], in0=a2[:], scalar1=float(shift), scalar2=None, op0=ALU.add),
             reads=[a2], writes=[r])
        k.op("dve", lambda e: e.tensor_single_scalar(out=m[:], in_=r[:], scalar=float(np.pi), op=ALU.is_gt),
             reads=[r], writes=[m])
        k.op("dve", lambda e: e.scalar_tensor_tensor(out=r[:], in0=m[:], scalar=-TWO_PI, in1=r[:], op0=ALU.mult,
                                                     op1=ALU.add), reads=[m, r], writes=[r])
        k.op("dve", lambda e: e.tensor_single_scalar(out=m[:], in_=r[:], scalar=float(-np.pi), op=ALU.is_lt),
             reads=[r], writes=[m])
        k.op("dve", lambda e: e.scalar_tensor_tensor(out=r[:], in0=m[:], scalar=TWO_PI, in1=r[:], op0=ALU.mult,
                                                     op1=ALU.add), reads=[m, r], writes=[r])
        k.op("dve", lambda e: e.tensor_scalar(out=r[:], in0=r[:], scalar1=float(np.pi), scalar2=float(-np.pi),
                                              op0=ALU.min, op1=ALU.max), reads=[r], writes=[r])
        k.op("act", lambda e: e.activation(out=dst[:], in_=r[:], func=AF.Sin), reads=[r], writes=[dst])


def stage_s5(k, c, L):
    import os as _os
    NT = 16
    pT = c.pT
    SH4 = [128, NT, 8, 16]
    with ExitStack() as stA:
        Tm = k.sb("s5_Tm", [128, 32, 128], BF16, stA)
        GstR = k.sb("s5_GstR", [128, NT, 128], BF16, stA)
        GstI = k.sb("s5_GstI", [128, NT, 128], BF16, stA)
        EfR = k.sb("s5_EfR", SH4, BF16, stA)
        EfnI = k.sb("s5_EfnI", SH4, BF16, stA)
        sc = k.sb("s5_sc", [128, 8, NT], F32, stA)
        LR, DT, ML, TH, MAG8, FRE, FIM, TMP = range(8)
        ca, ci_, cl = L.col["s5_are"], L.col["s5_aim"], L.col["s5_ldt"]
        AIM = L.prm[:, ci_:ci_ + NT]
        with ExitStack() as st:
            bsb = k.sb("s5_b", [128, NT, 2, 16], F32, st)
            csb = k.sb("s5_c", [128, NT, 2, 16], F32, st)
            k.dma("sp", bsb[:], L.s5b[:, :].rearrange("p (t r c) -> p t r c", t=NT, r=2), writes=[bsb])
            k.dma("sp", csb[:], L.s5c[:, :].rearrange("p (t r c) -> p t r c", t=NT, r=2), writes=[csb])
            k.op("dve", lambda e: e.tensor_scalar(out=sc[:, LR, :], in0=L.prm[:, ca:ca + NT], scalar1=-1e-4,
                                                  scalar2=None, op0=ALU.min), reads=[L.prm], writes=[sc])
            k.op("act", lambda e: e.activation(out=sc[:, DT, :], in_=L.prm[:, cl:cl + NT], func=AF.Exp),
                 reads=[L.prm], writes=[sc])
            k.op("dve", lambda e: e.tensor_tensor(out=sc[:, ML, :], in0=sc[:, DT, :], in1=sc[:, LR, :], op=ALU.mult),
                 reads=[sc], writes=[sc])
            k.op("dve", lambda e: e.tensor_tensor(out=sc[:, TH, :], in0=sc[:, DT, :], in1=AIM, op=ALU.mult),
                 reads=[sc, L.prm], writes=[sc])
            k.op("act", lambda e: e.activation(out=sc[:, MAG8, :], in_=sc[:, ML, :], func=AF.Exp, scale=8.0),
                 reads=[sc], writes=[sc])
            SH3 = [128, NT, 32]
            lm = k.sb("s5_lm", SH3, F32, st)
            an = k.sb("s5_an", SH3, F32, st)
            mg = k.sb("s5_mg", SH3, F32, st)
            sn = k.sb("s5_sn", SH3, F32, st)
            cs = k.sb("s5_cs", SH3, F32, st)
            zr = k.sb("s5_zr", SH3, F32, st)
            zi = k.sb("s5_zi", SH3, F32, st)
            tauB = c.cst[:, 8:40].unsqueeze(1).to_broadcast(SH3)
            k.op("dve", lambda e: e.tensor_tensor(out=lm[:], in0=sc[:, ML, :].unsqueeze(2).to_broadcast(SH3), in1=tauB,
                                                  op=ALU.mult), reads=[sc, c.cst], writes=[lm])
            k.op("dve", lambda e: e.tensor_tensor(out=an[:], in0=sc[:, TH, :].unsqueeze(2).to_broadcast(SH3), in1=tauB,
                                                  op=ALU.mult), reads=[sc, c.cst], writes=[an])
            k.op("act", lambda e: e.activation(out=mg[:], in_=lm[:], func=AF.Exp), reads=[lm], writes=[mg])
            sincos_tile(k, sincos_alloc(k, st, "s5sc0", SH3), an, sn, cs)
            k.op("dve", lambda e: e.tensor_tensor(out=zr[:], in0=mg[:], in1=cs[:], op=ALU.mult), reads=[mg, cs], writes=[zr])
            k.op("dve", lambda e: e.tensor_tensor(out=zi[:], in0=mg[:], in1=sn[:], op=ALU.mult), reads=[mg, sn], writes=[zi])
            if _os.environ.get("S5_STOP") == "a":
                k.barrier()
                return
            sm = k.sb("s5_sm", [128, 8, NT], F32, st)
            abr, abi = zr[:, :, Z_POS0 + 1], zi[:, :, Z_POS0 + 1]
            lr_ = sc[:, LR, :]

            def tt(out, a, b, op, rd, wr, E="dve"):
                k.op(E, lambda e: e.tensor_tensor(out=out, in0=a, in1=b, op=op), reads=rd, writes=wr)
            tt(sm[:, 0, :], lr_, lr_, ALU.mult, [sc], [sm])
            tt(sm[:, 1, :], AIM, AIM, ALU.mult, [L.prm], [sm])
            tt(sm[:, 0, :], sm[:, 0, :], sm[:, 1, :], ALU.add, [sm], [sm])
            k.op("dve", lambda e: e.reciprocal(out=sm[:, 0, :], in_=sm[:, 0, :]), reads=[sm], writes=[sm])
            k.op("dve", lambda e: e.tensor_scalar(out=sm[:, 1, :], in0=abr, scalar1=-1.0, scalar2=None, op0=ALU.add),
                 reads=[zr], writes=[sm])
            tt(sm[:, 2, :], sm[:, 1, :], lr_, ALU.mult, [sm, sc], [sm])
            tt(sm[:, 3, :], abi, AIM, ALU.mult, [zi, L.prm], [sm])
            tt(sm[:, 2, :], sm[:, 2, :], sm[:, 3, :], ALU.add, [sm], [sm])
            tt(sc[:, FRE, :], sm[:, 2, :], sm[:, 0, :], ALU.mult, [sm], [sc])
            tt(sm[:, 4, :], abi, lr_, ALU.mult, [zi, sc], [sm])
            tt(sm[:, 5, :], sm[:, 1, :], AIM, ALU.mult, [sm, L.prm], [sm])
            tt(sm[:, 4, :], sm[:, 4, :], sm[:, 5, :], ALU.subtract, [sm], [sm])
            tt(sc[:, FIM, :], sm[:, 4, :], sm[:, 0, :], ALU.mult, [sm], [sc])
            if _os.environ.get("S5_STOP") == "b":
                k.barrier()
                return
            SHB = [128, NT, 16]
            bbr = k.sb("s5_bbr", SHB, F32, st)
            bbi = k.sb("s5_bbi", SHB, F32, st)
            t1 = k.sb("s5_t1", SHB, F32, st)
            fre = sc[:, FRE, :].unsqueeze(2).to_broadcast(SHB)
            fim = sc[:, FIM, :].unsqueeze(2).to_broadcast(SHB)
            br_, bi_ = bsb[:, :, 0, :], bsb[:, :, 1, :]
            tt(bbr[:], fre, br_, ALU.mult, [sc, bsb], [bbr])
            tt(t1[:], fim, bi_, ALU.mult, [sc, bsb], [t1])
            tt(bbr[:], bbr[:], t1[:], ALU.subtract, [bbr, t1], [bbr])
            tt(bbi[:], fre, bi_, ALU.mult, [sc, bsb], [bbi])
            tt(t1[:], fim, br_, ALU.mult, [sc, bsb], [t1])
            tt(bbi[:], bbi[:], t1[:], ALU.add, [bbi, t1], [bbi])
            if _os.environ.get("S5_STOP") == "c":
                k.barrier()
                return
            BfR = k.sb("s5_BfR", SH4, BF16, st)
            BfnI = k.sb("s5_BfnI", SH4, BF16, st)
            CfR = k.sb("s5_CfR", SH4, BF16, st)
            CfI = k.sb("s5_CfI", SH4, BF16, st)
            GfR = k.sb("s5_GfR", SH4, BF16, st)
            GfI = k.sb("s5_GfI", SH4, BF16, st)
            u1 = [k.sb(f"s5_u1{i}", SH4, F32, st) for i in range(2)]
            u2 = [k.sb(f"s5_u2{i}", SH4, F32, st) for i in range(2)]

            def cmul(oR, oI, z0, xr, xi, xdeps, neg_im, i):
                E = "dve" if i % 2 == 0 else "pool"
                zR = zr[:, :, z0:z0 + 8].unsqueeze(3).to_broadcast(SH4)
                zI = zi[:, :, z0:z0 + 8].unsqueeze(3).to_broadcast(SH4)
                xR = xr.unsqueeze(2).to_broadcast(SH4)
                xI = xi.unsqueeze(2).to_broadcast(SH4)
                a, b_ = u1[i % 2], u2[i % 2]
                tt(a[:], zR, xR, ALU.mult, [zr, ] + xdeps, [a], E)
                tt(b_[:], zI, xI, ALU.mult, [zi, ] + xdeps, [b_], E)
                tt(oR[:], a[:], b_[:], ALU.subtract, [a, b_], [oR], E)
                tt(a[:], zR, xI, ALU.mult, [zr, ] + xdeps, [a], E)
                tt(b_[:], zI, xR, ALU.mult, [zi, ] + xdeps, [b_], E)
                if neg_im:
                    k.op("dve", lambda e: e.scalar_tensor_tensor(out=oI[:], in0=a[:], scalar=-1.0, in1=b_[:],
                                                                 op0=ALU.mult, op1=ALU.subtract),
                         reads=[a, b_], writes=[oI])
                else:
                    tt(oI[:], a[:], b_[:], ALU.add, [a, b_], [oI], E)
            cr_, ci2 = csb[:, :, 0, :], csb[:, :, 1, :]
            cmul(BfR, BfnI, Z_NEG0, bbr[:], bbi[:], [bbr, bbi], True, 0)
            cmul(CfR, CfI, Z_POS0, cr_, ci2, [csb], False, 1)
            cmul(GfR, GfI, Z_REV0, bbr[:], bbi[:], [bbr, bbi], False, 0)
            cmul(EfR, EfnI, Z_POS0 + 1, cr_, ci2, [csb], True, 1)
            if _os.environ.get("S5_STOP") == "d":
                k.barrier()
                return
            for t4 in range(4):
                for hh in range(2):
                    ps = c.next_pf()
                    hs = slice(hh * 64, (hh + 1) * 64)
                    for q in range(4):
                        t = t4 * 4 + q
                        k.op("pe", lambda e: e.matmul(ps[:, q * 128:(q + 1) * 128],
                                                      lhsT=BfR[hs, t, :, :].rearrange("p a b -> p (a b)"),
                                                      rhs=CfR[hs, t, :, :].rearrange("p a b -> p (a b)"), start=True, stop=False),
                             reads=[BfR, CfR], writes=[ps], inc=False)
                        k.op("pe", lambda e: e.matmul(ps[:, q * 128:(q + 1) * 128],
                                                      lhsT=BfnI[hs, t, :, :].rearrange("p a b -> p (a b)"),
                                                      rhs=CfI[hs, t, :, :].rearrange("p a b -> p (a b)"), start=False, stop=True),
                             reads=[BfnI, CfI], writes=[ps], inc=(q == 3))
                    for q in range(4):
                        g = 2 * (t4 * 4 + q) + hh
                        k.op("dve", lambda e: e.tensor_tensor(out=Tm[:, g, :], in0=ps[:, q * 128:(q + 1) * 128],
                                                              in1=c.tmask[:], op=ALU.mult),
                             reads=[ps, c.tmask], writes=[Tm])
            if _os.environ.get("S5_STOP") == "e":
                k.barrier()
                return
            for (src, dst) in ((GfR, GstR), (GfI, GstI)):
                for t8 in range(2):
                    pb = c.next_pb()
                    for q in range(8):
                        t = t8 * 8 + q
                        k.op("pe", lambda e: e.transpose(pb[:, q * 128:(q + 1) * 128],
                                                         src[:, t, :, :].rearrange("p a b -> p (a b)"), c.ident[:]),
                             reads=[src, c.ident], writes=[pb], inc=(q == 7))
                    k.op("act", lambda e: e.copy(out=dst[:, t8 * 8:(t8 + 1) * 8, :],
                                                 in_=pb[:].rearrange("p (q n) -> p q n", q=8)),
                         reads=[pb], writes=[dst])
            k.barrier()
        import os as _os
        if _os.environ.get("S5_STOP") == "1":
            return
        with ExitStack() as st:
            ug = [[k.sb(f"s5_u{i}{j}", [128, TCH], BF16, st) for j in range(2)] for i in range(2)]
            ang = k.sb("s5_ang", [128, TCH], F32, st)
            sn = k.sb("s5_snj", [128, TCH], F32, st)
            cs = k.sb("s5_csj", [128, TCH], F32, st)
            a_ = k.sb("s5_a", [128, TCH], F32, st)
            b_ = k.sb("s5_bq", [128, TCH], F32, st)
            mre = k.sb("s5_mre", [128, TCH], F32, st)
            mim = k.sb("s5_mim", [128, TCH], F32, st)
            hre = k.sb("s5_hre", [128, TCH], F32, st)
            him = k.sb("s5_him", [128, TCH], F32, st)
            Hp = [[k.sb(f"s5_Hp{i}{j}", [128, TCH + 1], BF16, st) for j in range(2)] for i in range(2)]
            yst = [k.sb(f"s5_y{i}", [128, TCH], F32, st) for i in range(3)]
            for i in range(2):
                for j in range(2):
                    k.op("pool", lambda e: e.memset(Hp[i][j][:, 0:1], 0.0), writes=[Hp[i][j]])
            sctmp = sincos_alloc(k, st, "s5scj", [128, TCH])
            jv = c.cst[:, 40:552]
            nyi = 0

            def tt(out, a, b, op, rd, wr, E="dve"):
                k.op(E, lambda e: e.tensor_tensor(out=out, in0=a, in1=b, op=op), reads=rd, writes=wr)
            if True:
                for t in range(NT):
                    pp = t % 2
                    for hh in range(2):
                        k.dma("sp", ug[pp][hh][:], c.us5[2 * t + hh, :, :], reads=[c.us5], writes=[ug[pp][hh]])
                    Xre, Xim = c.pf[0], c.pf[1]
                    for hh in range(2):
                        hs = slice(hh * 64, (hh + 1) * 64)
                        k.op("pe", lambda e: e.matmul(Xre[hs, :], lhsT=GstR[:, t, hs], rhs=ug[pp][hh][:], start=True, stop=True),
                             reads=[GstR, ug[pp][hh]], writes=[Xre], inc=(hh == 1))
                    for hh in range(2):
                        hs = slice(hh * 64, (hh + 1) * 64)
                        k.op("pe", lambda e: e.matmul(Xim[hs, :], lhsT=GstI[:, t, hs], rhs=ug[pp][hh][:], start=True, stop=True),
                             reads=[GstI, ug[pp][hh]], writes=[Xim], inc=(hh == 1))
                    k.op("pool", lambda e: e.tensor_scalar(out=ang[:], in0=jv, scalar1=sc[:, TH, t:t + 1], scalar2=None,
                                                           op0=ALU.mult), reads=[c.cst, sc], writes=[ang])
                    sincos_tile(k, sctmp, ang, sn, cs)
                    tt(a_[:], Xre[:, :], cs[:], ALU.mult, [Xre, cs], [a_])
                    tt(b_[:], Xim[:, :], sn[:], ALU.mult, [Xim, sn], [b_])
                    tt(mre[:], a_[:], b_[:], ALU.add, [a_, b_], [mre], "pool")
                    tt(a_[:], Xim[:, :], cs[:], ALU.mult, [Xim, cs], [a_])
                    tt(b_[:], Xre[:, :], sn[:], ALU.mult, [Xre, sn], [b_])
                    tt(mim[:], a_[:], b_[:], ALU.subtract, [a_, b_], [mim], "pool")
                    m8 = sc[:, MAG8, t:t + 1].to_broadcast([128, TCH])
                    k.op("dve", lambda e: e.tensor_tensor_scan(out=hre[:], data0=m8, data1=mre[:], initial=0.0,
                                                               op0=ALU.mult, op1=ALU.add), reads=[sc, mre], writes=[hre])
                    k.op("dve", lambda e: e.tensor_tensor_scan(out=him[:], data0=m8, data1=mim[:], initial=0.0,
                                                               op0=ALU.mult, op1=ALU.add), reads=[sc, mim], writes=[him])
                    hr, hi_ = Hp[pp][0], Hp[pp][1]
                    tt(a_[:], hre[:], cs[:], ALU.mult, [hre, cs], [a_])
                    tt(b_[:], him[:], sn[:], ALU.mult, [him, sn], [b_], "pool")
                    tt(hr[:, 1:TCH + 1], a_[:], b_[:], ALU.subtract, [a_, b_], [hr])
                    tt(mre[:], hre[:], sn[:], ALU.mult, [hre, sn], [mre])
                    tt(mim[:], him[:], cs[:], ALU.mult, [him, cs], [mim], "pool")
                    tt(hi_[:, 1:TCH + 1], mre[:], mim[:], ALU.add, [mre, mim], [hi_])
                    for hh in range(2):
                        g = 2 * t + hh
                        hs = slice(hh * 64, (hh + 1) * 64)
                        py = c.next_pf(2, 6)
                        k.op("pe", lambda e: e.matmul(py[:, :], lhsT=Tm[:, g, :], rhs=ug[pp][hh][:], start=True, stop=False),
                             reads=[Tm, ug[pp][hh]], writes=[py], inc=False)
                        k.op("pe", lambda e: e.matmul(py[:, :], lhsT=EfR[hs, t, :, :].rearrange("p a b -> p (a b)"),
                                                      rhs=hr[hs, 0:TCH], start=False, stop=False),
                             reads=[EfR, hr], writes=[py], inc=False)
                        k.op("pe", lambda e: e.matmul(py[:, :], lhsT=EfnI[hs, t, :, :].rearrange("p a b -> p (a b)"),
                                                      rhs=hi_[hs, 0:TCH], start=False, stop=True),
                             reads=[EfnI, hi_], writes=[py], inc=True)
                        ys = yst[nyi % 3]
                        nyi += 1
                        k.op("act", lambda e: e.copy(out=ys[:], in_=py[:, :]), reads=[py], writes=[ys])
                        k.dma("pool", c.ys5[g, :, :], ys[:], reads=[ys], writes=[c.ys5], waw=False)
    k.barrier()
    if _os.environ.get("S5_STOP") == "2":
        return
    cd = L.col["s5_d"]
    with ExitStack() as st:
        Y = k.sb("s5p_Y", [128, 8, TCH], F32, st)
        U = k.sb("s5p_U", [128, S], F32, st)
        yy = k.sb("s5p_yy", [128, S], F32, st)
        q1 = k.sb("s5p_q1", [128, S], F32, st)
        gb = k.sb("s5p_gb", [128, S], BF16, st)
        for ct in range(4):
            for gl in range(8):
                k.dma("sp", Y[gl * 16:(gl + 1) * 16, :, :],
                      c.ys5[ct * 8 + gl, :, :].rearrange("(t c) j -> c t j", c=16), reads=[c.ys5], writes=[Y],
                      waw=(gl == 0))
            k.dma("sp", U[:], pT[R_U + ct * 128:R_U + (ct + 1) * 128, :], reads=[pT], writes=[U])
            k.op("dve", lambda e: e.scalar_tensor_tensor(out=yy[:].rearrange("p (j t) -> p j t", t=8),
                                                         in0=U[:].rearrange("p (j t) -> p j t", t=8),
                                                         scalar=L.prm[:, cd + ct:cd + ct + 1],
                                                         in1=Y[:].rearrange("p t j -> p j t"),
                                                         op0=ALU.mult, op1=ALU.add), reads=[U, Y, L.prm], writes=[yy])
            k.op("act", lambda e: e.activation(out=q1[:], in_=yy[:], func=AF.Square), reads=[yy], writes=[q1])
            k.op("dve", lambda e: e.tensor_scalar(out=q1[:], in0=q1[:], scalar1=0.044715, scalar2=1.0, op0=ALU.mult,
                                                  op1=ALU.add), reads=[q1], writes=[q1])
            k.op("pool", lambda e: e.tensor_tensor(out=q1[:], in0=q1[:], in1=yy[:], op=ALU.mult), reads=[q1, yy], writes=[q1])
            k.op("act", lambda e: e.activation(out=q1[:], in_=q1[:], func=AF.Sigmoid, scale=GELU_C), reads=[q1], writes=[q1])
            k.op("dve", lambda e: e.tensor_tensor(out=yy[:], in0=yy[:], in1=q1[:], op=ALU.mult), reads=[yy, q1], writes=[yy])
            k.op("pool", lambda e: e.tensor_copy(out=gb[:], in_=yy[:]), reads=[yy], writes=[gb])
            k.dma("pool", c.gT[ct * 128:(ct + 1) * 128, :], gb[:], reads=[gb], writes=[c.gT], waw=False)
            if ct < 2:
                k.dma("pool", c.gF[ct * 128:(ct + 1) * 128, :], yy[:], reads=[yy], writes=[c.gF], waw=False)
    k.barrier()
    cb = L.col["s5_bg"]
    with ExitStack() as st:
        wbf, wd = load_w_bf16(k, st, lambda kc: L.wglu[kc * 128:(kc + 1) * 128, :], 4, 256, "wglu")
        gin = [k.sb(f"s5g_g{i}", [128, TCH], F32, st) for i in range(3)]
        sg = [k.sb(f"s5g_s{i}", [128, TCH], F32, st) for i in range(3)]
        n = [0]

        def epi(tc, ni, ps, ncol):
            i = n[0] % 3
            n[0] += 1
            tsl = slice(tc * TCH, (tc + 1) * TCH)
            k.dma("sp", gin[i][:], c.gF[ni * 128:(ni + 1) * 128, tsl], reads=[c.gF], writes=[gin[i]])
            k.op("act", lambda e: e.activation(out=sg[i][:], in_=ps[:, :], func=AF.Sigmoid,
                                               bias=L.prm[:, cb + ni:cb + ni + 1], scale=1.0),
                 reads=[ps, L.prm], writes=[sg[i]])
            k.op("dve", lambda e: e.tensor_tensor(out=sg[i][:], in0=sg[i][:], in1=gin[i][:], op=ALU.mult),
                 reads=[sg[i], gin[i]], writes=[sg[i]])
            k.dma("pool", c.mixT[256 + ni * 128:256 + (ni + 1) * 128, tsl], sg[i][:], reads=[sg[i]], writes=[c.mixT],
                  waw=False)
        linear_fm(k, c, st, c.gT, 4, wbf, wd, [(0, 128), (128, 128)], epi, "s5g")
    k.barrier()


for _n, _w in (("rw_mur", 2), ("rw_muk", 2), ("rw_muv", 2), ("rw_mul", 1), ("rw_w0", 2), ("rw_a0", 2),
               ("rw_kk", 2), ("rw_ka", 2), ("rw_rk", 2), ("rw_lng", 2), ("rw_lnb", 2)):
    _reg(_n, _w)
R_RV = 3584
SEG = 1024
NCK = SEG // 64
RW_EPS = 64e-5
LDC = -0.6065306597126334


def stage_rwkv(k, c, L):
    pT = c.pT
    col = L.col
    with ExitStack() as stA:
        P = lambda nm, j: L.prm[:, col[nm] + j:col[nm] + j + 1]
        wl = k.sb("rw_wl", [128, 256], BF16, stA)
        omka = k.sb("rw_omka", [128, 2], F32, stA)
        rmask = k.sb("rw_rmask", [128, SEG], F32, stA)
        with ExitStack() as st0:
            wlf = k.sb("rw_wlf", [128, 256], F32, st0)
            k.dma("sp", wlf[:], L.w2a2[:, :], writes=[wlf])
            k.op("dve", lambda e: e.tensor_copy(out=wl[:], in_=wlf[:]), reads=[wlf], writes=[wl])
            k.op("dve", lambda e: e.tensor_scalar(out=omka[:], in0=L.prm[:, col["rw_ka"]:col["rw_ka"] + 2], scalar1=-1.0,
                                                  scalar2=1.0, op0=ALU.mult, op1=ALU.add), reads=[L.prm], writes=[omka])
            k.op("pool", lambda e: e.memset(rmask[:], 1.0), writes=[rmask])
            k.op("pool", lambda e: e.memset(rmask[:].rearrange("p (c i) -> p c i", i=64)[:, :, 0:1], 0.0), writes=[rmask])
            k.barrier()
        F = lambda nm: k.sb("rw_" + nm, [128, SEG], F32, stA)
        rz = k.sb("rw_rz", [128, SEG + 1], F32, stA)
        kz = k.sb("rw_kz", [128, SEG + 1], F32, stA)
        vz = k.sb("rw_vz", [128, SEG + 1], F32, stA)
        lz = k.sb("rw_lz", [128, SEG + 1], F32, stA)
        rm, km, vm, lm, t1, t2, sgw, av, cl, Epos, Eneg, Eex, Eh, kkr, kk, k2, bv = (F(n) for n in (
            "rm", "km", "vm", "lm", "t1", "t2", "sgw", "av", "cl", "Epos", "Eneg", "Eex", "Eh", "kkr", "kk", "k2", "bv"))
        lbf = k.sb("rw_lbf", [128, SEG], BF16, stA)
        sqb = k.sb("rw_sqb", [128, SEG], BF16, stA)
        BDn = ("AT", "BT", "KT", "RT", "bhT", "khT", "vT", "Bh", "Kh", "Vb", "Lst", "Mst", "LakT", "ArbT", "ArkT", "TT")
        BD = {n: k.sb("rw_bd_" + n, [128, NCK, 128], BF16, stA) for n in BDn}
        Ln = [k.sb(f"rw_Ln{i}", [128, 8, 128], BF16, stA) for i in range(2)]
        Mn = [k.sb(f"rw_Mn{i}", [128, 8, 128], BF16, stA) for i in range(2)]
        Pn = [k.sb(f"rw_Pn{i}", [128, 8, 128], BF16, stA) for i in range(2)]
        Sf = k.sb("rw_Sf", [128, 128], F32, stA)
        Sb = [k.sb(f"rw_Sb{i}", [128, 128], BF16, stA) for i in range(2)]
        RHSb = k.sb("rw_RHSb", [128, 128], BF16, stA)
        Ub = k.sb("rw_Ub", [128, 128], BF16, stA)
        OT = k.sb("rw_OT", [128, SEG], F32, stA)
        ob = k.sb("rw_ob", [128, SEG], BF16, stA)
        yo = [k.sb(f"rw_yo{i}", [128, SEG], F32, stA) for i in range(2)]
        for n in ("AT", "BT", "KT", "RT", "bhT", "khT", "vT"):
            k.op("pool", lambda e: e.memset(BD[n][:], 0.0), writes=[BD[n]])

        def tt(out, a, b, op, rd, wr, E="dve"):
            k.op(E, lambda e: e.tensor_tensor(out=out, in0=a, in1=b, op=op), reads=rd, writes=wr)

        def act(out, in_, func, rd, wr, **kw):
            k.op("act", lambda e: e.activation(out=out, in_=in_, func=func, **kw), reads=rd, writes=wr)

        def bdw(dst, a, b, rd, E="dve", scalar=None):
            for h in range(2):
                hs = slice(h * 64, (h + 1) * 64)
                o = dst[hs, :, h * 64:(h + 1) * 64]
                av_ = a[hs, :].rearrange("p (c i) -> p c i", i=64)
                if b is None:
                    k.op(E, lambda e: e.tensor_copy(out=o, in_=av_), reads=rd, writes=[dst])
                    continue
                bv_ = b[hs, :].rearrange("p (c i) -> p c i", i=64)
                if scalar is None:
                    k.op(E, lambda e: e.tensor_tensor(out=o, in0=av_, in1=bv_, op=ALU.mult), reads=rd, writes=[dst])
                else:
                    k.op("dve", lambda e: e.scalar_tensor_tensor(out=o, in0=av_, scalar=float(scalar), in1=bv_,
                                                                 op0=ALU.mult, op1=ALU.mult), reads=rd, writes=[dst])

        for hp in range(2):
            k.op("dve", lambda e: e.memset(Sf[:], 0.0), writes=[Sf])
            k.op("dve", lambda e: e.memset(Sb[0][:], 0.0), writes=[Sb[0]])
            sbi = 0
            for seg in range(S // SEG):
                t0 = seg * SEG
                for (zt, r0, mu, dst) in ((rz, R_R + hp * 128, P("rw_mur", hp), rm), (kz, R_K + hp * 128, P("rw_muk", hp), km),
                                          (vz, R_RV + hp * 128, P("rw_muv", hp), vm), (lz, R_LORA, P("rw_mul", 0), lm)):
                    if seg == 0:
                        k.op("pool", lambda e: e.memset(zt[:, 0:1], 0.0), writes=[zt])
                        k.dma("sp", zt[:, 1:SEG + 1], pT[r0:r0 + 128, 0:SEG], reads=[pT], writes=[zt])
                    else:
                        k.dma("sp", zt[:, 0:SEG + 1], pT[r0:r0 + 128, t0 - 1:t0 + SEG], reads=[pT], writes=[zt])
                    tt(t1[:], zt[:, 0:SEG], zt[:, 1:SEG + 1], ALU.subtract, [zt], [t1], "pool")
                    k.op("dve", lambda e: e.scalar_tensor_tensor(out=dst[:], in0=t1[:], scalar=mu, in1=zt[:, 1:SEG + 1],
                                                                 op0=ALU.mult, op1=ALU.add), reads=[t1, zt, L.prm], writes=[dst])
                act(lbf[0:64, :], lm[0:64, :], AF.Tanh, [lm], [lbf])
                k.op("dve", lambda e: e.tensor_copy(out=lbf[64:128, :], in_=lm[64:128, :]), reads=[lm], writes=[lbf])
                for hb in range(SEG // TCH):
                    cs_ = slice(hb * TCH, (hb + 1) * TCH)
                    pw, pa = c.pf[0], c.pf[1]
                    k.op("pe", lambda e: e.matmul(pw[:, :], lhsT=wl[0:64, hp * 128:(hp + 1) * 128], rhs=lbf[0:64, cs_],
                                                  start=True, stop=True), reads=[wl, lbf], writes=[pw])
                    k.op("pe", lambda e: e.matmul(pa[:, :], lhsT=wl[64:128, hp * 128:(hp + 1) * 128], rhs=lbf[64:128, cs_],
                                                  start=True, stop=True), reads=[wl, lbf], writes=[pa])
                    act(sgw[:, cs_], pw[:, :], AF.Sigmoid, [pw, L.prm], [sgw], bias=P("rw_w0", hp), scale=1.0)
                    act(av[:, cs_], pa[:, :], AF.Sigmoid, [pa, L.prm], [av], bias=P("rw_a0", hp), scale=1.0)
                k.op("pool", lambda e: e.tensor_scalar(out=sgw[:], in0=sgw[:], scalar1=LDC, scalar2=None, op0=ALU.mult),
                     reads=[sgw], writes=[sgw])
                k.op("dve", lambda e: e.tensor_tensor_scan(out=cl[:], data0=rmask[:], data1=sgw[:], initial=0.0,
                                                           op0=ALU.mult, op1=ALU.add), reads=[rmask, sgw], writes=[cl])
                act(Epos[:], cl[:], AF.Exp, [cl], [Epos])
                act(Eneg[:], cl[:], AF.Exp, [cl], [Eneg], scale=-1.0)
                tt(t1[:], cl[:], sgw[:], ALU.subtract, [cl, sgw], [t1], "pool")
                act(Eex[:], t1[:], AF.Exp, [t1], [Eex])
                clC = cl[:].rearrange("p (c i) -> p c i", i=64)[:, :, 63:64].to_broadcast([128, NCK, 64])
                tt(t2[:].rearrange("p (c i) -> p c i", i=64), clC, cl[:].rearrange("p (c i) -> p c i", i=64),
                   ALU.subtract, [cl], [t2])
                act(Eh[:], t2[:], AF.Exp, [t2], [Eh])
                k.op("dve", lambda e: e.tensor_scalar(out=kkr[:], in0=km[:], scalar1=P("rw_kk", hp), scalar2=None,
                                                      op0=ALU.mult), reads=[km, L.prm], writes=[kkr])
                act(sqb[:], kkr[:], AF.Square, [kkr], [sqb])
                for hb in range(SEG // TCH):
                    cs_ = slice(hb * TCH, (hb + 1) * TCH)
                    pn = c.next_pf(2, 6)
                    k.op("pe", lambda e: e.matmul(pn[:, :], lhsT=c.blk1[:], rhs=sqb[:, cs_], start=True, stop=True),
                         reads=[c.blk1, sqb], writes=[pn])
                    act(t1[:, cs_], pn[:, :], AF.Sqrt, [pn], [t1])
                k.op("dve", lambda e: e.tensor_scalar(out=t1[:], in0=t1[:], scalar1=1e-12, scalar2=None, op0=ALU.max),
                     reads=[t1], writes=[t1])
                k.op("dve", lambda e: e.reciprocal(out=t1[:], in_=t1[:]), reads=[t1], writes=[t1])
                tt(kk[:], kkr[:], t1[:], ALU.mult, [kkr, t1], [kk])
                k.op("dve", lambda e: e.tensor_scalar(out=t2[:], in0=av[:], scalar1=P("rw_ka", hp), scalar2=omka[:, hp:hp + 1],
                                                      op0=ALU.mult, op1=ALU.add), reads=[av, L.prm, omka], writes=[t2])
                tt(k2[:], km[:], t2[:], ALU.mult, [km, t2], [k2], "pool")
                tt(bv[:], kk[:], av[:], ALU.mult, [kk, av], [bv], "pool")
                bdw(BD["AT"], kk, Eex, [kk, Eex], scalar=-1.0)
                bdw(BD["BT"], bv, Eneg, [bv, Eneg], "pool")
                bdw(BD["KT"], k2, Eneg, [k2, Eneg], "dve")
                bdw(BD["RT"], rm, Epos, [rm, Epos], "pool")
                bdw(BD["bhT"], bv, Eh, [bv, Eh], "dve")
                bdw(BD["khT"], k2, Eh, [k2, Eh], "pool")
                bdw(BD["vT"], vm, None, [vm], "dve")
                for oc in range(NCK // 8):
                    c8 = slice(oc * 8, (oc + 1) * 8)
                    prods = (("AT", "BT", "Lst", c.mSL), ("BT", "AT", "Mst", c.mSU), ("KT", "AT", "LakT", c.mSU),
                             ("BT", "RT", "ArbT", c.mUI), ("KT", "RT", "ArkT", c.mUI))
                    for (la, rb, dn, mk) in prods:
                        for g4 in range(2):
                            ps = c.next_pf()
                            for q in range(4):
                                cc = oc * 8 + g4 * 4 + q
                                k.op("pe", lambda e: e.matmul(ps[:, q * 128:(q + 1) * 128], lhsT=BD[la][:, cc, :],
                                                              rhs=BD[rb][:, cc, :], start=True, stop=True),
                                     reads=[BD[la], BD[rb]], writes=[ps], inc=(q == 3))
                            c4 = slice(oc * 8 + g4 * 4, oc * 8 + g4 * 4 + 4)
                            tt(BD[dn][:, c4, :].rearrange("p a b -> p (a b)"), ps[:, :], mk[:], ALU.mult, [ps, mk], [BD[dn]])
                    for (src, dst) in (("bhT", "Bh"), ("khT", "Kh"), ("vT", "Vb")):
                        pb = c.next_pb()
                        for q in range(8):
                            cc = oc * 8 + q
                            k.op("pe", lambda e: e.transpose(pb[:, q * 128:(q + 1) * 128], BD[src][:, cc, :], c.ident[:]),
                                 reads=[BD[src], c.ident], writes=[pb], inc=(q == 7))
                        k.op("act", lambda e: e.copy(out=BD[dst][:, c8, :].rearrange("p a b -> p (a b)"), in_=pb[:]),
                             reads=[pb], writes=[BD[dst]])
                    Lc, Mc, Pc = BD["Lst"][:, c8, :], BD["Mst"][:, c8, :], None
                    Ld, Md = BD["Lst"], BD["Mst"]
                    k.op("dve", lambda e: e.tensor_tensor(out=Pn[0][:].rearrange("p a b -> p (a b)"),
                                                          in0=BD["Mst"][:, c8, :].rearrange("p a b -> p (a b)"),
                                                          in1=c.ident8[:], op=ALU.add), reads=[BD["Mst"], c.ident8], writes=[Pn[0]])
                    pcur = 0
                    for n in range(1, 6):
                        di = n % 2
                        psL = [c.pf[0], c.pf[1]]
                        psM = [c.pf[2], c.pf[3]]
                        for g4 in range(2):
                            for q in range(4):
                                qq = g4 * 4 + q
                                k.op("pe", lambda e: e.matmul(psL[g4][:, q * 128:(q + 1) * 128], lhsT=Mc[:, qq, :], rhs=Lc[:, qq, :],
                                                              start=True, stop=True), reads=[Md, Ld], writes=[psL[g4]], inc=(q == 3))
                            if n < 5:
                                for q in range(4):
                                    qq = g4 * 4 + q
                                    k.op("pe", lambda e: e.matmul(psM[g4][:, q * 128:(q + 1) * 128], lhsT=Lc[:, qq, :], rhs=Mc[:, qq, :],
                                                                  start=True, stop=True), reads=[Ld, Md], writes=[psM[g4]], inc=(q == 3))
                        for g4 in range(2):
                            k.op("act", lambda e: e.copy(out=Ln[di][:, g4 * 4:(g4 + 1) * 4, :].rearrange("p a b -> p (a b)"),
                                                         in_=psL[g4][:, :]), reads=[psL[g4]], writes=[Ln[di]])
                            if n < 5:
                                k.op("dve", lambda e: e.tensor_copy(out=Mn[di][:, g4 * 4:(g4 + 1) * 4, :].rearrange("p a b -> p (a b)"),
                                                                    in_=psM[g4][:, :]), reads=[psM[g4]], writes=[Mn[di]])
                        Lc, Mc, Ld, Md = Ln[di][:, :, :], Mn[di][:, :, :], Ln[di], Mn[di]
                        psP = [c.pf[4], c.pf[5]]
                        pnx = 1 - pcur
                        for g4 in range(2):
                            for q in range(4):
                                qq = g4 * 4 + q
                                k.op("pe", lambda e: e.matmul(psP[g4][:, q * 128:(q + 1) * 128], lhsT=Lc[:, qq, :], rhs=Pn[pcur][:, qq, :],
                                                              start=True, stop=True), reads=[Ld, Pn[pcur]], writes=[psP[g4]], inc=(q == 3))
                        for g4 in range(2):
                            g4s = slice(g4 * 4, (g4 + 1) * 4)
                            if n < 5:
                                o_ = Pn[pnx][:, g4s, :].rearrange("p a b -> p (a b)")
                                wr = Pn[pnx]
                            else:
                                o_ = BD["TT"][:, oc * 8 + g4 * 4:oc * 8 + g4 * 4 + 4, :].rearrange("p a b -> p (a b)")
                                wr = BD["TT"]
                            tt(o_, psP[g4][:, :], Pn[pcur][:, g4s, :].rearrange("p a b -> p (a b)"), ALU.add,
                               [psP[g4], Pn[pcur]], [wr])
                        pcur = pnx
                for cc in range(NCK):
                    So = Sb[sbi]
                    Sn = Sb[1 - sbi]
                    pR, pU, pS, pO = c.pf[0], c.pf[1], c.pf[2], c.pf[3]
                    k.op("pe", lambda e: e.matmul(pR[:, 0:128], lhsT=BD["LakT"][:, cc, :], rhs=BD["Vb"][:, cc, :], start=True, stop=False),
                         reads=[BD["LakT"], BD["Vb"]], writes=[pR], inc=False)
                    k.op("pe", lambda e: e.matmul(pR[:, 0:128], lhsT=BD["AT"][:, cc, :], rhs=So[:], start=False, stop=True),
                         reads=[BD["AT"], So], writes=[pR])
                    k.op("act", lambda e: e.copy(out=RHSb[:], in_=pR[:, 0:128]), reads=[pR], writes=[RHSb])
                    k.op("pe", lambda e: e.matmul(pU[:, 0:128], lhsT=BD["TT"][:, cc, :], rhs=RHSb[:], start=True, stop=True),
                         reads=[BD["TT"], RHSb], writes=[pU])
                    k.op("dve", lambda e: e.tensor_copy(out=Ub[:], in_=pU[:, 0:128]), reads=[pU], writes=[Ub])
                    k.op("pe", lambda e: e.matmul(pS[:, 0:128], lhsT=BD["Kh"][:, cc, :], rhs=BD["Vb"][:, cc, :], start=True, stop=False),
                         reads=[BD["Kh"], BD["Vb"]], writes=[pS], inc=False)
                    k.op("pe", lambda e: e.matmul(pS[:, 0:128], lhsT=BD["Bh"][:, cc, :], rhs=Ub[:], start=False, stop=True),
                         reads=[BD["Bh"], Ub], writes=[pS])
                    gC = Epos[:, cc * 64 + 63:cc * 64 + 64]
                    k.op("dve", lambda e: e.scalar_tensor_tensor(out=Sf[:], in0=Sf[:], scalar=gC, in1=pS[:, 0:128],
                                                                 op0=ALU.mult, op1=ALU.add), reads=[Sf, Epos, pS], writes=[Sf])
                    k.op("act", lambda e: e.copy(out=Sn[:], in_=Sf[:]), reads=[Sf], writes=[Sn])
                    k.op("pe", lambda e: e.matmul(pO[:, 0:128], lhsT=BD["Vb"][:, cc, :], rhs=BD["ArkT"][:, cc, :], start=True, stop=False),
                         reads=[BD["Vb"], BD["ArkT"]], writes=[pO], inc=False)
                    k.op("pe", lambda e: e.matmul(pO[:, 0:128], lhsT=So[:], rhs=BD["RT"][:, cc, :], start=False, stop=False),
                         reads=[So, BD["RT"]], writes=[pO], inc=False)
                    k.op("pe", lambda e: e.matmul(pO[:, 0:128], lhsT=Ub[:], rhs=BD["ArbT"][:, cc, :], start=False, stop=True),
                         reads=[Ub, BD["ArbT"]], writes=[pO])
                    for h in range(2):
                        hs = slice(h * 64, (h + 1) * 64)
                        k.op("pool" if False else "act", lambda e: e.copy(out=OT[hs, cc * 64:(cc + 1) * 64], in_=pO[hs, h * 64:(h + 1) * 64]),
                             reads=[pO], writes=[OT])
                    sbi = 1 - sbi
                y = yo[seg % 2]
                k.op("pool", lambda e: e.tensor_copy(out=ob[:], in_=OT[:]), reads=[OT], writes=[ob])
                for hb in range(SEG // TCH):
                    cs_ = slice(hb * TCH, (hb + 1) * TCH)
                    pm = c.next_pf(4, 6)
                    k.op("pe", lambda e: e.matmul(pm[:, :], lhsT=c.blk64[:], rhs=ob[:, cs_], start=True, stop=True),
                         reads=[c.blk64, ob], writes=[pm])
                    tt(t1[:, cs_], OT[:, cs_], pm[:, :], ALU.subtract, [OT, pm], [t1])
                act(sqb[:], t1[:], AF.Square, [t1], [sqb])
                for hb in range(SEG // TCH):
                    cs_ = slice(hb * TCH, (hb + 1) * TCH)
                    pm = c.next_pf(4, 6)
                    k.op("pe", lambda e: e.matmul(pm[:, :], lhsT=c.blk64[:], rhs=sqb[:, cs_], start=True, stop=True),
                         reads=[c.blk64, sqb], writes=[pm])
                    k.op("dve", lambda e: e.tensor_scalar(out=t2[:, cs_], in0=pm[:, :], scalar1=RW_EPS, scalar2=None, op0=ALU.add),
                         reads=[pm], writes=[t2])
                act(t2[:], t2[:], AF.Sqrt, [t2], [t2])
                k.op("dve", lambda e: e.reciprocal(out=t2[:], in_=t2[:]), reads=[t2], writes=[t2])
                tt(t1[:], t1[:], t2[:], ALU.mult, [t1, t2], [t1])
                k.op("dve", lambda e: e.tensor_scalar(out=y[:], in0=t1[:], scalar1=P("rw_lng", hp), scalar2=P("rw_lnb", hp),
                                                      op0=ALU.mult, op1=ALU.add), reads=[t1, L.prm], writes=[y])
                k.op("dve", lambda e: e.scalar_tensor_tensor(out=sqb[:], in0=rm[:], scalar=P("rw_rk", hp), in1=k2[:],
                                                             op0=ALU.mult, op1=ALU.mult), reads=[rm, k2, L.prm], writes=[sqb])
                for hb in range(SEG // TCH):
                    cs_ = slice(hb * TCH, (hb + 1) * TCH)
                    pm = c.next_pf(4, 6)
                    k.op("pe", lambda e: e.matmul(pm[:, :], lhsT=c.blk1[:], rhs=sqb[:, cs_], start=True, stop=True),
                         reads=[c.blk1, sqb], writes=[pm])
                    tt(t2[:, cs_], pm[:, :], vm[:, cs_], ALU.mult, [pm, vm], [t2])
                tt(y[:], y[:], t2[:], ALU.add, [y, t2], [y], "pool")
                k.dma("pool", c.mixT[512 + hp * 128:512 + (hp + 1) * 128, t0:t0 + SEG], y[:], reads=[y], writes=[c.mixT], waw=False)
        k.barrier()


def stage_outproj(k, c, L, xa, xb, y, xa_deps=None, cc=None):
    pT = c.pT
    with ExitStack() as st:
        wbf, wd = load_w_bf16(k, st, lambda kc: L.wout[kc * 128:(kc + 1) * 128, :], 8, D, "wout")
        mx = [k.sb(f"op_mx{i}", [128, 8, TCH], F32, st) for i in range(2)]
        gt = [k.sb(f"op_gt{i}", [128, 8, TCH], F32, st) for i in range(2)]
        sg = k.sb("op_sg", [128, 8, TCH], F32, st)
        mg = [k.sb(f"op_mg{i}", [128, 8, TCH], BF16, st) for i in range(2)]
        xt = [k.sb(f"op_xa{i}", [128, D], F32, st) for i in range(2)]
        xu = [k.sb(f"op_xb{i}", [128, D], F32, st) for i in range(2)]
        yo = [k.sb(f"op_y{i}", [128, D], F32, st) for i in range(2)]
        ydeps = [Dep() for _ in range(NTC)] if cc is not None else [y.dep] * NTC

        def emit_cc(i):
            xs_out, xs_deps, groups = cc
            rs = slice(i * TCH, (i + 1) * TCH)
            k.collective(y[rs, :], xs_out[rs, :], reads=[ydeps[i]], writes=[xs_deps[i]], groups=groups)
        for tc in range(NTC):
            b = tc % 2
            tsl = slice(tc * TCH, (tc + 1) * TCH)
            k.dma("sp", mx[b][:], c.mixT[:, tsl].rearrange("(kc p) t -> p kc t", p=128), reads=[c.mixT], writes=[mx[b]])
            k.dma("sp", gt[b][:], pT[R_GATE:R_GATE + 1024, tsl].rearrange("(kc p) t -> p kc t", p=128), reads=[pT],
                  writes=[gt[b]])
            k.op("act", lambda e: e.activation(out=sg[:], in_=gt[b][:], func=AF.Sigmoid), reads=[gt[b]], writes=[sg])
            k.op("pool", lambda e: e.tensor_tensor(out=gt[b][:], in0=gt[b][:], in1=mx[b][:], op=ALU.mult),
                 reads=[gt[b], mx[b]], writes=[gt[b]])
            k.op("dve", lambda e: e.tensor_tensor(out=mg[b][:], in0=gt[b][:], in1=sg[:], op=ALU.mult),
                 reads=[gt[b], sg], writes=[mg[b]])
            for sub in range(4):
                tt_ = tc * 4 + sub
                xb_i = tt_ % 2
                rows = slice(tt_ * 128, (tt_ + 1) * 128)
                k.dma("sp", xt[xb_i][:], xa[rows, :], reads=[xa_deps[tc] if xa_deps else xa], writes=[xt[xb_i]])
                if xb is not None:
                    k.dma("sp", xu[xb_i][:], xb[rows, :], reads=[xb], writes=[xu[xb_i]])
                    k.op("pool", lambda e: e.tensor_tensor(out=xt[xb_i][:], in0=xt[xb_i][:], in1=xu[xb_i][:], op=ALU.add),
                         reads=[xt[xb_i], xu[xb_i]], writes=[xt[xb_i]])
                yb = yo[xb_i]
                for n in range(4):
                    ps = c.next_pf()
                    for kc in range(8):
                        k.op("pe", lambda e: e.matmul(ps[:, :], lhsT=mg[b][:, kc, sub * 128:(sub + 1) * 128],
                                                      rhs=wbf[:, kc, n * 512:(n + 1) * 512], start=(kc == 0), stop=(kc == 7)),
                             reads=[mg[b], wd[kc]], writes=[ps], inc=(kc == 7))
                    k.op("dve", lambda e: e.scalar_tensor_tensor(out=yb[:, n * 512:(n + 1) * 512], in0=xt[xb_i][:, n * 512:(n + 1) * 512],
                                                                 scalar=0.5, in1=ps[:, :], op0=ALU.mult, op1=ALU.add),
                         reads=[xt[xb_i], ps], writes=[yb])
                k.dma("pool", y[rows, :], yb[:], reads=[yb], writes=[ydeps[tc]], waw=False)
            if cc is not None and tc >= 1:
                emit_cc(tc - 1)
        if cc is not None:
            emit_cc(NTC - 1)
    k.barrier()


def stage_final(k, c, xa, xb, g_bc_d, out, xa_deps=None):
    with ExitStack() as st:
        gbc = k.sb("f_gbc", [128, D], F32, st)
        k.dma("sp", gbc[:], g_bc_d.partition_broadcast(128), writes=[gbc])
        xt = [k.sb(f"f_xt{i}", [128, D], F32, st) for i in range(2)]
        xu = [k.sb(f"f_xu{i}", [128, D], F32, st) for i in range(2)]
        junk = k.sb("f_junk", [128, D], BF16, st)
        yo = [k.sb(f"f_y{i}", [128, D], F32, st) for i in range(2)]
        ss = [k.sb(f"f_ss{i}", [128, 4], F32, st) for i in range(2)]
        for tt_ in range(S // 128):
            b = tt_ % 2
            rows = slice(tt_ * 128, (tt_ + 1) * 128)
            k.dma("sp", xt[b][:], xa[rows, :], reads=[xa_deps[tt_ // 4] if xa_deps else xa], writes=[xt[b]])
            if xb is not None:
                k.dma("sp", xu[b][:], xb[rows, :], reads=[xb], writes=[xu[b]])
                k.op("pool", lambda e: e.tensor_tensor(out=xt[b][:], in0=xt[b][:], in1=xu[b][:], op=ALU.add),
                     reads=[xt[b], xu[b]], writes=[xt[b]])
            k.op("act", lambda e: e.activation(out=junk[:], in_=xt[b][:], func=AF.Square, accum_out=ss[b][:, 0:1]),
                 reads=[xt[b]], writes=[junk, ss[b]])
            k.op("dve", lambda e: e.tensor_scalar(out=ss[b][:, 1:2], in0=ss[b][:, 0:1], scalar1=1.0 / D, scalar2=EPS,
                                                  op0=ALU.mult, op1=ALU.add), reads=[ss[b]], writes=[ss[b]])
            k.op("act", lambda e: e.activation(out=ss[b][:, 2:3], in_=ss[b][:, 1:2], func=AF.Sqrt), reads=[ss[b]], writes=[ss[b]])
            k.op("dve", lambda e: e.reciprocal(out=ss[b][:, 3:4], in_=ss[b][:, 2:3]), reads=[ss[b]], writes=[ss[b]])
            k.op("dve", lambda e: e.scalar_tensor_tensor(out=yo[b][:], in0=xt[b][:], scalar=ss[b][:, 3:4], in1=gbc[:],
                                                         op0=ALU.mult, op1=ALU.mult), reads=[xt[b], ss[b], gbc], writes=[yo[b]])
            k.dma("pool", out[rows, :], yo[b][:], reads=[yo[b]], writes=[out], waw=False)
    k.barrier()


class LayerIO:
    pass


def declare_layer_inputs(k, l):
    EI = "ExternalInput"
    L = LayerIO()
    L.col = PRM_COLS
    L.norm_g = k.dram(f"norm_g{l}", [1, D], F32, kind=EI)
    L.w_in = k.dram(f"w_in{l}", [D, NF + NV], F32, kind=EI)
    L.wuq = k.dram(f"wuq{l}", [512, 384], F32, kind=EI)
    L.wukv = k.dram(f"wukv{l}", [256, 512], F32, kind=EI)
    L.prm_d = k.dram(f"prm{l}", [128, _pc], F32, kind=EI)
    L.s5b = k.dram(f"s5b{l}", [128, 512], F32, kind=EI)
    L.s5c = k.dram(f"s5c{l}", [128, 512], F32, kind=EI)
    L.wglu = k.dram(f"wglu{l}", [512, 256], F32, kind=EI)
    L.w2a2 = k.dram(f"w2a2{l}", [128, 256], F32, kind=EI)
    L.wout = k.dram(f"wout{l}", [1024, D], F32, kind=EI)
    return L


def layer_input_arrays(inp, l, half, suffix):
    a = core_layer_arrays(inp, l, half)
    out = {
        f"norm_g{suffix}": np.ascontiguousarray(inp["norm_g"][l][None, :]),
        f"w_in{suffix}": a["w_in"], f"wuq{suffix}": a["wuq"], f"wukv{suffix}": a["wukv"], f"prm{suffix}": a["prm"],
        f"s5b{suffix}": a["s5b"], f"s5c{suffix}": a["s5c"], f"wglu{suffix}": a["wglu"], f"w2a2{suffix}": a["w2a2"],
    }
    rows = np.concatenate([b * 512 + half * 256 + np.arange(256) for b in range(4)])
    out[f"wout{suffix}"] = np.ascontiguousarray(inp["w_out"][l][rows, :])
    return out


def declare_scratch(k, c):
    c.hT = k.dram("sc_hT", [D, S], BF16)
    c.pT = k.dram("sc_pT", [NF, S], F32)
    c.pV = k.dram("sc_pV", [S, NV], F32)
    c.us5 = k.dram("sc_us5", [32, 128, 512], BF16)
    c.ys5 = k.dram("sc_ys5", [32, 128, 512], F32)
    c.gT = k.dram("sc_gT", [512, S], BF16)
    c.gF = k.dram("sc_gF", [256, S], F32)
    c.mixT = k.dram("sc_mixT", [1024, S], F32)
    c.cqnT = k.dram("sc_cqnT", [512, S], BF16)
    c.ckvnT = k.dram("sc_ckvnT", [256, S], BF16)
    c.qT = k.dram("sc_qT", [384, S], BF16)
    c.knT = k.dram("sc_knT", [256, S], BF16)
    c.vA = k.dram("sc_vA", [S, 256], BF16)
    c.krT = k.dram("sc_krT", [128, S], BF16)
    c.qdT = k.dram("sc_qdT", [256, S], BF16)
    c.kdT = k.dram("sc_kdT", [256, S], BF16)
    c.ropeA_cos = k.dram("sc_rAc", [128, S], F32)
    c.ropeA_sin = k.dram("sc_rAs", [128, S], F32)
    c.ropeD_cos = k.dram("sc_rDc", [128, S], F32)
    c.ropeD_sin = k.dram("sc_rDs", [128, S], F32)


def emit_layer(k, c, L, xa, xb, y, xa_deps=None, cc=None):
    with ExitStack() as st:
        L.prm = k.sb("prm_sb", [128, _pc], F32, st)
        k.dma("sp", L.prm[:], L.prm_d[:, :], writes=[L.prm])
        import os as _os
        sel = _os.environ.get("LAYER_STAGES", "nimsrdo")
        if "n" in sel:
            stage_norm(k, c, xa, xb, L.norm_g[0:1, :], c.hT, xa_deps)
            k.barrier()
        if "i" in sel:
            stage_inproj(k, c, c.hT, L.w_in, NF, NV, c.pT, c.pV)
        if "m" in sel:
            stage_mla(k, c, L)
        if "s" in sel:
            stage_s5(k, c, L)
        if "r" in sel:
            stage_rwkv(k, c, L)
        if "d" in sel:
            stage_diff(k, c, L)
        if "o" in sel:
            stage_outproj(k, c, L, xa, xb, y, xa_deps, cc)
        k.barrier()


def build_layer_program():
    nc = bass.Bass("TRN2", target_bir_lowering=False)
    k = KB(nc)
    c = Ctx(k)
    EI = "ExternalInput"
    cmat = k.dram("cmat", [128, 768], F32, kind=EI)
    cmask = k.dram("cmask", [128, 2048], F32, kind=EI)
    cst = k.dram("cst", [128, NCST], F32, kind=EI)
    cm2 = k.dram("cm2", [128, 2560], F32, kind=EI)
    pos = k.dram("pos", [1, S], I32, kind=EI)
    xa = k.dram("xa", [S, D], F32, kind=EI)
    xb = k.dram("xb", [S, D], F32, kind=EI)
    y = k.dram("y", [S, D], F32, kind="ExternalOutput")
    L = declare_layer_inputs(k, "")
    declare_scratch(k, c)
    load_all_consts(k, c, cmat, cmask, cst, cm2)
    make_rope_tables(k, c, pos, c.cst, 0, 1, c.ropeA_cos, c.ropeA_sin, "rtA")
    make_rope_tables(k, c, pos, c.cst, 2, 3, c.ropeD_cos, c.ropeD_sin, "rtD")
    emit_layer(k, c, L, xa, xb, y)
    k.finish()
    return nc


def build_final_program():
    nc = bass.Bass("TRN2", target_bir_lowering=False)
    k = KB(nc)
    c = Ctx(k)
    xa = k.dram("xa", [S, D], F32, kind="ExternalInput")
    xb = k.dram("xb", [S, D], F32, kind="ExternalInput")
    g = k.dram("fg", [1, D], F32, kind="ExternalInput")
    out = k.dram("out", [S, D], F32, kind="ExternalOutput")
    stage_final(k, c, xa, xb, g[0:1, :], out)
    k.finish()
    return nc


def const_inputs():
    cmat, cmask, cst = const_mats()
    return {"cmat": cmat, "cmask": cmask, "cst": cst, "cm2": const_mats2()}


def kernel_unfused(**inp):
    inp = {k_: np.asarray(v) for k_, v in inp.items()}
    x = inp["x"]
    B = x.shape[0]
    consts = const_inputs()
    ncl = build_layer_program()
    cur_a = [np.ascontiguousarray(x[cid // 2]) for cid in range(8)]
    cur_b = [np.zeros((S, D), np.float32) for _ in range(8)]
    for l in range(4):
        in_maps = []
        for cid in range(8):
            b, half = divmod(cid, 2)
            m = dict(consts)
            m["pos"] = np.ascontiguousarray(inp["positions"][b:b + 1].astype(np.int32))
            m["xa"] = cur_a[cid]
            m["xb"] = cur_b[cid]
            m.update(layer_input_arrays(inp, l, half, ""))
            in_maps.append(m)
        res = run_bass_kernel_spmd(ncl, in_maps, core_ids=list(range(8)))
        ys = [np.asarray(r["y"]) for r in res.results]
        cur_a = [ys[cid] for cid in range(8)]
        cur_b = [ys[cid ^ 1] for cid in range(8)]
    ncf = build_final_program()
    fg = np.ascontiguousarray(inp["final_norm_g"][None, :])
    in_maps = [{"xa": cur_a[cid], "xb": cur_b[cid], "fg": fg} for cid in range(8)]
    res = run_bass_kernel_spmd(ncf, in_maps, core_ids=list(range(8)))
    out = np.stack([np.asarray(res.results[2 * b]["out"]) for b in range(B)], axis=0)
    return out.astype(np.float32)


from concourse.bass_utils import run_bass_kernel_spmd


PAIR_GROUPS = [[0, 1], [2, 3], [4, 5], [6, 7]]
DEPTH = 4


def build_fused_program(depth=DEPTH):
    nc = bass.Bass("TRN2", target_bir_lowering=False)
    k = KB(nc)
    c = Ctx(k)
    EI = "ExternalInput"
    cmat = k.dram("cmat", [128, 768], F32, kind=EI)
    cmask = k.dram("cmask", [128, 2048], F32, kind=EI)
    cst = k.dram("cst", [128, NCST], F32, kind=EI)
    cm2 = k.dram("cm2", [128, 2560], F32, kind=EI)
    pos = k.dram("pos", [1, S], I32, kind=EI)
    x_in = k.dram("xa", [S, D], F32, kind=EI)
    fg = k.dram("fg", [1, D], F32, kind=EI)
    out = k.dram("out", [S, D], F32, kind="ExternalOutput")
    Ls = [declare_layer_inputs(k, l) for l in range(depth)]
    declare_scratch(k, c)
    ybuf = k.dram("sc_y", [S, D], F32)
    xs = [k.dram(f"sc_xs{i}", [S, D], F32) for i in range(2)]
    xs_deps = [[Dep() for _ in range(NTC)] for _ in range(2)]
    load_all_consts(k, c, cmat, cmask, cst, cm2)
    make_rope_tables(k, c, pos, c.cst, 0, 1, c.ropeA_cos, c.ropeA_sin, "rtA")
    make_rope_tables(k, c, pos, c.cst, 2, 3, c.ropeD_cos, c.ropeD_sin, "rtD")
    cur, cur_deps = x_in, None
    for l in range(depth):
        o = l % 2
        emit_layer(k, c, Ls[l], cur, None, ybuf, cur_deps, (xs[o], xs_deps[o], PAIR_GROUPS))
        cur, cur_deps = xs[o], xs_deps[o]
    stage_final(k, c, cur, None, fg[0:1, :], out, cur_deps)
    k.finish()
    return nc


def kernel_fused(**inp):
    inp = {k_: np.asarray(v) for k_, v in inp.items()}
    x = inp["x"]
    B = x.shape[0]
    consts = const_inputs()
    nc = build_fused_program()
    fg = np.ascontiguousarray(inp["final_norm_g"][None, :])
    in_maps = []
    for cid in range(8):
        b, half = divmod(cid, 2)
        m = dict(consts)
        m["pos"] = np.ascontiguousarray(inp["positions"][b:b + 1].astype(np.int32))
        m["xa"] = np.ascontiguousarray(x[b])
        m["fg"] = fg
        for l in range(DEPTH):
            m.update(layer_input_arrays(inp, l, half, str(l)))
        in_maps.append(m)
    res = run_bass_kernel_spmd(nc, in_maps, core_ids=list(range(8)))
    out = np.stack([np.asarray(res.results[2 * b]["out"]) for b in range(B)], axis=0)
    return out.astype(np.float32)


def kernel(**inputs):
    return kernel_fused(**inputs)
```

```python
from contextlib import ExitStack
import math
import numpy as np
import concourse.bass as bass
import concourse.mybir as mybir

F32 = mybir.dt.float32
BF16 = mybir.dt.bfloat16
I32 = mybir.dt.int32
AF = mybir.ActivationFunctionType
ALU = mybir.AluOpType
AX = mybir.AxisListType

NDS = 44
NDS_HW = 24


class Dep:
    __slots__ = ("w", "r", "excl")

    def __init__(self):
        self.excl = False
        self.w = {}
        self.r = {}


class T:
    def __init__(self, t, dep=None):
        self.t = t
        self.dep = dep or Dep()

    def __getitem__(self, k):
        return self.t[k]

    def ap(self):
        return self.t.ap() if hasattr(self.t, "ap") else self.t[:]


def _deps(x):
    return x.dep if isinstance(x, T) else x


class KB:
    def __init__(self, nc):
        self.nc = nc
        self.es = ExitStack()
        self.eng = {"pe": nc.tensor, "act": nc.scalar, "dve": nc.vector,
                    "pool": nc.gpsimd, "sp": nc.sync}
        self.sem = {}
        for e in ("pe", "act", "dve", "pool"):
            self.sem[e] = self.es.enter_context(nc.semaphore("s_" + e))
        self.cnt = {e: 0 for e in self.sem}
        self.dsem = [self.es.enter_context(nc.semaphore(f"sd{i}")) for i in range(NDS)]
        self.dcnt = [0] * NDS
        self.dnext = 0
        self.dnext_sw = 0
        self.seen = {e: {} for e in self.eng}
        self.ccsem = self.es.enter_context(nc.semaphore("s_cc"))
        self.cccnt = 0
        self.ninst = 0
        self.scopes = []

    def sb(self, name, shape, dtype, stack=None):
        self.uid = getattr(self, "uid", 0) + 1
        name = f"{name}_u{self.uid}"
        t = (stack or self.es).enter_context(self.nc.sbuf_tensor(name, list(shape), dtype))
        return T(t)

    def ps(self, name, shape, dtype, stack=None):
        t = (stack or self.es).enter_context(self.nc.psum_tensor(name, list(shape), dtype))
        r = T(t)
        r.dep.excl = True
        return r

    def dram(self, name, shape, dtype, kind="Internal"):
        t = self.nc.dram_tensor(name, list(shape), dtype, kind=kind)
        return T(t)

    def _wait(self, E, tok):
        if tok is None:
            return
        kind, key, val = tok
        if kind == "e" and key == E and E in ("pe", "sp"):
            return
        sk = (kind, key)
        if self.seen[E].get(sk, 0) >= val:
            return
        sem = self.sem[key] if kind == "e" else (self.dsem[key] if kind == "d" else self.ccsem)
        self.eng[E].wait_ge(sem, val)
        self.ninst += 1
        self.seen[E][sk] = val

    def _collect(self, E, reads, writes, waw=True):
        for d in reads:
            d = _deps(d)
            for (kd, ky), v in list(d.w.items()):
                self._wait(E, (kd, ky, v))
            if d.excl:
                for (kd, ky), v in list(d.r.items()):
                    if ky != E:
                        self._wait(E, (kd, ky, v))
        for d in writes:
            d = _deps(d)
            if waw:
                for (kd, ky), v in list(d.w.items()):
                    self._wait(E, (kd, ky, v))
            for (kd, ky), v in list(d.r.items()):
                self._wait(E, (kd, ky, v))

    def _update(self, tok, reads, writes, waw=True):
        kk = (tok[0], tok[1])
        for d in writes:
            d = _deps(d)
            if waw:
                d.w = {kk: tok[2]}
                d.r = {}
            else:
                d.w[kk] = max(d.w.get(kk, 0), tok[2])
        for d in reads:
            d = _deps(d)
            d.r[kk] = max(d.r.get(kk, 0), tok[2])

    def op(self, E, fn, reads=(), writes=(), inc=True):
        self._collect(E, reads, writes)
        inst = fn(self.eng[E])
        self.ninst += 1
        if inc:
            self.cnt[E] += 1
            inst.then_inc(self.sem[E], 1)
            tok = ("e", E, self.cnt[E])
        else:
            tok = ("e", E, self.cnt[E] + 1)
        self._update(tok, reads, writes)
        return inst

    def dma(self, Q, out, in_, reads=(), writes=(), waw=True, **kw):
        self._collect(Q, reads, writes, waw)
        if Q == "pool":
            i = NDS_HW + self.dnext_sw
            self.dnext_sw = (self.dnext_sw + 1) % (NDS - NDS_HW)
        else:
            i = self.dnext
            self.dnext = (i + 1) % NDS_HW
        if self.dcnt[i] > 0:
            self._wait(Q, ("d", i, 16 * self.dcnt[i]))
        inst = self.eng[Q].dma_start(out=out, in_=in_, **kw)
        inst.then_inc(self.dsem[i], 16)
        self.ninst += 1
        self.dcnt[i] += 1
        tok = ("d", i, 16 * self.dcnt[i])
        self._update(tok, reads, writes, waw)
        return tok

    def collective(self, in_ap, out_ap, reads=(), writes=(), groups=None):
        self._collect("pool", reads, writes, True)
        inst = self.nc.gpsimd.collective_compute("AllReduce", ALU.add, replica_groups=groups,
                                                 ins=[in_ap], outs=[out_ap])
        self.cccnt += 1
        inst.then_inc(self.ccsem, 1)
        self.ninst += 1
        tok = ("c", 0, self.cccnt)
        self._update(tok, reads, writes, True)
        return tok

    def barrier(self, engines=("pe", "act", "dve", "pool", "sp")):
        for E in engines:
            if self.cccnt > 0:
                self._wait(E, ("c", 0, self.cccnt))
            for p in self.sem:
                if p != E and self.cnt[p] > 0:
                    self._wait(E, ("e", p, self.cnt[p]))
            for i in range(NDS):
                if self.dcnt[i] > 0:
                    self._wait(E, ("d", i, 16 * self.dcnt[i]))
        for E in ("act", "dve", "pool"):
            if E in engines and self.cnt[E] > 0:
                self._wait(E, ("e", E, self.cnt[E]))

    def finish(self):
        self.barrier(engines=("sp",))


S = 4096
D = 2048
TCH = 512
NTC = S // TCH
EPS = 1e-6


class Ctx:
    def __init__(self, k):
        self.k = k
        self.pf = [k.ps(f"pf{i}", [128, 512], F32) for i in range(6)]
        self.pb = [k.ps(f"pb{i}", [128, 1024], BF16) for i in range(2)]
        self.pfi = 0
        self.pbi = 0
        self.rr = 0
        self.us5 = None

    def next_pf(self, lo=0, hi=6):
        n = hi - lo
        p = self.pf[lo + (self.pfi % n)]
        self.pfi += 1
        return p

    def next_pb(self):
        p = self.pb[self.pbi % 2]
        self.pbi += 1
        return p

    def evac_eng(self):
        self.rr += 1
        return "act" if self.rr % 2 else "dve"


def copy_op(k, E, out, in_, reads, writes):
    if E == "act":
        k.op("act", lambda e: e.copy(out=out, in_=in_), reads=reads, writes=writes)
    else:
        k.op(E, lambda e: e.tensor_copy(out=out, in_=in_), reads=reads, writes=writes)


def load_consts(k, c, ident_d, stack):
    idf = k.sb("idf", [128, 128], F32, stack)
    c.ident = k.sb("c_ident", [128, 128], BF16)
    c.ones = k.sb("c_ones", [128, 128], BF16)
    k.dma("sp", idf[:], ident_d[:, :], writes=[idf])
    k.op("dve", lambda e: e.tensor_copy(out=c.ident[:], in_=idf[:]), reads=[idf], writes=[c.ident])
    k.op("dve", lambda e: e.memset(c.ones[:], 1.0), writes=[c.ones])


def stage_norm(k, c, xa, xb, g_bc_d, hT, xa_deps=None):
    with ExitStack() as st:
        gbc = k.sb("n_gbc", [128, D], F32, st)
        k.dma("sp", gbc[:], g_bc_d.partition_broadcast(128), writes=[gbc])
        xt = [k.sb(f"n_xt{i}", [128, D], F32, st) for i in range(2)]
        xt2 = [k.sb(f"n_xu{i}", [128, D], F32, st) for i in range(2)]
        junk = k.sb("n_junk", [128, D], BF16, st)
        xn = [k.sb(f"n_xn{i}", [128, D], BF16, st) for i in range(2)]
        ss = [k.sb(f"n_ss{i}", [128, 4], F32, st) for i in range(2)]
        hst = [k.sb(f"n_hst{i}", [128, 16, TCH], BF16, st) for i in range(2)]
        for tt in range(S // 128):
            b = tt % 2
            tcn, tl = divmod(tt, 4)
            hs = hst[tcn % 2]
            rows = slice(tt * 128, (tt + 1) * 128)
            k.dma("sp", xt[b][:], xa[rows, :], reads=[xa_deps[tt // 4] if xa_deps else xa], writes=[xt[b]])
            if xb is not None:
                k.dma("sp", xt2[b][:], xb[rows, :], reads=[xb], writes=[xt2[b]])
                k.op("pool", lambda e: e.tensor_tensor(out=xt[b][:], in0=xt[b][:], in1=xt2[b][:], op=ALU.add),
                     reads=[xt[b], xt2[b]], writes=[xt[b]])
            k.op("act", lambda e: e.activation(out=junk[:], in_=xt[b][:], func=AF.Square,
                                               accum_out=ss[b][:, 0:1]),
                 reads=[xt[b]], writes=[junk, ss[b]])
            k.op("dve", lambda e: e.tensor_scalar(out=ss[b][:, 1:2], in0=ss[b][:, 0:1], scalar1=1.0 / D,
                                                  scalar2=EPS, op0=ALU.mult, op1=ALU.add),
                 reads=[ss[b]], writes=[ss[b]])
            k.op("act", lambda e: e.activation(out=ss[b][:, 2:3], in_=ss[b][:, 1:2], func=AF.Sqrt),
                 reads=[ss[b]], writes=[ss[b]])
            k.op("dve", lambda e: e.reciprocal(out=ss[b][:, 3:4], in_=ss[b][:, 2:3]),
                 reads=[ss[b]], writes=[ss[b]])
            k.op("dve", lambda e: e.scalar_tensor_tensor(out=xn[b][:], in0=xt[b][:], scalar=ss[b][:, 3:4],
                                                         in1=gbc[:], op0=ALU.mult, op1=ALU.mult),
                 reads=[xt[b], ss[b], gbc], writes=[xn[b]])
            for half in range(2):
                pb = c.next_pb()
                for j in range(8):
                    kc = half * 8 + j
                    k.op("pe", lambda e: e.transpose(pb[:, j * 128:(j + 1) * 128],
                                                     xn[b][:, kc * 128:(kc + 1) * 128], c.ident[:]),
                         reads=[xn[b], c.ident], writes=[pb], inc=(j == 7))
                k.op("act", lambda e: e.copy(out=hs[:, half * 8:(half + 1) * 8, tl * 128:(tl + 1) * 128],
                                             in_=pb[:].rearrange("p (a b) -> p a b", a=8)),
                     reads=[pb], writes=[hs])
            if tl == 3:
                k.dma("pool", hT[:, tcn * TCH:(tcn + 1) * TCH].rearrange("(kc p) t -> p kc t", p=128),
                      hs[:], reads=[hs], writes=[hT], waw=False)


def load_w_bf16(k, st, w_ap_fn, KC, N, name):
    wbf = k.sb(name, [128, KC, N], BF16, st)
    stg = [k.sb(f"{name}_s{i}", [128, N], F32, st) for i in range(2)]
    deps = [Dep() for _ in range(KC)]
    for kc in range(KC):
        s = stg[kc % 2]
        k.dma("sp", s[:], w_ap_fn(kc), writes=[s])
        E = "pool" if kc % 2 == 0 else "dve"
        k.op(E, lambda e: e.tensor_copy(out=wbf[:, kc, :], in_=s[:]), reads=[s], writes=[deps[kc]])
    return wbf, deps


def linear_fm(k, c, st, inT, KC, wbf, wdeps, n_tiles, epilogue, name, in_cast=False):
    xin = [k.sb(f"{name}_x{i}", [128, KC, TCH], BF16, st) for i in range(2)]
    for tc in range(NTC):
        xi = xin[tc % 2]
        k.dma("sp", xi[:], inT[:, tc * TCH:(tc + 1) * TCH].rearrange("(kc p) t -> p kc t", p=128),
              reads=[inT], writes=[xi])
        for ni, (c0, ncol) in enumerate(n_tiles):
            ps = c.next_pf()
            for kc in range(KC):
                k.op("pe", lambda e: e.matmul(ps[:ncol, :], lhsT=wbf[:, kc, c0:c0 + ncol], rhs=xi[:, kc, :],
                                              start=(kc == 0), stop=(kc == KC - 1)),
                     reads=[wdeps[kc], xi], writes=[ps], inc=(kc == KC - 1))
            epilogue(tc, ni, ps, ncol)


def linear_tm(k, c, st, inT, KC, wbf, wdeps, c0, N, epilogue, name):
    xin = [k.sb(f"{name}_x{i}", [128, KC, TCH], BF16, st) for i in range(2)]
    for tc in range(NTC):
        xi = xin[tc % 2]
        k.dma("sp", xi[:], inT[:, tc * TCH:(tc + 1) * TCH].rearrange("(kc p) t -> p kc t", p=128),
              reads=[inT], writes=[xi])
        for sub in range(4):
            ps = c.next_pf()
            for kc in range(KC):
                k.op("pe", lambda e: e.matmul(ps[:, :N], lhsT=xi[:, kc, sub * 128:(sub + 1) * 128],
                                              rhs=wbf[:, kc, c0:c0 + N],
                                              start=(kc == 0), stop=(kc == KC - 1)),
                     reads=[wdeps[kc], xi], writes=[ps], inc=(kc == KC - 1))
            epilogue(tc * 4 + sub, ps)


class Stager:
    def __init__(self, k, st, name, shape, dtype, n=4):
        self.k = k
        self.bufs = [k.sb(f"{name}{i}", shape, dtype, st) for i in range(n)]
        self.i = 0

    def next(self):
        b = self.bufs[self.i % len(self.bufs)]
        self.i += 1
        return b


def epi_store_fm(k, c, stg, outT, row0_of):
    def epi(tc, ni, ps, ncol):
        s = stg.next()
        copy_op(k, c.evac_eng(), s[:ncol, :], ps[:ncol, :], [ps], [s])
        r0 = row0_of(ni)
        k.dma("pool", outT[r0:r0 + ncol, tc * TCH:(tc + 1) * TCH], s[:ncol, :], reads=[s], writes=[outT], waw=False)
        return s
    return epi


def stage_inproj(k, c, hT, w_in_d, NF, NV, pT, pV):
    KC = D // 128
    tiles = []
    c0 = 0
    while c0 < NF:
        tiles.append((c0, min(128, NF - c0)))
        c0 += 128
    half = (len(tiles) + 1) // 2
    groups = [tiles[:half], tiles[half:]]
    for gi, grp in enumerate(groups):
        with ExitStack() as st:
            g0 = grp[0][0]
            gN = grp[-1][0] + grp[-1][1] - g0
            wbf, wd = load_w_bf16(k, st, lambda kc: w_in_d[kc * 128:(kc + 1) * 128, g0:g0 + gN], KC, gN, f"ip_w{gi}")
            stg = Stager(k, st, f"ip_o{gi}_", [128, TCH], F32, 4)
            rel = [(a - g0, b) for a, b in grp]
            base_epi = epi_store_fm(k, c, stg, pT, lambda ni: grp[ni][0])
            stu = Stager(k, st, f"ip_u{gi}_", [128, 8, 64], BF16, 3)

            def epi(tc, ni, ps, ncol, grp=grp, base_epi=base_epi, stu=stu):
                sst = base_epi(tc, ni, ps, ncol)
                r0 = grp[ni][0]
                import os as _os
                if R_U <= r0 < R_U + 512 and c.us5 is not None and _os.environ.get('NOHOOK') != '1':
                    su = stu.next()
                    k.op("pool", lambda e: e.tensor_copy(out=su[:],
                                                         in_=sst[:, :].rearrange("p (j t) -> p t j", t=8)),
                         reads=[sst], writes=[su])
                    g0 = (r0 - R_U) // 16
                    hq = _os.environ.get("HOOKDMA", "pool")
                    for gl in range(8 if hq != "none" else 0):
                        k.dma(hq, c.us5[g0 + gl, :, tc * 64:(tc + 1) * 64].rearrange("(t c) j -> c t j", c=16),
                              su[gl * 16:(gl + 1) * 16, :, :], reads=[su], writes=[c.us5], waw=False)
            linear_fm(k, c, st, hT, KC, wbf, wd, rel, epi, f"ip{gi}")
        k.barrier()
    with ExitStack() as st:
        wbf, wd = load_w_bf16(k, st, lambda kc: w_in_d[kc * 128:(kc + 1) * 128, NF:NF + NV], KC, NV, "ip_wv")
        stg = Stager(k, st, "ip_ov_", [128, NV], F32, 4)

        def epi(tt, ps):
            s = stg.next()
            copy_op(k, c.evac_eng(), s[:, :], ps[:, :NV], [ps], [s])
            k.dma("pool", pV[tt * 128:(tt + 1) * 128, :], s[:, :], reads=[s], writes=[pV], waw=False)
        linear_tm(k, c, st, hT, KC, wbf, wd, 0, NV, epi, "ipv")
    k.barrier()


TWO_PI = 2.0 * np.pi
CW1 = 6.28125
CW2 = float(np.float32(TWO_PI - 6.28125))
CW3 = float(TWO_PI - 6.28125 - float(np.float32(TWO_PI - 6.28125)))


def make_rope_tables(k, c, pos_d, cst, col_inv, col_sgn, cosT, sinT, name):
    HS = S // 2
    with ExitStack() as st:
        posi = k.sb(name + "_pi", [128, HS], I32, st)
        ang = k.sb(name + "_ang", [128, HS], F32, st)
        a2 = k.sb(name + "_a2", [128, HS], F32, st)
        ni = k.sb(name + "_ni", [128, HS], I32, st)
        nf = k.sb(name + "_nf", [128, HS], F32, st)
        r = k.sb(name + "_r", [128, HS], F32, st)
        m = k.sb(name + "_m", [128, HS], F32, st)
        o = k.sb(name + "_o", [128, HS], F32, st)
        for hh in range(2):
            sl = slice(hh * HS, (hh + 1) * HS)
            k.dma("sp", posi[:], pos_d[0:1, sl].partition_broadcast(128), writes=[posi])
            k.op("dve", lambda e: e.tensor_copy(out=ang[:], in_=posi[:]), reads=[posi], writes=[ang])
            k.op("dve", lambda e: e.tensor_scalar(out=ang[:], in0=ang[:], scalar1=cst[:, col_inv:col_inv + 1],
                                                  scalar2=None, op0=ALU.mult), reads=[ang, cst], writes=[ang])
            for which, shift, dst in (("s", 0.0, sinT), ("c", np.pi / 2, cosT)):
                if which == "s":
                    k.op("dve", lambda e: e.tensor_scalar(out=ni[:], in0=ang[:], scalar1=float(1.0 / TWO_PI),
                                                          scalar2=None, op0=ALU.mult), reads=[ang], writes=[ni])
                    k.op("dve", lambda e: e.tensor_copy(out=nf[:], in_=ni[:]), reads=[ni], writes=[nf])
                    k.op("dve", lambda e: e.scalar_tensor_tensor(out=a2[:], in0=nf[:], scalar=-CW1, in1=ang[:],
                                                                 op0=ALU.mult, op1=ALU.add), reads=[nf, ang], writes=[a2])
                    k.op("dve", lambda e: e.scalar_tensor_tensor(out=a2[:], in0=nf[:], scalar=-CW2, in1=a2[:],
                                                                 op0=ALU.mult, op1=ALU.add), reads=[nf, a2], writes=[a2])
                    k.op("dve", lambda e: e.scalar_tensor_tensor(out=a2[:], in0=nf[:], scalar=-CW3, in1=a2[:],
                                                                 op0=ALU.mult, op1=ALU.add), reads=[nf, a2], writes=[a2])
                k.op("dve", lambda e: e.tensor_scalar(out=r[:], in0=a2[:], scalar1=float(shift), scalar2=None,
                                                      op0=ALU.add), reads=[a2], writes=[r])
                k.op("dve", lambda e: e.tensor_single_scalar(out=m[:], in_=r[:], scalar=float(np.pi), op=ALU.is_gt),
                     reads=[r], writes=[m])
                k.op("dve", lambda e: e.scalar_tensor_tensor(out=r[:], in0=m[:], scalar=-TWO_PI, in1=r[:],
                                                             op0=ALU.mult, op1=ALU.add), reads=[m, r], writes=[r])
                k.op("dve", lambda e: e.tensor_single_scalar(out=m[:], in_=r[:], scalar=float(-np.pi), op=ALU.is_lt),
                     reads=[r], writes=[m])
                k.op("dve", lambda e: e.scalar_tensor_tensor(out=r[:], in0=m[:], scalar=TWO_PI, in1=r[:],
                                                             op0=ALU.mult, op1=ALU.add), reads=[m, r], writes=[r])
                k.op("dve", lambda e: e.tensor_scalar(out=r[:], in0=r[:], scalar1=float(np.pi), scalar2=float(-np.pi),
                                                      op0=ALU.min, op1=ALU.max), reads=[r], writes=[r])
                k.op("act", lambda e: e.activation(out=o[:], in_=r[:], func=AF.Sin), reads=[r], writes=[o])
                if which == "s":
                    k.op("dve", lambda e: e.tensor_scalar(out=o[:], in0=o[:], scalar1=cst[:, col_sgn:col_sgn + 1],
                                                          scalar2=None, op0=ALU.mult), reads=[o, cst], writes=[o])
                k.dma("sp", dst[:, sl], o[:], reads=[o], writes=[dst], waw=False)
    k.barrier()


def rmsnorm_fm(k, c, srcT, r0, nt, g_sb, gcol0, dstT, eps, name):
    n = nt * 128
    with ExitStack() as st:
        xin = [k.sb(f"{name}_x{i}", [128, nt, TCH], F32, st) for i in range(2)]
        sq = [k.sb(f"{name}_q{i}", [128, nt, TCH], BF16, st) for i in range(2)]
        rs = [k.sb(f"{name}_r{i}", [128, TCH], F32, st) for i in range(2)]
        ob = [k.sb(f"{name}_o{i}", [128, nt, TCH], BF16, st) for i in range(2)]
        for tc in range(NTC):
            b = tc % 2
            tsl = slice(tc * TCH, (tc + 1) * TCH)
            k.dma("sp", xin[b][:], srcT[r0:r0 + n, tsl].rearrange("(t p) s -> p t s", p=128),
                  reads=[srcT], writes=[xin[b]])
            k.op("act", lambda e: e.activation(out=sq[b][:], in_=xin[b][:], func=AF.Square),
                 reads=[xin[b]], writes=[sq[b]])
            ps = c.next_pf()
            for t in range(nt):
                k.op("pe", lambda e: e.matmul(ps[:, :], lhsT=c.ones[:], rhs=sq[b][:, t, :],
                                              start=(t == 0), stop=(t == nt - 1)),
                     reads=[c.ones, sq[b]], writes=[ps], inc=(t == nt - 1))
            k.op("dve", lambda e: e.tensor_scalar(out=rs[b][:], in0=ps[:, :], scalar1=1.0 / n, scalar2=float(eps),
                                                  op0=ALU.mult, op1=ALU.add), reads=[ps], writes=[rs[b]])
            k.op("act", lambda e: e.activation(out=rs[b][:], in_=rs[b][:], func=AF.Ln),
                 reads=[rs[b]], writes=[rs[b]])
            k.op("act", lambda e: e.activation(out=rs[b][:], in_=rs[b][:], func=AF.Exp, scale=-0.5),
                 reads=[rs[b]], writes=[rs[b]])
            for t in range(nt):
                k.op("dve", lambda e: e.scalar_tensor_tensor(out=ob[b][:, t, :], in0=xin[b][:, t, :],
                                                             scalar=g_sb[:, gcol0 + t:gcol0 + t + 1], in1=rs[b][:],
                                                             op0=ALU.mult, op1=ALU.mult),
                     reads=[xin[b], g_sb, rs[b]], writes=[ob[b]])
            k.dma("pool", dstT[0:n, tsl].rearrange("(t p) s -> p t s", p=128), ob[b][:],
                  reads=[ob[b]], writes=[dstT], waw=False)
    k.barrier()


def rope_tile(k, c, st_bufs, src_ap, src_dep, perm, cos_sb, sin_sb, out_ap, out_dep):
    xb, xf, t1 = st_bufs["xb"], st_bufs["xf"], st_bufs["t1"]
    k.op("act", lambda e: e.copy(out=xf[:], in_=src_ap), reads=[src_dep], writes=[xf])
    k.op("dve", lambda e: e.tensor_copy(out=xb[:], in_=xf[:]), reads=[xf], writes=[xb])
    ps = c.next_pf()
    k.op("pe", lambda e: e.matmul(ps[:, :], lhsT=perm[:], rhs=xb[:], start=True, stop=True),
         reads=[perm, xb], writes=[ps])
    k.op("dve", lambda e: e.tensor_tensor(out=t1[:], in0=ps[:, :], in1=sin_sb, op=ALU.mult),
         reads=[ps, st_bufs["tab"]], writes=[t1])
    k.op("pool", lambda e: e.tensor_tensor(out=xf[:], in0=xf[:], in1=cos_sb, op=ALU.mult),
         reads=[xf, st_bufs["tab"]], writes=[xf])
    k.op("dve", lambda e: e.tensor_tensor(out=out_ap, in0=xf[:], in1=t1[:], op=ALU.add),
         reads=[xf, t1], writes=[out_dep])


def attention_head(k, c, st, name, maps, Vsb, vdep, dv, scale, masks, post):
    raise NotImplementedError


def attn_qchunk(k, c, j, maps, Vfn, vdep, dv, scale, masks, ptbufs, acc_banks):
    nkt = 4 * j + 4
    items = [(mi, kt) for mi in range(len(maps)) for kt in range(nkt)]
    tri = masks[0]

    def c0_of(kt):
        return 128 * (kt - 4 * j) if kt >= 4 * j else 0

    def emit_qk(i):
        mi, kt = items[i]
        parts = maps[mi]
        ps = c.pf[i % 2]
        c0 = c0_of(kt)
        for pi, p in enumerate(parts):
            k.op("pe", lambda e: e.matmul(ps[:, c0:TCH], lhsT=p["K"](kt), rhs=p["Q"](c0),
                                          start=(pi == 0), stop=(pi == len(parts) - 1)),
                 reads=[p["kd"], p["qd"]], writes=[ps], inc=(pi == len(parts) - 1))

    emit_qk(0)
    for i, (mi, kt) in enumerate(items):
        if i + 1 < len(items):
            emit_qk(i + 1)
        oacc, sacc = acc_banks[mi]
        ps = c.pf[i % 2]
        c0 = c0_of(kt)
        pt = ptbufs[i % len(ptbufs)]
        k.op("act", lambda e: e.activation(out=pt[:, c0:TCH], in_=ps[:, c0:TCH], func=AF.Exp, scale=float(scale)),
             reads=[ps], writes=[pt])
        if kt >= 4 * j:
            k.op("pool", lambda e: e.tensor_tensor(out=pt[:, c0:c0 + 128], in0=pt[:, c0:c0 + 128], in1=tri[:, 0:128],
                                                   op=ALU.mult), reads=[pt, tri], writes=[pt])
        k.op("pe", lambda e: e.matmul(oacc[:dv, c0:TCH], lhsT=Vfn(kt), rhs=pt[:, c0:TCH],
                                      start=(kt == 0), stop=(kt == nkt - 1)),
             reads=[vdep, pt], writes=[oacc], inc=False)
        k.op("pe", lambda e: e.matmul(sacc[:, c0:TCH], lhsT=c.ones[:], rhs=pt[:, c0:TCH],
                                      start=(kt == 0), stop=(kt == nkt - 1)),
             reads=[c.ones, pt], writes=[sacc], inc=True)


R_CQ, R_CKV, R_U, R_R, R_K, R_QD, R_KD, R_GATE, R_KROPE, R_LORA = 0, 512, 768, 1280, 1536, 1792, 2048, 2304, 3328, 3456
NF = 3840
NV = 256


class RopeCtx:
    def __init__(self, k, c, st, name, cosT, sinT, perm):
        self.k, self.c = k, c
        self.cosT, self.sinT, self.perm = cosT, sinT, perm
        self.tab = [k.sb(f"{name}_tab{i}", [128, 2, TCH], F32, st) for i in range(2)]
        self.xb = [k.sb(f"{name}_xb{i}", [128, TCH], BF16, st) for i in range(2)]
        self.xf = [k.sb(f"{name}_xf{i}", [128, TCH], F32, st) for i in range(2)]
        self.t1 = [k.sb(f"{name}_t1{i}", [128, TCH], F32, st) for i in range(2)]
        self.cur = None
        self.n = 0

    def load(self, tc):
        k = self.k
        tb = self.tab[tc % 2]
        tsl = slice(tc * TCH, (tc + 1) * TCH)
        k.dma("sp", tb[:, 0, :], self.cosT[:, tsl], reads=[self.cosT], writes=[tb])
        k.dma("sp", tb[:, 1, :], self.sinT[:, tsl], reads=[self.cosT], writes=[tb], waw=False)
        self.cur = tb

    def apply(self, src_ap, src_dep, out_ap, out_dep):
        k, c = self.k, self.c
        i = self.n % 2
        self.n += 1
        xb, xf, t1, tb = self.xb[i], self.xf[i], self.t1[i], self.cur
        k.op("act", lambda e: e.copy(out=xf[:], in_=src_ap), reads=[src_dep], writes=[xf])
        k.op("dve", lambda e: e.tensor_copy(out=xb[:], in_=xf[:]), reads=[xf], writes=[xb])
        ps = c.next_pf(0, 2)
        k.op("pe", lambda e: e.matmul(ps[:, :], lhsT=self.perm[:], rhs=xb[:], start=True, stop=True),
             reads=[self.perm, xb], writes=[ps])
        k.op("dve", lambda e: e.tensor_tensor(out=t1[:], in0=ps[:, :], in1=tb[:, 1, :], op=ALU.mult),
             reads=[ps, tb], writes=[t1])
        k.op("pool", lambda e: e.tensor_tensor(out=xf[:], in0=xf[:], in1=tb[:, 0, :], op=ALU.mult),
             reads=[xf, tb], writes=[xf])
        k.op("dve", lambda e: e.tensor_tensor(out=out_ap, in0=xf[:], in1=t1[:], op=ALU.add),
             reads=[xf, t1], writes=[out_dep])


def stage_mla(k, c, L):
    pT = c.pT
    rmsnorm_fm(k, c, pT, R_CQ, 4, L.prm, L.col["gq"], c.cqnT, EPS, "nq")
    rmsnorm_fm(k, c, pT, R_CKV, 2, L.prm, L.col["gkv"], c.ckvnT, EPS, "nkv")
    with ExitStack() as st:
        wbf, wd = load_w_bf16(k, st, lambda kc: L.wuq[kc * 128:(kc + 1) * 128, :], 4, 384, "wuq")
        stg = Stager(k, st, "mq_o", [128, TCH], BF16, 4)
        rp = RopeCtx(k, c, st, "mqr", c.ropeA_cos, c.ropeA_sin, c.permA)

        def epi(tc, ni, ps, ncol):
            s = stg.next()
            if ni < 2:
                copy_op(k, c.evac_eng(), s[:, :], ps[:, :], [ps], [s])
            else:
                rp.load(tc)
                rp.apply(ps[:, :], ps, s[:, :], s)
            k.dma("pool", c.qT[ni * 128:(ni + 1) * 128, tc * TCH:(tc + 1) * TCH], s[:, :], reads=[s],
                  writes=[c.qT], waw=False)
        linear_fm(k, c, st, c.cqnT, 4, wbf, wd, [(0, 128), (128, 128), (256, 128)], epi, "mq")
    k.barrier()
    with ExitStack() as st:
        wbf, wd = load_w_bf16(k, st, lambda kc: L.wukv[kc * 128:(kc + 1) * 128, :], 2, 512, "wukv")
        stg = Stager(k, st, "mk_o", [128, TCH], BF16, 4)
        epi = epi_store_fm(k, c, stg, c.knT, lambda ni: ni * 128)
        linear_fm(k, c, st, c.ckvnT, 2, wbf, wd, [(0, 128), (128, 128)], epi, "mk")
        stgv = Stager(k, st, "mv_o", [128, 256], BF16, 4)

        def epiv(tt, ps):
            s = stgv.next()
            copy_op(k, c.evac_eng(), s[:, :], ps[:, :256], [ps], [s])
            k.dma("pool", c.vA[tt * 128:(tt + 1) * 128, :], s[:, :], reads=[s], writes=[c.vA], waw=False)
        linear_tm(k, c, st, c.ckvnT, 2, wbf, wd, 256, 256, epiv, "mv")
        rp = RopeCtx(k, c, st, "mkr", c.ropeA_cos, c.ropeA_sin, c.permA)
        kin = [k.sb(f"mkr_in{i}", [128, TCH], F32, st) for i in range(2)]
        for tc in range(NTC):
            tsl = slice(tc * TCH, (tc + 1) * TCH)
            ki = kin[tc % 2]
            k.dma("sp", ki[:], pT[R_KROPE:R_KROPE + 128, tsl], reads=[pT], writes=[ki])
            rp.load(tc)
            s = stg.next()
            rp.apply(ki[:], ki, s[:, :], s)
            k.dma("pool", c.krT[:, tsl], s[:, :], reads=[s], writes=[c.krT], waw=False)
    k.barrier()
    scale = (128 + 64) ** -0.5
    for h in range(2):
        with ExitStack() as st:
            Kn = k.sb("ma_kn", [128, S], BF16, st)
            Kr = k.sb("ma_kr", [128, S], BF16, st)
            Vs = k.sb("ma_v", [128, S // 128, 128], BF16, st)
            k.dma("sp", Kn[:], c.knT[h * 128:(h + 1) * 128, :], reads=[c.knT], writes=[Kn])
            k.dma("sp", Kr[:], c.krT[:, :], reads=[c.krT], writes=[Kr])
            k.dma("sp", Vs[:], c.vA[:, h * 128:(h + 1) * 128].rearrange("(kt p) d -> p kt d", p=128),
                  reads=[c.vA], writes=[Vs])
            Qn = [k.sb(f"ma_qn{i}", [128, TCH], BF16, st) for i in range(2)]
            Qr = [k.sb(f"ma_qr{i}", [128, TCH], BF16, st) for i in range(2)]
            ptb = [k.sb(f"ma_pt{i}", [128, TCH], BF16, st) for i in range(3)]
            rec = [k.sb(f"ma_rec{i}", [128, TCH], F32, st) for i in range(2)]
            ost = [k.sb(f"ma_o{i}", [128, TCH], F32, st) for i in range(2)]
            hs = slice(h * 64, (h + 1) * 64)
            for j in range(NTC):
                b = j % 2
                tsl = slice(j * TCH, (j + 1) * TCH)
                k.dma("sp", Qn[b][:], c.qT[h * 128:(h + 1) * 128, tsl], reads=[c.qT], writes=[Qn[b]])
                k.dma("sp", Qr[b][:], c.qT[256:384, tsl], reads=[c.qT], writes=[Qr[b]])
                parts = [dict(K=lambda kt: Kn[:, kt * 128:(kt + 1) * 128], Q=lambda c0: Qn[b][:, c0:TCH], kd=Kn, qd=Qn[b]),
                         dict(K=lambda kt: Kr[hs, kt * 128:(kt + 1) * 128], Q=lambda c0: Qr[b][hs, c0:TCH], kd=Kr, qd=Qr[b])]
                oacc, sacc = c.pf[2 + 2 * b], c.pf[3 + 2 * b]
                attn_qchunk(k, c, j, [parts], lambda kt: Vs[:, kt, :], Vs, 128, scale, c.masks, ptb, [(oacc, sacc)])
                k.op("act", lambda e: e.activation(out=rec[b][:], in_=sacc[:, :], func=AF.Ln), reads=[sacc], writes=[rec[b]])
                k.op("act", lambda e: e.activation(out=rec[b][:], in_=rec[b][:], func=AF.Exp, scale=-1.0), reads=[rec[b]], writes=[rec[b]])
                k.op("dve", lambda e: e.tensor_tensor(out=ost[b][:], in0=oacc[:, :], in1=rec[b][:], op=ALU.mult),
                     reads=[oacc, rec[b]], writes=[ost[b]])
                k.dma("pool", c.mixT[h * 128:(h + 1) * 128, tsl], ost[b][:], reads=[ost[b]], writes=[c.mixT],
                      waw=False)
        k.barrier()


PRM_COLS = {}
_pc = 0


def _reg(name, n):
    global _pc
    PRM_COLS[name] = _pc
    _pc += n


_reg("gq", 4)
_reg("gkv", 2)


def const_mats():
    ident = np.eye(128, dtype=np.float32)
    permA = np.zeros((128, 128), np.float32)
    for r in range(128):
        d = r % 64
        permA[r, r + 32 if d < 32 else r - 32] = 1.0
    permD = np.zeros((128, 128), np.float32)
    for r in range(128):
        d = r % 64
        if d < 8:
            permD[r, r + 8] = 1.0
        elif d < 16:
            permD[r, r - 8] = 1.0
    masks = np.zeros((128, 4, 512), np.float32)
    kk = np.arange(128)[:, None]
    qq = np.arange(512)[None, :]
    for r in range(4):
        masks[:, r, :] = (qq >= 128 * r + kk)
    tmask = np.zeros((128, 128), np.float32)
    ti = np.arange(128) // 16
    tmask[:, :] = (ti[None, :] >= ti[:, None])
    hidx = np.arange(128) // 64
    same = (hidx[:, None] == hidx[None, :]).astype(np.float32)
    blk1 = same.copy()
    blk64 = same / 64.0
    cmat = np.concatenate([ident, permA, permD, tmask, blk1, blk64], axis=1)
    cst = np.zeros((128, 8 + 32 + 512), np.float32)
    invA = (500000.0 ** (-np.arange(0, 64, 2, dtype=np.float32) / np.float32(64))).astype(np.float32)
    invD = (500000.0 ** (-np.arange(0, 16, 2, dtype=np.float32) / np.float32(16))).astype(np.float32)
    for r in range(128):
        d = r % 64
        cst[r, 0] = invA[d % 32]
        cst[r, 1] = -1.0 if d < 32 else 1.0
        cst[r, 2] = invD[d % 8] if d < 16 else 0.0
        cst[r, 3] = (-1.0 if d < 8 else 1.0) if d < 16 else 0.0
    tau = np.zeros(32, np.float32)
    tau[0:8] = -np.arange(8)
    tau[8:17] = np.arange(9)
    tau[17:25] = 7 - np.arange(8)
    cst[:, 8:40] = tau[None, :]
    cst[:, 40:552] = (8.0 * (np.arange(512) + 1))[None, :]
    return cmat, masks.reshape(128, 2048), cst


def const_mats2():
    hidx = np.arange(128) // 64
    idx = np.arange(128) % 64
    same = (hidx[:, None] == hidx[None, :])
    mSL = (same & (idx[:, None] > idx[None, :])).astype(np.float32)
    mSU = (same & (idx[:, None] < idx[None, :])).astype(np.float32)
    mUI = (same & (idx[:, None] <= idx[None, :])).astype(np.float32)
    ident = np.eye(128, dtype=np.float32)
    return np.concatenate([np.tile(mSL, (1, 4)), np.tile(mSU, (1, 4)), np.tile(mUI, (1, 4)), np.tile(ident, (1, 8))], axis=1)


NCST = 552


def load_all_consts(k, c, cmat_d, cmask_d, cst_d, cm2_d=None):
    c.ident = k.sb("c_ident", [128, 128], BF16)
    c.permA = k.sb("c_permA", [128, 128], BF16)
    c.permD = k.sb("c_permD", [128, 128], BF16)
    c.ones = k.sb("c_ones", [128, 128], BF16)
    c.cst = k.sb("c_cst", [128, NCST], F32)
    c.tmask = k.sb("c_tmask", [128, 128], F32)
    c.blk1 = k.sb("c_blk1", [128, 128], BF16)
    c.blk64 = k.sb("c_blk64", [128, 128], BF16)
    c.mSL = k.sb("c_mSL", [128, 512], BF16)
    c.mSU = k.sb("c_mSU", [128, 512], BF16)
    c.mUI = k.sb("c_mUI", [128, 512], BF16)
    c.ident8 = k.sb("c_ident8", [128, 1024], BF16)
    c.masks = [k.sb(f"c_mask{r}", [128, 512], BF16) for r in range(4)]
    with ExitStack() as st:
        f = k.sb("lc_f", [128, 768], F32, st)
        m2 = k.sb("lc_m2", [128, 2560], F32, st)
        m = k.sb("lc_m", [128, 2048], F32, st)
        k.dma("sp", f[:], cmat_d[:, :], writes=[f])
        k.dma("sp", m[:], cmask_d[:, :], writes=[m])
        k.dma("sp", c.cst[:], cst_d[:, :], writes=[c.cst])
        k.op("dve", lambda e: e.tensor_copy(out=c.ident[:], in_=f[:, 0:128]), reads=[f], writes=[c.ident])
        k.op("dve", lambda e: e.tensor_copy(out=c.permA[:], in_=f[:, 128:256]), reads=[f], writes=[c.permA])
        k.op("dve", lambda e: e.tensor_copy(out=c.permD[:], in_=f[:, 256:384]), reads=[f], writes=[c.permD])
        k.op("dve", lambda e: e.tensor_copy(out=c.tmask[:], in_=f[:, 384:512]), reads=[f], writes=[c.tmask])
        k.op("dve", lambda e: e.tensor_copy(out=c.blk1[:], in_=f[:, 512:640]), reads=[f], writes=[c.blk1])
        k.op("dve", lambda e: e.tensor_copy(out=c.blk64[:], in_=f[:, 640:768]), reads=[f], writes=[c.blk64])
        if cm2_d is not None:
            k.dma("sp", m2[:], cm2_d[:, :], writes=[m2])
            k.op("dve", lambda e: e.tensor_copy(out=c.mSL[:], in_=m2[:, 0:512]), reads=[m2], writes=[c.mSL])
            k.op("dve", lambda e: e.tensor_copy(out=c.mSU[:], in_=m2[:, 512:1024]), reads=[m2], writes=[c.mSU])
            k.op("dve", lambda e: e.tensor_copy(out=c.mUI[:], in_=m2[:, 1024:1536]), reads=[m2], writes=[c.mUI])
            k.op("dve", lambda e: e.tensor_copy(out=c.ident8[:], in_=m2[:, 1536:2560]), reads=[m2], writes=[c.ident8])
        k.op("dve", lambda e: e.memset(c.ones[:], 1.0), writes=[c.ones])
        for r in range(4):
            k.op("dve", lambda e: e.tensor_copy(out=c.masks[r][:], in_=m[:, r * 512:(r + 1) * 512]),
                 reads=[m], writes=[c.masks[r]])
        k.barrier()


O_CQ, O_CKV, O_KR, O_U, O_Z, O_QD, O_KD, O_VD, O_GATE = 0, 512, 768, 832, 1344, 3008, 3520, 4032, 4544


def s5_gperm(half):
    return np.concatenate([half * 16 + np.arange(16), (1 - half) * 16 + np.arange(16)])


def s5_chperm(half):
    return (s5_gperm(half)[:, None] * 16 + np.arange(16)[None, :]).reshape(-1)


def fm_cols(half):
    a = np.arange
    cols = [O_CQ + a(512), O_CKV + a(256), O_U + s5_chperm(half),
            O_Z + half * 256 + a(256), O_Z + 512 + half * 256 + a(256),
            O_QD + half * 256 + a(256), O_KD + half * 256 + a(256)]
    for b in range(4):
        cols.append(O_GATE + b * 512 + half * 256 + a(256))
    cols += [O_KR + a(64), O_KR + a(64), O_Z + 1536 + a(64), O_Z + 1600 + a(64), O_Z + 1024 + half * 256 + a(256)]
    return np.concatenate(cols)


def tm_cols(half):
    a = np.arange
    return O_VD + half * 256 + a(256)


def pt_layout(v, nt):
    return np.ascontiguousarray(np.asarray(v).reshape(nt, 128).T)


def core_layer_arrays(inp, l, half):
    out = {}
    w_in = inp["w_in"][l]
    out["w_in"] = np.ascontiguousarray(w_in[:, np.concatenate([fm_cols(half), tm_cols(half)])])
    hs = [2 * half, 2 * half + 1]
    wuq = inp["mla_w_uq"][l]
    out["wuq"] = np.ascontiguousarray(np.concatenate(
        [wuq[:, h * 192:h * 192 + 128] for h in hs] + [wuq[:, h * 192 + 128:(h + 1) * 192] for h in hs], axis=1))
    wukv = inp["mla_w_ukv"][l]
    out["wukv"] = np.ascontiguousarray(np.concatenate(
        [wukv[:, h * 256:h * 256 + 128] for h in hs] + [wukv[:, h * 256 + 128:(h + 1) * 256] for h in hs], axis=1))
    prm = np.zeros((128, _pc), np.float32)

    def put(name, arr):
        arr = np.asarray(arr, np.float32)
        if arr.ndim == 1:
            arr = arr[:, None]
        prm[:arr.shape[0], PRM_COLS[name]:PRM_COLS[name] + arr.shape[1]] = arr
    put("gq", pt_layout(inp["mla_q_norm_g"][l], 4))
    put("gkv", pt_layout(inp["mla_kv_norm_g"][l], 2))
    for nm in ("lq1", "lk1", "lq2", "lk2"):
        put(nm, np.broadcast_to(inp["diff_" + nm][l][None, :], (128, 64)))
    put("gsub", inp["diff_subln_g"][l])
    lam_init = 0.8 - 0.6 * math.exp(-0.3 * l)
    put("lam_init", np.full((128,), lam_init, np.float32))
    put("omlam", np.full((128,), 1.0 - lam_init, np.float32))
    fill_more(inp, l, half, put, out)
    out["prm"] = prm
    return out


def st_layout(a):
    a = np.asarray(a)
    rest = a.shape[2:]
    a = a.reshape((16, 128) + rest)
    return np.ascontiguousarray(np.moveaxis(a, 0, 1))


def fill_more(inp, l, half, put, out=None):
    gp = s5_gperm(half)
    chp = s5_chperm(half)
    put("s5_are", st_layout(inp["s5_a_re"][l][gp]))
    put("s5_aim", st_layout(inp["s5_a_im"][l][gp]))
    put("s5_ldt", st_layout(np.broadcast_to(inp["s5_log_dt"][l][gp][:, None], (32, 64))))
    put("s5_d", pt_layout(inp["s5_d"][l][chp], 4))
    put("s5_bg", pt_layout(inp["s5_b_glu"][l][half * 256:(half + 1) * 256], 2))
    mu = inp["rwkv_mu"][l]
    my = slice(half * 256, (half + 1) * 256)
    put("rw_mur", pt_layout(mu[0:512][my], 2))
    put("rw_muk", pt_layout(mu[512:1024][my], 2))
    put("rw_muv", pt_layout(mu[1024:1536][my], 2))
    put("rw_mul", mu[1536:1664])
    put("rw_w0", pt_layout(inp["rwkv_w0"][l][my], 2))
    put("rw_a0", pt_layout(inp["rwkv_a0"][l][my], 2))
    put("rw_kk", pt_layout(inp["rwkv_k_k"][l][my], 2))
    put("rw_ka", pt_layout(inp["rwkv_k_a"][l][my], 2))
    put("rw_rk", pt_layout(inp["rwkv_r_k"][l].reshape(512)[my], 2))
    put("rw_lng", pt_layout(inp["rwkv_ln_g"][l][my], 2))
    put("rw_lnb", pt_layout(inp["rwkv_ln_b"][l][my], 2))
    if out is not None:
        out["w2a2"] = np.ascontiguousarray(np.concatenate([inp["rwkv_w2"][l][:, my], inp["rwkv_a2"][l][:, my]], axis=0))
        b = np.stack([inp["s5_b_re"][l][gp], inp["s5_b_im"][l][gp]], axis=2)
        out["s5b"] = st_layout(b).reshape(128, 512).astype(np.float32)
        cc = np.stack([np.swapaxes(inp["s5_c_re"][l][gp], 1, 2), np.swapaxes(inp["s5_c_im"][l][gp], 1, 2)], axis=2)
        out["s5c"] = st_layout(cc).reshape(128, 512).astype(np.float32)
        out["wglu"] = np.ascontiguousarray(inp["s5_w_glu"][l][chp][:, half * 256:(half + 1) * 256])


_reg("lq1", 64)
_reg("lk1", 64)
_reg("lq2", 64)
_reg("lk2", 64)
_reg("gsub", 1)
_reg("lam_init", 1)
_reg("omlam", 1)
DIFF_EPS = 1e-5


def stage_diff(k, c, L):
    pT, pV = c.pT, c.pV
    with ExitStack() as st:
        rp = RopeCtx(k, c, st, "dr", c.ropeD_cos, c.ropeD_sin, c.permD)
        xin = [k.sb(f"dr_in{i}", [128, TCH], F32, st) for i in range(3)]
        stg = Stager(k, st, "dr_o", [128, TCH], BF16, 4)
        n = 0
        for tc in range(NTC):
            tsl = slice(tc * TCH, (tc + 1) * TCH)
            rp.load(tc)
            for (r0, dst) in ((R_QD, c.qdT), (R_KD, c.kdT)):
                for hd in range(2):
                    xi = xin[n % 3]
                    n += 1
                    k.dma("sp", xi[:], pT[r0 + hd * 128:r0 + (hd + 1) * 128, tsl], reads=[pT], writes=[xi])
                    s = stg.next()
                    rp.apply(xi[:], xi, s[:, :], s)
                    k.dma("pool", dst[hd * 128:(hd + 1) * 128, tsl], s[:, :], reads=[s], writes=[dst], waw=False)
    k.barrier()
    with ExitStack() as st0:
        sm = k.sb("df_sm", [128, 8], F32, st0)
        tmp = k.sb("df_tmp", [128, 64], F32, st0)
        cq1, ck1, cq2, ck2, cg = (L.col[n_] for n_ in ("lq1", "lk1", "lq2", "lk2", "gsub"))
        for i, (a, b_) in enumerate(((cq1, ck1), (cq2, ck2))):
            k.op("dve", lambda e: e.tensor_tensor(out=tmp[:], in0=L.prm[:, a:a + 64], in1=L.prm[:, b_:b_ + 64],
                                                  op=ALU.mult), reads=[L.prm], writes=[tmp])
            k.op("dve", lambda e: e.reduce_sum(out=sm[:, i:i + 1], in_=tmp[:], axis=AX.X), reads=[tmp], writes=[sm])
            k.op("act", lambda e: e.activation(out=sm[:, 2 + i:3 + i], in_=sm[:, i:i + 1], func=AF.Exp),
                 reads=[sm], writes=[sm])
        k.op("dve", lambda e: e.tensor_tensor(out=sm[:, 4:5], in0=sm[:, 3:4], in1=sm[:, 2:3], op=ALU.subtract),
             reads=[sm], writes=[sm])
        cli, col_ = L.col["lam_init"], L.col["omlam"]
        k.op("dve", lambda e: e.tensor_tensor(out=sm[:, 5:6], in0=sm[:, 4:5], in1=L.prm[:, cli:cli + 1], op=ALU.subtract),
             reads=[sm, L.prm], writes=[sm])
        k.op("dve", lambda e: e.tensor_tensor(out=sm[:, 6:7], in0=L.prm[:, cg:cg + 1], in1=L.prm[:, col_:col_ + 1], op=ALU.mult),
             reads=[L.prm], writes=[sm])
        nlam = sm[:, 5:6]
        gs = sm[:, 6:7]
        scale = 64 ** -0.5
        for hd in range(2):
            with ExitStack() as st:
                Kd = k.sb("da_k", [128, S], BF16, st)
                Vf = k.sb("da_vf", [128, S // 128, 128], F32, st)
                Vs = k.sb("da_v", [128, S // 128, 128], BF16, st)
                k.dma("sp", Kd[:], c.kdT[hd * 128:(hd + 1) * 128, :], reads=[c.kdT], writes=[Kd])
                k.dma("sp", Vf[:], pV[:, hd * 128:(hd + 1) * 128].rearrange("(kt p) d -> p kt d", p=128),
                      reads=[pV], writes=[Vf])
                k.op("pool", lambda e: e.tensor_copy(out=Vs[:], in_=Vf[:]), reads=[Vf], writes=[Vs])
                Qd = [k.sb(f"da_q{i}", [128, TCH], BF16, st) for i in range(2)]
                ptb = [k.sb(f"da_pt{i}", [128, TCH], BF16, st) for i in range(3)]
                rec = k.sb("da_rec", [128, TCH], F32, st)
                o1 = k.sb("da_o1", [128, TCH], F32, st)
                o2 = k.sb("da_o2", [128, TCH], F32, st)
                sq = k.sb("da_sq", [128, TCH], BF16, st)
                rs = k.sb("da_rs", [128, TCH], F32, st)
                ost = [k.sb(f"da_o{i}", [128, TCH], F32, st) for i in range(2)]
                for j in range(NTC):
                    b = j % 2
                    tsl = slice(j * TCH, (j + 1) * TCH)
                    k.dma("sp", Qd[b][:], c.qdT[hd * 128:(hd + 1) * 128, tsl], reads=[c.qdT], writes=[Qd[b]])
                    maps = []
                    for m in range(2):
                        ms = slice(m * 64, (m + 1) * 64)
                        maps.append([dict(K=(lambda kt, ms=ms: Kd[ms, kt * 128:(kt + 1) * 128]),
                                          Q=(lambda c0, ms=ms: Qd[b][ms, c0:TCH]), kd=Kd, qd=Qd[b])])
                    accs = [(c.pf[2], c.pf[3]), (c.pf[4], c.pf[5])]
                    attn_qchunk(k, c, j, maps, lambda kt: Vs[:, kt, :], Vs, 128, scale, c.masks, ptb, accs)
                    (oa1, sa1), (oa2, sa2) = accs
                    k.op("act", lambda e: e.activation(out=rec[:], in_=sa1[:, :], func=AF.Ln), reads=[sa1], writes=[rec])
                    k.op("act", lambda e: e.activation(out=rec[:], in_=rec[:], func=AF.Exp, scale=-1.0), reads=[rec], writes=[rec])
                    k.op("dve", lambda e: e.tensor_tensor(out=o1[:], in0=oa1[:, :], in1=rec[:], op=ALU.mult),
                         reads=[oa1, rec], writes=[o1])
                    k.op("act", lambda e: e.activation(out=rec[:], in_=sa2[:, :], func=AF.Ln), reads=[sa2], writes=[rec])
                    k.op("act", lambda e: e.activation(out=rec[:], in_=rec[:], func=AF.Exp, scale=-1.0), reads=[rec], writes=[rec])
                    k.op("dve", lambda e: e.tensor_tensor(out=o2[:], in0=oa2[:, :], in1=rec[:], op=ALU.mult),
                         reads=[oa2, rec], writes=[o2])
                    k.op("dve", lambda e: e.scalar_tensor_tensor(out=o1[:], in0=o2[:], scalar=nlam, in1=o1[:],
                                                                 op0=ALU.mult, op1=ALU.add),
                         reads=[o2, o1, sm], writes=[o1])
                    k.op("pool", lambda e: e.tensor_tensor(out=sq[:], in0=o1[:], in1=o1[:], op=ALU.mult), reads=[o1], writes=[sq])
                    ps = c.next_pf(0, 2)
                    k.op("pe", lambda e: e.matmul(ps[:, :], lhsT=c.ones[:], rhs=sq[:], start=True, stop=True),
                         reads=[c.ones, sq], writes=[ps])
                    k.op("dve", lambda e: e.tensor_scalar(out=rs[:], in0=ps[:, :], scalar1=1.0 / 128, scalar2=DIFF_EPS,
                                                          op0=ALU.mult, op1=ALU.add), reads=[ps], writes=[rs])
                    k.op("act", lambda e: e.activation(out=rs[:], in_=rs[:], func=AF.Ln), reads=[rs], writes=[rs])
                    k.op("act", lambda e: e.activation(out=rs[:], in_=rs[:], func=AF.Exp, scale=-0.5), reads=[rs], writes=[rs])
                    k.op("dve", lambda e: e.scalar_tensor_tensor(out=ost[b][:], in0=o1[:], scalar=gs, in1=rs[:],
                                                                 op0=ALU.mult, op1=ALU.mult),
                         reads=[o1, rs, sm], writes=[ost[b]])
                    k.dma("pool", c.mixT[768 + hd * 128:768 + (hd + 1) * 128, tsl], ost[b][:], reads=[ost[b]],
                          writes=[c.mixT], waw=False)
            k.barrier()


_reg("s5_are", 16)
_reg("s5_aim", 16)
_reg("s5_ldt", 16)
_reg("s5_d", 4)
_reg("s5_bg", 2)
Z_NEG0, Z_POS0, Z_REV0 = 0, 8, 17
GELU_C = 1.5957691216057308


def sincos_alloc(k, st, name, shape):
    return dict(ni=k.sb(name + "_ni", shape, I32, st), nf=k.sb(name + "_nf", shape, F32, st),
                a2=k.sb(name + "_a2", shape, F32, st), r=k.sb(name + "_r", shape, F32, st),
                m=k.sb(name + "_m", shape, F32, st))


def sincos_tile(k, tmp, ang, sin_out, cos_out):
    ni, nf, a2, r, m = tmp["ni"], tmp["nf"], tmp["a2"], tmp["r"], tmp["m"]
    k.op("dve", lambda e: e.tensor_scalar(out=ni[:], in0=ang[:], scalar1=float(1.0 / TWO_PI), scalar2=None,
                                          op0=ALU.mult), reads=[ang], writes=[ni])
    k.op("dve", lambda e: e.tensor_copy(out=nf[:], in_=ni[:]), reads=[ni], writes=[nf])
    k.op("dve", lambda e: e.scalar_tensor_tensor(out=a2[:], in0=nf[:], scalar=-CW1, in1=ang[:], op0=ALU.mult,
                                                 op1=ALU.add), reads=[nf, ang], writes=[a2])
    k.op("dve", lambda e: e.scalar_tensor_tensor(out=a2[:], in0=nf[:], scalar=-CW2, in1=a2[:], op0=ALU.mult,
                                                 op1=ALU.add), reads=[nf, a2], writes=[a2])
    k.op("dve", lambda e: e.scalar_tensor_tensor(out=a2[:], in0=nf[:], scalar=-CW3, in1=a2[:], op0=ALU.mult,
                                                 op1=ALU.add), reads=[nf, a2], writes=[a2])
    for shift, dst in ((0.0, sin_out), (np.pi / 2, cos_out)):
        k.op("dve", lambda e: e.tensor_scalar(out=r[:], in0=a2[:], scalar1=float(shift), scalar2=None, op0=ALU.add),
             reads=[a2], writes=[r])
        k.op("dve", lambda e: e.tensor_single_scalar(out=m[:], in_=r[:], scalar=float(np.pi), op=ALU.is_gt),
             reads=[r], writes=[m])
        k.op("dve", lambda e: e.scalar_tensor_tensor(out=r[:], in0=m[:], scalar=-TWO_PI, in1=r[:], op0=ALU.mult,
                                                     op1=ALU.add), reads=[m, r], writes=[r])
        k.op("dve", lambda e: e.tensor_single_scalar(out=m[:], in_=r[:], scalar=float(-np.pi), op=ALU.is_lt),
             reads=[r], writes=[m])
        k.op("dve", lambda e: e.scalar_tensor_tensor(out=r[:], in0=m[:], scalar=TWO_PI, in1=r[:], op0=ALU.mult,
                                                     op1=ALU.add), reads=[m, r], writes=[r])
        k.op("dve", lambda e: e.tensor_scalar(out=r[:], in0=r[:], scalar1=float(np.pi), scalar2=float(-np.pi),
                                              op0=ALU.min, op1=ALU.max), reads=[r], writes=[r])
        k.op("act", lambda e: e.activation(out=dst[:], in_=r[:], func=AF.Sin), reads=[r], writes=[dst])


def stage_s5(k, c, L):
    import os as _os
    NT = 16
    pT = c.pT
    SH4 = [128, NT, 8, 16]
    with ExitStack() as stA:
        Tm = k.sb("s5_Tm", [128, 32, 128], BF16, stA)
        GstR = k.sb("s5_GstR", [128, NT, 128], BF16, stA)
        GstI = k.sb("s5_GstI", [128, NT, 128], BF16, stA)
        EfR = k.sb("s5_EfR", SH4, BF16, stA)
        EfnI = k.sb("s5_EfnI", SH4, BF16, stA)
        sc = k.sb("s5_sc", [128, 8, NT], F32, stA)
        LR, DT, ML, TH, MAG8, FRE, FIM, TMP = range(8)
        ca, ci_, cl = L.col["s5_are"], L.col["s5_aim"], L.col["s5_ldt"]
        AIM = L.prm[:, ci_:ci_ + NT]
        with ExitStack() as st:
            bsb = k.sb("s5_b", [128, NT, 2, 16], F32, st)
            csb = k.sb("s5_c", [128, NT, 2, 16], F32, st)
            k.dma("sp", bsb[:], L.s5b[:, :].rearrange("p (t r c) -> p t r c", t=NT, r=2), writes=[bsb])
            k.dma("sp", csb[:], L.s5c[:, :].rearrange("p (t r c) -> p t r c", t=NT, r=2), writes=[csb])
            k.op("dve", lambda e: e.tensor_scalar(out=sc[:, LR, :], in0=L.prm[:, ca:ca + NT], scalar1=-1e-4,
                                                  scalar2=None, op0=ALU.min), reads=[L.prm], writes=[sc])
            k.op("act", lambda e: e.activation(out=sc[:, DT, :], in_=L.prm[:, cl:cl + NT], func=AF.Exp),
                 reads=[L.prm], writes=[sc])
            k.op("dve", lambda e: e.tensor_tensor(out=sc[:, ML, :], in0=sc[:, DT, :], in1=sc[:, LR, :], op=ALU.mult),
                 reads=[sc], writes=[sc])
            k.op("dve", lambda e: e.tensor_tensor(out=sc[:, TH, :], in0=sc[:, DT, :], in1=AIM, op=ALU.mult),
                 reads=[sc, L.prm], writes=[sc])
            k.op("act", lambda e: e.activation(out=sc[:, MAG8, :], in_=sc[:, ML, :], func=AF.Exp, scale=8.0),
                 reads=[sc], writes=[sc])
            SH3 = [128, NT, 32]
            lm = k.sb("s5_lm", SH3, F32, st)
            an = k.sb("s5_an", SH3, F32, st)
            mg = k.sb("s5_mg", SH3, F32, st)
            sn = k.sb("s5_sn", SH3, F32, st)
            cs = k.sb("s5_cs", SH3, F32, st)
            zr = k.sb("s5_zr", SH3, F32, st)
            zi = k.sb("s5_zi", SH3, F32, st)
            tauB = c.cst[:, 8:40].unsqueeze(1).to_broadcast(SH3)
            k.op("dve", lambda e: e.tensor_tensor(out=lm[:], in0=sc[:, ML, :].unsqueeze(2).to_broadcast(SH3), in1=tauB,
                                                  op=ALU.mult), reads=[sc, c.cst], writes=[lm])
            k.op("dve", lambda e: e.tensor_tensor(out=an[:], in0=sc[:, TH, :].unsqueeze(2).to_broadcast(SH3), in1=tauB,
                                                  op=ALU.mult), reads=[sc, c.cst], writes=[an])
            k.op("act", lambda e: e.activation(out=mg[:], in_=lm[:], func=AF.Exp), reads=[lm], writes=[mg])
            sincos_tile(k, sincos_alloc(k, st, "s5sc0", SH3), an, sn, cs)
            k.op("dve", lambda e: e.tensor_tensor(out=zr[:], in0=mg[:], in1=cs[:], op=ALU.mult), reads=[mg, cs], writes=[zr])
            k.op("dve", lambda e: e.tensor_tensor(out=zi[:], in0=mg[:], in1=sn[:], op=ALU.mult), reads=[mg, sn], writes=[zi])
            if _os.environ.get("S5_STOP") == "a":
                k.barrier()
                return
            sm = k.sb("s5_sm", [128, 8, NT], F32, st)
            abr, abi = zr[:, :, Z_POS0 + 1], zi[:, :, Z_POS0 + 1]
            lr_ = sc[:, LR, :]

            def tt(out, a, b, op, rd, wr, E="dve"):
                k.op(E, lambda e: e.tensor_tensor(out=out, in0=a, in1=b, op=op), reads=rd, writes=wr)
            tt(sm[:, 0, :], lr_, lr_, ALU.mult, [sc], [sm])
            tt(sm[:, 1, :], AIM, AIM, ALU.mult, [L.prm], [sm])
            tt(sm[:, 0, :], sm[:, 0, :], sm[:, 1, :], ALU.add, [sm], [sm])
            k.op("dve", lambda e: e.reciprocal(out=sm[:, 0, :], in_=sm[:, 0, :]), reads=[sm], writes=[sm])
            k.op("dve", lambda e: e.tensor_scalar(out=sm[:, 1, :], in0=abr, scalar1=-1.0, scalar2=None, op0=ALU.add),
                 reads=[zr], writes=[sm])
            tt(sm[:, 2, :], sm[:, 1, :], lr_, ALU.mult, [sm, sc], [sm])
            tt(sm[:, 3, :], abi, AIM, ALU.mult, [zi, L.prm], [sm])
            tt(sm[:, 2, :], sm[:, 2, :], sm[:, 3, :], ALU.add, [sm], [sm])
            tt(sc[:, FRE, :], sm[:, 2, :], sm[:, 0, :], ALU.mult, [sm], [sc])
            tt(sm[:, 4, :], abi, lr_, ALU.mult, [zi, sc], [sm])
            tt(sm[:, 5, :], sm[:, 1, :], AIM, ALU.mult, [sm, L.prm], [sm])
            tt(sm[:, 4, :], sm[:, 4, :], sm[:, 5, :], ALU.subtract, [sm], [sm])
            tt(sc[:, FIM, :], sm[:, 4, :], sm[:, 0, :], ALU.mult, [sm], [sc])
            if _os.environ.get("S5_STOP") == "b":
                k.barrier()
                return
            SHB = [128, NT, 16]
            bbr = k.sb("s5_bbr", SHB, F32, st)
            bbi = k.sb("s5_bbi", SHB, F32, st)
            t1 = k.sb("s5_t1", SHB, F32, st)
            fre = sc[:, FRE, :].unsqueeze(2).to_broadcast(SHB)
            fim = sc[:, FIM, :].unsqueeze(2).to_broadcast(SHB)
            br_, bi_ = bsb[:, :, 0, :], bsb[:, :, 1, :]
            tt(bbr[:], fre, br_, ALU.mult, [sc, bsb], [bbr])
            tt(t1[:], fim, bi_, ALU.mult, [sc, bsb], [t1])
            tt(bbr[:], bbr[:], t1[:], ALU.subtract, [bbr, t1], [bbr])
            tt(bbi[:], fre, bi_, ALU.mult, [sc, bsb], [bbi])
            tt(t1[:], fim, br_, ALU.mult, [sc, bsb], [t1])
            tt(bbi[:], bbi[:], t1[:], ALU.add, [bbi, t1], [bbi])
            if _os.environ.get("S5_STOP") == "c":
                k.barrier()
                return
            BfR = k.sb("s5_BfR", SH4, BF16, st)
            BfnI = k.sb("s5_BfnI", SH4, BF16, st)
            CfR = k.sb("s5_CfR", SH4, BF16, st)
            CfI = k.sb("s5_CfI", SH4, BF16, st)
            GfR = k.sb("s5_GfR", SH4, BF16, st)
            GfI = k.sb("s5_GfI", SH4, BF16, st)
            u1 = [k.sb(f"s5_u1{i}", SH4, F32, st) for i in range(2)]
            u2 = [k.sb(f"s5_u2{i}", SH4, F32, st) for i in range(2)]

            def cmul(oR, oI, z0, xr, xi, xdeps, neg_im, i):
                E = "dve" if i % 2 == 0 else "pool"
                zR = zr[:, :, z0:z0 + 8].unsqueeze(3).to_broadcast(SH4)
                zI = zi[:, :, z0:z0 + 8].unsqueeze(3).to_broadcast(SH4)
                xR = xr.unsqueeze(2).to_broadcast(SH4)
                xI = xi.unsqueeze(2).to_broadcast(SH4)
                a, b_ = u1[i % 2], u2[i % 2]
                tt(a[:], zR, xR, ALU.mult, [zr, ] + xdeps, [a], E)
                tt(b_[:], zI, xI, ALU.mult, [zi, ] + xdeps, [b_], E)
                tt(oR[:], a[:], b_[:], ALU.subtract, [a, b_], [oR], E)
                tt(a[:], zR, xI, ALU.mult, [zr, ] + xdeps, [a], E)
                tt(b_[:], zI, xR, ALU.mult, [zi, ] + xdeps, [b_], E)
                if neg_im:
                    k.op("dve", lambda e: e.scalar_tensor_tensor(out=oI[:], in0=a[:], scalar=-1.0, in1=b_[:],
                                                                 op0=ALU.mult, op1=ALU.subtract),
                         reads=[a, b_], writes=[oI])
                else:
                    tt(oI[:], a[:], b_[:], ALU.add, [a, b_], [oI], E)
            cr_, ci2 = csb[:, :, 0, :], csb[:, :, 1, :]
            cmul(BfR, BfnI, Z_NEG0, bbr[:], bbi[:], [bbr, bbi], True, 0)
            cmul(CfR, CfI, Z_POS0, cr_, ci2, [csb], False, 1)
            cmul(GfR, GfI, Z_REV0, bbr[:], bbi[:], [bbr, bbi], False, 0)
            cmul(EfR, EfnI, Z_POS0 + 1, cr_, ci2, [csb], True, 1)
            if _os.environ.get("S5_STOP") == "d":
                k.barrier()
                return
            for t4 in range(4):
                for hh in range(2):
                    ps = c.next_pf()
                    hs = slice(hh * 64, (hh + 1) * 64)
                    for q in range(4):
                        t = t4 * 4 + q
                        k.op("pe", lambda e: e.matmul(ps[:, q * 128:(q + 1) * 128],
                                                      lhsT=BfR[hs, t, :, :].rearrange("p a b -> p (a b)"),
                                                      rhs=CfR[hs, t, :, :].rearrange("p a b -> p (a b)"), start=True, stop=False),
                             reads=[BfR, CfR], writes=[ps], inc=False)
                        k.op("pe", lambda e: e.matmul(ps[:, q * 128:(q + 1) * 128],
                                                      lhsT=BfnI[hs, t, :, :].rearrange("p a b -> p (a b)"),
                                                      rhs=CfI[hs, t, :, :].rearrange("p a b -> p (a b)"), start=False, stop=True),
                             reads=[BfnI, CfI], writes=[ps], inc=(q == 3))
                    for q in range(4):
                        g = 2 * (t4 * 4 + q) + hh
                        k.op("dve", lambda e: e.tensor_tensor(out=Tm[:, g, :], in0=ps[:, q * 128:(q + 1) * 128],
                                                              in1=c.tmask[:], op=ALU.mult),
                             reads=[ps, c.tmask], writes=[Tm])
            if _os.environ.get("S5_STOP") == "e":
                k.barrier()
                return
            for (src, dst) in ((GfR, GstR), (GfI, GstI)):
                for t8 in range(2):
                    pb = c.next_pb()
                    for q in range(8):
                        t = t8 * 8 + q
                        k.op("pe", lambda e: e.transpose(pb[:, q * 128:(q + 1) * 128],
                                                         src[:, t, :, :].rearrange("p a b -> p (a b)"), c.ident[:]),
                             reads=[src, c.ident], writes=[pb], inc=(q == 7))
                    k.op("act", lambda e: e.copy(out=dst[:, t8 * 8:(t8 + 1) * 8, :],
                                                 in_=pb[:].rearrange("p (q n) -> p q n", q=8)),
                         reads=[pb], writes=[dst])
            k.barrier()
        import os as _os
        if _os.environ.get("S5_STOP") == "1":
            return
        with ExitStack() as st:
            ug = [[k.sb(f"s5_u{i}{j}", [128, TCH], BF16, st) for j in range(2)] for i in range(2)]
            ang = k.sb("s5_ang", [128, TCH], F32, st)
            sn = k.sb("s5_snj", [128, TCH], F32, st)
            cs = k.sb("s5_csj", [128, TCH], F32, st)
            a_ = k.sb("s5_a", [128, TCH], F32, st)
            b_ = k.sb("s5_bq", [128, TCH], F32, st)
            mre = k.sb("s5_mre", [128, TCH], F32, st)
            mim = k.sb("s5_mim", [128, TCH], F32, st)
            hre = k.sb("s5_hre", [128, TCH], F32, st)
            him = k.sb("s5_him", [128, TCH], F32, st)
            Hp = [[k.sb(f"s5_Hp{i}{j}", [128, TCH + 1], BF16, st) for j in range(2)] for i in range(2)]
            yst = [k.sb(f"s5_y{i}", [128, TCH], F32, st) for i in range(3)]
            for i in range(2):
                for j in range(2):
                    k.op("pool", lambda e: e.memset(Hp[i][j][:, 0:1], 0.0), writes=[Hp[i][j]])
            sctmp = sincos_alloc(k, st, "s5scj", [128, TCH])
            jv = c.cst[:, 40:552]
            nyi = 0

            def tt(out, a, b, op, rd, wr, E="dve"):
                k.op(E, lambda e: e.tensor_tensor(out=out, in0=a, in1=b, op=op), reads=rd, writes=wr)
            if True:
                for t in range(NT):
                    pp = t % 2
                    for hh in range(2):
                        k.dma("sp", ug[pp][hh][:], c.us5[2 * t + hh, :, :], reads=[c.us5], writes=[ug[pp][hh]])
                    Xre, Xim = c.pf[0], c.pf[1]
                    for hh in range(2):
                        hs = slice(hh * 64, (hh + 1) * 64)
                        k.op("pe", lambda e: e.matmul(Xre[hs, :], lhsT=GstR[:, t, hs], rhs=ug[pp][hh][:], start=True, stop=True),
                             reads=[GstR, ug[pp][hh]], writes=[Xre], inc=(hh == 1))
                    for hh in range(2):
                        hs = slice(hh * 64, (hh + 1) * 64)
                        k.op("pe", lambda e: e.matmul(Xim[hs, :], lhsT=GstI[:, t, hs], rhs=ug[pp][hh][:], start=True, stop=True),
                             reads=[GstI, ug[pp][hh]], writes=[Xim], inc=(hh == 1))
                    k.op("pool", lambda e: e.tensor_scalar(out=ang[:], in0=jv, scalar1=sc[:, TH, t:t + 1], scalar2=None,
                                                           op0=ALU.mult), reads=[c.cst, sc], writes=[ang])
                    sincos_tile(k, sctmp, ang, sn, cs)
                    tt(a_[:], Xre[:, :], cs[:], ALU.mult, [Xre, cs], [a_])
                    tt(b_[:], Xim[:, :], sn[:], ALU.mult, [Xim, sn], [b_])
                    tt(mre[:], a_[:], b_[:], ALU.add, [a_, b_], [mre], "pool")
                    tt(a_[:], Xim[:, :], cs[:], ALU.mult, [Xim, cs], [a_])
                    tt(b_[:], Xre[:, :], sn[:], ALU.mult, [Xre, sn], [b_])
                    tt(mim[:], a_[:], b_[:], ALU.subtract, [a_, b_], [mim], "pool")
                    m8 = sc[:, MAG8, t:t + 1].to_broadcast([128, TCH])
                    k.op("dve", lambda e: e.tensor_tensor_scan(out=hre[:], data0=m8, data1=mre[:], initial=0.0,
                                                               op0=ALU.mult, op1=ALU.add), reads=[sc, mre], writes=[hre])
                    k.op("dve", lambda e: e.tensor_tensor_scan(out=him[:], data0=m8, data1=mim[:], initial=0.0,
                                                               op0=ALU.mult, op1=ALU.add), reads=[sc, mim], writes=[him])
                    hr, hi_ = Hp[pp][0], Hp[pp][1]
                    tt(a_[:], hre[:], cs[:], ALU.mult, [hre, cs], [a_])
                    tt(b_[:], him[:], sn[:], ALU.mult, [him, sn], [b_], "pool")
                    tt(hr[:, 1:TCH + 1], a_[:], b_[:], ALU.subtract, [a_, b_], [hr])
                    tt(mre[:], hre[:], sn[:], ALU.mult, [hre, sn], [mre])
                    tt(mim[:], him[:], cs[:], ALU.mult, [him, cs], [mim], "pool")
                    tt(hi_[:, 1:TCH + 1], mre[:], mim[:], ALU.add, [mre, mim], [hi_])
                    for hh in range(2):
                        g = 2 * t + hh
                        hs = slice(hh * 64, (hh + 1) * 64)
                        py = c.next_pf(2, 6)
                        k.op("pe", lambda e: e.matmul(py[:, :], lhsT=Tm[:, g, :], rhs=ug[pp][hh][:], start=True, stop=False),
                             reads=[Tm, ug[pp][hh]], writes=[py], inc=False)
                        k.op("pe", lambda e: e.matmul(py[:, :], lhsT=EfR[hs, t, :, :].rearrange("p a b -> p (a b)"),
                                                      rhs=hr[hs, 0:TCH], start=False, stop=False),
                             reads=[EfR, hr], writes=[py], inc=False)
                        k.op("pe", lambda e: e.matmul(py[:, :], lhsT=EfnI[hs, t, :, :].rearrange("p a b -> p (a b)"),
                                                      rhs=hi_[hs, 0:TCH], start=False, stop=True),
                             reads=[EfnI, hi_], writes=[py], inc=True)
                        ys = yst[nyi % 3]
                        nyi += 1
                        k.op("act", lambda e: e.copy(out=ys[:], in_=py[:, :]), reads=[py], writes=[ys])
                        k.dma("pool", c.ys5[g, :, :], ys[:], reads=[ys], writes=[c.ys5], waw=False)
    k.barrier()
    if _os.environ.get("S5_STOP") == "2":
        return
    cd = L.col["s5_d"]
    with ExitStack() as st:
        Y = k.sb("s5p_Y", [128, 8, TCH], F32, st)
        U = k.sb("s5p_U", [128, S], F32, st)
        yy = k.sb("s5p_yy", [128, S], F32, st)
        q1 = k.sb("s5p_q1", [128, S], F32, st)
        gb = k.sb("s5p_gb", [128, S], BF16, st)
        for ct in range(4):
            for gl in range(8):
                k.dma("sp", Y[gl * 16:(gl + 1) * 16, :, :],
                      c.ys5[ct * 8 + gl, :, :].rearrange("(t c) j -> c t j", c=16), reads=[c.ys5], writes=[Y],
                      waw=(gl == 0))
            k.dma("sp", U[:], pT[R_U + ct * 128:R_U + (ct + 1) * 128, :], reads=[pT], writes=[U])
            k.op("dve", lambda e: e.scalar_tensor_tensor(out=yy[:].rearrange("p (j t) -> p j t", t=8),
                                                         in0=U[:].rearrange("p (j t) -> p j t", t=8),
                                                         scalar=L.prm[:, cd + ct:cd + ct + 1],
                                                         in1=Y[:].rearrange("p t j -> p j t"),
                                                         op0=ALU.mult, op1=ALU.add), reads=[U, Y, L.prm], writes=[yy])
            k.op("act", lambda e: e.activation(out=q1[:], in_=yy[:], func=AF.Square), reads=[yy], writes=[q1])
            k.op("dve", lambda e: e.tensor_scalar(out=q1[:], in0=q1[:], scalar1=0.044715, scalar2=1.0, op0=ALU.mult,
                                                  op1=ALU.add), reads=[q1], writes=[q1])
            k.op("pool", lambda e: e.tensor_tensor(out=q1[:], in0=q1[:], in1=yy[:], op=ALU.mult), reads=[q1, yy], writes=[q1])
            k.op("act", lambda e: e.activation(out=q1[:], in_=q1[:], func=AF.Sigmoid, scale=GELU_C), reads=[q1], writes=[q1])
            k.op("dve", lambda e: e.tensor_tensor(out=yy[:], in0=yy[:], in1=q1[:], op=ALU.mult), reads=[yy, q1], writes=[yy])
            k.op("pool", lambda e: e.tensor_copy(out=gb[:], in_=yy[:]), reads=[yy], writes=[gb])
            k.dma("pool", c.gT[ct * 128:(ct + 1) * 128, :], gb[:], reads=[gb], writes=[c.gT], waw=False)
            if ct < 2:
                k.dma("pool", c.gF[ct * 128:(ct + 1) * 128, :], yy[:], reads=[yy], writes=[c.gF], waw=False)
    k.barrier()
    cb = L.col["s5_bg"]
    with ExitStack() as st:
        wbf, wd = load_w_bf16(k, st, lambda kc: L.wglu[kc * 128:(kc + 1) * 128, :], 4, 256, "wglu")
        gin = [k.sb(f"s5g_g{i}", [128, TCH], F32, st) for i in range(3)]
        sg = [k.sb(f"s5g_s{i}", [128, TCH], F32, st) for i in range(3)]
        n = [0]

        def epi(tc, ni, ps, ncol):
            i = n[0] % 3
            n[0] += 1
            tsl = slice(tc * TCH, (tc + 1) * TCH)
            k.dma("sp", gin[i][:], c.gF[ni * 128:(ni + 1) * 128, tsl], reads=[c.gF], writes=[gin[i]])
            k.op("act", lambda e: e.activation(out=sg[i][:], in_=ps[:, :], func=AF.Sigmoid,
                                               bias=L.prm[:, cb + ni:cb + ni + 1], scale=1.0),
                 reads=[ps, L.prm], writes=[sg[i]])
            k.op("dve", lambda e: e.tensor_tensor(out=sg[i][:], in0=sg[i][:], in1=gin[i][:], op=ALU.mult),
                 reads=[sg[i], gin[i]], writes=[sg[i]])
            k.dma("pool", c.mixT[256 + ni * 128:256 + (ni + 1) * 128, tsl], sg[i][:], reads=[sg[i]], writes=[c.mixT],
                  waw=False)
        linear_fm(k, c, st, c.gT, 4, wbf, wd, [(0, 128), (128, 128)], epi, "s5g")
    k.barrier()


for _n, _w in (("rw_mur", 2), ("rw_muk", 2), ("rw_muv", 2), ("rw_mul", 1), ("rw_w0", 2), ("rw_a0", 2),
               ("rw_kk", 2), ("rw_ka", 2), ("rw_rk", 2), ("rw_lng", 2), ("rw_lnb", 2)):
    _reg(_n, _w)
R_RV = 3584
SEG = 1024
NCK = SEG // 64
RW_EPS = 64e-5
LDC = -0.6065306597126334


def stage_rwkv(k, c, L):
    pT = c.pT
    col = L.col
    with ExitStack() as stA:
        P = lambda nm, j: L.prm[:, col[nm] + j:col[nm] + j + 1]
        wl = k.sb("rw_wl", [128, 256], BF16, stA)
        omka = k.sb("rw_omka", [128, 2], F32, stA)
        rmask = k.sb("rw_rmask", [128, SEG], F32, stA)
        with ExitStack() as st0:
            wlf = k.sb("rw_wlf", [128, 256], F32, st0)
            k.dma("sp", wlf[:], L.w2a2[:, :], writes=[wlf])
            k.op("dve", lambda e: e.tensor_copy(out=wl[:], in_=wlf[:]), reads=[wlf], writes=[wl])
            k.op("dve", lambda e: e.tensor_scalar(out=omka[:], in0=L.prm[:, col["rw_ka"]:col["rw_ka"] + 2], scalar1=-1.0,
                                                  scalar2=1.0, op0=ALU.mult, op1=ALU.add), reads=[L.prm], writes=[omka])
            k.op("pool", lambda e: e.memset(rmask[:], 1.0), writes=[rmask])
            k.op("pool", lambda e: e.memset(rmask[:].rearrange("p (c i) -> p c i", i=64)[:, :, 0:1], 0.0), writes=[rmask])
            k.barrier()
        F = lambda nm: k.sb("rw_" + nm, [128, SEG], F32, stA)
        rz = k.sb("rw_rz", [128, SEG + 1], F32, stA)
        kz = k.sb("rw_kz", [128, SEG + 1], F32, stA)
        vz = k.sb("rw_vz", [128, SEG + 1], F32, stA)
        lz = k.sb("rw_lz", [128, SEG + 1], F32, stA)
        rm, km, vm, lm, t1, t2, sgw, av, cl, Epos, Eneg, Eex, Eh, kkr, kk, k2, bv = (F(n) for n in (
            "rm", "km", "vm", "lm", "t1", "t2", "sgw", "av", "cl", "Epos", "Eneg", "Eex", "Eh", "kkr", "kk", "k2", "bv"))
        lbf = k.sb("rw_lbf", [128, SEG], BF16, stA)
        sqb = k.sb("rw_sqb", [128, SEG], BF16, stA)
        BDn = ("AT", "BT", "KT", "RT", "bhT", "khT", "vT", "Bh", "Kh", "Vb", "Lst", "Mst", "LakT", "ArbT", "ArkT", "TT")
        BD = {n: k.sb("rw_bd_" + n, [128, NCK, 128], BF16, stA) for n in BDn}
        Ln = [k.sb(f"rw_Ln{i}", [128, 8, 128], BF16, stA) for i in range(2)]
        Mn = [k.sb(f"rw_Mn{i}", [128, 8, 128], BF16, stA) for i in range(2)]
        Pn = [k.sb(f"rw_Pn{i}", [128, 8, 128], BF16, stA) for i in range(2)]
        Sf = k.sb("rw_Sf", [128, 128], F32, stA)
        Sb = [k.sb(f"rw_Sb{i}", [128, 128], BF16, stA) for i in range(2)]
        RHSb = k.sb("rw_RHSb", [128, 128], BF16, stA)
        Ub = k.sb("rw_Ub", [128, 128], BF16, stA)
        OT = k.sb("rw_OT", [128, SEG], F32, stA)
        ob = k.sb("rw_ob", [128, SEG], BF16, stA)
        yo = [k.sb(f"rw_yo{i}", [128, SEG], F32, stA) for i in range(2)]
        for n in ("AT", "BT", "KT", "RT", "bhT", "khT", "vT"):
            k.op("pool", lambda e: e.memset(BD[n][:], 0.0), writes=[BD[n]])

        def tt(out, a, b, op, rd, wr, E="dve"):
            k.op(E, lambda e: e.tensor_tensor(out=out, in0=a, in1=b, op=op), reads=rd, writes=wr)

        def act(out, in_, func, rd, wr, **kw):
            k.op("act", lambda e: e.activation(out=out, in_=in_, func=func, **kw), reads=rd, writes=wr)

        def bdw(dst, a, b, rd, E="dve", scalar=None):
            for h in range(2):
                hs = slice(h * 64, (h + 1) * 64)
                o = dst[hs, :, h * 64:(h + 1) * 64]
                av_ = a[hs, :].rearrange("p (c i) -> p c i", i=64)
                if b is None:
                    k.op(E, lambda e: e.tensor_copy(out=o, in_=av_), reads=rd, writes=[dst])
                    continue
                bv_ = b[hs, :].rearrange("p (c i) -> p c i", i=64)
                if scalar is None:
                    k.op(E, lambda e: e.tensor_tensor(out=o, in0=av_, in1=bv_, op=ALU.mult), reads=rd, writes=[dst])
                else:
                    k.op("dve", lambda e: e.scalar_tensor_tensor(out=o, in0=av_, scalar=float(scalar), in1=bv_,
                                                                 op0=ALU.mult, op1=ALU.mult), reads=rd, writes=[dst])

        for hp in range(2):
            k.op("dve", lambda e: e.memset(Sf[:], 0.0), writes=[Sf])
            k.op("dve", lambda e: e.memset(Sb[0][:], 0.0), writes=[Sb[0]])
            sbi = 0
            for seg in range(S // SEG):
                t0 = seg * SEG
                for (zt, r0, mu, dst) in ((rz, R_R + hp * 128, P("rw_mur", hp), rm), (kz, R_K + hp * 128, P("rw_muk", hp), km),
                                          (vz, R_RV + hp * 128, P("rw_muv", hp), vm), (lz, R_LORA, P("rw_mul", 0), lm)):
                    if seg == 0:
                        k.op("pool", lambda e: e.memset(zt[:, 0:1], 0.0), writes=[zt])
                        k.dma("sp", zt[:, 1:SEG + 1], pT[r0:r0 + 128, 0:SEG], reads=[pT], writes=[zt])
                    else:
                        k.dma("sp", zt[:, 0:SEG + 1], pT[r0:r0 + 128, t0 - 1:t0 + SEG], reads=[pT], writes=[zt])
                    tt(t1[:], zt[:, 0:SEG], zt[:, 1:SEG + 1], ALU.subtract, [zt], [t1], "pool")
                    k.op("dve", lambda e: e.scalar_tensor_tensor(out=dst[:], in0=t1[:], scalar=mu, in1=zt[:, 1:SEG + 1],
                                                                 op0=ALU.mult, op1=ALU.add), reads=[t1, zt, L.prm], writes=[dst])
                act(lbf[0:64, :], lm[0:64, :], AF.Tanh, [lm], [lbf])
                k.op("dve", lambda e: e.tensor_copy(out=lbf[64:128, :], in_=lm[64:128, :]), reads=[lm], writes=[lbf])
                for hb in range(SEG // TCH):
                    cs_ = slice(hb * TCH, (hb + 1) * TCH)
                    pw, pa = c.pf[0], c.pf[1]
                    k.op("pe", lambda e: e.matmul(pw[:, :], lhsT=wl[0:64, hp * 128:(hp + 1) * 128], rhs=lbf[0:64, cs_],
                                                  start=True, stop=True), reads=[wl, lbf], writes=[pw])
                    k.op("pe", lambda e: e.matmul(pa[:, :], lhsT=wl[64:128, hp * 128:(hp + 1) * 128], rhs=lbf[64:128, cs_],
                                                  start=True, stop=True), reads=[wl, lbf], writes=[pa])
                    act(sgw[:, cs_], pw[:, :], AF.Sigmoid, [pw, L.prm], [sgw], bias=P("rw_w0", hp), scale=1.0)
                    act(av[:, cs_], pa[:, :], AF.Sigmoid, [pa, L.prm], [av], bias=P("rw_a0", hp), scale=1.0)
                k.op("pool", lambda e: e.tensor_scalar(out=sgw[:], in0=sgw[:], scalar1=LDC, scalar2=None, op0=ALU.mult),
                     reads=[sgw], writes=[sgw])
                k.op("dve", lambda e: e.tensor_tensor_scan(out=cl[:], data0=rmask[:], data1=sgw[:], initial=0.0,
                                                           op0=ALU.mult, op1=ALU.add), reads=[rmask, sgw], writes=[cl])
                act(Epos[:], cl[:], AF.Exp, [cl], [Epos])
                act(Eneg[:], cl[:], AF.Exp, [cl], [Eneg], scale=-1.0)
                tt(t1[:], cl[:], sgw[:], ALU.subtract, [cl, sgw], [t1], "pool")
                act(Eex[:], t1[:], AF.Exp, [t1], [Eex])
                clC = cl[:].rearrange("p (c i) -> p c i", i=64)[:, :, 63:64].to_broadcast([128, NCK, 64])
                tt(t2[:].rearrange("p (c i) -> p c i", i=64), clC, cl[:].rearrange("p (c i) -> p c i", i=64),
                   ALU.subtract, [cl], [t2])
                act(Eh[:], t2[:], AF.Exp, [t2], [Eh])
                k.op("dve", lambda e: e.tensor_scalar(out=kkr[:], in0=km[:], scalar1=P("rw_kk", hp), scalar2=None,
                                                      op0=ALU.mult), reads=[km, L.prm], writes=[kkr])
                act(sqb[:], kkr[:], AF.Square, [kkr], [sqb])
                for hb in range(SEG // TCH):
                    cs_ = slice(hb * TCH, (hb + 1) * TCH)
                    pn = c.next_pf(2, 6)
                    k.op("pe", lambda e: e.matmul(pn[:, :], lhsT=c.blk1[:], rhs=sqb[:, cs_], start=True, stop=True),
                         reads=[c.blk1, sqb], writes=[pn])
                    k.op("dve", lambda e: e.tensor_scalar(out=t1[:, cs_], in0=pn[:, :], scalar1=1e-24, scalar2=None, op0=ALU.max),
                         reads=[pn], writes=[t1])
                act(t1[:], t1[:], AF.Ln, [t1], [t1])
                act(t1[:], t1[:], AF.Exp, [t1], [t1], scale=-0.5)
                tt(kk[:], kkr[:], t1[:], ALU.mult, [kkr, t1], [kk])
                k.op("dve", lambda e: e.tensor_scalar(out=t2[:], in0=av[:], scalar1=P("rw_ka", hp), scalar2=omka[:, hp:hp + 1],
                                                      op0=ALU.mult, op1=ALU.add), reads=[av, L.prm, omka], writes=[t2])
                tt(k2[:], km[:], t2[:], ALU.mult, [km, t2], [k2], "pool")
                tt(bv[:], kk[:], av[:], ALU.mult, [kk, av], [bv], "pool")
                bdw(BD["AT"], kk, Eex, [kk, Eex], scalar=-1.0)
                bdw(BD["BT"], bv, Eneg, [bv, Eneg], "pool")
                bdw(BD["KT"], k2, Eneg, [k2, Eneg], "dve")
                bdw(BD["RT"], rm, Epos, [rm, Epos], "pool")
                bdw(BD["bhT"], bv, Eh, [bv, Eh], "dve")
                bdw(BD["khT"], k2, Eh, [k2, Eh], "pool")
                bdw(BD["vT"], vm, None, [vm], "dve")
                for oc in range(NCK // 8):
                    c8 = slice(oc * 8, (oc + 1) * 8)
                    prods = (("AT", "BT", "Lst", c.mSL), ("BT", "AT", "Mst", c.mSU), ("KT", "AT", "LakT", c.mSU),
                             ("BT", "RT", "ArbT", c.mUI), ("KT", "RT", "ArkT", c.mUI))
                    for (la, rb, dn, mk) in prods:
                        for g4 in range(2):
                            ps = c.next_pf()
                            for q in range(4):
                                cc = oc * 8 + g4 * 4 + q
                                k.op("pe", lambda e: e.matmul(ps[:, q * 128:(q + 1) * 128], lhsT=BD[la][:, cc, :],
                                                              rhs=BD[rb][:, cc, :], start=True, stop=True),
                                     reads=[BD[la], BD[rb]], writes=[ps], inc=(q == 3))
                            c4 = slice(oc * 8 + g4 * 4, oc * 8 + g4 * 4 + 4)
                            tt(BD[dn][:, c4, :].rearrange("p a b -> p (a b)"), ps[:, :], mk[:], ALU.mult, [ps, mk], [BD[dn]])
                    for (src, dst) in (("bhT", "Bh"), ("khT", "Kh"), ("vT", "Vb")):
                        pb = c.next_pb()
                        for q in range(8):
                            cc = oc * 8 + q
                            k.op("pe", lambda e: e.transpose(pb[:, q * 128:(q + 1) * 128], BD[src][:, cc, :], c.ident[:]),
                                 reads=[BD[src], c.ident], writes=[pb], inc=(q == 7))
                        k.op("act", lambda e: e.copy(out=BD[dst][:, c8, :].rearrange("p a b -> p (a b)"), in_=pb[:]),
                             reads=[pb], writes=[BD[dst]])
                    Lc, Mc, Pc = BD["Lst"][:, c8, :], BD["Mst"][:, c8, :], None
                    Ld, Md = BD["Lst"], BD["Mst"]
                    k.op("dve", lambda e: e.tensor_tensor(out=Pn[0][:].rearrange("p a b -> p (a b)"),
                                                          in0=BD["Mst"][:, c8, :].rearrange("p a b -> p (a b)"),
                                                          in1=c.ident8[:], op=ALU.add), reads=[BD["Mst"], c.ident8], writes=[Pn[0]])
                    pcur = 0
                    for n in range(1, 6):
                        di = n % 2
                        psL = [c.pf[0], c.pf[1]]
                        psM = [c.pf[2], c.pf[3]]
                        for g4 in range(2):
                            for q in range(4):
                                qq = g4 * 4 + q
                                k.op("pe", lambda e: e.matmul(psL[g4][:, q * 128:(q + 1) * 128], lhsT=Mc[:, qq, :], rhs=Lc[:, qq, :],
                                                              start=True, stop=True), reads=[Md, Ld], writes=[psL[g4]], inc=(q == 3))
                            if n < 5:
                                for q in range(4):
                                    qq = g4 * 4 + q
                                    k.op("pe", lambda e: e.matmul(psM[g4][:, q * 128:(q + 1) * 128], lhsT=Lc[:, qq, :], rhs=Mc[:, qq, :],
                                                                  start=True, stop=True), reads=[Ld, Md], writes=[psM[g4]], inc=(q == 3))
                        for g4 in range(2):
                            k.op("act", lambda e: e.copy(out=Ln[di][:, g4 * 4:(g4 + 1) * 4, :].rearrange("p a b -> p (a b)"),
                                                         in_=psL[g4][:, :]), reads=[psL[g4]], writes=[Ln[di]])
                            if n < 5:
                                k.op("dve", lambda e: e.tensor_copy(out=Mn[di][:, g4 * 4:(g4 + 1) * 4, :].rearrange("p a b -> p (a b)"),
                                                                    in_=psM[g4][:, :]), reads=[psM[g4]], writes=[Mn[di]])
                        Lc, Mc, Ld, Md = Ln[di][:, :, :], Mn[di][:, :, :], Ln[di], Mn[di]
                        psP = [c.pf[4], c.pf[5]]
                        pnx = 1 - pcur
                        for g4 in range(2):
                            for q in range(4):
                                qq = g4 * 4 + q
                                k.op("pe", lambda e: e.matmul(psP[g4][:, q * 128:(q + 1) * 128], lhsT=Lc[:, qq, :], rhs=Pn[pcur][:, qq, :],
                                                              start=True, stop=True), reads=[Ld, Pn[pcur]], writes=[psP[g4]], inc=(q == 3))
                        for g4 in range(2):
                            g4s = slice(g4 * 4, (g4 + 1) * 4)
                            if n < 5:
                                o_ = Pn[pnx][:, g4s, :].rearrange("p a b -> p (a b)")
                                wr = Pn[pnx]
                            else:
                                o_ = BD["TT"][:, oc * 8 + g4 * 4:oc * 8 + g4 * 4 + 4, :].rearrange("p a b -> p (a b)")
                                wr = BD["TT"]
                            tt(o_, psP[g4][:, :], Pn[pcur][:, g4s, :].rearrange("p a b -> p (a b)"), ALU.add,
                               [psP[g4], Pn[pcur]], [wr])
                        pcur = pnx
                for cc in range(NCK):
                    So = Sb[sbi]
                    Sn = Sb[1 - sbi]
                    pR, pU, pS, pO = c.pf[0], c.pf[1], c.pf[2], c.pf[3]
                    k.op("pe", lambda e: e.matmul(pR[:, 0:128], lhsT=BD["LakT"][:, cc, :], rhs=BD["Vb"][:, cc, :], start=True, stop=False),
                         reads=[BD["LakT"], BD["Vb"]], writes=[pR], inc=False)
                    k.op("pe", lambda e: e.matmul(pR[:, 0:128], lhsT=BD["AT"][:, cc, :], rhs=So[:], start=False, stop=True),
                         reads=[BD["AT"], So], writes=[pR])
                    k.op("act", lambda e: e.copy(out=RHSb[:], in_=pR[:, 0:128]), reads=[pR], writes=[RHSb])
                    k.op("pe", lambda e: e.matmul(pU[:, 0:128], lhsT=BD["TT"][:, cc, :], rhs=RHSb[:], start=True, stop=True),
                         reads=[BD["TT"], RHSb], writes=[pU])
                    k.op("dve", lambda e: e.tensor_copy(out=Ub[:], in_=pU[:, 0:128]), reads=[pU], writes=[Ub])
                    k.op("pe", lambda e: e.matmul(pS[:, 0:128], lhsT=BD["Kh"][:, cc, :], rhs=BD["Vb"][:, cc, :], start=True, stop=False),
                         reads=[BD["Kh"], BD["Vb"]], writes=[pS], inc=False)
                    k.op("pe", lambda e: e.matmul(pS[:, 0:128], lhsT=BD["Bh"][:, cc, :], rhs=Ub[:], start=False, stop=True),
                         reads=[BD["Bh"], Ub], writes=[pS])
                    gC = Epos[:, cc * 64 + 63:cc * 64 + 64]
                    k.op("dve", lambda e: e.scalar_tensor_tensor(out=Sf[:], in0=Sf[:], scalar=gC, in1=pS[:, 0:128],
                                                                 op0=ALU.mult, op1=ALU.add), reads=[Sf, Epos, pS], writes=[Sf])
                    k.op("act", lambda e: e.copy(out=Sn[:], in_=Sf[:]), reads=[Sf], writes=[Sn])
                    k.op("pe", lambda e: e.matmul(pO[:, 0:128], lhsT=BD["Vb"][:, cc, :], rhs=BD["ArkT"][:, cc, :], start=True, stop=False),
                         reads=[BD["Vb"], BD["ArkT"]], writes=[pO], inc=False)
                    k.op("pe", lambda e: e.matmul(pO[:, 0:128], lhsT=So[:], rhs=BD["RT"][:, cc, :], start=False, stop=False),
                         reads=[So, BD["RT"]], writes=[pO], inc=False)
                    k.op("pe", lambda e: e.matmul(pO[:, 0:128], lhsT=Ub[:], rhs=BD["ArbT"][:, cc, :], start=False, stop=True),
                         reads=[Ub, BD["ArbT"]], writes=[pO])
                    for h in range(2):
                        hs = slice(h * 64, (h + 1) * 64)
                        k.op("pool" if False else "act", lambda e: e.copy(out=OT[hs, cc * 64:(cc + 1) * 64], in_=pO[hs, h * 64:(h + 1) * 64]),
                             reads=[pO], writes=[OT])
                    sbi = 1 - sbi
                y = yo[seg % 2]
                k.op("pool", lambda e: e.tensor_copy(out=ob[:], in_=OT[:]), reads=[OT], writes=[ob])
                for hb in range(SEG // TCH):
                    cs_ = slice(hb * TCH, (hb + 1) * TCH)
                    pm = c.next_pf(4, 6)
                    k.op("pe", lambda e: e.matmul(pm[:, :], lhsT=c.blk64[:], rhs=ob[:, cs_], start=True, stop=True),
                         reads=[c.blk64, ob], writes=[pm])
                    tt(t1[:, cs_], OT[:, cs_], pm[:, :], ALU.subtract, [OT, pm], [t1])
                act(sqb[:], t1[:], AF.Square, [t1], [sqb])
                for hb in range(SEG // TCH):
                    cs_ = slice(hb * TCH, (hb + 1) * TCH)
                    pm = c.next_pf(4, 6)
                    k.op("pe", lambda e: e.matmul(pm[:, :], lhsT=c.blk64[:], rhs=sqb[:, cs_], start=True, stop=True),
                         reads=[c.blk64, sqb], writes=[pm])
                    k.op("dve", lambda e: e.tensor_scalar(out=t2[:, cs_], in0=pm[:, :], scalar1=RW_EPS, scalar2=None, op0=ALU.add),
                         reads=[pm], writes=[t2])
                act(t2[:], t2[:], AF.Ln, [t2], [t2])
                act(t2[:], t2[:], AF.Exp, [t2], [t2], scale=-0.5)
                tt(t1[:], t1[:], t2[:], ALU.mult, [t1, t2], [t1])
                k.op("dve", lambda e: e.tensor_scalar(out=y[:], in0=t1[:], scalar1=P("rw_lng", hp), scalar2=P("rw_lnb", hp),
                                                      op0=ALU.mult, op1=ALU.add), reads=[t1, L.prm], writes=[y])
                k.op("dve", lambda e: e.scalar_tensor_tensor(out=sqb[:], in0=rm[:], scalar=P("rw_rk", hp), in1=k2[:],
                                                             op0=ALU.mult, op1=ALU.mult), reads=[rm, k2, L.prm], writes=[sqb])
                for hb in range(SEG // TCH):
                    cs_ = slice(hb * TCH, (hb + 1) * TCH)
                    pm = c.next_pf(4, 6)
                    k.op("pe", lambda e: e.matmul(pm[:, :], lhsT=c.blk1[:], rhs=sqb[:, cs_], start=True, stop=True),
                         reads=[c.blk1, sqb], writes=[pm])
                    tt(t2[:, cs_], pm[:, :], vm[:, cs_], ALU.mult, [pm, vm], [t2])
                tt(y[:], y[:], t2[:], ALU.add, [y, t2], [y], "pool")
                k.dma("pool", c.mixT[512 + hp * 128:512 + (hp + 1) * 128, t0:t0 + SEG], y[:], reads=[y], writes=[c.mixT], waw=False)
        k.barrier()


def stage_outproj(k, c, L, xa, xb, y, xa_deps=None, cc=None):
    pT = c.pT
    with ExitStack() as st:
        wbf, wd = load_w_bf16(k, st, lambda kc: L.wout[kc * 128:(kc + 1) * 128, :], 8, D, "wout")
        mx = [k.sb(f"op_mx{i}", [128, 8, TCH], F32, st) for i in range(2)]
        gt = [k.sb(f"op_gt{i}", [128, 8, TCH], F32, st) for i in range(2)]
        sg = k.sb("op_sg", [128, 8, TCH], F32, st)
        mg = [k.sb(f"op_mg{i}", [128, 8, TCH], BF16, st) for i in range(2)]
        xt = [k.sb(f"op_xa{i}", [128, D], F32, st) for i in range(2)]
        xu = [k.sb(f"op_xb{i}", [128, D], F32, st) for i in range(2)]
        yo = [k.sb(f"op_y{i}", [128, D], F32, st) for i in range(2)]
        ydeps = [Dep() for _ in range(NTC)] if cc is not None else [y.dep] * NTC

        def emit_cc(i):
            xs_out, xs_deps, groups = cc
            rs = slice(i * TCH, (i + 1) * TCH)
            k.collective(y[rs, :], xs_out[rs, :], reads=[ydeps[i]], writes=[xs_deps[i]], groups=groups)
        for tc in range(NTC):
            b = tc % 2
            tsl = slice(tc * TCH, (tc + 1) * TCH)
            k.dma("sp", mx[b][:], c.mixT[:, tsl].rearrange("(kc p) t -> p kc t", p=128), reads=[c.mixT], writes=[mx[b]])
            k.dma("sp", gt[b][:], pT[R_GATE:R_GATE + 1024, tsl].rearrange("(kc p) t -> p kc t", p=128), reads=[pT],
                  writes=[gt[b]])
            k.op("act", lambda e: e.activation(out=sg[:], in_=gt[b][:], func=AF.Sigmoid), reads=[gt[b]], writes=[sg])
            k.op("pool", lambda e: e.tensor_tensor(out=gt[b][:], in0=gt[b][:], in1=mx[b][:], op=ALU.mult),
                 reads=[gt[b], mx[b]], writes=[gt[b]])
            k.op("dve", lambda e: e.tensor_tensor(out=mg[b][:], in0=gt[b][:], in1=sg[:], op=ALU.mult),
                 reads=[gt[b], sg], writes=[mg[b]])
            for sub in range(4):
                tt_ = tc * 4 + sub
                xb_i = tt_ % 2
                rows = slice(tt_ * 128, (tt_ + 1) * 128)
                k.dma("sp", xt[xb_i][:], xa[rows, :], reads=[xa_deps[tc] if xa_deps else xa], writes=[xt[xb_i]])
                if xb is not None:
                    k.dma("sp", xu[xb_i][:], xb[rows, :], reads=[xb], writes=[xu[xb_i]])
                    k.op("pool", lambda e: e.tensor_tensor(out=xt[xb_i][:], in0=xt[xb_i][:], in1=xu[xb_i][:], op=ALU.add),
                         reads=[xt[xb_i], xu[xb_i]], writes=[xt[xb_i]])
                yb = yo[xb_i]
                for n in range(4):
                    ps = c.next_pf()
                    for kc in range(8):
                        k.op("pe", lambda e: e.matmul(ps[:, :], lhsT=mg[b][:, kc, sub * 128:(sub + 1) * 128],
                                                      rhs=wbf[:, kc, n * 512:(n + 1) * 512], start=(kc == 0), stop=(kc == 7)),
                             reads=[mg[b], wd[kc]], writes=[ps], inc=(kc == 7))
                    k.op("dve", lambda e: e.scalar_tensor_tensor(out=yb[:, n * 512:(n + 1) * 512], in0=xt[xb_i][:, n * 512:(n + 1) * 512],
                                                                 scalar=0.5, in1=ps[:, :], op0=ALU.mult, op1=ALU.add),
                         reads=[xt[xb_i], ps], writes=[yb])
                k.dma("pool", y[rows, :], yb[:], reads=[yb], writes=[ydeps[tc]], waw=False)
            if cc is not None and tc >= 1:
                emit_cc(tc - 1)
        if cc is not None:
            emit_cc(NTC - 1)
    k.barrier()


def stage_final(k, c, xa, xb, g_bc_d, out, xa_deps=None):
    with ExitStack() as st:
        gbc = k.sb("f_gbc", [128, D], F32, st)
        k.dma("sp", gbc[:], g_bc_d.partition_broadcast(128), writes=[gbc])
        xt = [k.sb(f"f_xt{i}", [128, D], F32, st) for i in range(2)]
        xu = [k.sb(f"f_xu{i}", [128, D], F32, st) for i in range(2)]
        junk = k.sb("f_junk", [128, D], BF16, st)
        yo = [k.sb(f"f_y{i}", [128, D], F32, st) for i in range(2)]
        ss = [k.sb(f"f_ss{i}", [128, 4], F32, st) for i in range(2)]
        for tt_ in range(S // 128):
            b = tt_ % 2
            rows = slice(tt_ * 128, (tt_ + 1) * 128)
            k.dma("sp", xt[b][:], xa[rows, :], reads=[xa_deps[tt_ // 4] if xa_deps else xa], writes=[xt[b]])
            if xb is not None:
                k.dma("sp", xu[b][:], xb[rows, :], reads=[xb], writes=[xu[b]])
                k.op("pool", lambda e: e.tensor_tensor(out=xt[b][:], in0=xt[b][:], in1=xu[b][:], op=ALU.add),
                     reads=[xt[b], xu[b]], writes=[xt[b]])
            k.op("act", lambda e: e.activation(out=junk[:], in_=xt[b][:], func=AF.Square, accum_out=ss[b][:, 0:1]),
                 reads=[xt[b]], writes=[junk, ss[b]])
            k.op("dve", lambda e: e.tensor_scalar(out=ss[b][:, 1:2], in0=ss[b][:, 0:1], scalar1=1.0 / D, scalar2=EPS,
                                                  op0=ALU.mult, op1=ALU.add), reads=[ss[b]], writes=[ss[b]])
            k.op("act", lambda e: e.activation(out=ss[b][:, 2:3], in_=ss[b][:, 1:2], func=AF.Sqrt), reads=[ss[b]], writes=[ss[b]])
            k.op("dve", lambda e: e.reciprocal(out=ss[b][:, 3:4], in_=ss[b][:, 2:3]), reads=[ss[b]], writes=[ss[b]])
            k.op("dve", lambda e: e.scalar_tensor_tensor(out=yo[b][:], in0=xt[b][:], scalar=ss[b][:, 3:4], in1=gbc[:],
                                                         op0=ALU.mult, op1=ALU.mult), reads=[xt[b], ss[b], gbc], writes=[yo[b]])
            k.dma("pool", out[rows, :], yo[b][:], reads=[yo[b]], writes=[out], waw=False)
    k.barrier()


class LayerIO:
    pass


def declare_layer_inputs(k, l):
    EI = "ExternalInput"
    L = LayerIO()
    L.col = PRM_COLS
    L.norm_g = k.dram(f"norm_g{l}", [1, D], F32, kind=EI)
    L.w_in = k.dram(f"w_in{l}", [D, NF + NV], F32, kind=EI)
    L.wuq = k.dram(f"wuq{l}", [512, 384], F32, kind=EI)
    L.wukv = k.dram(f"wukv{l}", [256, 512], F32, kind=EI)
    L.prm_d = k.dram(f"prm{l}", [128, _pc], F32, kind=EI)
    L.s5b = k.dram(f"s5b{l}", [128, 512], F32, kind=EI)
    L.s5c = k.dram(f"s5c{l}", [128, 512], F32, kind=EI)
    L.wglu = k.dram(f"wglu{l}", [512, 256], F32, kind=EI)
    L.w2a2 = k.dram(f"w2a2{l}", [128, 256], F32, kind=EI)
    L.wout = k.dram(f"wout{l}", [1024, D], F32, kind=EI)
    return L


def layer_input_arrays(inp, l, half, suffix):
    a = core_layer_arrays(inp, l, half)
    out = {
        f"norm_g{suffix}": np.ascontiguousarray(inp["norm_g"][l][None, :]),
        f"w_in{suffix}": a["w_in"], f"wuq{suffix}": a["wuq"], f"wukv{suffix}": a["wukv"], f"prm{suffix}": a["prm"],
        f"s5b{suffix}": a["s5b"], f"s5c{suffix}": a["s5c"], f"wglu{suffix}": a["wglu"], f"w2a2{suffix}": a["w2a2"],
    }
    rows = np.concatenate([b * 512 + half * 256 + np.arange(256) for b in range(4)])
    out[f"wout{suffix}"] = np.ascontiguousarray(inp["w_out"][l][rows, :])
    return out


def declare_scratch(k, c):
    c.hT = k.dram("sc_hT", [D, S], BF16)
    c.pT = k.dram("sc_pT", [NF, S], F32)
    c.pV = k.dram("sc_pV", [S, NV], F32)
    c.us5 = k.dram("sc_us5", [32, 128, 512], BF16)
    c.ys5 = k.dram("sc_ys5", [32, 128, 512], F32)
    c.gT = k.dram("sc_gT", [512, S], BF16)
    c.gF = k.dram("sc_gF", [256, S], F32)
    c.mixT = k.dram("sc_mixT", [1024, S], F32)
    c.cqnT = k.dram("sc_cqnT", [512, S], BF16)
    c.ckvnT = k.dram("sc_ckvnT", [256, S], BF16)
    c.qT = k.dram("sc_qT", [384, S], BF16)
    c.knT = k.dram("sc_knT", [256, S], BF16)
    c.vA = k.dram("sc_vA", [S, 256], BF16)
    c.krT = k.dram("sc_krT", [128, S], BF16)
    c.qdT = k.dram("sc_qdT", [256, S], BF16)
    c.kdT = k.dram("sc_kdT", [256, S], BF16)
    c.ropeA_cos = k.dram("sc_rAc", [128, S], F32)
    c.ropeA_sin = k.dram("sc_rAs", [128, S], F32)
    c.ropeD_cos = k.dram("sc_rDc", [128, S], F32)
    c.ropeD_sin = k.dram("sc_rDs", [128, S], F32)


def emit_layer(k, c, L, xa, xb, y, xa_deps=None, cc=None):
    with ExitStack() as st:
        L.prm = k.sb("prm_sb", [128, _pc], F32, st)
        k.dma("sp", L.prm[:], L.prm_d[:, :], writes=[L.prm])
        import os as _os
        sel = _os.environ.get("LAYER_STAGES", "nimsrdo")
        if "n" in sel:
            stage_norm(k, c, xa, xb, L.norm_g[0:1, :], c.hT, xa_deps)
            k.barrier()
        if "i" in sel:
            stage_inproj(k, c, c.hT, L.w_in, NF, NV, c.pT, c.pV)
        if "m" in sel:
            stage_mla(k, c, L)
        if "s" in sel:
            stage_s5(k, c, L)
        if "r" in sel:
            stage_rwkv(k, c, L)
        if "d" in sel:
            stage_diff(k, c, L)
        if "o" in sel:
            stage_outproj(k, c, L, xa, xb, y, xa_deps, cc)
        k.barrier()


def build_layer_program():
    nc = bass.Bass("TRN2", target_bir_lowering=False)
    k = KB(nc)
    c = Ctx(k)
    EI = "ExternalInput"
    cmat = k.dram("cmat", [128, 768], F32, kind=EI)
    cmask = k.dram("cmask", [128, 2048], F32, kind=EI)
    cst = k.dram("cst", [128, NCST], F32, kind=EI)
    cm2 = k.dram("cm2", [128, 2560], F32, kind=EI)
    pos = k.dram("pos", [1, S], I32, kind=EI)
    xa = k.dram("xa", [S, D], F32, kind=EI)
    xb = k.dram("xb", [S, D], F32, kind=EI)
    y = k.dram("y", [S, D], F32, kind="ExternalOutput")
    L = declare_layer_inputs(k, "")
    declare_scratch(k, c)
    load_all_consts(k, c, cmat, cmask, cst, cm2)
    make_rope_tables(k, c, pos, c.cst, 0, 1, c.ropeA_cos, c.ropeA_sin, "rtA")
    make_rope_tables(k, c, pos, c.cst, 2, 3, c.ropeD_cos, c.ropeD_sin, "rtD")
    emit_layer(k, c, L, xa, xb, y)
    k.finish()
    return nc


def build_final_program():
    nc = bass.Bass("TRN2", target_bir_lowering=False)
    k = KB(nc)
    c = Ctx(k)
    xa = k.dram("xa", [S, D], F32, kind="ExternalInput")
    xb = k.dram("xb", [S, D], F32, kind="ExternalInput")
    g = k.dram("fg", [1, D], F32, kind="ExternalInput")
    out = k.dram("out", [S, D], F32, kind="ExternalOutput")
    stage_final(k, c, xa, xb, g[0:1, :], out)
    k.finish()
    return nc


def const_inputs():
    cmat, cmask, cst = const_mats()
    return {"cmat": cmat, "cmask": cmask, "cst": cst, "cm2": const_mats2()}


def kernel_unfused(**inp):
    inp = {k_: np.asarray(v) for k_, v in inp.items()}
    x = inp["x"]
    B = x.shape[0]
    consts = const_inputs()
    ncl = build_layer_program()
    cur_a = [np.ascontiguousarray(x[cid // 2]) for cid in range(8)]
    cur_b = [np.zeros((S, D), np.float32) for _ in range(8)]
    for l in range(4):
        in_maps = []
        for cid in range(8):
            b, half = divmod(cid, 2)
            m = dict(consts)
            m["pos"] = np.ascontiguousarray(inp["positions"][b:b + 1].astype(np.int32))
            m["xa"] = cur_a[cid]
            m["xb"] = cur_b[cid]
            m.update(layer_input_arrays(inp, l, half, ""))
            in_maps.append(m)
        res = run_bass_kernel_spmd(ncl, in_maps, core_ids=list(range(8)))
        ys = [np.asarray(r["y"]) for r in res.results]
        cur_a = [ys[cid] for cid in range(8)]
        cur_b = [ys[cid ^ 1] for cid in range(8)]
    ncf = build_final_program()
    fg = np.ascontiguousarray(inp["final_norm_g"][None, :])
    in_maps = [{"xa": cur_a[cid], "xb": cur_b[cid], "fg": fg} for cid in range(8)]
    res = run_bass_kernel_spmd(ncf, in_maps, core_ids=list(range(8)))
    out = np.stack([np.asarray(res.results[2 * b]["out"]) for b in range(B)], axis=0)
    return out.astype(np.float32)


from concourse.bass_utils import run_bass_kernel_spmd


PAIR_GROUPS = [[0, 1], [2, 3], [4, 5], [6, 7]]
DEPTH = 4


def build_fused_program(depth=DEPTH):
    nc = bass.Bass("TRN2", target_bir_lowering=False)
    k = KB(nc)
    c = Ctx(k)
    EI = "ExternalInput"
    cmat = k.dram("cmat", [128, 768], F32, kind=EI)
    cmask = k.dram("cmask", [128, 2048], F32, kind=EI)
    cst = k.dram("cst", [128, NCST], F32, kind=EI)
    cm2 = k.dram("cm2", [128, 2560], F32, kind=EI)
    pos = k.dram("pos", [1, S], I32, kind=EI)
    x_in = k.dram("xa", [S, D], F32, kind=EI)
    fg = k.dram("fg", [1, D], F32, kind=EI)
    out = k.dram("out", [S, D], F32, kind="ExternalOutput")
    Ls = [declare_layer_inputs(k, l) for l in range(depth)]
    declare_scratch(k, c)
    ybuf = k.dram("sc_y", [S, D], F32)
    xs = [k.dram(f"sc_xs{i}", [S, D], F32) for i in range(2)]
    xs_deps = [[Dep() for _ in range(NTC)] for _ in range(2)]
    load_all_consts(k, c, cmat, cmask, cst, cm2)
    make_rope_tables(k, c, pos, c.cst, 0, 1, c.ropeA_cos, c.ropeA_sin, "rtA")
    make_rope_tables(k, c, pos, c.cst, 2, 3, c.ropeD_cos, c.ropeD_sin, "rtD")
    cur, cur_deps = x_in, None
    for l in range(depth):
        o = l % 2
        emit_layer(k, c, Ls[l], cur, None, ybuf, cur_deps, (xs[o], xs_deps[o], PAIR_GROUPS))
        cur, cur_deps = xs[o], xs_deps[o]
    stage_final(k, c, cur, None, fg[0:1, :], out, cur_deps)
    k.finish()
    return nc


def kernel_fused(**inp):
    inp = {k_: np.asarray(v) for k_, v in inp.items()}
    x = inp["x"]
    B = x.shape[0]
    consts = const_inputs()
    nc = build_fused_program()
    fg = np.ascontiguousarray(inp["final_norm_g"][None, :])
    in_maps = []
    for cid in range(8):
        b, half = divmod(cid, 2)
        m = dict(consts)
        m["pos"] = np.ascontiguousarray(inp["positions"][b:b + 1].astype(np.int32))
        m["xa"] = np.ascontiguousarray(x[b])
        m["fg"] = fg
        for l in range(DEPTH):
            m.update(layer_input_arrays(inp, l, half, str(l)))
        in_maps.append(m)
    res = run_bass_kernel_spmd(nc, in_maps, core_ids=list(range(8)))
    out = np.stack([np.asarray(res.results[2 * b]["out"]) for b in range(B)], axis=0)
    return out.astype(np.float32)


def kernel(**inputs):
    return kernel_fused(**inputs)
```

```python
from contextlib import ExitStack
import math
import numpy as np
import concourse.bass as bass
import concourse.mybir as mybir

F32 = mybir.dt.float32
BF16 = mybir.dt.bfloat16
I32 = mybir.dt.int32
AF = mybir.ActivationFunctionType
ALU = mybir.AluOpType
AX = mybir.AxisListType

NDS = 44
NDS_HW = 24
DQ_POOLS = {"sp": (0, 16), "act": (16, 30), "pool": (30, 44)}
STQ = "act"


class Dep:
    __slots__ = ("w", "r", "excl")

    def __init__(self):
        self.excl = False
        self.w = {}
        self.r = {}


class T:
    def __init__(self, t, dep=None):
        self.t = t
        self.dep = dep or Dep()

    def __getitem__(self, k):
        return self.t[k]

    def ap(self):
        return self.t.ap() if hasattr(self.t, "ap") else self.t[:]


def _deps(x):
    return x.dep if isinstance(x, T) else x


class KB:
    def __init__(self, nc):
        self.nc = nc
        self.es = ExitStack()
        self.eng = {"pe": nc.tensor, "act": nc.scalar, "dve": nc.vector,
                    "pool": nc.gpsimd, "sp": nc.sync}
        self.sem = {}
        for e in ("pe", "act", "dve", "pool"):
            self.sem[e] = self.es.enter_context(nc.semaphore("s_" + e))
        self.cnt = {e: 0 for e in self.sem}
        self.dsem = [self.es.enter_context(nc.semaphore(f"sd{i}")) for i in range(NDS)]
        self.dcnt = [0] * NDS
        self.dq_next = {}
        self.seen = {e: {} for e in self.eng}
        self.ccsem = self.es.enter_context(nc.semaphore("s_cc"))
        self.cccnt = 0
        self.ninst = 0
        self.scopes = []

    def sb(self, name, shape, dtype, stack=None):
        self.uid = getattr(self, "uid", 0) + 1
        name = f"{name}_u{self.uid}"
        t = (stack or self.es).enter_context(self.nc.sbuf_tensor(name, list(shape), dtype))
        return T(t)

    def ps(self, name, shape, dtype, stack=None):
        t = (stack or self.es).enter_context(self.nc.psum_tensor(name, list(shape), dtype))
        r = T(t)
        r.dep.excl = True
        return r

    def dram(self, name, shape, dtype, kind="Internal"):
        t = self.nc.dram_tensor(name, list(shape), dtype, kind=kind)
        return T(t)

    def _wait(self, E, tok):
        if tok is None:
            return
        kind, key, val = tok
        if kind == "e" and key == E and E in ("pe", "sp"):
            return
        sk = (kind, key)
        if self.seen[E].get(sk, 0) >= val:
            return
        sem = self.sem[key] if kind == "e" else (self.dsem[key] if kind == "d" else self.ccsem)
        self.eng[E].wait_ge(sem, val)
        self.ninst += 1
        self.seen[E][sk] = val

    def _collect(self, E, reads, writes, waw=True):
        for d in reads:
            d = _deps(d)
            for (kd, ky), v in list(d.w.items()):
                self._wait(E, (kd, ky, v))
            if d.excl:
                for (kd, ky), v in list(d.r.items()):
                    if ky != E:
                        self._wait(E, (kd, ky, v))
        for d in writes:
            d = _deps(d)
            if waw:
                for (kd, ky), v in list(d.w.items()):
                    self._wait(E, (kd, ky, v))
            for (kd, ky), v in list(d.r.items()):
                self._wait(E, (kd, ky, v))

    def _update(self, tok, reads, writes, waw=True):
        kk = (tok[0], tok[1])
        for d in writes:
            d = _deps(d)
            if waw:
                d.w = {kk: tok[2]}
                d.r = {}
            else:
                d.w[kk] = max(d.w.get(kk, 0), tok[2])
        for d in reads:
            d = _deps(d)
            d.r[kk] = max(d.r.get(kk, 0), tok[2])

    def op(self, E, fn, reads=(), writes=(), inc=True):
        self._collect(E, reads, writes)
        inst = fn(self.eng[E])
        self.ninst += 1
        if inc:
            self.cnt[E] += 1
            inst.then_inc(self.sem[E], 1)
            tok = ("e", E, self.cnt[E])
        else:
            tok = ("e", E, self.cnt[E] + 1)
        self._update(tok, reads, writes)
        return inst

    def dma(self, Q, out, in_, reads=(), writes=(), waw=True, **kw):
        self._collect(Q, reads, writes, waw)
        lo, hi = DQ_POOLS[Q]
        i = lo + self.dq_next.get(Q, 0)
        self.dq_next[Q] = (self.dq_next.get(Q, 0) + 1) % (hi - lo)
        if self.dcnt[i] > 0:
            self._wait(Q, ("d", i, 16 * self.dcnt[i]))
        inst = self.eng[Q].dma_start(out=out, in_=in_, **kw)
        inst.then_inc(self.dsem[i], 16)
        self.ninst += 1
        self.dcnt[i] += 1
        tok = ("d", i, 16 * self.dcnt[i])
        self._update(tok, reads, writes, waw)
        return tok

    def collective(self, in_ap, out_ap, reads=(), writes=(), groups=None):
        self._collect("pool", reads, writes, True)
        inst = self.nc.gpsimd.collective_compute("AllReduce", ALU.add, replica_groups=groups,
                                                 ins=[in_ap], outs=[out_ap])
        self.cccnt += 1
        inst.then_inc(self.ccsem, 1)
        self.ninst += 1
        tok = ("c", 0, self.cccnt)
        self._update(tok, reads, writes, True)
        return tok

    def barrier(self, engines=("pe", "act", "dve", "pool", "sp")):
        for E in engines:
            if self.cccnt > 0:
                self._wait(E, ("c", 0, self.cccnt))
            for p in self.sem:
                if p != E and self.cnt[p] > 0:
                    self._wait(E, ("e", p, self.cnt[p]))
            for i in range(NDS):
                if self.dcnt[i] > 0:
                    self._wait(E, ("d", i, 16 * self.dcnt[i]))
        for E in ("act", "dve", "pool"):
            if E in engines and self.cnt[E] > 0:
                self._wait(E, ("e", E, self.cnt[E]))

    def finish(self):
        self.barrier(engines=("sp",))


S = 4096
D = 2048
TCH = 512
NTC = S // TCH
EPS = 1e-6


class Ctx:
    def __init__(self, k):
        self.k = k
        self.pf = [k.ps(f"pf{i}", [128, 512], F32) for i in range(6)]
        self.pb = [k.ps(f"pb{i}", [128, 1024], BF16) for i in range(2)]
        self.pfi = 0
        self.pbi = 0
        self.rr = 0
        self.us5 = None

    def next_pf(self, lo=0, hi=6):
        n = hi - lo
        p = self.pf[lo + (self.pfi % n)]
        self.pfi += 1
        return p

    def next_pb(self):
        p = self.pb[self.pbi % 2]
        self.pbi += 1
        return p

    def evac_eng(self):
        self.rr += 1
        return "act" if self.rr % 2 else "dve"


def copy_op(k, E, out, in_, reads, writes):
    if E == "act":
        k.op("act", lambda e: e.copy(out=out, in_=in_), reads=reads, writes=writes)
    else:
        k.op(E, lambda e: e.tensor_copy(out=out, in_=in_), reads=reads, writes=writes)


def load_consts(k, c, ident_d, stack):
    idf = k.sb("idf", [128, 128], F32, stack)
    c.ident = k.sb("c_ident", [128, 128], BF16)
    c.ones = k.sb("c_ones", [128, 128], BF16)
    k.dma("sp", idf[:], ident_d[:, :], writes=[idf])
    k.op("dve", lambda e: e.tensor_copy(out=c.ident[:], in_=idf[:]), reads=[idf], writes=[c.ident])
    k.op("dve", lambda e: e.memset(c.ones[:], 1.0), writes=[c.ones])


def stage_norm(k, c, xa, xb, g_bc_d, hT, xa_deps=None):
    with ExitStack() as st:
        gbc = k.sb("n_gbc", [128, D], F32, st)
        k.dma("sp", gbc[:], g_bc_d.partition_broadcast(128), writes=[gbc])
        xt = [k.sb(f"n_xt{i}", [128, D], F32, st) for i in range(2)]
        xt2 = [k.sb(f"n_xu{i}", [128, D], F32, st) for i in range(2)]
        junk = k.sb("n_junk", [128, D], BF16, st)
        xn = [k.sb(f"n_xn{i}", [128, D], BF16, st) for i in range(2)]
        ss = [k.sb(f"n_ss{i}", [128, 4], F32, st) for i in range(2)]
        hst = [k.sb(f"n_hst{i}", [128, 16, TCH], BF16, st) for i in range(2)]
        for tt in range(S // 128):
            b = tt % 2
            tcn, tl = divmod(tt, 4)
            hs = hst[tcn % 2]
            rows = slice(tt * 128, (tt + 1) * 128)
            k.dma("sp", xt[b][:], xa[rows, :], reads=[xa_deps[tt // 4] if xa_deps else xa], writes=[xt[b]])
            if xb is not None:
                k.dma("sp", xt2[b][:], xb[rows, :], reads=[xb], writes=[xt2[b]])
                k.op("pool", lambda e: e.tensor_tensor(out=xt[b][:], in0=xt[b][:], in1=xt2[b][:], op=ALU.add),
                     reads=[xt[b], xt2[b]], writes=[xt[b]])
            k.op("act", lambda e: e.activation(out=junk[:], in_=xt[b][:], func=AF.Square,
                                               accum_out=ss[b][:, 0:1]),
                 reads=[xt[b]], writes=[junk, ss[b]])
            k.op("dve", lambda e: e.tensor_scalar(out=ss[b][:, 1:2], in0=ss[b][:, 0:1], scalar1=1.0 / D,
                                                  scalar2=EPS, op0=ALU.mult, op1=ALU.add),
                 reads=[ss[b]], writes=[ss[b]])
            k.op("act", lambda e: e.activation(out=ss[b][:, 2:3], in_=ss[b][:, 1:2], func=AF.Sqrt),
                 reads=[ss[b]], writes=[ss[b]])
            k.op("dve", lambda e: e.reciprocal(out=ss[b][:, 3:4], in_=ss[b][:, 2:3]),
                 reads=[ss[b]], writes=[ss[b]])
            k.op("dve", lambda e: e.scalar_tensor_tensor(out=xn[b][:], in0=xt[b][:], scalar=ss[b][:, 3:4],
                                                         in1=gbc[:], op0=ALU.mult, op1=ALU.mult),
                 reads=[xt[b], ss[b], gbc], writes=[xn[b]])
            for half in range(2):
                pb = c.next_pb()
                for j in range(8):
                    kc = half * 8 + j
                    k.op("pe", lambda e: e.transpose(pb[:, j * 128:(j + 1) * 128],
                                                     xn[b][:, kc * 128:(kc + 1) * 128], c.ident[:]),
                         reads=[xn[b], c.ident], writes=[pb], inc=(j == 7))
                k.op("act", lambda e: e.copy(out=hs[:, half * 8:(half + 1) * 8, tl * 128:(tl + 1) * 128],
                                             in_=pb[:].rearrange("p (a b) -> p a b", a=8)),
                     reads=[pb], writes=[hs])
            if tl == 3:
                k.dma(STQ, hT[:, tcn * TCH:(tcn + 1) * TCH].rearrange("(kc p) t -> p kc t", p=128),
                      hs[:], reads=[hs], writes=[hT], waw=False)


def load_w_bf16(k, st, w_ap_fn, KC, N, name, nstg=3):
    wbf = k.sb(name, [128, KC, N], BF16, st)
    stg = [k.sb(f"{name}_s{i}", [128, N], F32, st) for i in range(nstg)]
    deps = [Dep() for _ in range(KC)]
    for kc in range(KC):
        s = stg[kc % nstg]
        k.dma("sp", s[:], w_ap_fn(kc), writes=[s])
        E = ("dve", "act", "dve", "pool")[kc % 4]
        copy_op(k, E, wbf[:, kc, :], s[:], [s], [deps[kc]])
    return wbf, deps


def linear_fm(k, c, st, inT, KC, wbf, wdeps, n_tiles, epilogue, name, in_cast=False):
    xin = [k.sb(f"{name}_x{i}", [128, KC, TCH], BF16, st) for i in range(2)]
    for tc in range(NTC):
        xi = xin[tc % 2]
        k.dma("sp", xi[:], inT[:, tc * TCH:(tc + 1) * TCH].rearrange("(kc p) t -> p kc t", p=128),
              reads=[inT], writes=[xi])
        for ni, (c0, ncol) in enumerate(n_tiles):
            ps = c.next_pf()
            for kc in range(KC):
                k.op("pe", lambda e: e.matmul(ps[:ncol, :], lhsT=wbf[:, kc, c0:c0 + ncol], rhs=xi[:, kc, :],
                                              start=(kc == 0), stop=(kc == KC - 1)),
                     reads=[wdeps[kc], xi], writes=[ps], inc=(kc == KC - 1))
            epilogue(tc, ni, ps, ncol)


def linear_tm(k, c, st, inT, KC, wbf, wdeps, c0, N, epilogue, name):
    xin = [k.sb(f"{name}_x{i}", [128, KC, TCH], BF16, st) for i in range(2)]
    for tc in range(NTC):
        xi = xin[tc % 2]
        k.dma("sp", xi[:], inT[:, tc * TCH:(tc + 1) * TCH].rearrange("(kc p) t -> p kc t", p=128),
              reads=[inT], writes=[xi])
        for sub in range(4):
            ps = c.next_pf()
            for kc in range(KC):
                k.op("pe", lambda e: e.matmul(ps[:, :N], lhsT=xi[:, kc, sub * 128:(sub + 1) * 128],
                                              rhs=wbf[:, kc, c0:c0 + N],
                                              start=(kc == 0), stop=(kc == KC - 1)),
                     reads=[wdeps[kc], xi], writes=[ps], inc=(kc == KC - 1))
            epilogue(tc * 4 + sub, ps)


class Stager:
    def __init__(self, k, st, name, shape, dtype, n=4):
        self.k = k
        self.bufs = [k.sb(f"{name}{i}", shape, dtype, st) for i in range(n)]
        self.i = 0

    def next(self):
        b = self.bufs[self.i % len(self.bufs)]
        self.i += 1
        return b


def epi_store_fm(k, c, stg, outT, row0_of):
    def epi(tc, ni, ps, ncol):
        s = stg.next()
        copy_op(k, c.evac_eng(), s[:ncol, :], ps[:ncol, :], [ps], [s])
        r0 = row0_of(ni)
        k.dma(STQ, outT[r0:r0 + ncol, tc * TCH:(tc + 1) * TCH], s[:ncol, :], reads=[s], writes=[outT], waw=False)
        return s
    return epi


def stage_inproj(k, c, hT, w_in_d, NF, NV, pT, pV):
    KC = D // 128
    tiles = []
    c0 = 0
    while c0 < NF:
        tiles.append((c0, min(128, NF - c0)))
        c0 += 128
    half = (len(tiles) + 1) // 2
    groups = [tiles[:half], tiles[half:]]
    for gi, grp in enumerate(groups):
        with ExitStack() as st:
            g0 = grp[0][0]
            gN = grp[-1][0] + grp[-1][1] - g0
            wbf, wd = load_w_bf16(k, st, lambda kc: w_in_d[kc * 128:(kc + 1) * 128, g0:g0 + gN], KC, gN, f"ip_w{gi}")
            stg = Stager(k, st, f"ip_o{gi}_", [128, TCH], F32, 4)
            rel = [(a - g0, b) for a, b in grp]
            base_epi = epi_store_fm(k, c, stg, pT, lambda ni: grp[ni][0])
            stu = Stager(k, st, f"ip_u{gi}_", [128, 8, 64], BF16, 3)

            def epi(tc, ni, ps, ncol, grp=grp, base_epi=base_epi, stu=stu):
                sst = base_epi(tc, ni, ps, ncol)
                r0 = grp[ni][0]
                import os as _os
                if R_U <= r0 < R_U + 512 and c.us5 is not None and _os.environ.get('NOHOOK') != '1':
                    su = stu.next()
                    k.op("pool", lambda e: e.tensor_copy(out=su[:],
                                                         in_=sst[:, :].rearrange("p (j t) -> p t j", t=8)),
                         reads=[sst], writes=[su])
                    g0 = (r0 - R_U) // 16
                    hq = _os.environ.get("HOOKDMA", STQ)
                    for gl in range(8 if hq != "none" else 0):
                        k.dma(hq, c.us5[g0 + gl, :, tc * 64:(tc + 1) * 64].rearrange("(t c) j -> c t j", c=16),
                              su[gl * 16:(gl + 1) * 16, :, :], reads=[su], writes=[c.us5], waw=False)
            linear_fm(k, c, st, hT, KC, wbf, wd, rel, epi, f"ip{gi}")
        k.barrier()
    with ExitStack() as st:
        wbf, wd = load_w_bf16(k, st, lambda kc: w_in_d[kc * 128:(kc + 1) * 128, NF:NF + NV], KC, NV, "ip_wv")
        stg = Stager(k, st, "ip_ov_", [128, NV], F32, 4)

        def epi(tt, ps):
            s = stg.next()
            copy_op(k, c.evac_eng(), s[:, :], ps[:, :NV], [ps], [s])
            k.dma(STQ, pV[tt * 128:(tt + 1) * 128, :], s[:, :], reads=[s], writes=[pV], waw=False)
        linear_tm(k, c, st, hT, KC, wbf, wd, 0, NV, epi, "ipv")
    k.barrier()


TWO_PI = 2.0 * np.pi
CW1 = 6.28125
CW2 = float(np.float32(TWO_PI - 6.28125))
CW3 = float(TWO_PI - 6.28125 - float(np.float32(TWO_PI - 6.28125)))


def make_rope_tables(k, c, pos_d, cst, col_inv, col_sgn, cosT, sinT, name):
    HS = S // 2
    with ExitStack() as st:
        posi = k.sb(name + "_pi", [128, HS], I32, st)
        ang = k.sb(name + "_ang", [128, HS], F32, st)
        a2 = k.sb(name + "_a2", [128, HS], F32, st)
        ni = k.sb(name + "_ni", [128, HS], I32, st)
        nf = k.sb(name + "_nf", [128, HS], F32, st)
        r = k.sb(name + "_r", [128, HS], F32, st)
        m = k.sb(name + "_m", [128, HS], F32, st)
        o = k.sb(name + "_o", [128, HS], F32, st)
        for hh in range(2):
            sl = slice(hh * HS, (hh + 1) * HS)
            k.dma("sp", posi[:], pos_d[0:1, sl].partition_broadcast(128), writes=[posi])
            k.op("dve", lambda e: e.tensor_copy(out=ang[:], in_=posi[:]), reads=[posi], writes=[ang])
            k.op("dve", lambda e: e.tensor_scalar(out=ang[:], in0=ang[:], scalar1=cst[:, col_inv:col_inv + 1],
                                                  scalar2=None, op0=ALU.mult), reads=[ang, cst], writes=[ang])
            for which, shift, dst in (("s", 0.0, sinT), ("c", np.pi / 2, cosT)):
                if which == "s":
                    k.op("dve", lambda e: e.tensor_scalar(out=ni[:], in0=ang[:], scalar1=float(1.0 / TWO_PI),
                                                          scalar2=None, op0=ALU.mult), reads=[ang], writes=[ni])
                    k.op("dve", lambda e: e.tensor_copy(out=nf[:], in_=ni[:]), reads=[ni], writes=[nf])
                    k.op("dve", lambda e: e.scalar_tensor_tensor(out=a2[:], in0=nf[:], scalar=-CW1, in1=ang[:],
                                                                 op0=ALU.mult, op1=ALU.add), reads=[nf, ang], writes=[a2])
                    k.op("dve", lambda e: e.scalar_tensor_tensor(out=a2[:], in0=nf[:], scalar=-CW2, in1=a2[:],
                                                                 op0=ALU.mult, op1=ALU.add), reads=[nf, a2], writes=[a2])
                    k.op("dve", lambda e: e.scalar_tensor_tensor(out=a2[:], in0=nf[:], scalar=-CW3, in1=a2[:],
                                                                 op0=ALU.mult, op1=ALU.add), reads=[nf, a2], writes=[a2])
                k.op("dve", lambda e: e.tensor_scalar(out=r[:], in0=a2[:], scalar1=float(shift), scalar2=None,
                                                      op0=ALU.add), reads=[a2], writes=[r])
                k.op("dve", lambda e: e.tensor_single_scalar(out=m[:], in_=r[:], scalar=float(np.pi), op=ALU.is_gt),
                     reads=[r], writes=[m])
                k.op("dve", lambda e: e.scalar_tensor_tensor(out=r[:], in0=m[:], scalar=-TWO_PI, in1=r[:],
                                                             op0=ALU.mult, op1=ALU.add), reads=[m, r], writes=[r])
                k.op("dve", lambda e: e.tensor_single_scalar(out=m[:], in_=r[:], scalar=float(-np.pi), op=ALU.is_lt),
                     reads=[r], writes=[m])
                k.op("dve", lambda e: e.scalar_tensor_tensor(out=r[:], in0=m[:], scalar=TWO_PI, in1=r[:],
                                                             op0=ALU.mult, op1=ALU.add), reads=[m, r], writes=[r])
                k.op("dve", lambda e: e.tensor_scalar(out=r[:], in0=r[:], scalar1=float(np.pi), scalar2=float(-np.pi),
                                                      op0=ALU.min, op1=ALU.max), reads=[r], writes=[r])
                k.op("act", lambda e: e.activation(out=o[:], in_=r[:], func=AF.Sin), reads=[r], writes=[o])
                if which == "s":
                    k.op("dve", lambda e: e.tensor_scalar(out=o[:], in0=o[:], scalar1=cst[:, col_sgn:col_sgn + 1],
                                                          scalar2=None, op0=ALU.mult), reads=[o, cst], writes=[o])
                k.dma("sp", dst[:, sl], o[:], reads=[o], writes=[dst], waw=False)
    k.barrier()


def rmsnorm_fm(k, c, srcT, r0, nt, g_sb, gcol0, dstT, eps, name):
    n = nt * 128
    with ExitStack() as st:
        xin = [k.sb(f"{name}_x{i}", [128, nt, TCH], F32, st) for i in range(2)]
        sq = [k.sb(f"{name}_q{i}", [128, nt, TCH], BF16, st) for i in range(2)]
        rs = [k.sb(f"{name}_r{i}", [128, TCH], F32, st) for i in range(2)]
        ob = [k.sb(f"{name}_o{i}", [128, nt, TCH], BF16, st) for i in range(2)]
        for tc in range(NTC):
            b = tc % 2
            tsl = slice(tc * TCH, (tc + 1) * TCH)
            k.dma("sp", xin[b][:], srcT[r0:r0 + n, tsl].rearrange("(t p) s -> p t s", p=128),
                  reads=[srcT], writes=[xin[b]])
            k.op("act", lambda e: e.activation(out=sq[b][:], in_=xin[b][:], func=AF.Square),
                 reads=[xin[b]], writes=[sq[b]])
            ps = c.next_pf()
            for t in range(nt):
                k.op("pe", lambda e: e.matmul(ps[:, :], lhsT=c.ones[:], rhs=sq[b][:, t, :],
                                              start=(t == 0), stop=(t == nt - 1)),
                     reads=[c.ones, sq[b]], writes=[ps], inc=(t == nt - 1))
            k.op("dve", lambda e: e.tensor_scalar(out=rs[b][:], in0=ps[:, :], scalar1=1.0 / n, scalar2=float(eps),
                                                  op0=ALU.mult, op1=ALU.add), reads=[ps], writes=[rs[b]])
            k.op("act", lambda e: e.activation(out=rs[b][:], in_=rs[b][:], func=AF.Ln),
                 reads=[rs[b]], writes=[rs[b]])
            k.op("act", lambda e: e.activation(out=rs[b][:], in_=rs[b][:], func=AF.Exp, scale=-0.5),
                 reads=[rs[b]], writes=[rs[b]])
            for t in range(nt):
                k.op("dve", lambda e: e.scalar_tensor_tensor(out=ob[b][:, t, :], in0=xin[b][:, t, :],
                                                             scalar=g_sb[:, gcol0 + t:gcol0 + t + 1], in1=rs[b][:],
                                                             op0=ALU.mult, op1=ALU.mult),
                     reads=[xin[b], g_sb, rs[b]], writes=[ob[b]])
            k.dma(STQ, dstT[0:n, tsl].rearrange("(t p) s -> p t s", p=128), ob[b][:],
                  reads=[ob[b]], writes=[dstT], waw=False)
    k.barrier()


def rope_tile(k, c, st_bufs, src_ap, src_dep, perm, cos_sb, sin_sb, out_ap, out_dep):
    xb, xf, t1 = st_bufs["xb"], st_bufs["xf"], st_bufs["t1"]
    k.op("act", lambda e: e.copy(out=xf[:], in_=src_ap), reads=[src_dep], writes=[xf])
    k.op("dve", lambda e: e.tensor_copy(out=xb[:], in_=xf[:]), reads=[xf], writes=[xb])
    ps = c.next_pf()
    k.op("pe", lambda e: e.matmul(ps[:, :], lhsT=perm[:], rhs=xb[:], start=True, stop=True),
         reads=[perm, xb], writes=[ps])
    k.op("dve", lambda e: e.tensor_tensor(out=t1[:], in0=ps[:, :], in1=sin_sb, op=ALU.mult),
         reads=[ps, st_bufs["tab"]], writes=[t1])
    k.op("pool", lambda e: e.tensor_tensor(out=xf[:], in0=xf[:], in1=cos_sb, op=ALU.mult),
         reads=[xf, st_bufs["tab"]], writes=[xf])
    k.op("dve", lambda e: e.tensor_tensor(out=out_ap, in0=xf[:], in1=t1[:], op=ALU.add),
         reads=[xf, t1], writes=[out_dep])


def attention_head(k, c, st, name, maps, Vsb, vdep, dv, scale, masks, post):
    raise NotImplementedError


def attn_qchunk(k, c, j, maps, Vfn, vdep, dv, scale, masks, ptbufs, acc_banks):
    nkt = 4 * j + 4
    items = [(mi, kt) for mi in range(len(maps)) for kt in range(nkt)]
    tri = masks[0]

    def c0_of(kt):
        return 128 * (kt - 4 * j) if kt >= 4 * j else 0

    def emit_qk(i):
        mi, kt = items[i]
        parts = maps[mi]
        ps = c.pf[i % 2]
        c0 = c0_of(kt)
        for pi, p in enumerate(parts):
            k.op("pe", lambda e: e.matmul(ps[:, c0:TCH], lhsT=p["K"](kt), rhs=p["Q"](c0),
                                          start=(pi == 0), stop=(pi == len(parts) - 1)),
                 reads=[p["kd"], p["qd"]], writes=[ps], inc=(pi == len(parts) - 1))

    emit_qk(0)
    for i, (mi, kt) in enumerate(items):
        if i + 1 < len(items):
            emit_qk(i + 1)
        oacc, sacc = acc_banks[mi]
        ps = c.pf[i % 2]
        c0 = c0_of(kt)
        pt = ptbufs[i % len(ptbufs)]
        k.op("act", lambda e: e.activation(out=pt[:, c0:TCH], in_=ps[:, c0:TCH], func=AF.Exp, scale=float(scale)),
             reads=[ps], writes=[pt])
        if kt >= 4 * j:
            k.op("pool", lambda e: e.tensor_tensor(out=pt[:, c0:c0 + 128], in0=pt[:, c0:c0 + 128], in1=tri[:, 0:128],
                                                   op=ALU.mult), reads=[pt, tri], writes=[pt])
        k.op("pe", lambda e: e.matmul(oacc[:dv, c0:TCH], lhsT=Vfn(kt), rhs=pt[:, c0:TCH],
                                      start=(kt == 0), stop=(kt == nkt - 1)),
             reads=[vdep, pt], writes=[oacc], inc=False)
        k.op("pe", lambda e: e.matmul(sacc[:, c0:TCH], lhsT=c.ones[:], rhs=pt[:, c0:TCH],
                                      start=(kt == 0), stop=(kt == nkt - 1)),
             reads=[c.ones, pt], writes=[sacc], inc=True)


R_CQ, R_CKV, R_U, R_R, R_K, R_QD, R_KD, R_GATE, R_KROPE, R_LORA = 0, 512, 768, 1280, 1536, 1792, 2048, 2304, 3328, 3456
NF = 3840
NV = 256


class RopeCtx:
    def __init__(self, k, c, st, name, cosT, sinT, perm):
        self.k, self.c = k, c
        self.cosT, self.sinT, self.perm = cosT, sinT, perm
        self.tab = [k.sb(f"{name}_tab{i}", [128, 2, TCH], F32, st) for i in range(2)]
        self.xb = [k.sb(f"{name}_xb{i}", [128, TCH], BF16, st) for i in range(2)]
        self.xf = [k.sb(f"{name}_xf{i}", [128, TCH], F32, st) for i in range(2)]
        self.t1 = [k.sb(f"{name}_t1{i}", [128, TCH], F32, st) for i in range(2)]
        self.cur = None
        self.n = 0

    def load(self, tc):
        k = self.k
        tb = self.tab[tc % 2]
        tsl = slice(tc * TCH, (tc + 1) * TCH)
        k.dma("sp", tb[:, 0, :], self.cosT[:, tsl], reads=[self.cosT], writes=[tb])
        k.dma("sp", tb[:, 1, :], self.sinT[:, tsl], reads=[self.cosT], writes=[tb], waw=False)
        self.cur = tb

    def apply(self, src_ap, src_dep, out_ap, out_dep):
        k, c = self.k, self.c
        i = self.n % 2
        self.n += 1
        xb, xf, t1, tb = self.xb[i], self.xf[i], self.t1[i], self.cur
        k.op("act", lambda e: e.copy(out=xf[:], in_=src_ap), reads=[src_dep], writes=[xf])
        k.op("dve", lambda e: e.tensor_copy(out=xb[:], in_=xf[:]), reads=[xf], writes=[xb])
        ps = c.next_pf(0, 2)
        k.op("pe", lambda e: e.matmul(ps[:, :], lhsT=self.perm[:], rhs=xb[:], start=True, stop=True),
             reads=[self.perm, xb], writes=[ps])
        k.op("dve", lambda e: e.tensor_tensor(out=t1[:], in0=ps[:, :], in1=tb[:, 1, :], op=ALU.mult),
             reads=[ps, tb], writes=[t1])
        k.op("pool", lambda e: e.tensor_tensor(out=xf[:], in0=xf[:], in1=tb[:, 0, :], op=ALU.mult),
             reads=[xf, tb], writes=[xf])
        k.op("dve", lambda e: e.tensor_tensor(out=out_ap, in0=xf[:], in1=t1[:], op=ALU.add),
             reads=[xf, t1], writes=[out_dep])


def stage_mla(k, c, L):
    pT = c.pT
    rmsnorm_fm(k, c, pT, R_CQ, 4, L.prm, L.col["gq"], c.cqnT, EPS, "nq")
    rmsnorm_fm(k, c, pT, R_CKV, 2, L.prm, L.col["gkv"], c.ckvnT, EPS, "nkv")
    with ExitStack() as st:
        wbf, wd = load_w_bf16(k, st, lambda kc: L.wuq[kc * 128:(kc + 1) * 128, :], 4, 384, "wuq")
        stg = Stager(k, st, "mq_o", [128, TCH], BF16, 4)
        rp = RopeCtx(k, c, st, "mqr", c.ropeA_cos, c.ropeA_sin, c.permA)

        def epi(tc, ni, ps, ncol):
            s = stg.next()
            if ni < 2:
                copy_op(k, c.evac_eng(), s[:, :], ps[:, :], [ps], [s])
            else:
                rp.load(tc)
                rp.apply(ps[:, :], ps, s[:, :], s)
            k.dma(STQ, c.qT[ni * 128:(ni + 1) * 128, tc * TCH:(tc + 1) * TCH], s[:, :], reads=[s],
                  writes=[c.qT], waw=False)
        linear_fm(k, c, st, c.cqnT, 4, wbf, wd, [(0, 128), (128, 128), (256, 128)], epi, "mq")
    k.barrier()
    with ExitStack() as st:
        wbf, wd = load_w_bf16(k, st, lambda kc: L.wukv[kc * 128:(kc + 1) * 128, :], 2, 512, "wukv")
        stg = Stager(k, st, "mk_o", [128, TCH], BF16, 4)
        epi = epi_store_fm(k, c, stg, c.knT, lambda ni: ni * 128)
        linear_fm(k, c, st, c.ckvnT, 2, wbf, wd, [(0, 128), (128, 128)], epi, "mk")
        stgv = Stager(k, st, "mv_o", [128, 256], BF16, 4)

        def epiv(tt, ps):
            s = stgv.next()
            copy_op(k, c.evac_eng(), s[:, :], ps[:, :256], [ps], [s])
            k.dma(STQ, c.vA[tt * 128:(tt + 1) * 128, :], s[:, :], reads=[s], writes=[c.vA], waw=False)
        linear_tm(k, c, st, c.ckvnT, 2, wbf, wd, 256, 256, epiv, "mv")
        rp = RopeCtx(k, c, st, "mkr", c.ropeA_cos, c.ropeA_sin, c.permA)
        kin = [k.sb(f"mkr_in{i}", [128, TCH], F32, st) for i in range(2)]
        for tc in range(NTC):
            tsl = slice(tc * TCH, (tc + 1) * TCH)
            ki = kin[tc % 2]
            k.dma("sp", ki[:], pT[R_KROPE:R_KROPE + 128, tsl], reads=[pT], writes=[ki])
            rp.load(tc)
            s = stg.next()
            rp.apply(ki[:], ki, s[:, :], s)
            k.dma(STQ, c.krT[:, tsl], s[:, :], reads=[s], writes=[c.krT], waw=False)
    k.barrier()
    scale = (128 + 64) ** -0.5
    for h in range(2):
        with ExitStack() as st:
            Kn = k.sb("ma_kn", [128, S], BF16, st)
            Kr = k.sb("ma_kr", [128, S], BF16, st)
            Vs = k.sb("ma_v", [128, S // 128, 128], BF16, st)
            k.dma("sp", Kn[:], c.knT[h * 128:(h + 1) * 128, :], reads=[c.knT], writes=[Kn])
            k.dma("sp", Kr[:], c.krT[:, :], reads=[c.krT], writes=[Kr])
            k.dma("sp", Vs[:], c.vA[:, h * 128:(h + 1) * 128].rearrange("(kt p) d -> p kt d", p=128),
                  reads=[c.vA], writes=[Vs])
            Qn = [k.sb(f"ma_qn{i}", [128, TCH], BF16, st) for i in range(2)]
            Qr = [k.sb(f"ma_qr{i}", [128, TCH], BF16, st) for i in range(2)]
            ptb = [k.sb(f"ma_pt{i}", [128, TCH], BF16, st) for i in range(3)]
            rec = [k.sb(f"ma_rec{i}", [128, TCH], F32, st) for i in range(2)]
            ost = [k.sb(f"ma_o{i}", [128, TCH], F32, st) for i in range(2)]
            hs = slice(h * 64, (h + 1) * 64)
            for j in range(NTC):
                b = j % 2
                tsl = slice(j * TCH, (j + 1) * TCH)
                k.dma("sp", Qn[b][:], c.qT[h * 128:(h + 1) * 128, tsl], reads=[c.qT], writes=[Qn[b]])
                k.dma("sp", Qr[b][:], c.qT[256:384, tsl], reads=[c.qT], writes=[Qr[b]])
                parts = [dict(K=lambda kt: Kn[:, kt * 128:(kt + 1) * 128], Q=lambda c0: Qn[b][:, c0:TCH], kd=Kn, qd=Qn[b]),
                         dict(K=lambda kt: Kr[hs, kt * 128:(kt + 1) * 128], Q=lambda c0: Qr[b][hs, c0:TCH], kd=Kr, qd=Qr[b])]
                oacc, sacc = c.pf[2 + 2 * b], c.pf[3 + 2 * b]
                attn_qchunk(k, c, j, [parts], lambda kt: Vs[:, kt, :], Vs, 128, scale, c.masks, ptb, [(oacc, sacc)])
                k.op("act", lambda e: e.activation(out=rec[b][:], in_=sacc[:, :], func=AF.Ln), reads=[sacc], writes=[rec[b]])
                k.op("act", lambda e: e.activation(out=rec[b][:], in_=rec[b][:], func=AF.Exp, scale=-1.0), reads=[rec[b]], writes=[rec[b]])
                k.op("dve", lambda e: e.tensor_tensor(out=ost[b][:], in0=oacc[:, :], in1=rec[b][:], op=ALU.mult),
                     reads=[oacc, rec[b]], writes=[ost[b]])
                k.dma(STQ, c.mixT[h * 128:(h + 1) * 128, tsl], ost[b][:], reads=[ost[b]], writes=[c.mixT],
                      waw=False)
        k.barrier()


PRM_COLS = {}
_pc = 0


def _reg(name, n):
    global _pc
    PRM_COLS[name] = _pc
    _pc += n


_reg("gq", 4)
_reg("gkv", 2)


def const_mats():
    ident = np.eye(128, dtype=np.float32)
    permA = np.zeros((128, 128), np.float32)
    for r in range(128):
        d = r % 64
        permA[r, r + 32 if d < 32 else r - 32] = 1.0
    permD = np.zeros((128, 128), np.float32)
    for r in range(128):
        d = r % 64
        if d < 8:
            permD[r, r + 8] = 1.0
        elif d < 16:
            permD[r, r - 8] = 1.0
    masks = np.zeros((128, 4, 512), np.float32)
    kk = np.arange(128)[:, None]
    qq = np.arange(512)[None, :]
    for r in range(4):
        masks[:, r, :] = (qq >= 128 * r + kk)
    tmask = np.zeros((128, 128), np.float32)
    ti = np.arange(128) // 16
    tmask[:, :] = (ti[None, :] >= ti[:, None])
    hidx = np.arange(128) // 64
    same = (hidx[:, None] == hidx[None, :]).astype(np.float32)
    blk1 = same.copy()
    blk64 = same / 64.0
    cmat = np.concatenate([ident, permA, permD, tmask, blk1, blk64], axis=1)
    cst = np.zeros((128, 8 + 32 + 512), np.float32)
    invA = (500000.0 ** (-np.arange(0, 64, 2, dtype=np.float32) / np.float32(64))).astype(np.float32)
    invD = (500000.0 ** (-np.arange(0, 16, 2, dtype=np.float32) / np.float32(16))).astype(np.float32)
    for r in range(128):
        d = r % 64
        cst[r, 0] = invA[d % 32]
        cst[r, 1] = -1.0 if d < 32 else 1.0
        cst[r, 2] = invD[d % 8] if d < 16 else 0.0
        cst[r, 3] = (-1.0 if d < 8 else 1.0) if d < 16 else 0.0
    tau = np.zeros(32, np.float32)
    tau[0:8] = -np.arange(8)
    tau[8:17] = np.arange(9)
    tau[17:25] = 7 - np.arange(8)
    cst[:, 8:40] = tau[None, :]
    cst[:, 40:552] = (8.0 * (np.arange(512) + 1))[None, :]
    return cmat, masks.reshape(128, 2048), cst


def const_mats2():
    hidx = np.arange(128) // 64
    idx = np.arange(128) % 64
    same = (hidx[:, None] == hidx[None, :])
    mSL = (same & (idx[:, None] > idx[None, :])).astype(np.float32)
    mSU = (same & (idx[:, None] < idx[None, :])).astype(np.float32)
    mUI = (same & (idx[:, None] <= idx[None, :])).astype(np.float32)
    ident = np.eye(128, dtype=np.float32)
    return np.concatenate([np.tile(mSL, (1, 4)), np.tile(mSU, (1, 4)), np.tile(mUI, (1, 4)), np.tile(ident, (1, 8))], axis=1)


NCST = 552


def load_all_consts(k, c, cmat_d, cmask_d, cst_d, cm2_d=None):
    c.ident = k.sb("c_ident", [128, 128], BF16)
    c.permA = k.sb("c_permA", [128, 128], BF16)
    c.permD = k.sb("c_permD", [128, 128], BF16)
    c.ones = k.sb("c_ones", [128, 128], BF16)
    c.cst = k.sb("c_cst", [128, NCST], F32)
    c.tmask = k.sb("c_tmask", [128, 128], F32)
    c.blk1 = k.sb("c_blk1", [128, 128], BF16)
    c.blk64 = k.sb("c_blk64", [128, 128], BF16)
    c.mSL = k.sb("c_mSL", [128, 512], BF16)
    c.mSU = k.sb("c_mSU", [128, 512], BF16)
    c.mUI = k.sb("c_mUI", [128, 512], BF16)
    c.ident8 = k.sb("c_ident8", [128, 1024], BF16)
    c.masks = [k.sb(f"c_mask{r}", [128, 512], BF16) for r in range(4)]
    with ExitStack() as st:
        f = k.sb("lc_f", [128, 768], F32, st)
        m2 = k.sb("lc_m2", [128, 2560], F32, st)
        m = k.sb("lc_m", [128, 2048], F32, st)
        k.dma("sp", f[:], cmat_d[:, :], writes=[f])
        k.dma("sp", m[:], cmask_d[:, :], writes=[m])
        k.dma("sp", c.cst[:], cst_d[:, :], writes=[c.cst])
        k.op("dve", lambda e: e.tensor_copy(out=c.ident[:], in_=f[:, 0:128]), reads=[f], writes=[c.ident])
        k.op("dve", lambda e: e.tensor_copy(out=c.permA[:], in_=f[:, 128:256]), reads=[f], writes=[c.permA])
        k.op("dve", lambda e: e.tensor_copy(out=c.permD[:], in_=f[:, 256:384]), reads=[f], writes=[c.permD])
        k.op("dve", lambda e: e.tensor_copy(out=c.tmask[:], in_=f[:, 384:512]), reads=[f], writes=[c.tmask])
        k.op("dve", lambda e: e.tensor_copy(out=c.blk1[:], in_=f[:, 512:640]), reads=[f], writes=[c.blk1])
        k.op("dve", lambda e: e.tensor_copy(out=c.blk64[:], in_=f[:, 640:768]), reads=[f], writes=[c.blk64])
        if cm2_d is not None:
            k.dma("sp", m2[:], cm2_d[:, :], writes=[m2])
            k.op("dve", lambda e: e.tensor_copy(out=c.mSL[:], in_=m2[:, 0:512]), reads=[m2], writes=[c.mSL])
            k.op("dve", lambda e: e.tensor_copy(out=c.mSU[:], in_=m2[:, 512:1024]), reads=[m2], writes=[c.mSU])
            k.op("dve", lambda e: e.tensor_copy(out=c.mUI[:], in_=m2[:, 1024:1536]), reads=[m2], writes=[c.mUI])
            k.op("dve", lambda e: e.tensor_copy(out=c.ident8[:], in_=m2[:, 1536:2560]), reads=[m2], writes=[c.ident8])
        k.op("dve", lambda e: e.memset(c.ones[:], 1.0), writes=[c.ones])
        for r in range(4):
            k.op("dve", lambda e: e.tensor_copy(out=c.masks[r][:], in_=m[:, r * 512:(r + 1) * 512]),
                 reads=[m], writes=[c.masks[r]])
        k.barrier()


O_CQ, O_CKV, O_KR, O_U, O_Z, O_QD, O_KD, O_VD, O_GATE = 0, 512, 768, 832, 1344, 3008, 3520, 4032, 4544


def s5_gperm(half):
    return np.concatenate([half * 16 + np.arange(16), (1 - half) * 16 + np.arange(16)])


def s5_chperm(half):
    return (s5_gperm(half)[:, None] * 16 + np.arange(16)[None, :]).reshape(-1)


def fm_cols(half):
    a = np.arange
    cols = [O_CQ + a(512), O_CKV + a(256), O_U + s5_chperm(half),
            O_Z + half * 256 + a(256), O_Z + 512 + half * 256 + a(256),
            O_QD + half * 256 + a(256), O_KD + half * 256 + a(256)]
    for b in range(4):
        cols.append(O_GATE + b * 512 + half * 256 + a(256))
    cols += [O_KR + a(64), O_KR + a(64), O_Z + 1536 + a(64), O_Z + 1600 + a(64), O_Z + 1024 + half * 256 + a(256)]
    return np.concatenate(cols)


def tm_cols(half):
    a = np.arange
    return O_VD + half * 256 + a(256)


def pt_layout(v, nt):
    return np.ascontiguousarray(np.asarray(v).reshape(nt, 128).T)


def core_layer_arrays(inp, l, half):
    out = {}
    w_in = inp["w_in"][l]
    out["w_in"] = np.ascontiguousarray(w_in[:, np.concatenate([fm_cols(half), tm_cols(half)])])
    hs = [2 * half, 2 * half + 1]
    wuq = inp["mla_w_uq"][l]
    out["wuq"] = np.ascontiguousarray(np.concatenate(
        [wuq[:, h * 192:h * 192 + 128] for h in hs] + [wuq[:, h * 192 + 128:(h + 1) * 192] for h in hs], axis=1))
    wukv = inp["mla_w_ukv"][l]
    out["wukv"] = np.ascontiguousarray(np.concatenate(
        [wukv[:, h * 256:h * 256 + 128] for h in hs] + [wukv[:, h * 256 + 128:(h + 1) * 256] for h in hs], axis=1))
    prm = np.zeros((128, _pc), np.float32)

    def put(name, arr):
        arr = np.asarray(arr, np.float32)
        if arr.ndim == 1:
            arr = arr[:, None]
        prm[:arr.shape[0], PRM_COLS[name]:PRM_COLS[name] + arr.shape[1]] = arr
    put("gq", pt_layout(inp["mla_q_norm_g"][l], 4))
    put("gkv", pt_layout(inp["mla_kv_norm_g"][l], 2))
    for nm in ("lq1", "lk1", "lq2", "lk2"):
        put(nm, np.broadcast_to(inp["diff_" + nm][l][None, :], (128, 64)))
    put("gsub", inp["diff_subln_g"][l])
    lam_init = 0.8 - 0.6 * math.exp(-0.3 * l)
    put("lam_init", np.full((128,), lam_init, np.float32))
    put("omlam", np.full((128,), 1.0 - lam_init, np.float32))
    fill_more(inp, l, half, put, out)
    out["prm"] = prm
    return out


def st_layout(a):
    a = np.asarray(a)
    rest = a.shape[2:]
    a = a.reshape((16, 128) + rest)
    return np.ascontiguousarray(np.moveaxis(a, 0, 1))


def fill_more(inp, l, half, put, out=None):
    gp = s5_gperm(half)
    chp = s5_chperm(half)
    put("s5_are", st_layout(inp["s5_a_re"][l][gp]))
    put("s5_aim", st_layout(inp["s5_a_im"][l][gp]))
    put("s5_ldt", st_layout(np.broadcast_to(inp["s5_log_dt"][l][gp][:, None], (32, 64))))
    put("s5_d", pt_layout(inp["s5_d"][l][chp], 4))
    put("s5_bg", pt_layout(inp["s5_b_glu"][l][half * 256:(half + 1) * 256], 2))
    mu = inp["rwkv_mu"][l]
    my = slice(half * 256, (half + 1) * 256)
    put("rw_mur", pt_layout(mu[0:512][my], 2))
    put("rw_muk", pt_layout(mu[512:1024][my], 2))
    put("rw_muv", pt_layout(mu[1024:1536][my], 2))
    put("rw_mul", mu[1536:1664])
    put("rw_w0", pt_layout(inp["rwkv_w0"][l][my], 2))
    put("rw_a0", pt_layout(inp["rwkv_a0"][l][my], 2))
    put("rw_kk", pt_layout(inp["rwkv_k_k"][l][my], 2))
    put("rw_ka", pt_layout(inp["rwkv_k_a"][l][my], 2))
    put("rw_rk", pt_layout(inp["rwkv_r_k"][l].reshape(512)[my], 2))
    put("rw_lng", pt_layout(inp["rwkv_ln_g"][l][my], 2))
    put("rw_lnb", pt_layout(inp["rwkv_ln_b"][l][my], 2))
    if out is not None:
        out["w2a2"] = np.ascontiguousarray(np.concatenate([inp["rwkv_w2"][l][:, my], inp["rwkv_a2"][l][:, my]], axis=0))
        b = np.stack([inp["s5_b_re"][l][gp], inp["s5_b_im"][l][gp]], axis=2)
        out["s5b"] = st_layout(b).reshape(128, 512).astype(np.float32)
        cc = np.stack([np.swapaxes(inp["s5_c_re"][l][gp], 1, 2), np.swapaxes(inp["s5_c_im"][l][gp], 1, 2)], axis=2)
        out["s5c"] = st_layout(cc).reshape(128, 512).astype(np.float32)
        out["wglu"] = np.ascontiguousarray(inp["s5_w_glu"][l][chp][:, half * 256:(half + 1) * 256])


_reg("lq1", 64)
_reg("lk1", 64)
_reg("lq2", 64)
_reg("lk2", 64)
_reg("gsub", 1)
_reg("lam_init", 1)
_reg("omlam", 1)
DIFF_EPS = 1e-5


def stage_diff(k, c, L):
    pT, pV = c.pT, c.pV
    with ExitStack() as st:
        rp = RopeCtx(k, c, st, "dr", c.ropeD_cos, c.ropeD_sin, c.permD)
        xin = [k.sb(f"dr_in{i}", [128, TCH], F32, st) for i in range(3)]
        stg = Stager(k, st, "dr_o", [128, TCH], BF16, 4)
        n = 0
        for tc in range(NTC):
            tsl = slice(tc * TCH, (tc + 1) * TCH)
            rp.load(tc)
            for (r0, dst) in ((R_QD, c.qdT), (R_KD, c.kdT)):
                for hd in range(2):
                    xi = xin[n % 3]
                    n += 1
                    k.dma("sp", xi[:], pT[r0 + hd * 128:r0 + (hd + 1) * 128, tsl], reads=[pT], writes=[xi])
                    s = stg.next()
                    rp.apply(xi[:], xi, s[:, :], s)
                    k.dma(STQ, dst[hd * 128:(hd + 1) * 128, tsl], s[:, :], reads=[s], writes=[dst], waw=False)
    k.barrier()
    with ExitStack() as st0:
        sm = k.sb("df_sm", [128, 8], F32, st0)
        tmp = k.sb("df_tmp", [128, 64], F32, st0)
        cq1, ck1, cq2, ck2, cg = (L.col[n_] for n_ in ("lq1", "lk1", "lq2", "lk2", "gsub"))
        for i, (a, b_) in enumerate(((cq1, ck1), (cq2, ck2))):
            k.op("dve", lambda e: e.tensor_tensor(out=tmp[:], in0=L.prm[:, a:a + 64], in1=L.prm[:, b_:b_ + 64],
                                                  op=ALU.mult), reads=[L.prm], writes=[tmp])
            k.op("dve", lambda e: e.reduce_sum(out=sm[:, i:i + 1], in_=tmp[:], axis=AX.X), reads=[tmp], writes=[sm])
            k.op("act", lambda e: e.activation(out=sm[:, 2 + i:3 + i], in_=sm[:, i:i + 1], func=AF.Exp),
                 reads=[sm], writes=[sm])
        k.op("dve", lambda e: e.tensor_tensor(out=sm[:, 4:5], in0=sm[:, 3:4], in1=sm[:, 2:3], op=ALU.subtract),
             reads=[sm], writes=[sm])
        cli, col_ = L.col["lam_init"], L.col["omlam"]
        k.op("dve", lambda e: e.tensor_tensor(out=sm[:, 5:6], in0=sm[:, 4:5], in1=L.prm[:, cli:cli + 1], op=ALU.subtract),
             reads=[sm, L.prm], writes=[sm])
        k.op("dve", lambda e: e.tensor_tensor(out=sm[:, 6:7], in0=L.prm[:, cg:cg + 1], in1=L.prm[:, col_:col_ + 1], op=ALU.mult),
             reads=[L.prm], writes=[sm])
        nlam = sm[:, 5:6]
        gs = sm[:, 6:7]
        scale = 64 ** -0.5
        for hd in range(2):
            with ExitStack() as st:
                Kd = k.sb("da_k", [128, S], BF16, st)
                Vf = k.sb("da_vf", [128, S // 128, 128], F32, st)
                Vs = k.sb("da_v", [128, S // 128, 128], BF16, st)
                k.dma("sp", Kd[:], c.kdT[hd * 128:(hd + 1) * 128, :], reads=[c.kdT], writes=[Kd])
                k.dma("sp", Vf[:], pV[:, hd * 128:(hd + 1) * 128].rearrange("(kt p) d -> p kt d", p=128),
                      reads=[pV], writes=[Vf])
                k.op("pool", lambda e: e.tensor_copy(out=Vs[:], in_=Vf[:]), reads=[Vf], writes=[Vs])
                Qd = [k.sb(f"da_q{i}", [128, TCH], BF16, st) for i in range(2)]
                ptb = [k.sb(f"da_pt{i}", [128, TCH], BF16, st) for i in range(3)]
                rec = k.sb("da_rec", [128, TCH], F32, st)
                o1 = k.sb("da_o1", [128, TCH], F32, st)
                o2 = k.sb("da_o2", [128, TCH], F32, st)
                sq = k.sb("da_sq", [128, TCH], BF16, st)
                rs = k.sb("da_rs", [128, TCH], F32, st)
                ost = [k.sb(f"da_o{i}", [128, TCH], F32, st) for i in range(2)]
                for j in range(NTC):
                    b = j % 2
                    tsl = slice(j * TCH, (j + 1) * TCH)
                    k.dma("sp", Qd[b][:], c.qdT[hd * 128:(hd + 1) * 128, tsl], reads=[c.qdT], writes=[Qd[b]])
                    maps = []
                    for m in range(2):
                        ms = slice(m * 64, (m + 1) * 64)
                        maps.append([dict(K=(lambda kt, ms=ms: Kd[ms, kt * 128:(kt + 1) * 128]),
                                          Q=(lambda c0, ms=ms: Qd[b][ms, c0:TCH]), kd=Kd, qd=Qd[b])])
                    accs = [(c.pf[2], c.pf[3]), (c.pf[4], c.pf[5])]
                    attn_qchunk(k, c, j, maps, lambda kt: Vs[:, kt, :], Vs, 128, scale, c.masks, ptb, accs)
                    (oa1, sa1), (oa2, sa2) = accs
                    k.op("act", lambda e: e.activation(out=rec[:], in_=sa1[:, :], func=AF.Ln), reads=[sa1], writes=[rec])
                    k.op("act", lambda e: e.activation(out=rec[:], in_=rec[:], func=AF.Exp, scale=-1.0), reads=[rec], writes=[rec])
                    k.op("dve", lambda e: e.tensor_tensor(out=o1[:], in0=oa1[:, :], in1=rec[:], op=ALU.mult),
                         reads=[oa1, rec], writes=[o1])
                    k.op("act", lambda e: e.activation(out=rec[:], in_=sa2[:, :], func=AF.Ln), reads=[sa2], writes=[rec])
                    k.op("act", lambda e: e.activation(out=rec[:], in_=rec[:], func=AF.Exp, scale=-1.0), reads=[rec], writes=[rec])
                    k.op("dve", lambda e: e.tensor_tensor(out=o2[:], in0=oa2[:, :], in1=rec[:], op=ALU.mult),
                         reads=[oa2, rec], writes=[o2])
                    k.op("dve", lambda e: e.scalar_tensor_tensor(out=o1[:], in0=o2[:], scalar=nlam, in1=o1[:],
                                                                 op0=ALU.mult, op1=ALU.add),
                         reads=[o2, o1, sm], writes=[o1])
                    k.op("pool", lambda e: e.tensor_tensor(out=sq[:], in0=o1[:], in1=o1[:], op=ALU.mult), reads=[o1], writes=[sq])
                    ps = c.next_pf(0, 2)
                    k.op("pe", lambda e: e.matmul(ps[:, :], lhsT=c.ones[:], rhs=sq[:], start=True, stop=True),
                         reads=[c.ones, sq], writes=[ps])
                    k.op("dve", lambda e: e.tensor_scalar(out=rs[:], in0=ps[:, :], scalar1=1.0 / 128, scalar2=DIFF_EPS,
                                                          op0=ALU.mult, op1=ALU.add), reads=[ps], writes=[rs])
                    k.op("act", lambda e: e.activation(out=rs[:], in_=rs[:], func=AF.Ln), reads=[rs], writes=[rs])
                    k.op("act", lambda e: e.activation(out=rs[:], in_=rs[:], func=AF.Exp, scale=-0.5), reads=[rs], writes=[rs])
                    k.op("dve", lambda e: e.scalar_tensor_tensor(out=ost[b][:], in0=o1[:], scalar=gs, in1=rs[:],
                                                                 op0=ALU.mult, op1=ALU.mult),
                         reads=[o1, rs, sm], writes=[ost[b]])
                    k.dma(STQ, c.mixT[768 + hd * 128:768 + (hd + 1) * 128, tsl], ost[b][:], reads=[ost[b]],
                          writes=[c.mixT], waw=False)
            k.barrier()


_reg("s5_are", 16)
_reg("s5_aim", 16)
_reg("s5_ldt", 16)
_reg("s5_d", 4)
_reg("s5_bg", 2)
Z_NEG0, Z_POS0, Z_REV0 = 0, 8, 17
GELU_C = 1.5957691216057308


def sincos_alloc(k, st, name, shape):
    return dict(ni=k.sb(name + "_ni", shape, I32, st), nf=k.sb(name + "_nf", shape, F32, st),
                a2=k.sb(name + "_a2", shape, F32, st), r=k.sb(name + "_r", shape, F32, st),
                m=k.sb(name + "_m", shape, F32, st))


def sincos_tile(k, tmp, ang, sin_out, cos_out):
    ni, nf, a2, r, m = tmp["ni"], tmp["nf"], tmp["a2"], tmp["r"], tmp["m"]
    k.op("dve", lambda e: e.tensor_scalar(out=ni[:], in0=ang[:], scalar1=float(1.0 / TWO_PI), scalar2=None,
                                          op0=ALU.mult), reads=[ang], writes=[ni])
    k.op("dve", lambda e: e.tensor_copy(out=nf[:], in_=ni[:]), reads=[ni], writes=[nf])
    k.op("dve", lambda e: e.scalar_tensor_tensor(out=a2[:], in0=nf[:], scalar=-CW1, in1=ang[:], op0=ALU.mult,
                                                 op1=ALU.add), reads=[nf, ang], writes=[a2])
    k.op("dve", lambda e: e.scalar_tensor_tensor(out=a2[:], in0=nf[:], scalar=-CW2, in1=a2[:], op0=ALU.mult,
                                                 op1=ALU.add), reads=[nf, a2], writes=[a2])
    k.op("dve", lambda e: e.scalar_tensor_tensor(out=a2[:], in0=nf[:], scalar=-CW3, in1=a2[:], op0=ALU.mult,
                                                 op1=ALU.add), reads=[nf, a2], writes=[a2])
    for shift, dst in ((0.0, sin_out), (np.pi / 2, cos_out)):
        k.op("dve", lambda e: e.tensor_scalar(out=r[:], in0=a2[:], scalar1=float(shift), scalar2=None, op0=ALU.add),
             reads=[a2], writes=[r])
        k.op("dve", lambda e: e.tensor_single_scalar(out=m[:], in_=r[:], scalar=float(np.pi), op=ALU.is_gt),
             reads=[r], writes=[m])
        k.op("dve", lambda e: e.scalar_tensor_tensor(out=r[:], in0=m[:], scalar=-TWO_PI, in1=r[:], op0=ALU.mult,
                                                     op1=ALU.add), reads=[m, r], writes=[r])
        k.op("dve", lambda e: e.tensor_single_scalar(out=m[:], in_=r[:], scalar=float(-np.pi), op=ALU.is_lt),
             reads=[r], writes=[m])
        k.op("dve", lambda e: e.scalar_tensor_tensor(out=r[:], in0=m[:], scalar=TWO_PI, in1=r[:], op0=ALU.mult,
                                                     op1=ALU.add), reads=[m, r], writes=[r])
        k.op("dve", lambda e: e.tensor_scalar(out=r[:], in0=r[:], scalar1=float(np.pi), scalar2=float(-np.pi),
                                              op0=ALU.min, op1=ALU.max), reads=[r], writes=[r])
        k.op("act", lambda e: e.activation(out=dst[:], in_=r[:], func=AF.Sin), reads=[r], writes=[dst])


def stage_s5(k, c, L):
    import os as _os
    NT = 16
    pT = c.pT
    SH4 = [128, NT, 8, 16]
    with ExitStack() as stA:
        Tm = k.sb("s5_Tm", [128, 32, 128], BF16, stA)
        GstR = k.sb("s5_GstR", [128, NT, 128], BF16, stA)
        GstI = k.sb("s5_GstI", [128, NT, 128], BF16, stA)
        EfR = k.sb("s5_EfR", SH4, BF16, stA)
        EfnI = k.sb("s5_EfnI", SH4, BF16, stA)
        sc = k.sb("s5_sc", [128, 8, NT], F32, stA)
        LR, DT, ML, TH, MAG8, FRE, FIM, TMP = range(8)
        ca, ci_, cl = L.col["s5_are"], L.col["s5_aim"], L.col["s5_ldt"]
        AIM = L.prm[:, ci_:ci_ + NT]
        with ExitStack() as st:
            bsb = k.sb("s5_b", [128, NT, 2, 16], F32, st)
            csb = k.sb("s5_c", [128, NT, 2, 16], F32, st)
            k.dma("sp", bsb[:], L.s5b[:, :].rearrange("p (t r c) -> p t r c", t=NT, r=2), writes=[bsb])
            k.dma("sp", csb[:], L.s5c[:, :].rearrange("p (t r c) -> p t r c", t=NT, r=2), writes=[csb])
            k.op("dve", lambda e: e.tensor_scalar(out=sc[:, LR, :], in0=L.prm[:, ca:ca + NT], scalar1=-1e-4,
                                                  scalar2=None, op0=ALU.min), reads=[L.prm], writes=[sc])
            k.op("act", lambda e: e.activation(out=sc[:, DT, :], in_=L.prm[:, cl:cl + NT], func=AF.Exp),
                 reads=[L.prm], writes=[sc])
            k.op("dve", lambda e: e.tensor_tensor(out=sc[:, ML, :], in0=sc[:, DT, :], in1=sc[:, LR, :], op=ALU.mult),
                 reads=[sc], writes=[sc])
            k.op("dve", lambda e: e.tensor_tensor(out=sc[:, TH, :], in0=sc[:, DT, :], in1=AIM, op=ALU.mult),
                 reads=[sc, L.prm], writes=[sc])
            k.op("act", lambda e: e.activation(out=sc[:, MAG8, :], in_=sc[:, ML, :], func=AF.Exp, scale=8.0),
                 reads=[sc], writes=[sc])
            SH3 = [128, NT, 32]
            lm = k.sb("s5_lm", SH3, F32, st)
            an = k.sb("s5_an", SH3, F32, st)
            mg = k.sb("s5_mg", SH3, F32, st)
            sn = k.sb("s5_sn", SH3, F32, st)
            cs = k.sb("s5_cs", SH3, F32, st)
            zr = k.sb("s5_zr", SH3, F32, st)
            zi = k.sb("s5_zi", SH3, F32, st)
            tauB = c.cst[:, 8:40].unsqueeze(1).to_broadcast(SH3)
            k.op("dve", lambda e: e.tensor_tensor(out=lm[:], in0=sc[:, ML, :].unsqueeze(2).to_broadcast(SH3), in1=tauB,
                                                  op=ALU.mult), reads=[sc, c.cst], writes=[lm])
            k.op("dve", lambda e: e.tensor_tensor(out=an[:], in0=sc[:, TH, :].unsqueeze(2).to_broadcast(SH3), in1=tauB,
                                                  op=ALU.mult), reads=[sc, c.cst], writes=[an])
            k.op("act", lambda e: e.activation(out=mg[:], in_=lm[:], func=AF.Exp), reads=[lm], writes=[mg])
            sincos_tile(k, sincos_alloc(k, st, "s5sc0", SH3), an, sn, cs)
            k.op("dve", lambda e: e.tensor_tensor(out=zr[:], in0=mg[:], in1=cs[:], op=ALU.mult), reads=[mg, cs], writes=[zr])
            k.op("dve", lambda e: e.tensor_tensor(out=zi[:], in0=mg[:], in1=sn[:], op=ALU.mult), reads=[mg, sn], writes=[zi])
            if _os.environ.get("S5_STOP") == "a":
                k.barrier()
                return
            sm = k.sb("s5_sm", [128, 8, NT], F32, st)
            abr, abi = zr[:, :, Z_POS0 + 1], zi[:, :, Z_POS0 + 1]
            lr_ = sc[:, LR, :]

            def tt(out, a, b, op, rd, wr, E="dve"):
                k.op(E, lambda e: e.tensor_tensor(out=out, in0=a, in1=b, op=op), reads=rd, writes=wr)
            tt(sm[:, 0, :], lr_, lr_, ALU.mult, [sc], [sm])
            tt(sm[:, 1, :], AIM, AIM, ALU.mult, [L.prm], [sm])
            tt(sm[:, 0, :], sm[:, 0, :], sm[:, 1, :], ALU.add, [sm], [sm])
            k.op("dve", lambda e: e.reciprocal(out=sm[:, 0, :], in_=sm[:, 0, :]), reads=[sm], writes=[sm])
            k.op("dve", lambda e: e.tensor_scalar(out=sm[:, 1, :], in0=abr, scalar1=-1.0, scalar2=None, op0=ALU.add),
                 reads=[zr], writes=[sm])
            tt(sm[:, 2, :], sm[:, 1, :], lr_, ALU.mult, [sm, sc], [sm])
            tt(sm[:, 3, :], abi, AIM, ALU.mult, [zi, L.prm], [sm])
            tt(sm[:, 2, :], sm[:, 2, :], sm[:, 3, :], ALU.add, [sm], [sm])
            tt(sc[:, FRE, :], sm[:, 2, :], sm[:, 0, :], ALU.mult, [sm], [sc])
            tt(sm[:, 4, :], abi, lr_, ALU.mult, [zi, sc], [sm])
            tt(sm[:, 5, :], sm[:, 1, :], AIM, ALU.mult, [sm, L.prm], [sm])
            tt(sm[:, 4, :], sm[:, 4, :], sm[:, 5, :], ALU.subtract, [sm], [sm])
            tt(sc[:, FIM, :], sm[:, 4, :], sm[:, 0, :], ALU.mult, [sm], [sc])
            if _os.environ.get("S5_STOP") == "b":
                k.barrier()
                return
            SHB = [128, NT, 16]
            bbr = k.sb("s5_bbr", SHB, F32, st)
            bbi = k.sb("s5_bbi", SHB, F32, st)
            t1 = k.sb("s5_t1", SHB, F32, st)
            fre = sc[:, FRE, :].unsqueeze(2).to_broadcast(SHB)
            fim = sc[:, FIM, :].unsqueeze(2).to_broadcast(SHB)
            br_, bi_ = bsb[:, :, 0, :], bsb[:, :, 1, :]
            tt(bbr[:], fre, br_, ALU.mult, [sc, bsb], [bbr])
            tt(t1[:], fim, bi_, ALU.mult, [sc, bsb], [t1])
            tt(bbr[:], bbr[:], t1[:], ALU.subtract, [bbr, t1], [bbr])
            tt(bbi[:], fre, bi_, ALU.mult, [sc, bsb], [bbi])
            tt(t1[:], fim, br_, ALU.mult, [sc, bsb], [t1])
            tt(bbi[:], bbi[:], t1[:], ALU.add, [bbi, t1], [bbi])
            if _os.environ.get("S5_STOP") == "c":
                k.barrier()
                return
            BfR = k.sb("s5_BfR", SH4, BF16, st)
            BfnI = k.sb("s5_BfnI", SH4, BF16, st)
            CfR = k.sb("s5_CfR", SH4, BF16, st)
            CfI = k.sb("s5_CfI", SH4, BF16, st)
            GfR = k.sb("s5_GfR", SH4, BF16, st)
            GfI = k.sb("s5_GfI", SH4, BF16, st)
            u1 = [k.sb(f"s5_u1{i}", SH4, F32, st) for i in range(2)]
            u2 = [k.sb(f"s5_u2{i}", SH4, F32, st) for i in range(2)]

            def cmul(oR, oI, z0, xr, xi, xdeps, neg_im, i):
                E = "dve" if i % 2 == 0 else "pool"
                zR = zr[:, :, z0:z0 + 8].unsqueeze(3).to_broadcast(SH4)
                zI = zi[:, :, z0:z0 + 8].unsqueeze(3).to_broadcast(SH4)
                xR = xr.unsqueeze(2).to_broadcast(SH4)
                xI = xi.unsqueeze(2).to_broadcast(SH4)
                a, b_ = u1[i % 2], u2[i % 2]
                tt(a[:], zR, xR, ALU.mult, [zr, ] + xdeps, [a], E)
                tt(b_[:], zI, xI, ALU.mult, [zi, ] + xdeps, [b_], E)
                tt(oR[:], a[:], b_[:], ALU.subtract, [a, b_], [oR], E)
                tt(a[:], zR, xI, ALU.mult, [zr, ] + xdeps, [a], E)
                tt(b_[:], zI, xR, ALU.mult, [zi, ] + xdeps, [b_], E)
                if neg_im:
                    k.op("dve", lambda e: e.scalar_tensor_tensor(out=oI[:], in0=a[:], scalar=-1.0, in1=b_[:],
                                                                 op0=ALU.mult, op1=ALU.subtract),
                         reads=[a, b_], writes=[oI])
                else:
                    tt(oI[:], a[:], b_[:], ALU.add, [a, b_], [oI], E)
            cr_, ci2 = csb[:, :, 0, :], csb[:, :, 1, :]
            cmul(BfR, BfnI, Z_NEG0, bbr[:], bbi[:], [bbr, bbi], True, 0)
            cmul(CfR, CfI, Z_POS0, cr_, ci2, [csb], False, 1)
            cmul(GfR, GfI, Z_REV0, bbr[:], bbi[:], [bbr, bbi], False, 0)
            cmul(EfR, EfnI, Z_POS0 + 1, cr_, ci2, [csb], True, 1)
            if _os.environ.get("S5_STOP") == "d":
                k.barrier()
                return
            for t4 in range(4):
                for hh in range(2):
                    ps = c.next_pf()
                    hs = slice(hh * 64, (hh + 1) * 64)
                    for q in range(4):
                        t = t4 * 4 + q
                        k.op("pe", lambda e: e.matmul(ps[:, q * 128:(q + 1) * 128],
                                                      lhsT=BfR[hs, t, :, :].rearrange("p a b -> p (a b)"),
                                                      rhs=CfR[hs, t, :, :].rearrange("p a b -> p (a b)"), start=True, stop=False),
                             reads=[BfR, CfR], writes=[ps], inc=False)
                        k.op("pe", lambda e: e.matmul(ps[:, q * 128:(q + 1) * 128],
                                                      lhsT=BfnI[hs, t, :, :].rearrange("p a b -> p (a b)"),
                                                      rhs=CfI[hs, t, :, :].rearrange("p a b -> p (a b)"), start=False, stop=True),
                             reads=[BfnI, CfI], writes=[ps], inc=(q == 3))
                    for q in range(4):
                        g = 2 * (t4 * 4 + q) + hh
                        k.op("dve", lambda e: e.tensor_tensor(out=Tm[:, g, :], in0=ps[:, q * 128:(q + 1) * 128],
                                                              in1=c.tmask[:], op=ALU.mult),
                             reads=[ps, c.tmask], writes=[Tm])
            if _os.environ.get("S5_STOP") == "e":
                k.barrier()
                return
            for (src, dst) in ((GfR, GstR), (GfI, GstI)):
                for t8 in range(2):
                    pb = c.next_pb()
                    for q in range(8):
                        t = t8 * 8 + q
                        k.op("pe", lambda e: e.transpose(pb[:, q * 128:(q + 1) * 128],
                                                         src[:, t, :, :].rearrange("p a b -> p (a b)"), c.ident[:]),
                             reads=[src, c.ident], writes=[pb], inc=(q == 7))
                    k.op("act", lambda e: e.copy(out=dst[:, t8 * 8:(t8 + 1) * 8, :],
                                                 in_=pb[:].rearrange("p (q n) -> p q n", q=8)),
                         reads=[pb], writes=[dst])
            k.barrier()
        import os as _os
        if _os.environ.get("S5_STOP") == "1":
            return
        with ExitStack() as st:
            ug = [[k.sb(f"s5_u{i}{j}", [128, TCH], BF16, st) for j in range(2)] for i in range(2)]
            ang = k.sb("s5_ang", [128, TCH], F32, st)
            sn = k.sb("s5_snj", [128, TCH], F32, st)
            cs = k.sb("s5_csj", [128, TCH], F32, st)
            a_ = k.sb("s5_a", [128, TCH], F32, st)
            b_ = k.sb("s5_bq", [128, TCH], F32, st)
            mre = k.sb("s5_mre", [128, TCH], F32, st)
            mim = k.sb("s5_mim", [128, TCH], F32, st)
            hre = k.sb("s5_hre", [128, TCH], F32, st)
            him = k.sb("s5_him", [128, TCH], F32, st)
            Hp = [[k.sb(f"s5_Hp{i}{j}", [128, TCH + 1], BF16, st) for j in range(2)] for i in range(2)]
            yst = [k.sb(f"s5_y{i}", [128, TCH], F32, st) for i in range(3)]
            for i in range(2):
                for j in range(2):
                    k.op("pool", lambda e: e.memset(Hp[i][j][:, 0:1], 0.0), writes=[Hp[i][j]])
            sctmp = sincos_alloc(k, st, "s5scj", [128, TCH])
            jv = c.cst[:, 40:552]
            nyi = 0

            def tt(out, a, b, op, rd, wr, E="dve"):
                k.op(E, lambda e: e.tensor_tensor(out=out, in0=a, in1=b, op=op), reads=rd, writes=wr)
            if True:
                for t in range(NT):
                    pp = t % 2
                    for hh in range(2):
                        k.dma("sp", ug[pp][hh][:], c.us5[2 * t + hh, :, :], reads=[c.us5], writes=[ug[pp][hh]])
                    Xre, Xim = c.pf[0], c.pf[1]
                    for hh in range(2):
                        hs = slice(hh * 64, (hh + 1) * 64)
                        k.op("pe", lambda e: e.matmul(Xre[hs, :], lhsT=GstR[:, t, hs], rhs=ug[pp][hh][:], start=True, stop=True),
                             reads=[GstR, ug[pp][hh]], writes=[Xre], inc=(hh == 1))
                    for hh in range(2):
                        hs = slice(hh * 64, (hh + 1) * 64)
                        k.op("pe", lambda e: e.matmul(Xim[hs, :], lhsT=GstI[:, t, hs], rhs=ug[pp][hh][:], start=True, stop=True),
                             reads=[GstI, ug[pp][hh]], writes=[Xim], inc=(hh == 1))
                    k.op("pool", lambda e: e.tensor_scalar(out=ang[:], in0=jv, scalar1=sc[:, TH, t:t + 1], scalar2=None,
                                                           op0=ALU.mult), reads=[c.cst, sc], writes=[ang])
                    sincos_tile(k, sctmp, ang, sn, cs)
                    tt(a_[:], Xre[:, :], cs[:], ALU.mult, [Xre, cs], [a_])
                    tt(b_[:], Xim[:, :], sn[:], ALU.mult, [Xim, sn], [b_])
                    tt(mre[:], a_[:], b_[:], ALU.add, [a_, b_], [mre], "pool")
                    tt(a_[:], Xim[:, :], cs[:], ALU.mult, [Xim, cs], [a_])
                    tt(b_[:], Xre[:, :], sn[:], ALU.mult, [Xre, sn], [b_])
                    tt(mim[:], a_[:], b_[:], ALU.subtract, [a_, b_], [mim], "pool")
                    m8 = sc[:, MAG8, t:t + 1].to_broadcast([128, TCH])
                    k.op("dve", lambda e: e.tensor_tensor_scan(out=hre[:], data0=m8, data1=mre[:], initial=0.0,
                                                               op0=ALU.mult, op1=ALU.add), reads=[sc, mre], writes=[hre])
                    k.op("dve", lambda e: e.tensor_tensor_scan(out=him[:], data0=m8, data1=mim[:], initial=0.0,
                                                               op0=ALU.mult, op1=ALU.add), reads=[sc, mim], writes=[him])
                    hr, hi_ = Hp[pp][0], Hp[pp][1]
                    tt(a_[:], hre[:], cs[:], ALU.mult, [hre, cs], [a_])
                    tt(b_[:], him[:], sn[:], ALU.mult, [him, sn], [b_], "pool")
                    tt(hr[:, 1:TCH + 1], a_[:], b_[:], ALU.subtract, [a_, b_], [hr])
                    tt(mre[:], hre[:], sn[:], ALU.mult, [hre, sn], [mre])
                    tt(mim[:], him[:], cs[:], ALU.mult, [him, cs], [mim], "pool")
                    tt(hi_[:, 1:TCH + 1], mre[:], mim[:], ALU.add, [mre, mim], [hi_])
                    for hh in range(2):
                        g = 2 * t + hh
                        hs = slice(hh * 64, (hh + 1) * 64)
                        py = c.next_pf(2, 6)
                        k.op("pe", lambda e: e.matmul(py[:, :], lhsT=Tm[:, g, :], rhs=ug[pp][hh][:], start=True, stop=False),
                             reads=[Tm, ug[pp][hh]], writes=[py], inc=False)
                        k.op("pe", lambda e: e.matmul(py[:, :], lhsT=EfR[hs, t, :, :].rearrange("p a b -> p (a b)"),
                                                      rhs=hr[hs, 0:TCH], start=False, stop=False),
                             reads=[EfR, hr], writes=[py], inc=False)
                        k.op("pe", lambda e: e.matmul(py[:, :], lhsT=EfnI[hs, t, :, :].rearrange("p a b -> p (a b)"),
                                                      rhs=hi_[hs, 0:TCH], start=False, stop=True),
                             reads=[EfnI, hi_], writes=[py], inc=True)
                        ys = yst[nyi % 3]
                        nyi += 1
                        k.op("act", lambda e: e.copy(out=ys[:], in_=py[:, :]), reads=[py], writes=[ys])
                        k.dma(STQ, c.ys5[g, :, :], ys[:], reads=[ys], writes=[c.ys5], waw=False)
    k.barrier()
    if _os.environ.get("S5_STOP") == "2":
        return
    cd = L.col["s5_d"]
    with ExitStack() as st:
        Y = k.sb("s5p_Y", [128, 8, TCH], F32, st)
        U = k.sb("s5p_U", [128, S], F32, st)
        yy = k.sb("s5p_yy", [128, S], F32, st)
        q1 = k.sb("s5p_q1", [128, S], F32, st)
        gb = k.sb("s5p_gb", [128, S], BF16, st)
        for ct in range(4):
            for gl in range(8):
                k.dma("sp", Y[gl * 16:(gl + 1) * 16, :, :],
                      c.ys5[ct * 8 + gl, :, :].rearrange("(t c) j -> c t j", c=16), reads=[c.ys5], writes=[Y],
                      waw=(gl == 0))
            k.dma("sp", U[:], pT[R_U + ct * 128:R_U + (ct + 1) * 128, :], reads=[pT], writes=[U])
            k.op("dve", lambda e: e.scalar_tensor_tensor(out=yy[:].rearrange("p (j t) -> p j t", t=8),
                                                         in0=U[:].rearrange("p (j t) -> p j t", t=8),
                                                         scalar=L.prm[:, cd + ct:cd + ct + 1],
                                                         in1=Y[:].rearrange("p t j -> p j t"),
                                                         op0=ALU.mult, op1=ALU.add), reads=[U, Y, L.prm], writes=[yy])
            k.op("act", lambda e: e.activation(out=q1[:], in_=yy[:], func=AF.Square), reads=[yy], writes=[q1])
            k.op("dve", lambda e: e.tensor_scalar(out=q1[:], in0=q1[:], scalar1=0.044715, scalar2=1.0, op0=ALU.mult,
                                                  op1=ALU.add), reads=[q1], writes=[q1])
            k.op("pool", lambda e: e.tensor_tensor(out=q1[:], in0=q1[:], in1=yy[:], op=ALU.mult), reads=[q1, yy], writes=[q1])
            k.op("act", lambda e: e.activation(out=q1[:], in_=q1[:], func=AF.Sigmoid, scale=GELU_C), reads=[q1], writes=[q1])
            k.op("dve", lambda e: e.tensor_tensor(out=yy[:], in0=yy[:], in1=q1[:], op=ALU.mult), reads=[yy, q1], writes=[yy])
            k.op("pool", lambda e: e.tensor_copy(out=gb[:], in_=yy[:]), reads=[yy], writes=[gb])
            k.dma(STQ, c.gT[ct * 128:(ct + 1) * 128, :], gb[:], reads=[gb], writes=[c.gT], waw=False)
            if ct < 2:
                k.dma(STQ, c.gF[ct * 128:(ct + 1) * 128, :], yy[:], reads=[yy], writes=[c.gF], waw=False)
    k.barrier()
    cb = L.col["s5_bg"]
    with ExitStack() as st:
        wbf, wd = load_w_bf16(k, st, lambda kc: L.wglu[kc * 128:(kc + 1) * 128, :], 4, 256, "wglu")
        gin = [k.sb(f"s5g_g{i}", [128, TCH], F32, st) for i in range(3)]
        sg = [k.sb(f"s5g_s{i}", [128, TCH], F32, st) for i in range(3)]
        n = [0]

        def epi(tc, ni, ps, ncol):
            i = n[0] % 3
            n[0] += 1
            tsl = slice(tc * TCH, (tc + 1) * TCH)
            k.dma("sp", gin[i][:], c.gF[ni * 128:(ni + 1) * 128, tsl], reads=[c.gF], writes=[gin[i]])
            k.op("act", lambda e: e.activation(out=sg[i][:], in_=ps[:, :], func=AF.Sigmoid,
                                               bias=L.prm[:, cb + ni:cb + ni + 1], scale=1.0),
                 reads=[ps, L.prm], writes=[sg[i]])
            k.op("dve", lambda e: e.tensor_tensor(out=sg[i][:], in0=sg[i][:], in1=gin[i][:], op=ALU.mult),
                 reads=[sg[i], gin[i]], writes=[sg[i]])
            k.dma(STQ, c.mixT[256 + ni * 128:256 + (ni + 1) * 128, tsl], sg[i][:], reads=[sg[i]], writes=[c.mixT],
                  waw=False)
        linear_fm(k, c, st, c.gT, 4, wbf, wd, [(0, 128), (128, 128)], epi, "s5g")
    k.barrier()


for _n, _w in (("rw_mur", 2), ("rw_muk", 2), ("rw_muv", 2), ("rw_mul", 1), ("rw_w0", 2), ("rw_a0", 2),
               ("rw_kk", 2), ("rw_ka", 2), ("rw_rk", 2), ("rw_lng", 2), ("rw_lnb", 2)):
    _reg(_n, _w)
R_RV = 3584
SEG = 1024
NCK = SEG // 64
RW_EPS = 64e-5
LDC = -0.6065306597126334


def stage_rwkv(k, c, L):
    pT = c.pT
    col = L.col
    with ExitStack() as stA:
        P = lambda nm, j: L.prm[:, col[nm] + j:col[nm] + j + 1]
        wl = k.sb("rw_wl", [128, 256], BF16, stA)
        omka = k.sb("rw_omka", [128, 2], F32, stA)
        rmask = k.sb("rw_rmask", [128, SEG], F32, stA)
        with ExitStack() as st0:
            wlf = k.sb("rw_wlf", [128, 256], F32, st0)
            k.dma("sp", wlf[:], L.w2a2[:, :], writes=[wlf])
            k.op("dve", lambda e: e.tensor_copy(out=wl[:], in_=wlf[:]), reads=[wlf], writes=[wl])
            k.op("dve", lambda e: e.tensor_scalar(out=omka[:], in0=L.prm[:, col["rw_ka"]:col["rw_ka"] + 2], scalar1=-1.0,
                                                  scalar2=1.0, op0=ALU.mult, op1=ALU.add), reads=[L.prm], writes=[omka])
            k.op("pool", lambda e: e.memset(rmask[:], 1.0), writes=[rmask])
            k.op("pool", lambda e: e.memset(rmask[:].rearrange("p (c i) -> p c i", i=64)[:, :, 0:1], 0.0), writes=[rmask])
            k.barrier()
        F = lambda nm: k.sb("rw_" + nm, [128, SEG], F32, stA)
        rz = k.sb("rw_rz", [128, SEG + 1], F32, stA)
        kz = k.sb("rw_kz", [128, SEG + 1], F32, stA)
        vz = k.sb("rw_vz", [128, SEG + 1], F32, stA)
        lz = k.sb("rw_lz", [128, SEG + 1], F32, stA)
        rm, km, vm, lm, t1, t2, sgw, av, cl, Epos, Eneg, Eex, Eh, kkr, kk, k2, bv = (F(n) for n in (
            "rm", "km", "vm", "lm", "t1", "t2", "sgw", "av", "cl", "Epos", "Eneg", "Eex", "Eh", "kkr", "kk", "k2", "bv"))
        lbf = k.sb("rw_lbf", [128, SEG], BF16, stA)
        sqb = k.sb("rw_sqb", [128, SEG], BF16, stA)
        BDn = ("AT", "BT", "KT", "RT", "bhT", "khT", "vT", "Bh", "Kh", "Vb", "Lst", "Mst", "LakT", "ArbT", "ArkT", "TT")
        BD = {n: k.sb("rw_bd_" + n, [128, NCK, 128], BF16, stA) for n in BDn}
        Ln = [k.sb(f"rw_Ln{i}", [128, 8, 128], BF16, stA) for i in range(2)]
        Mn = [k.sb(f"rw_Mn{i}", [128, 8, 128], BF16, stA) for i in range(2)]
        Pn = [k.sb(f"rw_Pn{i}", [128, 8, 128], BF16, stA) for i in range(2)]
        Sf = k.sb("rw_Sf", [128, 128], F32, stA)
        Sb = [k.sb(f"rw_Sb{i}", [128, 128], BF16, stA) for i in range(2)]
        RHSb = k.sb("rw_RHSb", [128, 128], BF16, stA)
        Ub = k.sb("rw_Ub", [128, 128], BF16, stA)
        OT = k.sb("rw_OT", [128, SEG], F32, stA)
        ob = k.sb("rw_ob", [128, SEG], BF16, stA)
        yo = [k.sb(f"rw_yo{i}", [128, SEG], F32, stA) for i in range(2)]
        for n in ("AT", "BT", "KT", "RT", "bhT", "khT", "vT"):
            k.op("pool", lambda e: e.memset(BD[n][:], 0.0), writes=[BD[n]])

        def tt(out, a, b, op, rd, wr, E="dve"):
            k.op(E, lambda e: e.tensor_tensor(out=out, in0=a, in1=b, op=op), reads=rd, writes=wr)

        def act(out, in_, func, rd, wr, **kw):
            k.op("act", lambda e: e.activation(out=out, in_=in_, func=func, **kw), reads=rd, writes=wr)

        def bdw(dst, a, b, rd, E="dve", scalar=None):
            for h in range(2):
                hs = slice(h * 64, (h + 1) * 64)
                o = dst[hs, :, h * 64:(h + 1) * 64]
                av_ = a[hs, :].rearrange("p (c i) -> p c i", i=64)
                if b is None:
                    k.op(E, lambda e: e.tensor_copy(out=o, in_=av_), reads=rd, writes=[dst])
                    continue
                bv_ = b[hs, :].rearrange("p (c i) -> p c i", i=64)
                if scalar is None:
                    k.op(E, lambda e: e.tensor_tensor(out=o, in0=av_, in1=bv_, op=ALU.mult), reads=rd, writes=[dst])
                else:
                    k.op("dve", lambda e: e.scalar_tensor_tensor(out=o, in0=av_, scalar=float(scalar), in1=bv_,
                                                                 op0=ALU.mult, op1=ALU.mult), reads=rd, writes=[dst])

        for hp in range(2):
            k.op("dve", lambda e: e.memset(Sf[:], 0.0), writes=[Sf])
            k.op("dve", lambda e: e.memset(Sb[0][:], 0.0), writes=[Sb[0]])
            sbi = 0
            for seg in range(S // SEG):
                t0 = seg * SEG
                for (zt, r0, mu, dst) in ((rz, R_R + hp * 128, P("rw_mur", hp), rm), (kz, R_K + hp * 128, P("rw_muk", hp), km),
                                          (vz, R_RV + hp * 128, P("rw_muv", hp), vm), (lz, R_LORA, P("rw_mul", 0), lm)):
                    if seg == 0:
                        k.op("pool", lambda e: e.memset(zt[:, 0:1], 0.0), writes=[zt])
                        k.dma("sp", zt[:, 1:SEG + 1], pT[r0:r0 + 128, 0:SEG], reads=[pT], writes=[zt])
                    else:
                        k.dma("sp", zt[:, 0:SEG + 1], pT[r0:r0 + 128, t0 - 1:t0 + SEG], reads=[pT], writes=[zt])
                    tt(t1[:], zt[:, 0:SEG], zt[:, 1:SEG + 1], ALU.subtract, [zt], [t1], "pool")
                    k.op("dve", lambda e: e.scalar_tensor_tensor(out=dst[:], in0=t1[:], scalar=mu, in1=zt[:, 1:SEG + 1],
                                                                 op0=ALU.mult, op1=ALU.add), reads=[t1, zt, L.prm], writes=[dst])
                act(lbf[0:64, :], lm[0:64, :], AF.Tanh, [lm], [lbf])
                k.op("dve", lambda e: e.tensor_copy(out=lbf[64:128, :], in_=lm[64:128, :]), reads=[lm], writes=[lbf])
                for hb in range(SEG // TCH):
                    cs_ = slice(hb * TCH, (hb + 1) * TCH)
                    pw, pa = c.pf[0], c.pf[1]
                    k.op("pe", lambda e: e.matmul(pw[:, :], lhsT=wl[0:64, hp * 128:(hp + 1) * 128], rhs=lbf[0:64, cs_],
                                                  start=True, stop=True), reads=[wl, lbf], writes=[pw])
                    k.op("pe", lambda e: e.matmul(pa[:, :], lhsT=wl[64:128, hp * 128:(hp + 1) * 128], rhs=lbf[64:128, cs_],
                                                  start=True, stop=True), reads=[wl, lbf], writes=[pa])
                    act(sgw[:, cs_], pw[:, :], AF.Sigmoid, [pw, L.prm], [sgw], bias=P("rw_w0", hp), scale=1.0)
                    act(av[:, cs_], pa[:, :], AF.Sigmoid, [pa, L.prm], [av], bias=P("rw_a0", hp), scale=1.0)
                k.op("dve", lambda e: e.tensor_tensor_scan(out=cl[:], data0=rmask[:], data1=sgw[:], initial=0.0,
                                                           op0=ALU.mult, op1=ALU.add), reads=[rmask, sgw], writes=[cl])
                act(Epos[:], cl[:], AF.Exp, [cl], [Epos], scale=LDC)
                act(Eneg[:], cl[:], AF.Exp, [cl], [Eneg], scale=-LDC)
                tt(t1[:], cl[:], sgw[:], ALU.subtract, [cl, sgw], [t1], "dve")
                act(Eex[:], t1[:], AF.Exp, [t1], [Eex], scale=LDC)
                clC = cl[:].rearrange("p (c i) -> p c i", i=64)[:, :, 63:64].to_broadcast([128, NCK, 64])
                tt(t2[:].rearrange("p (c i) -> p c i", i=64), clC, cl[:].rearrange("p (c i) -> p c i", i=64),
                   ALU.subtract, [cl], [t2])
                act(Eh[:], t2[:], AF.Exp, [t2], [Eh], scale=LDC)
                k.op("dve", lambda e: e.tensor_scalar(out=kkr[:], in0=km[:], scalar1=P("rw_kk", hp), scalar2=None,
                                                      op0=ALU.mult), reads=[km, L.prm], writes=[kkr])
                act(sqb[:], kkr[:], AF.Square, [kkr], [sqb])
                for hb in range(SEG // TCH):
                    cs_ = slice(hb * TCH, (hb + 1) * TCH)
                    pn = c.next_pf(2, 6)
                    k.op("pe", lambda e: e.matmul(pn[:, :], lhsT=c.blk1[:], rhs=sqb[:, cs_], start=True, stop=True),
                         reads=[c.blk1, sqb], writes=[pn])
                    k.op("dve", lambda e: e.tensor_scalar(out=t1[:, cs_], in0=pn[:, :], scalar1=1e-24, scalar2=None, op0=ALU.max),
                         reads=[pn], writes=[t1])
                act(t1[:], t1[:], AF.Ln, [t1], [t1])
                act(t1[:], t1[:], AF.Exp, [t1], [t1], scale=-0.5)
                tt(kk[:], kkr[:], t1[:], ALU.mult, [kkr, t1], [kk])
                k.op("dve", lambda e: e.tensor_scalar(out=t2[:], in0=av[:], scalar1=P("rw_ka", hp), scalar2=omka[:, hp:hp + 1],
                                                      op0=ALU.mult, op1=ALU.add), reads=[av, L.prm, omka], writes=[t2])
                tt(k2[:], km[:], t2[:], ALU.mult, [km, t2], [k2], "pool")
                tt(bv[:], kk[:], av[:], ALU.mult, [kk, av], [bv], "pool")
                bdw(BD["AT"], kk, Eex, [kk, Eex], scalar=-1.0)
                bdw(BD["BT"], bv, Eneg, [bv, Eneg], "pool")
                bdw(BD["KT"], k2, Eneg, [k2, Eneg], "dve")
                bdw(BD["RT"], rm, Epos, [rm, Epos], "pool")
                bdw(BD["bhT"], bv, Eh, [bv, Eh], "dve")
                bdw(BD["khT"], k2, Eh, [k2, Eh], "pool")
                bdw(BD["vT"], vm, None, [vm], "dve")
                for oc in range(NCK // 8):
                    c8 = slice(oc * 8, (oc + 1) * 8)
                    prods = (("AT", "BT", "Lst", c.mSL), ("BT", "AT", "Mst", c.mSU), ("KT", "AT", "LakT", c.mSU),
                             ("BT", "RT", "ArbT", c.mUI), ("KT", "RT", "ArkT", c.mUI))
                    for (la, rb, dn, mk) in prods:
                        for g4 in range(2):
                            ps = c.next_pf()
                            for q in range(4):
                                cc = oc * 8 + g4 * 4 + q
                                k.op("pe", lambda e: e.matmul(ps[:, q * 128:(q + 1) * 128], lhsT=BD[la][:, cc, :],
                                                              rhs=BD[rb][:, cc, :], start=True, stop=True),
                                     reads=[BD[la], BD[rb]], writes=[ps], inc=(q == 3))
                            c4 = slice(oc * 8 + g4 * 4, oc * 8 + g4 * 4 + 4)
                            tt(BD[dn][:, c4, :].rearrange("p a b -> p (a b)"), ps[:, :], mk[:], ALU.mult, [ps, mk], [BD[dn]])
                    for (src, dst) in (("bhT", "Bh"), ("khT", "Kh"), ("vT", "Vb")):
                        pb = c.next_pb()
                        for q in range(8):
                            cc = oc * 8 + q
                            k.op("pe", lambda e: e.transpose(pb[:, q * 128:(q + 1) * 128], BD[src][:, cc, :], c.ident[:]),
                                 reads=[BD[src], c.ident], writes=[pb], inc=(q == 7))
                        k.op("act", lambda e: e.copy(out=BD[dst][:, c8, :].rearrange("p a b -> p (a b)"), in_=pb[:]),
                             reads=[pb], writes=[BD[dst]])
                    Lc, Mc, Pc = BD["Lst"][:, c8, :], BD["Mst"][:, c8, :], None
                    Ld, Md = BD["Lst"], BD["Mst"]
                    k.op("dve", lambda e: e.tensor_tensor(out=Pn[0][:].rearrange("p a b -> p (a b)"),
                                                          in0=BD["Mst"][:, c8, :].rearrange("p a b -> p (a b)"),
                                                          in1=c.ident8[:], op=ALU.add), reads=[BD["Mst"], c.ident8], writes=[Pn[0]])
                    pcur = 0
                    for n in range(1, 6):
                        di = n % 2
                        psL = [c.pf[0], c.pf[1]]
                        psM = [c.pf[2], c.pf[3]]
                        for g4 in range(2):
                            for q in range(4):
                                qq = g4 * 4 + q
                                k.op("pe", lambda e: e.matmul(psL[g4][:, q * 128:(q + 1) * 128], lhsT=Mc[:, qq, :], rhs=Lc[:, qq, :],
                                                              start=True, stop=True), reads=[Md, Ld], writes=[psL[g4]], inc=(q == 3))
                            if n < 5:
                                for q in range(4):
                                    qq = g4 * 4 + q
                                    k.op("pe", lambda e: e.matmul(psM[g4][:, q * 128:(q + 1) * 128], lhsT=Lc[:, qq, :], rhs=Mc[:, qq, :],
                                                                  start=True, stop=True), reads=[Ld, Md], writes=[psM[g4]], inc=(q == 3))
                        for g4 in range(2):
                            k.op("act", lambda e: e.copy(out=Ln[di][:, g4 * 4:(g4 + 1) * 4, :].rearrange("p a b -> p (a b)"),
                                                         in_=psL[g4][:, :]), reads=[psL[g4]], writes=[Ln[di]])
                            if n < 5:
                                k.op("dve", lambda e: e.tensor_copy(out=Mn[di][:, g4 * 4:(g4 + 1) * 4, :].rearrange("p a b -> p (a b)"),
                                                                    in_=psM[g4][:, :]), reads=[psM[g4]], writes=[Mn[di]])
                        Lc, Mc, Ld, Md = Ln[di][:, :, :], Mn[di][:, :, :], Ln[di], Mn[di]
                        psP = [c.pf[4], c.pf[5]]
                        pnx = 1 - pcur
                        for g4 in range(2):
                            for q in range(4):
                                qq = g4 * 4 + q
                                k.op("pe", lambda e: e.matmul(psP[g4][:, q * 128:(q + 1) * 128], lhsT=Lc[:, qq, :], rhs=Pn[pcur][:, qq, :],
                                                              start=True, stop=True), reads=[Ld, Pn[pcur]], writes=[psP[g4]], inc=(q == 3))
                        for g4 in range(2):
                            g4s = slice(g4 * 4, (g4 + 1) * 4)
                            if n < 5:
                                o_ = Pn[pnx][:, g4s, :].rearrange("p a b -> p (a b)")
                                wr = Pn[pnx]
                            else:
                                o_ = BD["TT"][:, oc * 8 + g4 * 4:oc * 8 + g4 * 4 + 4, :].rearrange("p a b -> p (a b)")
                                wr = BD["TT"]
                            tt(o_, psP[g4][:, :], Pn[pcur][:, g4s, :].rearrange("p a b -> p (a b)"), ALU.add,
                               [psP[g4], Pn[pcur]], [wr])
                        pcur = pnx
                for cc in range(NCK):
                    So = Sb[sbi]
                    Sn = Sb[1 - sbi]
                    pR, pU, pS, pO = c.pf[0], c.pf[1], c.pf[2], c.pf[3]
                    k.op("pe", lambda e: e.matmul(pR[:, 0:128], lhsT=BD["LakT"][:, cc, :], rhs=BD["Vb"][:, cc, :], start=True, stop=False),
                         reads=[BD["LakT"], BD["Vb"]], writes=[pR], inc=False)
                    k.op("pe", lambda e: e.matmul(pR[:, 0:128], lhsT=BD["AT"][:, cc, :], rhs=So[:], start=False, stop=True),
                         reads=[BD["AT"], So], writes=[pR])
                    k.op("act", lambda e: e.copy(out=RHSb[:], in_=pR[:, 0:128]), reads=[pR], writes=[RHSb])
                    k.op("pe", lambda e: e.matmul(pU[:, 0:128], lhsT=BD["TT"][:, cc, :], rhs=RHSb[:], start=True, stop=True),
                         reads=[BD["TT"], RHSb], writes=[pU])
                    k.op("dve", lambda e: e.tensor_copy(out=Ub[:], in_=pU[:, 0:128]), reads=[pU], writes=[Ub])
                    k.op("pe", lambda e: e.matmul(pS[:, 0:128], lhsT=BD["Kh"][:, cc, :], rhs=BD["Vb"][:, cc, :], start=True, stop=False),
                         reads=[BD["Kh"], BD["Vb"]], writes=[pS], inc=False)
                    k.op("pe", lambda e: e.matmul(pS[:, 0:128], lhsT=BD["Bh"][:, cc, :], rhs=Ub[:], start=False, stop=True),
                         reads=[BD["Bh"], Ub], writes=[pS])
                    gC = Epos[:, cc * 64 + 63:cc * 64 + 64]
                    k.op("dve", lambda e: e.scalar_tensor_tensor(out=Sf[:], in0=Sf[:], scalar=gC, in1=pS[:, 0:128],
                                                                 op0=ALU.mult, op1=ALU.add), reads=[Sf, Epos, pS], writes=[Sf])
                    k.op("act", lambda e: e.copy(out=Sn[:], in_=Sf[:]), reads=[Sf], writes=[Sn])
                    k.op("pe", lambda e: e.matmul(pO[:, 0:128], lhsT=BD["Vb"][:, cc, :], rhs=BD["ArkT"][:, cc, :], start=True, stop=False),
                         reads=[BD["Vb"], BD["ArkT"]], writes=[pO], inc=False)
                    k.op("pe", lambda e: e.matmul(pO[:, 0:128], lhsT=So[:], rhs=BD["RT"][:, cc, :], start=False, stop=False),
                         reads=[So, BD["RT"]], writes=[pO], inc=False)
                    k.op("pe", lambda e: e.matmul(pO[:, 0:128], lhsT=Ub[:], rhs=BD["ArbT"][:, cc, :], start=False, stop=True),
                         reads=[Ub, BD["ArbT"]], writes=[pO])
                    for h in range(2):
                        hs = slice(h * 64, (h + 1) * 64)
                        k.op("pool" if False else "act", lambda e: e.copy(out=OT[hs, cc * 64:(cc + 1) * 64], in_=pO[hs, h * 64:(h + 1) * 64]),
                             reads=[pO], writes=[OT])
                    sbi = 1 - sbi
                y = yo[seg % 2]
                k.op("pool", lambda e: e.tensor_copy(out=ob[:], in_=OT[:]), reads=[OT], writes=[ob])
                for hb in range(SEG // TCH):
                    cs_ = slice(hb * TCH, (hb + 1) * TCH)
                    pm = c.next_pf(4, 6)
                    k.op("pe", lambda e: e.matmul(pm[:, :], lhsT=c.blk64[:], rhs=ob[:, cs_], start=True, stop=True),
                         reads=[c.blk64, ob], writes=[pm])
                    tt(t1[:, cs_], OT[:, cs_], pm[:, :], ALU.subtract, [OT, pm], [t1])
                act(sqb[:], t1[:], AF.Square, [t1], [sqb])
                for hb in range(SEG // TCH):
                    cs_ = slice(hb * TCH, (hb + 1) * TCH)
                    pm = c.next_pf(4, 6)
                    k.op("pe", lambda e: e.matmul(pm[:, :], lhsT=c.blk64[:], rhs=sqb[:, cs_], start=True, stop=True),
                         reads=[c.blk64, sqb], writes=[pm])
                    k.op("dve", lambda e: e.tensor_scalar(out=t2[:, cs_], in0=pm[:, :], scalar1=RW_EPS, scalar2=None, op0=ALU.add),
                         reads=[pm], writes=[t2])
                act(t2[:], t2[:], AF.Ln, [t2], [t2])
                act(t2[:], t2[:], AF.Exp, [t2], [t2], scale=-0.5)
                tt(t1[:], t1[:], t2[:], ALU.mult, [t1, t2], [t1])
                k.op("dve", lambda e: e.tensor_scalar(out=y[:], in0=t1[:], scalar1=P("rw_lng", hp), scalar2=P("rw_lnb", hp),
                                                      op0=ALU.mult, op1=ALU.add), reads=[t1, L.prm], writes=[y])
                k.op("dve", lambda e: e.scalar_tensor_tensor(out=sqb[:], in0=rm[:], scalar=P("rw_rk", hp), in1=k2[:],
                                                             op0=ALU.mult, op1=ALU.mult), reads=[rm, k2, L.prm], writes=[sqb])
                for hb in range(SEG // TCH):
                    cs_ = slice(hb * TCH, (hb + 1) * TCH)
                    pm = c.next_pf(4, 6)
                    k.op("pe", lambda e: e.matmul(pm[:, :], lhsT=c.blk1[:], rhs=sqb[:, cs_], start=True, stop=True),
                         reads=[c.blk1, sqb], writes=[pm])
                    tt(t2[:, cs_], pm[:, :], vm[:, cs_], ALU.mult, [pm, vm], [t2])
                tt(y[:], y[:], t2[:], ALU.add, [y, t2], [y], "pool")
                k.dma(STQ, c.mixT[512 + hp * 128:512 + (hp + 1) * 128, t0:t0 + SEG], y[:], reads=[y], writes=[c.mixT], waw=False)
        k.barrier()


def stage_outproj(k, c, L, xa, xb, y, xa_deps=None, cc=None):
    pT = c.pT
    with ExitStack() as st:
        wbf, wd = load_w_bf16(k, st, lambda kc: L.wout[kc * 128:(kc + 1) * 128, :], 8, D, "wout", nstg=2)
        mx = [k.sb(f"op_mx{i}", [128, 8, TCH], F32, st) for i in range(2)]
        gt = [k.sb(f"op_gt{i}", [128, 8, TCH], F32, st) for i in range(2)]
        sg = k.sb("op_sg", [128, 8, TCH], F32, st)
        mg = [k.sb(f"op_mg{i}", [128, 8, TCH], BF16, st) for i in range(2)]
        xt = [k.sb(f"op_xa{i}", [128, D], F32, st) for i in range(2)]
        xu = [k.sb(f"op_xb{i}", [128, D], F32, st) for i in range(2)]
        yo = [k.sb(f"op_y{i}", [128, D], F32, st) for i in range(2)]
        ydeps = [Dep() for _ in range(NTC)] if cc is not None else [y.dep] * NTC

        def emit_cc(i):
            xs_out, xs_deps, groups = cc
            rs = slice(i * TCH, (i + 1) * TCH)
            k.collective(y[rs, :], xs_out[rs, :], reads=[ydeps[i]], writes=[xs_deps[i]], groups=groups)
        for tc in range(NTC):
            b = tc % 2
            tsl = slice(tc * TCH, (tc + 1) * TCH)
            k.dma("sp", mx[b][:], c.mixT[:, tsl].rearrange("(kc p) t -> p kc t", p=128), reads=[c.mixT], writes=[mx[b]])
            k.dma("sp", gt[b][:], pT[R_GATE:R_GATE + 1024, tsl].rearrange("(kc p) t -> p kc t", p=128), reads=[pT],
                  writes=[gt[b]])
            k.op("act", lambda e: e.activation(out=sg[:], in_=gt[b][:], func=AF.Sigmoid), reads=[gt[b]], writes=[sg])
            k.op("pool", lambda e: e.tensor_tensor(out=gt[b][:], in0=gt[b][:], in1=mx[b][:], op=ALU.mult),
                 reads=[gt[b], mx[b]], writes=[gt[b]])
            k.op("dve", lambda e: e.tensor_tensor(out=mg[b][:], in0=gt[b][:], in1=sg[:], op=ALU.mult),
                 reads=[gt[b], sg], writes=[mg[b]])
            for sub in range(4):
                tt_ = tc * 4 + sub
                xb_i = tt_ % 2
                rows = slice(tt_ * 128, (tt_ + 1) * 128)
                k.dma("sp", xt[xb_i][:], xa[rows, :], reads=[xa_deps[tc] if xa_deps else xa], writes=[xt[xb_i]])
                if xb is not None:
                    k.dma("sp", xu[xb_i][:], xb[rows, :], reads=[xb], writes=[xu[xb_i]])
                    k.op("pool", lambda e: e.tensor_tensor(out=xt[xb_i][:], in0=xt[xb_i][:], in1=xu[xb_i][:], op=ALU.add),
                         reads=[xt[xb_i], xu[xb_i]], writes=[xt[xb_i]])
                yb = yo[xb_i]
                for n in range(4):
                    ps = c.next_pf()
                    for kc in range(8):
                        k.op("pe", lambda e: e.matmul(ps[:, :], lhsT=mg[b][:, kc, sub * 128:(sub + 1) * 128],
                                                      rhs=wbf[:, kc, n * 512:(n + 1) * 512], start=(kc == 0), stop=(kc == 7)),
                             reads=[mg[b], wd[kc]], writes=[ps], inc=(kc == 7))
                    k.op("dve", lambda e: e.scalar_tensor_tensor(out=yb[:, n * 512:(n + 1) * 512], in0=xt[xb_i][:, n * 512:(n + 1) * 512],
                                                                 scalar=0.5, in1=ps[:, :], op0=ALU.mult, op1=ALU.add),
                         reads=[xt[xb_i], ps], writes=[yb])
                k.dma(STQ, y[rows, :], yb[:], reads=[yb], writes=[ydeps[tc]], waw=False)
            if cc is not None and tc >= 1:
                emit_cc(tc - 1)
        if cc is not None:
            emit_cc(NTC - 1)
    k.barrier()


def stage_final(k, c, xa, xb, g_bc_d, out, xa_deps=None):
    with ExitStack() as st:
        gbc = k.sb("f_gbc", [128, D], F32, st)
        k.dma("sp", gbc[:], g_bc_d.partition_broadcast(128), writes=[gbc])
        xt = [k.sb(f"f_xt{i}", [128, D], F32, st) for i in range(2)]
        xu = [k.sb(f"f_xu{i}", [128, D], F32, st) for i in range(2)]
        junk = k.sb("f_junk", [128, D], BF16, st)
        yo = [k.sb(f"f_y{i}", [128, D], F32, st) for i in range(2)]
        ss = [k.sb(f"f_ss{i}", [128, 4], F32, st) for i in range(2)]
        for tt_ in range(S // 128):
            b = tt_ % 2
            rows = slice(tt_ * 128, (tt_ + 1) * 128)
            k.dma("sp", xt[b][:], xa[rows, :], reads=[xa_deps[tt_ // 4] if xa_deps else xa], writes=[xt[b]])
            if xb is not None:
                k.dma("sp", xu[b][:], xb[rows, :], reads=[xb], writes=[xu[b]])
                k.op("pool", lambda e: e.tensor_tensor(out=xt[b][:], in0=xt[b][:], in1=xu[b][:], op=ALU.add),
                     reads=[xt[b], xu[b]], writes=[xt[b]])
            k.op("act", lambda e: e.activation(out=junk[:], in_=xt[b][:], func=AF.Square, accum_out=ss[b][:, 0:1]),
                 reads=[xt[b]], writes=[junk, ss[b]])
            k.op("dve", lambda e: e.tensor_scalar(out=ss[b][:, 1:2], in0=ss[b][:, 0:1], scalar1=1.0 / D, scalar2=EPS,
                                                  op0=ALU.mult, op1=ALU.add), reads=[ss[b]], writes=[ss[b]])
            k.op("act", lambda e: e.activation(out=ss[b][:, 2:3], in_=ss[b][:, 1:2], func=AF.Sqrt), reads=[ss[b]], writes=[ss[b]])
            k.op("dve", lambda e: e.reciprocal(out=ss[b][:, 3:4], in_=ss[b][:, 2:3]), reads=[ss[b]], writes=[ss[b]])
            k.op("dve", lambda e: e.scalar_tensor_tensor(out=yo[b][:], in0=xt[b][:], scalar=ss[b][:, 3:4], in1=gbc[:],
                                                         op0=ALU.mult, op1=ALU.mult), reads=[xt[b], ss[b], gbc], writes=[yo[b]])
            k.dma(STQ, out[rows, :], yo[b][:], reads=[yo[b]], writes=[out], waw=False)
    k.barrier()


class LayerIO:
    pass


def declare_layer_inputs(k, l):
    EI = "ExternalInput"
    L = LayerIO()
    L.col = PRM_COLS
    L.norm_g = k.dram(f"norm_g{l}", [1, D], F32, kind=EI)
    L.w_in = k.dram(f"w_in{l}", [D, NF + NV], F32, kind=EI)
    L.wuq = k.dram(f"wuq{l}", [512, 384], F32, kind=EI)
    L.wukv = k.dram(f"wukv{l}", [256, 512], F32, kind=EI)
    L.prm_d = k.dram(f"prm{l}", [128, _pc], F32, kind=EI)
    L.s5b = k.dram(f"s5b{l}", [128, 512], F32, kind=EI)
    L.s5c = k.dram(f"s5c{l}", [128, 512], F32, kind=EI)
    L.wglu = k.dram(f"wglu{l}", [512, 256], F32, kind=EI)
    L.w2a2 = k.dram(f"w2a2{l}", [128, 256], F32, kind=EI)
    L.wout = k.dram(f"wout{l}", [1024, D], F32, kind=EI)
    return L


def layer_input_arrays(inp, l, half, suffix):
    a = core_layer_arrays(inp, l, half)
    out = {
        f"norm_g{suffix}": np.ascontiguousarray(inp["norm_g"][l][None, :]),
        f"w_in{suffix}": a["w_in"], f"wuq{suffix}": a["wuq"], f"wukv{suffix}": a["wukv"], f"prm{suffix}": a["prm"],
        f"s5b{suffix}": a["s5b"], f"s5c{suffix}": a["s5c"], f"wglu{suffix}": a["wglu"], f"w2a2{suffix}": a["w2a2"],
    }
    rows = np.concatenate([b * 512 + half * 256 + np.arange(256) for b in range(4)])
    out[f"wout{suffix}"] = np.ascontiguousarray(inp["w_out"][l][rows, :])
    return out


def declare_scratch(k, c):
    c.hT = k.dram("sc_hT", [D, S], BF16)
    c.pT = k.dram("sc_pT", [NF, S], F32)
    c.pV = k.dram("sc_pV", [S, NV], F32)
    c.us5 = k.dram("sc_us5", [32, 128, 512], BF16)
    c.ys5 = k.dram("sc_ys5", [32, 128, 512], F32)
    c.gT = k.dram("sc_gT", [512, S], BF16)
    c.gF = k.dram("sc_gF", [256, S], F32)
    c.mixT = k.dram("sc_mixT", [1024, S], F32)
    c.cqnT = k.dram("sc_cqnT", [512, S], BF16)
    c.ckvnT = k.dram("sc_ckvnT", [256, S], BF16)
    c.qT = k.dram("sc_qT", [384, S], BF16)
    c.knT = k.dram("sc_knT", [256, S], BF16)
    c.vA = k.dram("sc_vA", [S, 256], BF16)
    c.krT = k.dram("sc_krT", [128, S], BF16)
    c.qdT = k.dram("sc_qdT", [256, S], BF16)
    c.kdT = k.dram("sc_kdT", [256, S], BF16)
    c.ropeA_cos = k.dram("sc_rAc", [128, S], F32)
    c.ropeA_sin = k.dram("sc_rAs", [128, S], F32)
    c.ropeD_cos = k.dram("sc_rDc", [128, S], F32)
    c.ropeD_sin = k.dram("sc_rDs", [128, S], F32)


def emit_layer(k, c, L, xa, xb, y, xa_deps=None, cc=None):
    with ExitStack() as st:
        L.prm = k.sb("prm_sb", [128, _pc], F32, st)
        k.dma("sp", L.prm[:], L.prm_d[:, :], writes=[L.prm])
        import os as _os
        sel = _os.environ.get("LAYER_STAGES", "nimsrdo")
        if "n" in sel:
            stage_norm(k, c, xa, xb, L.norm_g[0:1, :], c.hT, xa_deps)
            k.barrier()
        if "i" in sel:
            stage_inproj(k, c, c.hT, L.w_in, NF, NV, c.pT, c.pV)
        if "m" in sel:
            stage_mla(k, c, L)
        if "s" in sel:
            stage_s5(k, c, L)
        if "r" in sel:
            stage_rwkv(k, c, L)
        if "d" in sel:
            stage_diff(k, c, L)
        if "o" in sel:
            stage_outproj(k, c, L, xa, xb, y, xa_deps, cc)
        k.barrier()


def build_layer_program():
    nc = bass.Bass("TRN2", target_bir_lowering=False)
    k = KB(nc)
    c = Ctx(k)
    EI = "ExternalInput"
    cmat = k.dram("cmat", [128, 768], F32, kind=EI)
    cmask = k.dram("cmask", [128, 2048], F32, kind=EI)
    cst = k.dram("cst", [128, NCST], F32, kind=EI)
    cm2 = k.dram("cm2", [128, 2560], F32, kind=EI)
    pos = k.dram("pos", [1, S], I32, kind=EI)
    xa = k.dram("xa", [S, D], F32, kind=EI)
    xb = k.dram("xb", [S, D], F32, kind=EI)
    y = k.dram("y", [S, D], F32, kind="ExternalOutput")
    L = declare_layer_inputs(k, "")
    declare_scratch(k, c)
    load_all_consts(k, c, cmat, cmask, cst, cm2)
    make_rope_tables(k, c, pos, c.cst, 0, 1, c.ropeA_cos, c.ropeA_sin, "rtA")
    make_rope_tables(k, c, pos, c.cst, 2, 3, c.ropeD_cos, c.ropeD_sin, "rtD")
    emit_layer(k, c, L, xa, xb, y)
    k.finish()
    return nc


def build_final_program():
    nc = bass.Bass("TRN2", target_bir_lowering=False)
    k = KB(nc)
    c = Ctx(k)
    xa = k.dram("xa", [S, D], F32, kind="ExternalInput")
    xb = k.dram("xb", [S, D], F32, kind="ExternalInput")
    g = k.dram("fg", [1, D], F32, kind="ExternalInput")
    out = k.dram("out", [S, D], F32, kind="ExternalOutput")
    stage_final(k, c, xa, xb, g[0:1, :], out)
    k.finish()
    return nc


def const_inputs():
    cmat, cmask, cst = const_mats()
    return {"cmat": cmat, "cmask": cmask, "cst": cst, "cm2": const_mats2()}


def kernel_unfused(**inp):
    inp = {k_: np.asarray(v) for k_, v in inp.items()}
    x = inp["x"]
    B = x.shape[0]
    consts = const_inputs()
    ncl = build_layer_program()
    cur_a = [np.ascontiguousarray(x[cid // 2]) for cid in range(8)]
    cur_b = [np.zeros((S, D), np.float32) for _ in range(8)]
    for l in range(4):
        in_maps = []
        for cid in range(8):
            b, half = divmod(cid, 2)
            m = dict(consts)
            m["pos"] = np.ascontiguousarray(inp["positions"][b:b + 1].astype(np.int32))
            m["xa"] = cur_a[cid]
            m["xb"] = cur_b[cid]
            m.update(layer_input_arrays(inp, l, half, ""))
            in_maps.append(m)
        res = run_bass_kernel_spmd(ncl, in_maps, core_ids=list(range(8)))
        ys = [np.asarray(r["y"]) for r in res.results]
        cur_a = [ys[cid] for cid in range(8)]
        cur_b = [ys[cid ^ 1] for cid in range(8)]
    ncf = build_final_program()
    fg = np.ascontiguousarray(inp["final_norm_g"][None, :])
    in_maps = [{"xa": cur_a[cid], "xb": cur_b[cid], "fg": fg} for cid in range(8)]
    res = run_bass_kernel_spmd(ncf, in_maps, core_ids=list(range(8)))
    out = np.stack([np.asarray(res.results[2 * b]["out"]) for b in range(B)], axis=0)
    return out.astype(np.float32)


from concourse.bass_utils import run_bass_kernel_spmd


PAIR_GROUPS = [[0, 1], [2, 3], [4, 5], [6, 7]]
DEPTH = 4


def build_fused_program(depth=DEPTH):
    nc = bass.Bass("TRN2", target_bir_lowering=False)
    k = KB(nc)
    c = Ctx(k)
    EI = "ExternalInput"
    cmat = k.dram("cmat", [128, 768], F32, kind=EI)
    cmask = k.dram("cmask", [128, 2048], F32, kind=EI)
    cst = k.dram("cst", [128, NCST], F32, kind=EI)
    cm2 = k.dram("cm2", [128, 2560], F32, kind=EI)
    pos = k.dram("pos", [1, S], I32, kind=EI)
    x_in = k.dram("xa", [S, D], F32, kind=EI)
    fg = k.dram("fg", [1, D], F32, kind=EI)
    out = k.dram("out", [S, D], F32, kind="ExternalOutput")
    Ls = [declare_layer_inputs(k, l) for l in range(depth)]
    declare_scratch(k, c)
    ybuf = k.dram("sc_y", [S, D], F32)
    xs = [k.dram(f"sc_xs{i}", [S, D], F32) for i in range(2)]
    xs_deps = [[Dep() for _ in range(NTC)] for _ in range(2)]
    load_all_consts(k, c, cmat, cmask, cst, cm2)
    make_rope_tables(k, c, pos, c.cst, 0, 1, c.ropeA_cos, c.ropeA_sin, "rtA")
    make_rope_tables(k, c, pos, c.cst, 2, 3, c.ropeD_cos, c.ropeD_sin, "rtD")
    cur, cur_deps = x_in, None
    for l in range(depth):
        o = l % 2
        emit_layer(k, c, Ls[l], cur, None, ybuf, cur_deps, (xs[o], xs_deps[o], PAIR_GROUPS))
        cur, cur_deps = xs[o], xs_deps[o]
    stage_final(k, c, cur, None, fg[0:1, :], out, cur_deps)
    k.finish()
    return nc


def kernel_fused(**inp):
    inp = {k_: np.asarray(v) for k_, v in inp.items()}
    x = inp["x"]
    B = x.shape[0]
    consts = const_inputs()
    nc = build_fused_program()
    fg = np.ascontiguousarray(inp["final_norm_g"][None, :])
    in_maps = []
    for cid in range(8):
        b, half = divmod(cid, 2)
        m = dict(consts)
        m["pos"] = np.ascontiguousarray(inp["positions"][b:b + 1].astype(np.int32))
        m["xa"] = np.ascontiguousarray(x[b])
        m["fg"] = fg
        for l in range(DEPTH):
            m.update(layer_input_arrays(inp, l, half, str(l)))
        in_maps.append(m)
    res = run_bass_kernel_spmd(nc, in_maps, core_ids=list(range(8)))
    out = np.stack([np.asarray(res.results[2 * b]["out"]) for b in range(B)], axis=0)
    return out.astype(np.float32)


def kernel(**inputs):
    return kernel_fused(**inputs)
```

```python
from contextlib import ExitStack
import math
import numpy as np
import concourse.bass as bass
import concourse.mybir as mybir

F32 = mybir.dt.float32
BF16 = mybir.dt.bfloat16
I32 = mybir.dt.int32
AF = mybir.ActivationFunctionType
ALU = mybir.AluOpType
AX = mybir.AxisListType

NDS = 44
NDS_HW = 24
DQ_POOLS = {"sp": (0, 16), "act": (16, 30), "pool": (30, 44)}
STQ = "act"


class Dep:
    __slots__ = ("w", "r", "excl")

    def __init__(self):
        self.excl = False
        self.w = {}
        self.r = {}


class T:
    def __init__(self, t, dep=None):
        self.t = t
        self.dep = dep or Dep()

    def __getitem__(self, k):
        return self.t[k]

    def ap(self):
        return self.t.ap() if hasattr(self.t, "ap") else self.t[:]


def _deps(x):
    return x.dep if isinstance(x, T) else x


class KB:
    def __init__(self, nc):
        self.nc = nc
        self.es = ExitStack()
        self.eng = {"pe": nc.tensor, "act": nc.scalar, "dve": nc.vector,
                    "pool": nc.gpsimd, "sp": nc.sync}
        self.sem = {}
        for e in ("pe", "act", "dve", "pool"):
            self.sem[e] = self.es.enter_context(nc.semaphore("s_" + e))
        self.cnt = {e: 0 for e in self.sem}
        self.dsem = [self.es.enter_context(nc.semaphore(f"sd{i}")) for i in range(NDS)]
        self.dcnt = [0] * NDS
        self.dq_next = {}
        self.seen = {e: {} for e in self.eng}
        self.ccsem = self.es.enter_context(nc.semaphore("s_cc"))
        self.cccnt = 0
        self.ninst = 0
        self.scopes = []

    def sb(self, name, shape, dtype, stack=None):
        self.uid = getattr(self, "uid", 0) + 1
        name = f"{name}_u{self.uid}"
        t = (stack or self.es).enter_context(self.nc.sbuf_tensor(name, list(shape), dtype))
        return T(t)

    def ps(self, name, shape, dtype, stack=None):
        t = (stack or self.es).enter_context(self.nc.psum_tensor(name, list(shape), dtype))
        r = T(t)
        r.dep.excl = True
        return r

    def dram(self, name, shape, dtype, kind="Internal"):
        t = self.nc.dram_tensor(name, list(shape), dtype, kind=kind)
        return T(t)

    def _wait(self, E, tok):
        if tok is None:
            return
        kind, key, val = tok
        if kind == "e" and key == E and E in ("pe", "sp"):
            return
        sk = (kind, key)
        if self.seen[E].get(sk, 0) >= val:
            return
        sem = self.sem[key] if kind == "e" else (self.dsem[key] if kind == "d" else self.ccsem)
        self.eng[E].wait_ge(sem, val)
        self.ninst += 1
        self.seen[E][sk] = val

    def _collect(self, E, reads, writes, waw=True):
        for d in reads:
            d = _deps(d)
            for (kd, ky), v in list(d.w.items()):
                self._wait(E, (kd, ky, v))
            if d.excl:
                for (kd, ky), v in list(d.r.items()):
                    if ky != E:
                        self._wait(E, (kd, ky, v))
        for d in writes:
            d = _deps(d)
            if waw:
                for (kd, ky), v in list(d.w.items()):
                    self._wait(E, (kd, ky, v))
            for (kd, ky), v in list(d.r.items()):
                self._wait(E, (kd, ky, v))

    def _update(self, tok, reads, writes, waw=True):
        kk = (tok[0], tok[1])
        for d in writes:
            d = _deps(d)
            if waw:
                d.w = {kk: tok[2]}
                d.r = {}
            else:
                d.w[kk] = max(d.w.get(kk, 0), tok[2])
        for d in reads:
            d = _deps(d)
            d.r[kk] = max(d.r.get(kk, 0), tok[2])

    def op(self, E, fn, reads=(), writes=(), inc=True):
        self._collect(E, reads, writes)
        inst = fn(self.eng[E])
        self.ninst += 1
        if inc:
            self.cnt[E] += 1
            inst.then_inc(self.sem[E], 1)
            tok = ("e", E, self.cnt[E])
        else:
            tok = ("e", E, self.cnt[E] + 1)
        self._update(tok, reads, writes)
        return inst

    def dma(self, Q, out, in_, reads=(), writes=(), waw=True, **kw):
        self._collect(Q, reads, writes, waw)
        lo, hi = DQ_POOLS[Q]
        i = lo + self.dq_next.get(Q, 0)
        self.dq_next[Q] = (self.dq_next.get(Q, 0) + 1) % (hi - lo)
        if self.dcnt[i] > 0:
            self._wait(Q, ("d", i, 16 * self.dcnt[i]))
        inst = self.eng[Q].dma_start(out=out, in_=in_, **kw)
        inst.then_inc(self.dsem[i], 16)
        self.ninst += 1
        self.dcnt[i] += 1
        tok = ("d", i, 16 * self.dcnt[i])
        self._update(tok, reads, writes, waw)
        return tok

    def collective(self, in_ap, out_ap, reads=(), writes=(), groups=None):
        self._collect("pool", reads, writes, True)
        inst = self.nc.gpsimd.collective_compute("AllReduce", ALU.add, replica_groups=groups,
                                                 ins=[in_ap], outs=[out_ap])
        self.cccnt += 1
        inst.then_inc(self.ccsem, 1)
        self.ninst += 1
        tok = ("c", 0, self.cccnt)
        self._update(tok, reads, writes, True)
        return tok

    def barrier(self, engines=("pe", "act", "dve", "pool", "sp")):
        for E in engines:
            if self.cccnt > 0:
                self._wait(E, ("c", 0, self.cccnt))
            for p in self.sem:
                if p != E and self.cnt[p] > 0:
                    self._wait(E, ("e", p, self.cnt[p]))
            for i in range(NDS):
                if self.dcnt[i] > 0:
                    self._wait(E, ("d", i, 16 * self.dcnt[i]))
        for E in ("act", "dve", "pool"):
            if E in engines and self.cnt[E] > 0:
                self._wait(E, ("e", E, self.cnt[E]))

    def finish(self):
        self.barrier(engines=("sp",))


S = 4096
D = 2048
TCH = 512
NTC = S // TCH
EPS = 1e-6


class Ctx:
    def __init__(self, k):
        self.k = k
        self.pf = [k.ps(f"pf{i}", [128, 512], F32) for i in range(6)]
        self.pb = [k.ps(f"pb{i}", [128, 1024], BF16) for i in range(2)]
        self.pfi = 0
        self.pbi = 0
        self.rr = 0
        self.us5 = None

    def next_pf(self, lo=0, hi=6):
        n = hi - lo
        p = self.pf[lo + (self.pfi % n)]
        self.pfi += 1
        return p

    def next_pb(self):
        p = self.pb[self.pbi % 2]
        self.pbi += 1
        return p

    def evac_eng(self):
        self.rr += 1
        return "act" if self.rr % 2 else "dve"


def copy_op(k, E, out, in_, reads, writes):
    if E == "act":
        k.op("act", lambda e: e.copy(out=out, in_=in_), reads=reads, writes=writes)
    else:
        k.op(E, lambda e: e.tensor_copy(out=out, in_=in_), reads=reads, writes=writes)


def load_consts(k, c, ident_d, stack):
    idf = k.sb("idf", [128, 128], F32, stack)
    c.ident = k.sb("c_ident", [128, 128], BF16)
    c.ones = k.sb("c_ones", [128, 128], BF16)
    k.dma("sp", idf[:], ident_d[:, :], writes=[idf])
    k.op("dve", lambda e: e.tensor_copy(out=c.ident[:], in_=idf[:]), reads=[idf], writes=[c.ident])
    k.op("dve", lambda e: e.memset(c.ones[:], 1.0), writes=[c.ones])


def stage_norm(k, c, xa, xb, g_bc_d, hT, xa_deps=None):
    with ExitStack() as st:
        gbc = k.sb("n_gbc", [128, D], F32, st)
        k.dma("sp", gbc[:], g_bc_d.partition_broadcast(128), writes=[gbc])
        xt = [k.sb(f"n_xt{i}", [128, D], F32, st) for i in range(2)]
        xt2 = [k.sb(f"n_xu{i}", [128, D], F32, st) for i in range(2)]
        junk = k.sb("n_junk", [128, D], BF16, st)
        xn = [k.sb(f"n_xn{i}", [128, D], BF16, st) for i in range(2)]
        ss = [k.sb(f"n_ss{i}", [128, 4], F32, st) for i in range(2)]
        hst = [k.sb(f"n_hst{i}", [128, 16, TCH], BF16, st) for i in range(2)]
        for tt in range(S // 128):
            b = tt % 2
            tcn, tl = divmod(tt, 4)
            hs = hst[tcn % 2]
            rows = slice(tt * 128, (tt + 1) * 128)
            k.dma("sp", xt[b][:], xa[rows, :], reads=[xa_deps[tt // 4] if xa_deps else xa], writes=[xt[b]])
            if xb is not None:
                k.dma("sp", xt2[b][:], xb[rows, :], reads=[xb], writes=[xt2[b]])
                k.op("pool", lambda e: e.tensor_tensor(out=xt[b][:], in0=xt[b][:], in1=xt2[b][:], op=ALU.add),
                     reads=[xt[b], xt2[b]], writes=[xt[b]])
            k.op("act", lambda e: e.activation(out=junk[:], in_=xt[b][:], func=AF.Square,
                                               accum_out=ss[b][:, 0:1]),
                 reads=[xt[b]], writes=[junk, ss[b]])
            k.op("dve", lambda e: e.tensor_scalar(out=ss[b][:, 1:2], in0=ss[b][:, 0:1], scalar1=1.0 / D,
                                                  scalar2=EPS, op0=ALU.mult, op1=ALU.add),
                 reads=[ss[b]], writes=[ss[b]])
            k.op("act", lambda e: e.activation(out=ss[b][:, 2:3], in_=ss[b][:, 1:2], func=AF.Sqrt),
                 reads=[ss[b]], writes=[ss[b]])
            k.op("dve", lambda e: e.reciprocal(out=ss[b][:, 3:4], in_=ss[b][:, 2:3]),
                 reads=[ss[b]], writes=[ss[b]])
            k.op("dve", lambda e: e.scalar_tensor_tensor(out=xn[b][:], in0=xt[b][:], scalar=ss[b][:, 3:4],
                                                         in1=gbc[:], op0=ALU.mult, op1=ALU.mult),
                 reads=[xt[b], ss[b], gbc], writes=[xn[b]])
            for half in range(2):
                pb = c.next_pb()
                for j in range(8):
                    kc = half * 8 + j
                    k.op("pe", lambda e: e.transpose(pb[:, j * 128:(j + 1) * 128],
                                                     xn[b][:, kc * 128:(kc + 1) * 128], c.ident[:]),
                         reads=[xn[b], c.ident], writes=[pb], inc=(j == 7))
                k.op("act", lambda e: e.copy(out=hs[:, half * 8:(half + 1) * 8, tl * 128:(tl + 1) * 128],
                                             in_=pb[:].rearrange("p (a b) -> p a b", a=8)),
                     reads=[pb], writes=[hs])
            if tl == 3:
                k.dma(STQ, hT[:, tcn * TCH:(tcn + 1) * TCH].rearrange("(kc p) t -> p kc t", p=128),
                      hs[:], reads=[hs], writes=[hT], waw=False)


def load_w_bf16(k, st, w_ap_fn, KC, N, name, nstg=3):
    wbf = k.sb(name, [128, KC, N], BF16, st)
    stg = [k.sb(f"{name}_s{i}", [128, N], F32, st) for i in range(nstg)]
    deps = [Dep() for _ in range(KC)]
    for kc in range(KC):
        s = stg[kc % nstg]
        k.dma("sp", s[:], w_ap_fn(kc), writes=[s])
        E = ("dve", "act", "dve", "pool")[kc % 4]
        copy_op(k, E, wbf[:, kc, :], s[:], [s], [deps[kc]])
    return wbf, deps


def linear_fm(k, c, st, inT, KC, wbf, wdeps, n_tiles, epilogue, name, in_cast=False):
    xin = [k.sb(f"{name}_x{i}", [128, KC, TCH], BF16, st) for i in range(2)]
    for tc in range(NTC):
        xi = xin[tc % 2]
        k.dma("sp", xi[:], inT[:, tc * TCH:(tc + 1) * TCH].rearrange("(kc p) t -> p kc t", p=128),
              reads=[inT], writes=[xi])
        for ni, (c0, ncol) in enumerate(n_tiles):
            ps = c.next_pf()
            for kc in range(KC):
                k.op("pe", lambda e: e.matmul(ps[:ncol, :], lhsT=wbf[:, kc, c0:c0 + ncol], rhs=xi[:, kc, :],
                                              start=(kc == 0), stop=(kc == KC - 1)),
                     reads=[wdeps[kc], xi], writes=[ps], inc=(kc == KC - 1))
            epilogue(tc, ni, ps, ncol)


def linear_tm(k, c, st, inT, KC, wbf, wdeps, c0, N, epilogue, name):
    xin = [k.sb(f"{name}_x{i}", [128, KC, TCH], BF16, st) for i in range(2)]
    for tc in range(NTC):
        xi = xin[tc % 2]
        k.dma("sp", xi[:], inT[:, tc * TCH:(tc + 1) * TCH].rearrange("(kc p) t -> p kc t", p=128),
              reads=[inT], writes=[xi])
        for sub in range(4):
            ps = c.next_pf()
            for kc in range(KC):
                k.op("pe", lambda e: e.matmul(ps[:, :N], lhsT=xi[:, kc, sub * 128:(sub + 1) * 128],
                                              rhs=wbf[:, kc, c0:c0 + N],
                                              start=(kc == 0), stop=(kc == KC - 1)),
                     reads=[wdeps[kc], xi], writes=[ps], inc=(kc == KC - 1))
            epilogue(tc * 4 + sub, ps)


class Stager:
    def __init__(self, k, st, name, shape, dtype, n=4):
        self.k = k
        self.bufs = [k.sb(f"{name}{i}", shape, dtype, st) for i in range(n)]
        self.i = 0

    def next(self):
        b = self.bufs[self.i % len(self.bufs)]
        self.i += 1
        return b


def epi_store_fm(k, c, stg, outT, row0_of):
    def epi(tc, ni, ps, ncol):
        s = stg.next()
        copy_op(k, c.evac_eng(), s[:ncol, :], ps[:ncol, :], [ps], [s])
        r0 = row0_of(ni)
        k.dma(STQ, outT[r0:r0 + ncol, tc * TCH:(tc + 1) * TCH], s[:ncol, :], reads=[s], writes=[outT], waw=False)
        return s
    return epi


def stage_inproj(k, c, hT, w_in_d, NF, NV, pT, pV):
    KC = D // 128
    tiles = []
    c0 = 0
    while c0 < NF:
        tiles.append((c0, min(128, NF - c0)))
        c0 += 128
    half = (len(tiles) + 1) // 2
    groups = [tiles[:half], tiles[half:]]
    for gi, grp in enumerate(groups):
        with ExitStack() as st:
            g0 = grp[0][0]
            gN = grp[-1][0] + grp[-1][1] - g0
            wbf, wd = load_w_bf16(k, st, lambda kc: w_in_d[kc * 128:(kc + 1) * 128, g0:g0 + gN], KC, gN, f"ip_w{gi}")
            stg = Stager(k, st, f"ip_o{gi}_", [128, TCH], F32, 4)
            rel = [(a - g0, b) for a, b in grp]
            base_epi = epi_store_fm(k, c, stg, pT, lambda ni: grp[ni][0])
            stu = Stager(k, st, f"ip_u{gi}_", [128, 8, 64], BF16, 3)

            def epi(tc, ni, ps, ncol, grp=grp, base_epi=base_epi, stu=stu):
                sst = base_epi(tc, ni, ps, ncol)
                r0 = grp[ni][0]
                import os as _os
                if R_U <= r0 < R_U + 512 and c.us5 is not None and _os.environ.get('NOHOOK') != '1':
                    su = stu.next()
                    k.op("pool", lambda e: e.tensor_copy(out=su[:],
                                                         in_=sst[:, :].rearrange("p (j t) -> p t j", t=8)),
                         reads=[sst], writes=[su])
                    g0 = (r0 - R_U) // 16
                    hq = _os.environ.get("HOOKDMA", STQ)
                    for gl in range(8 if hq != "none" else 0):
                        k.dma(hq, c.us5[g0 + gl, :, tc * 64:(tc + 1) * 64].rearrange("(t c) j -> c t j", c=16),
                              su[gl * 16:(gl + 1) * 16, :, :], reads=[su], writes=[c.us5], waw=False)
            linear_fm(k, c, st, hT, KC, wbf, wd, rel, epi, f"ip{gi}")
        k.barrier()
    with ExitStack() as st:
        wbf, wd = load_w_bf16(k, st, lambda kc: w_in_d[kc * 128:(kc + 1) * 128, NF:NF + NV], KC, NV, "ip_wv")
        stg = Stager(k, st, "ip_ov_", [128, NV], F32, 4)

        def epi(tt, ps):
            s = stg.next()
            copy_op(k, c.evac_eng(), s[:, :], ps[:, :NV], [ps], [s])
            k.dma(STQ, pV[tt * 128:(tt + 1) * 128, :], s[:, :], reads=[s], writes=[pV], waw=False)
        linear_tm(k, c, st, hT, KC, wbf, wd, 0, NV, epi, "ipv")
    k.barrier()


TWO_PI = 2.0 * np.pi
CW1 = 6.28125
CW2 = float(np.float32(TWO_PI - 6.28125))
CW3 = float(TWO_PI - 6.28125 - float(np.float32(TWO_PI - 6.28125)))


def make_rope_tables(k, c, pos_d, cst, col_inv, col_sgn, cosT, sinT, name):
    HS = S // 2
    with ExitStack() as st:
        posi = k.sb(name + "_pi", [128, HS], I32, st)
        ang = k.sb(name + "_ang", [128, HS], F32, st)
        a2 = k.sb(name + "_a2", [128, HS], F32, st)
        ni = k.sb(name + "_ni", [128, HS], I32, st)
        nf = k.sb(name + "_nf", [128, HS], F32, st)
        r = k.sb(name + "_r", [128, HS], F32, st)
        m = k.sb(name + "_m", [128, HS], F32, st)
        o = k.sb(name + "_o", [128, HS], F32, st)
        for hh in range(2):
            sl = slice(hh * HS, (hh + 1) * HS)
            k.dma("sp", posi[:], pos_d[0:1, sl].partition_broadcast(128), writes=[posi])
            k.op("dve", lambda e: e.tensor_copy(out=ang[:], in_=posi[:]), reads=[posi], writes=[ang])
            k.op("dve", lambda e: e.tensor_scalar(out=ang[:], in0=ang[:], scalar1=cst[:, col_inv:col_inv + 1],
                                                  scalar2=None, op0=ALU.mult), reads=[ang, cst], writes=[ang])
            for which, shift, dst in (("s", 0.0, sinT), ("c", np.pi / 2, cosT)):
                if which == "s":
                    k.op("dve", lambda e: e.tensor_scalar(out=ni[:], in0=ang[:], scalar1=float(1.0 / TWO_PI),
                                                          scalar2=None, op0=ALU.mult), reads=[ang], writes=[ni])
                    k.op("dve", lambda e: e.tensor_copy(out=nf[:], in_=ni[:]), reads=[ni], writes=[nf])
                    k.op("dve", lambda e: e.scalar_tensor_tensor(out=a2[:], in0=nf[:], scalar=-CW1, in1=ang[:],
                                                                 op0=ALU.mult, op1=ALU.add), reads=[nf, ang], writes=[a2])
                    k.op("dve", lambda e: e.scalar_tensor_tensor(out=a2[:], in0=nf[:], scalar=-CW2, in1=a2[:],
                                                                 op0=ALU.mult, op1=ALU.add), reads=[nf, a2], writes=[a2])
                    k.op("dve", lambda e: e.scalar_tensor_tensor(out=a2[:], in0=nf[:], scalar=-CW3, in1=a2[:],
                                                                 op0=ALU.mult, op1=ALU.add), reads=[nf, a2], writes=[a2])
                k.op("dve", lambda e: e.tensor_scalar(out=r[:], in0=a2[:], scalar1=float(shift), scalar2=None,
                                                      op0=ALU.add), reads=[a2], writes=[r])
                k.op("dve", lambda e: e.tensor_single_scalar(out=m[:], in_=r[:], scalar=float(np.pi), op=ALU.is_gt),
                     reads=[r], writes=[m])
                k.op("dve", lambda e: e.scalar_tensor_tensor(out=r[:], in0=m[:], scalar=-TWO_PI, in1=r[:],
                                                             op0=ALU.mult, op1=ALU.add), reads=[m, r], writes=[r])
                k.op("dve", lambda e: e.tensor_single_scalar(out=m[:], in_=r[:], scalar=float(-np.pi), op=ALU.is_lt),
                     reads=[r], writes=[m])
                k.op("dve", lambda e: e.scalar_tensor_tensor(out=r[:], in0=m[:], scalar=TWO_PI, in1=r[:],
                                                             op0=ALU.mult, op1=ALU.add), reads=[m, r], writes=[r])
                k.op("dve", lambda e: e.tensor_scalar(out=r[:], in0=r[:], scalar1=float(np.pi), scalar2=float(-np.pi),
                                                      op0=ALU.min, op1=ALU.max), reads=[r], writes=[r])
                k.op("act", lambda e: e.activation(out=o[:], in_=r[:], func=AF.Sin), reads=[r], writes=[o])
                if which == "s":
                    k.op("dve", lambda e: e.tensor_scalar(out=o[:], in0=o[:], scalar1=cst[:, col_sgn:col_sgn + 1],
                                                          scalar2=None, op0=ALU.mult), reads=[o, cst], writes=[o])
                k.dma("sp", dst[:, sl], o[:], reads=[o], writes=[dst], waw=False)
    k.barrier()


def rmsnorm_fm(k, c, srcT, r0, nt, g_sb, gcol0, dstT, eps, name):
    n = nt * 128
    with ExitStack() as st:
        xin = [k.sb(f"{name}_x{i}", [128, nt, TCH], F32, st) for i in range(2)]
        sq = [k.sb(f"{name}_q{i}", [128, nt, TCH], BF16, st) for i in range(2)]
        rs = [k.sb(f"{name}_r{i}", [128, TCH], F32, st) for i in range(2)]
        ob = [k.sb(f"{name}_o{i}", [128, nt, TCH], BF16, st) for i in range(2)]
        for tc in range(NTC):
            b = tc % 2
            tsl = slice(tc * TCH, (tc + 1) * TCH)
            k.dma("sp", xin[b][:], srcT[r0:r0 + n, tsl].rearrange("(t p) s -> p t s", p=128),
                  reads=[srcT], writes=[xin[b]])
            k.op("act", lambda e: e.activation(out=sq[b][:], in_=xin[b][:], func=AF.Square),
                 reads=[xin[b]], writes=[sq[b]])
            ps = c.next_pf()
            for t in range(nt):
                k.op("pe", lambda e: e.matmul(ps[:, :], lhsT=c.ones[:], rhs=sq[b][:, t, :],
                                              start=(t == 0), stop=(t == nt - 1)),
                     reads=[c.ones, sq[b]], writes=[ps], inc=(t == nt - 1))
            k.op("dve", lambda e: e.tensor_scalar(out=rs[b][:], in0=ps[:, :], scalar1=1.0 / n, scalar2=float(eps),
                                                  op0=ALU.mult, op1=ALU.add), reads=[ps], writes=[rs[b]])
            k.op("act", lambda e: e.activation(out=rs[b][:], in_=rs[b][:], func=AF.Ln),
                 reads=[rs[b]], writes=[rs[b]])
            k.op("act", lambda e: e.activation(out=rs[b][:], in_=rs[b][:], func=AF.Exp, scale=-0.5),
                 reads=[rs[b]], writes=[rs[b]])
            for t in range(nt):
                k.op("dve", lambda e: e.scalar_tensor_tensor(out=ob[b][:, t, :], in0=xin[b][:, t, :],
                                                             scalar=g_sb[:, gcol0 + t:gcol0 + t + 1], in1=rs[b][:],
                                                             op0=ALU.mult, op1=ALU.mult),
                     reads=[xin[b], g_sb, rs[b]], writes=[ob[b]])
            k.dma(STQ, dstT[0:n, tsl].rearrange("(t p) s -> p t s", p=128), ob[b][:],
                  reads=[ob[b]], writes=[dstT], waw=False)
    k.barrier()


def rope_tile(k, c, st_bufs, src_ap, src_dep, perm, cos_sb, sin_sb, out_ap, out_dep):
    xb, xf, t1 = st_bufs["xb"], st_bufs["xf"], st_bufs["t1"]
    k.op("act", lambda e: e.copy(out=xf[:], in_=src_ap), reads=[src_dep], writes=[xf])
    k.op("dve", lambda e: e.tensor_copy(out=xb[:], in_=xf[:]), reads=[xf], writes=[xb])
    ps = c.next_pf()
    k.op("pe", lambda e: e.matmul(ps[:, :], lhsT=perm[:], rhs=xb[:], start=True, stop=True),
         reads=[perm, xb], writes=[ps])
    k.op("dve", lambda e: e.tensor_tensor(out=t1[:], in0=ps[:, :], in1=sin_sb, op=ALU.mult),
         reads=[ps, st_bufs["tab"]], writes=[t1])
    k.op("pool", lambda e: e.tensor_tensor(out=xf[:], in0=xf[:], in1=cos_sb, op=ALU.mult),
         reads=[xf, st_bufs["tab"]], writes=[xf])
    k.op("dve", lambda e: e.tensor_tensor(out=out_ap, in0=xf[:], in1=t1[:], op=ALU.add),
         reads=[xf, t1], writes=[out_dep])


def attention_head(k, c, st, name, maps, Vsb, vdep, dv, scale, masks, post):
    raise NotImplementedError


def attn_qchunk(k, c, j, maps, Vfn, vdep, dv, scale, masks, ptbufs, acc_banks):
    nkt = 4 * j + 4
    items = [(mi, kt) for mi in range(len(maps)) for kt in range(nkt)]
    tri = masks[0]

    def c0_of(kt):
        return 128 * (kt - 4 * j) if kt >= 4 * j else 0

    def emit_qk(i):
        mi, kt = items[i]
        parts = maps[mi]
        ps = c.pf[i % 2]
        c0 = c0_of(kt)
        for pi, p in enumerate(parts):
            k.op("pe", lambda e: e.matmul(ps[:, c0:TCH], lhsT=p["K"](kt), rhs=p["Q"](c0),
                                          start=(pi == 0), stop=(pi == len(parts) - 1)),
                 reads=[p["kd"], p["qd"]], writes=[ps], inc=(pi == len(parts) - 1))

    emit_qk(0)
    for i, (mi, kt) in enumerate(items):
        if i + 1 < len(items):
            emit_qk(i + 1)
        oacc, sacc = acc_banks[mi]
        ps = c.pf[i % 2]
        c0 = c0_of(kt)
        pt = ptbufs[i % len(ptbufs)]
        k.op("act", lambda e: e.activation(out=pt[:, c0:TCH], in_=ps[:, c0:TCH], func=AF.Exp, scale=float(scale)),
             reads=[ps], writes=[pt])
        if kt >= 4 * j:
            k.op("pool", lambda e: e.tensor_tensor(out=pt[:, c0:c0 + 128], in0=pt[:, c0:c0 + 128], in1=tri[:, 0:128],
                                                   op=ALU.mult), reads=[pt, tri], writes=[pt])
        k.op("pe", lambda e: e.matmul(oacc[:dv, c0:TCH], lhsT=Vfn(kt), rhs=pt[:, c0:TCH],
                                      start=(kt == 0), stop=(kt == nkt - 1)),
             reads=[vdep, pt], writes=[oacc], inc=False)
        k.op("pe", lambda e: e.matmul(sacc[:, c0:TCH], lhsT=c.ones[:], rhs=pt[:, c0:TCH],
                                      start=(kt == 0), stop=(kt == nkt - 1)),
             reads=[c.ones, pt], writes=[sacc], inc=True)


R_CQ, R_CKV, R_U, R_R, R_K, R_QD, R_KD, R_GATE, R_KROPE, R_LORA = 0, 512, 768, 1280, 1536, 1792, 2048, 2304, 3328, 3456
NF = 3840
NV = 256


class RopeCtx:
    def __init__(self, k, c, st, name, cosT, sinT, perm):
        self.k, self.c = k, c
        self.cosT, self.sinT, self.perm = cosT, sinT, perm
        self.tab = [k.sb(f"{name}_tab{i}", [128, 2, TCH], F32, st) for i in range(2)]
        self.xb = [k.sb(f"{name}_xb{i}", [128, TCH], BF16, st) for i in range(2)]
        self.xf = [k.sb(f"{name}_xf{i}", [128, TCH], F32, st) for i in range(2)]
        self.t1 = [k.sb(f"{name}_t1{i}", [128, TCH], F32, st) for i in range(2)]
        self.cur = None
        self.n = 0

    def load(self, tc):
        k = self.k
        tb = self.tab[tc % 2]
        tsl = slice(tc * TCH, (tc + 1) * TCH)
        k.dma("sp", tb[:, 0, :], self.cosT[:, tsl], reads=[self.cosT], writes=[tb])
        k.dma("sp", tb[:, 1, :], self.sinT[:, tsl], reads=[self.cosT], writes=[tb], waw=False)
        self.cur = tb

    def apply(self, src_ap, src_dep, out_ap, out_dep):
        k, c = self.k, self.c
        i = self.n % 2
        self.n += 1
        xb, xf, t1, tb = self.xb[i], self.xf[i], self.t1[i], self.cur
        k.op("act", lambda e: e.copy(out=xf[:], in_=src_ap), reads=[src_dep], writes=[xf])
        k.op("dve", lambda e: e.tensor_copy(out=xb[:], in_=xf[:]), reads=[xf], writes=[xb])
        ps = c.next_pf(0, 2)
        k.op("pe", lambda e: e.matmul(ps[:, :], lhsT=self.perm[:], rhs=xb[:], start=True, stop=True),
             reads=[self.perm, xb], writes=[ps])
        k.op("dve", lambda e: e.tensor_tensor(out=t1[:], in0=ps[:, :], in1=tb[:, 1, :], op=ALU.mult),
             reads=[ps, tb], writes=[t1])
        k.op("pool", lambda e: e.tensor_tensor(out=xf[:], in0=xf[:], in1=tb[:, 0, :], op=ALU.mult),
             reads=[xf, tb], writes=[xf])
        k.op("dve", lambda e: e.tensor_tensor(out=out_ap, in0=xf[:], in1=t1[:], op=ALU.add),
             reads=[xf, t1], writes=[out_dep])


def stage_mla(k, c, L):
    pT = c.pT
    rmsnorm_fm(k, c, pT, R_CQ, 4, L.prm, L.col["gq"], c.cqnT, EPS, "nq")
    rmsnorm_fm(k, c, pT, R_CKV, 2, L.prm, L.col["gkv"], c.ckvnT, EPS, "nkv")
    with ExitStack() as st:
        wbf, wd = load_w_bf16(k, st, lambda kc: L.wuq[kc * 128:(kc + 1) * 128, :], 4, 384, "wuq")
        stg = Stager(k, st, "mq_o", [128, TCH], BF16, 4)
        rp = RopeCtx(k, c, st, "mqr", c.ropeA_cos, c.ropeA_sin, c.permA)

        def epi(tc, ni, ps, ncol):
            s = stg.next()
            if ni < 2:
                copy_op(k, c.evac_eng(), s[:, :], ps[:, :], [ps], [s])
            else:
                rp.load(tc)
                rp.apply(ps[:, :], ps, s[:, :], s)
            k.dma(STQ, c.qT[ni * 128:(ni + 1) * 128, tc * TCH:(tc + 1) * TCH], s[:, :], reads=[s],
                  writes=[c.qT], waw=False)
        linear_fm(k, c, st, c.cqnT, 4, wbf, wd, [(0, 128), (128, 128), (256, 128)], epi, "mq")
    k.barrier()
    with ExitStack() as st:
        wbf, wd = load_w_bf16(k, st, lambda kc: L.wukv[kc * 128:(kc + 1) * 128, :], 2, 512, "wukv")
        stg = Stager(k, st, "mk_o", [128, TCH], BF16, 4)
        epi = epi_store_fm(k, c, stg, c.knT, lambda ni: ni * 128)
        linear_fm(k, c, st, c.ckvnT, 2, wbf, wd, [(0, 128), (128, 128)], epi, "mk")
        stgv = Stager(k, st, "mv_o", [128, 256], BF16, 4)

        def epiv(tt, ps):
            s = stgv.next()
            copy_op(k, c.evac_eng(), s[:, :], ps[:, :256], [ps], [s])
            k.dma(STQ, c.vA[tt * 128:(tt + 1) * 128, :], s[:, :], reads=[s], writes=[c.vA], waw=False)
        linear_tm(k, c, st, c.ckvnT, 2, wbf, wd, 256, 256, epiv, "mv")
        rp = RopeCtx(k, c, st, "mkr", c.ropeA_cos, c.ropeA_sin, c.permA)
        kin = [k.sb(f"mkr_in{i}", [128, TCH], F32, st) for i in range(2)]
        for tc in range(NTC):
            tsl = slice(tc * TCH, (tc + 1) * TCH)
            ki = kin[tc % 2]
            k.dma("sp", ki[:], pT[R_KROPE:R_KROPE + 128, tsl], reads=[pT], writes=[ki])
            rp.load(tc)
            s = stg.next()
            rp.apply(ki[:], ki, s[:, :], s)
            k.dma(STQ, c.krT[:, tsl], s[:, :], reads=[s], writes=[c.krT], waw=False)
    k.barrier()
    scale = (128 + 64) ** -0.5
    for h in range(2):
        with ExitStack() as st:
            Kn = k.sb("ma_kn", [128, S], BF16, st)
            Kr = k.sb("ma_kr", [128, S], BF16, st)
            Vs = k.sb("ma_v", [128, S // 128, 128], BF16, st)
            k.dma("sp", Kn[:], c.knT[h * 128:(h + 1) * 128, :], reads=[c.knT], writes=[Kn])
            k.dma("sp", Kr[:], c.krT[:, :], reads=[c.krT], writes=[Kr])
            k.dma("sp", Vs[:], c.vA[:, h * 128:(h + 1) * 128].rearrange("(kt p) d -> p kt d", p=128),
                  reads=[c.vA], writes=[Vs])
            Qn = [k.sb(f"ma_qn{i}", [128, TCH], BF16, st) for i in range(2)]
            Qr = [k.sb(f"ma_qr{i}", [128, TCH], BF16, st) for i in range(2)]
            ptb = [k.sb(f"ma_pt{i}", [128, TCH], BF16, st) for i in range(3)]
            rec = [k.sb(f"ma_rec{i}", [128, TCH], F32, st) for i in range(2)]
            ost = [k.sb(f"ma_o{i}", [128, TCH], F32, st) for i in range(2)]
            hs = slice(h * 64, (h + 1) * 64)
            for j in range(NTC):
                b = j % 2
                tsl = slice(j * TCH, (j + 1) * TCH)
                k.dma("sp", Qn[b][:], c.qT[h * 128:(h + 1) * 128, tsl], reads=[c.qT], writes=[Qn[b]])
                k.dma("sp", Qr[b][:], c.qT[256:384, tsl], reads=[c.qT], writes=[Qr[b]])
                parts = [dict(K=lambda kt: Kn[:, kt * 128:(kt + 1) * 128], Q=lambda c0: Qn[b][:, c0:TCH], kd=Kn, qd=Qn[b]),
                         dict(K=lambda kt: Kr[hs, kt * 128:(kt + 1) * 128], Q=lambda c0: Qr[b][hs, c0:TCH], kd=Kr, qd=Qr[b])]
                oacc, sacc = c.pf[2 + 2 * b], c.pf[3 + 2 * b]
                attn_qchunk(k, c, j, [parts], lambda kt: Vs[:, kt, :], Vs, 128, scale, c.masks, ptb, [(oacc, sacc)])
                k.op("act", lambda e: e.activation(out=rec[b][:], in_=sacc[:, :], func=AF.Ln), reads=[sacc], writes=[rec[b]])
                k.op("act", lambda e: e.activation(out=rec[b][:], in_=rec[b][:], func=AF.Exp, scale=-1.0), reads=[rec[b]], writes=[rec[b]])
                k.op("dve", lambda e: e.tensor_tensor(out=ost[b][:], in0=oacc[:, :], in1=rec[b][:], op=ALU.mult),
                     reads=[oacc, rec[b]], writes=[ost[b]])
                k.dma(STQ, c.mixT[h * 128:(h + 1) * 128, tsl], ost[b][:], reads=[ost[b]], writes=[c.mixT],
                      waw=False)
        k.barrier()


PRM_COLS = {}
_pc = 0


def _reg(name, n):
    global _pc
    PRM_COLS[name] = _pc
    _pc += n


_reg("gq", 4)
_reg("gkv", 2)


def const_mats():
    ident = np.eye(128, dtype=np.float32)
    permA = np.zeros((128, 128), np.float32)
    for r in range(128):
        d = r % 64
        permA[r, r + 32 if d < 32 else r - 32] = 1.0
    permD = np.zeros((128, 128), np.float32)
    for r in range(128):
        d = r % 64
        if d < 8:
            permD[r, r + 8] = 1.0
        elif d < 16:
            permD[r, r - 8] = 1.0
    masks = np.zeros((128, 4, 512), np.float32)
    kk = np.arange(128)[:, None]
    qq = np.arange(512)[None, :]
    for r in range(4):
        masks[:, r, :] = (qq >= 128 * r + kk)
    tmask = np.zeros((128, 128), np.float32)
    ti = np.arange(128) // 16
    tmask[:, :] = (ti[None, :] >= ti[:, None])
    hidx = np.arange(128) // 64
    same = (hidx[:, None] == hidx[None, :]).astype(np.float32)
    blk1 = same.copy()
    blk64 = same / 64.0
    cmat = np.concatenate([ident, permA, permD, tmask, blk1, blk64], axis=1)
    cst = np.zeros((128, 8 + 32 + 512), np.float32)
    invA = (500000.0 ** (-np.arange(0, 64, 2, dtype=np.float32) / np.float32(64))).astype(np.float32)
    invD = (500000.0 ** (-np.arange(0, 16, 2, dtype=np.float32) / np.float32(16))).astype(np.float32)
    for r in range(128):
        d = r % 64
        cst[r, 0] = invA[d % 32]
        cst[r, 1] = -1.0 if d < 32 else 1.0
        cst[r, 2] = invD[d % 8] if d < 16 else 0.0
        cst[r, 3] = (-1.0 if d < 8 else 1.0) if d < 16 else 0.0
    tau = np.zeros(32, np.float32)
    tau[0:8] = -np.arange(8)
    tau[8:17] = np.arange(9)
    tau[17:25] = 7 - np.arange(8)
    cst[:, 8:40] = tau[None, :]
    cst[:, 40:552] = (8.0 * (np.arange(512) + 1))[None, :]
    return cmat, masks.reshape(128, 2048), cst


def const_mats2():
    hidx = np.arange(128) // 64
    idx = np.arange(128) % 64
    same = (hidx[:, None] == hidx[None, :])
    mSL = (same & (idx[:, None] > idx[None, :])).astype(np.float32)
    mSU = (same & (idx[:, None] < idx[None, :])).astype(np.float32)
    mUI = (same & (idx[:, None] <= idx[None, :])).astype(np.float32)
    ident = np.eye(128, dtype=np.float32)
    return np.concatenate([np.tile(mSL, (1, 4)), np.tile(mSU, (1, 4)), np.tile(mUI, (1, 4)), np.tile(ident, (1, 8))], axis=1)


NCST = 552


def load_all_consts(k, c, cmat_d, cmask_d, cst_d, cm2_d=None):
    c.ident = k.sb("c_ident", [128, 128], BF16)
    c.permA = k.sb("c_permA", [128, 128], BF16)
    c.permD = k.sb("c_permD", [128, 128], BF16)
    c.ones = k.sb("c_ones", [128, 128], BF16)
    c.cst = k.sb("c_cst", [128, NCST], F32)
    c.tmask = k.sb("c_tmask", [128, 128], F32)
    c.blk1 = k.sb("c_blk1", [128, 128], BF16)
    c.blk64 = k.sb("c_blk64", [128, 128], BF16)
    c.mSL = k.sb("c_mSL", [128, 512], BF16)
    c.mSU = k.sb("c_mSU", [128, 512], BF16)
    c.mUI = k.sb("c_mUI", [128, 512], BF16)
    c.ident8 = k.sb("c_ident8", [128, 1024], BF16)
    c.masks = [k.sb(f"c_mask{r}", [128, 512], BF16) for r in range(4)]
    with ExitStack() as st:
        f = k.sb("lc_f", [128, 768], F32, st)
        m2 = k.sb("lc_m2", [128, 2560], F32, st)
        m = k.sb("lc_m", [128, 2048], F32, st)
        k.dma("sp", f[:], cmat_d[:, :], writes=[f])
        k.dma("sp", m[:], cmask_d[:, :], writes=[m])
        k.dma("sp", c.cst[:], cst_d[:, :], writes=[c.cst])
        k.op("dve", lambda e: e.tensor_copy(out=c.ident[:], in_=f[:, 0:128]), reads=[f], writes=[c.ident])
        k.op("dve", lambda e: e.tensor_copy(out=c.permA[:], in_=f[:, 128:256]), reads=[f], writes=[c.permA])
        k.op("dve", lambda e: e.tensor_copy(out=c.permD[:], in_=f[:, 256:384]), reads=[f], writes=[c.permD])
        k.op("dve", lambda e: e.tensor_copy(out=c.tmask[:], in_=f[:, 384:512]), reads=[f], writes=[c.tmask])
        k.op("dve", lambda e: e.tensor_copy(out=c.blk1[:], in_=f[:, 512:640]), reads=[f], writes=[c.blk1])
        k.op("dve", lambda e: e.tensor_copy(out=c.blk64[:], in_=f[:, 640:768]), reads=[f], writes=[c.blk64])
        if cm2_d is not None:
            k.dma("sp", m2[:], cm2_d[:, :], writes=[m2])
            k.op("dve", lambda e: e.tensor_copy(out=c.mSL[:], in_=m2[:, 0:512]), reads=[m2], writes=[c.mSL])
            k.op("dve", lambda e: e.tensor_copy(out=c.mSU[:], in_=m2[:, 512:1024]), reads=[m2], writes=[c.mSU])
            k.op("dve", lambda e: e.tensor_copy(out=c.mUI[:], in_=m2[:, 1024:1536]), reads=[m2], writes=[c.mUI])
            k.op("dve", lambda e: e.tensor_copy(out=c.ident8[:], in_=m2[:, 1536:2560]), reads=[m2], writes=[c.ident8])
        k.op("dve", lambda e: e.memset(c.ones[:], 1.0), writes=[c.ones])
        for r in range(4):
            k.op("dve", lambda e: e.tensor_copy(out=c.masks[r][:], in_=m[:, r * 512:(r + 1) * 512]),
                 reads=[m], writes=[c.masks[r]])
        k.barrier()


O_CQ, O_CKV, O_KR, O_U, O_Z, O_QD, O_KD, O_VD, O_GATE = 0, 512, 768, 832, 1344, 3008, 3520, 4032, 4544


def s5_gperm(half):
    return np.concatenate([half * 16 + np.arange(16), (1 - half) * 16 + np.arange(16)])


def s5_chperm(half):
    return (s5_gperm(half)[:, None] * 16 + np.arange(16)[None, :]).reshape(-1)


def fm_cols(half):
    a = np.arange
    cols = [O_CQ + a(512), O_CKV + a(256), O_U + s5_chperm(half),
            O_Z + half * 256 + a(256), O_Z + 512 + half * 256 + a(256),
            O_QD + half * 256 + a(256), O_KD + half * 256 + a(256)]
    for b in range(4):
        cols.append(O_GATE + b * 512 + half * 256 + a(256))
    cols += [O_KR + a(64), O_KR + a(64), O_Z + 1536 + a(64), O_Z + 1600 + a(64), O_Z + 1024 + half * 256 + a(256)]
    return np.concatenate(cols)


def tm_cols(half):
    a = np.arange
    return O_VD + half * 256 + a(256)


def pt_layout(v, nt):
    return np.ascontiguousarray(np.asarray(v).reshape(nt, 128).T)


def core_layer_arrays(inp, l, half):
    out = {}
    w_in = inp["w_in"][l]
    out["w_in"] = np.ascontiguousarray(w_in[:, np.concatenate([fm_cols(half), tm_cols(half)])])
    hs = [2 * half, 2 * half + 1]
    wuq = inp["mla_w_uq"][l]
    out["wuq"] = np.ascontiguousarray(np.concatenate(
        [wuq[:, h * 192:h * 192 + 128] for h in hs] + [wuq[:, h * 192 + 128:(h + 1) * 192] for h in hs], axis=1))
    wukv = inp["mla_w_ukv"][l]
    out["wukv"] = np.ascontiguousarray(np.concatenate(
        [wukv[:, h * 256:h * 256 + 128] for h in hs] + [wukv[:, h * 256 + 128:(h + 1) * 256] for h in hs], axis=1))
    prm = np.zeros((128, _pc), np.float32)

    def put(name, arr):
        arr = np.asarray(arr, np.float32)
        if arr.ndim == 1:
            arr = arr[:, None]
        prm[:arr.shape[0], PRM_COLS[name]:PRM_COLS[name] + arr.shape[1]] = arr
    put("gq", pt_layout(inp["mla_q_norm_g"][l], 4))
    put("gkv", pt_layout(inp["mla_kv_norm_g"][l], 2))
    for nm in ("lq1", "lk1", "lq2", "lk2"):
        put(nm, np.broadcast_to(inp["diff_" + nm][l][None, :], (128, 64)))
    put("gsub", inp["diff_subln_g"][l])
    lam_init = 0.8 - 0.6 * math.exp(-0.3 * l)
    put("lam_init", np.full((128,), lam_init, np.float32))
    put("omlam", np.full((128,), 1.0 - lam_init, np.float32))
    fill_more(inp, l, half, put, out)
    out["prm"] = prm
    return out


def st_layout(a):
    a = np.asarray(a)
    rest = a.shape[2:]
    a = a.reshape((16, 128) + rest)
    return np.ascontiguousarray(np.moveaxis(a, 0, 1))


def fill_more(inp, l, half, put, out=None):
    gp = s5_gperm(half)
    chp = s5_chperm(half)
    put("s5_are", st_layout(inp["s5_a_re"][l][gp]))
    put("s5_aim", st_layout(inp["s5_a_im"][l][gp]))
    put("s5_ldt", st_layout(np.broadcast_to(inp["s5_log_dt"][l][gp][:, None], (32, 64))))
    put("s5_d", pt_layout(inp["s5_d"][l][chp], 4))
    put("s5_bg", pt_layout(inp["s5_b_glu"][l][half * 256:(half + 1) * 256], 2))
    mu = inp["rwkv_mu"][l]
    my = slice(half * 256, (half + 1) * 256)
    put("rw_mur", pt_layout(mu[0:512][my], 2))
    put("rw_muk", pt_layout(mu[512:1024][my], 2))
    put("rw_muv", pt_layout(mu[1024:1536][my], 2))
    put("rw_mul", mu[1536:1664])
    put("rw_w0", pt_layout(inp["rwkv_w0"][l][my], 2))
    put("rw_a0", pt_layout(inp["rwkv_a0"][l][my], 2))
    put("rw_kk", pt_layout(inp["rwkv_k_k"][l][my], 2))
    put("rw_ka", pt_layout(inp["rwkv_k_a"][l][my], 2))
    put("rw_rk", pt_layout(inp["rwkv_r_k"][l].reshape(512)[my], 2))
    put("rw_lng", pt_layout(inp["rwkv_ln_g"][l][my], 2))
    put("rw_lnb", pt_layout(inp["rwkv_ln_b"][l][my], 2))
    if out is not None:
        out["w2a2"] = np.ascontiguousarray(np.concatenate([inp["rwkv_w2"][l][:, my], inp["rwkv_a2"][l][:, my]], axis=0))
        b = np.stack([inp["s5_b_re"][l][gp], inp["s5_b_im"][l][gp]], axis=2)
        out["s5b"] = st_layout(b).reshape(128, 512).astype(np.float32)
        cc = np.stack([np.swapaxes(inp["s5_c_re"][l][gp], 1, 2), np.swapaxes(inp["s5_c_im"][l][gp], 1, 2)], axis=2)
        out["s5c"] = st_layout(cc).reshape(128, 512).astype(np.float32)
        out["wglu"] = np.ascontiguousarray(inp["s5_w_glu"][l][chp][:, half * 256:(half + 1) * 256])


_reg("lq1", 64)
_reg("lk1", 64)
_reg("lq2", 64)
_reg("lk2", 64)
_reg("gsub", 1)
_reg("lam_init", 1)
_reg("omlam", 1)
DIFF_EPS = 1e-5


def stage_diff(k, c, L):
    pT, pV = c.pT, c.pV
    with ExitStack() as st:
        rp = RopeCtx(k, c, st, "dr", c.ropeD_cos, c.ropeD_sin, c.permD)
        xin = [k.sb(f"dr_in{i}", [128, TCH], F32, st) for i in range(3)]
        stg = Stager(k, st, "dr_o", [128, TCH], BF16, 4)
        n = 0
        for tc in range(NTC):
            tsl = slice(tc * TCH, (tc + 1) * TCH)
            rp.load(tc)
            for (r0, dst) in ((R_QD, c.qdT), (R_KD, c.kdT)):
                for hd in range(2):
                    xi = xin[n % 3]
                    n += 1
                    k.dma("sp", xi[:], pT[r0 + hd * 128:r0 + (hd + 1) * 128, tsl], reads=[pT], writes=[xi])
                    s = stg.next()
                    rp.apply(xi[:], xi, s[:, :], s)
                    k.dma(STQ, dst[hd * 128:(hd + 1) * 128, tsl], s[:, :], reads=[s], writes=[dst], waw=False)
    k.barrier()
    with ExitStack() as st0:
        sm = k.sb("df_sm", [128, 8], F32, st0)
        tmp = k.sb("df_tmp", [128, 64], F32, st0)
        cq1, ck1, cq2, ck2, cg = (L.col[n_] for n_ in ("lq1", "lk1", "lq2", "lk2", "gsub"))
        for i, (a, b_) in enumerate(((cq1, ck1), (cq2, ck2))):
            k.op("dve", lambda e: e.tensor_tensor(out=tmp[:], in0=L.prm[:, a:a + 64], in1=L.prm[:, b_:b_ + 64],
                                                  op=ALU.mult), reads=[L.prm], writes=[tmp])
            k.op("dve", lambda e: e.reduce_sum(out=sm[:, i:i + 1], in_=tmp[:], axis=AX.X), reads=[tmp], writes=[sm])
            k.op("act", lambda e: e.activation(out=sm[:, 2 + i:3 + i], in_=sm[:, i:i + 1], func=AF.Exp),
                 reads=[sm], writes=[sm])
        k.op("dve", lambda e: e.tensor_tensor(out=sm[:, 4:5], in0=sm[:, 3:4], in1=sm[:, 2:3], op=ALU.subtract),
             reads=[sm], writes=[sm])
        cli, col_ = L.col["lam_init"], L.col["omlam"]
        k.op("dve", lambda e: e.tensor_tensor(out=sm[:, 5:6], in0=sm[:, 4:5], in1=L.prm[:, cli:cli + 1], op=ALU.subtract),
             reads=[sm, L.prm], writes=[sm])
        k.op("dve", lambda e: e.tensor_tensor(out=sm[:, 6:7], in0=L.prm[:, cg:cg + 1], in1=L.prm[:, col_:col_ + 1], op=ALU.mult),
             reads=[L.prm], writes=[sm])
        nlam = sm[:, 5:6]
        gs = sm[:, 6:7]
        scale = 64 ** -0.5
        for hd in range(2):
            with ExitStack() as st:
                Kd = k.sb("da_k", [128, S], BF16, st)
                Vf = k.sb("da_vf", [128, S // 128, 128], F32, st)
                Vs = k.sb("da_v", [128, S // 128, 128], BF16, st)
                k.dma("sp", Kd[:], c.kdT[hd * 128:(hd + 1) * 128, :], reads=[c.kdT], writes=[Kd])
                k.dma("sp", Vf[:], pV[:, hd * 128:(hd + 1) * 128].rearrange("(kt p) d -> p kt d", p=128),
                      reads=[pV], writes=[Vf])
                k.op("pool", lambda e: e.tensor_copy(out=Vs[:], in_=Vf[:]), reads=[Vf], writes=[Vs])
                Qd = [k.sb(f"da_q{i}", [128, TCH], BF16, st) for i in range(2)]
                ptb = [k.sb(f"da_pt{i}", [128, TCH], BF16, st) for i in range(3)]
                rec = k.sb("da_rec", [128, TCH], F32, st)
                o1 = k.sb("da_o1", [128, TCH], F32, st)
                o2 = k.sb("da_o2", [128, TCH], F32, st)
                sq = k.sb("da_sq", [128, TCH], BF16, st)
                rs = k.sb("da_rs", [128, TCH], F32, st)
                ost = [k.sb(f"da_o{i}", [128, TCH], F32, st) for i in range(2)]
                for j in range(NTC):
                    b = j % 2
                    tsl = slice(j * TCH, (j + 1) * TCH)
                    k.dma("sp", Qd[b][:], c.qdT[hd * 128:(hd + 1) * 128, tsl], reads=[c.qdT], writes=[Qd[b]])
                    maps = []
                    for m in range(2):
                        ms = slice(m * 64, (m + 1) * 64)
                        maps.append([dict(K=(lambda kt, ms=ms: Kd[ms, kt * 128:(kt + 1) * 128]),
                                          Q=(lambda c0, ms=ms: Qd[b][ms, c0:TCH]), kd=Kd, qd=Qd[b])])
                    accs = [(c.pf[2], c.pf[3]), (c.pf[4], c.pf[5])]
                    attn_qchunk(k, c, j, maps, lambda kt: Vs[:, kt, :], Vs, 128, scale, c.masks, ptb, accs)
                    (oa1, sa1), (oa2, sa2) = accs
                    k.op("act", lambda e: e.activation(out=rec[:], in_=sa1[:, :], func=AF.Ln), reads=[sa1], writes=[rec])
                    k.op("act", lambda e: e.activation(out=rec[:], in_=rec[:], func=AF.Exp, scale=-1.0), reads=[rec], writes=[rec])
                    k.op("dve", lambda e: e.tensor_tensor(out=o1[:], in0=oa1[:, :], in1=rec[:], op=ALU.mult),
                         reads=[oa1, rec], writes=[o1])
                    k.op("act", lambda e: e.activation(out=rec[:], in_=sa2[:, :], func=AF.Ln), reads=[sa2], writes=[rec])
                    k.op("act", lambda e: e.activation(out=rec[:], in_=rec[:], func=AF.Exp, scale=-1.0), reads=[rec], writes=[rec])
                    k.op("dve", lambda e: e.tensor_tensor(out=o2[:], in0=oa2[:, :], in1=rec[:], op=ALU.mult),
                         reads=[oa2, rec], writes=[o2])
                    k.op("dve", lambda e: e.scalar_tensor_tensor(out=o1[:], in0=o2[:], scalar=nlam, in1=o1[:],
                                                                 op0=ALU.mult, op1=ALU.add),
                         reads=[o2, o1, sm], writes=[o1])
                    k.op("pool", lambda e: e.tensor_tensor(out=sq[:], in0=o1[:], in1=o1[:], op=ALU.mult), reads=[o1], writes=[sq])
                    ps = c.next_pf(0, 2)
                    k.op("pe", lambda e: e.matmul(ps[:, :], lhsT=c.ones[:], rhs=sq[:], start=True, stop=True),
                         reads=[c.ones, sq], writes=[ps])
                    k.op("dve", lambda e: e.tensor_scalar(out=rs[:], in0=ps[:, :], scalar1=1.0 / 128, scalar2=DIFF_EPS,
                                                          op0=ALU.mult, op1=ALU.add), reads=[ps], writes=[rs])
                    k.op("act", lambda e: e.activation(out=rs[:], in_=rs[:], func=AF.Ln), reads=[rs], writes=[rs])
                    k.op("act", lambda e: e.activation(out=rs[:], in_=rs[:], func=AF.Exp, scale=-0.5), reads=[rs], writes=[rs])
                    k.op("dve", lambda e: e.scalar_tensor_tensor(out=ost[b][:], in0=o1[:], scalar=gs, in1=rs[:],
                                                                 op0=ALU.mult, op1=ALU.mult),
                         reads=[o1, rs, sm], writes=[ost[b]])
                    k.dma(STQ, c.mixT[768 + hd * 128:768 + (hd + 1) * 128, tsl], ost[b][:], reads=[ost[b]],
                          writes=[c.mixT], waw=False)
            k.barrier()


_reg("s5_are", 16)
_reg("s5_aim", 16)
_reg("s5_ldt", 16)
_reg("s5_d", 4)
_reg("s5_bg", 2)
Z_NEG0, Z_POS0, Z_REV0 = 0, 8, 17
GELU_C = 1.5957691216057308


def sincos_alloc(k, st, name, shape):
    return dict(ni=k.sb(name + "_ni", shape, I32, st), nf=k.sb(name + "_nf", shape, F32, st),
                a2=k.sb(name + "_a2", shape, F32, st), r=k.sb(name + "_r", shape, F32, st),
                m=k.sb(name + "_m", shape, F32, st))


def sincos_tile(k, tmp, ang, sin_out, cos_out):
    ni, nf, a2, r, m = tmp["ni"], tmp["nf"], tmp["a2"], tmp["r"], tmp["m"]
    k.op("dve", lambda e: e.tensor_scalar(out=ni[:], in0=ang[:], scalar1=float(1.0 / TWO_PI), scalar2=None,
                                          op0=ALU.mult), reads=[ang], writes=[ni])
    k.op("dve", lambda e: e.tensor_copy(out=nf[:], in_=ni[:]), reads=[ni], writes=[nf])
    k.op("dve", lambda e: e.scalar_tensor_tensor(out=a2[:], in0=nf[:], scalar=-CW1, in1=ang[:], op0=ALU.mult,
                                                 op1=ALU.add), reads=[nf, ang], writes=[a2])
    k.op("dve", lambda e: e.scalar_tensor_tensor(out=a2[:], in0=nf[:], scalar=-CW2, in1=a2[:], op0=ALU.mult,
                                                 op1=ALU.add), reads=[nf, a2], writes=[a2])
    k.op("dve", lambda e: e.scalar_tensor_tensor(out=a2[:], in0=nf[:], scalar=-CW3, in1=a2[:], op0=ALU.mult,
                                                 op1=ALU.add), reads=[nf, a2], writes=[a2])
    for shift, dst in ((0.0, sin_out), (np.pi / 2, cos_out)):
        k.op("dve", lambda e: e.tensor_scalar(out=r[:], in0=a2[:], scalar1=float(shift), scalar2=None, op0=ALU.add),
             reads=[a2], writes=[r])
        k.op("dve", lambda e: e.tensor_single_scalar(out=m[:], in_=r[:], scalar=float(np.pi), op=ALU.is_gt),
             reads=[r], writes=[m])
        k.op("dve", lambda e: e.scalar_tensor_tensor(out=r[:], in0=m[:], scalar=-TWO_PI, in1=r[:], op0=ALU.mult,
                                                     op1=ALU.add), reads=[m, r], writes=[r])
        k.op("dve", lambda e: e.tensor_single_scalar(out=m[:], in_=r[:], scalar=float(-np.pi), op=ALU.is_lt),
             reads=[r], writes=[m])
        k.op("dve", lambda e: e.scalar_tensor_tensor(out=r[:], in0=m[:], scalar=TWO_PI, in1=r[:], op0=ALU.mult,
                                                     op1=ALU.add), reads=[m, r], writes=[r])
        k.op("dve", lambda e: e.tensor_scalar(out=r[:], in0=r[:], scalar1=float(np.pi), scalar2=float(-np.pi),
                                              op0=ALU.min, op1=ALU.max), reads=[r], writes=[r])
        k.op("act", lambda e: e.activation(out=dst[:], in_=r[:], func=AF.Sin), reads=[r], writes=[dst])


def stage_s5(k, c, L):
    import os as _os
    NT = 16
    pT = c.pT
    SH4 = [128, NT, 8, 16]
    with ExitStack() as stA:
        Tm = k.sb("s5_Tm", [128, 32, 128], BF16, stA)
        GstR = k.sb("s5_GstR", [128, NT, 128], BF16, stA)
        GstI = k.sb("s5_GstI", [128, NT, 128], BF16, stA)
        EfR = k.sb("s5_EfR", SH4, BF16, stA)
        EfnI = k.sb("s5_EfnI", SH4, BF16, stA)
        sc = k.sb("s5_sc", [128, 8, NT], F32, stA)
        LR, DT, ML, TH, MAG8, FRE, FIM, TMP = range(8)
        ca, ci_, cl = L.col["s5_are"], L.col["s5_aim"], L.col["s5_ldt"]
        AIM = L.prm[:, ci_:ci_ + NT]
        with ExitStack() as st:
            bsb = k.sb("s5_b", [128, NT, 2, 16], F32, st)
            csb = k.sb("s5_c", [128, NT, 2, 16], F32, st)
            k.dma("sp", bsb[:], L.s5b[:, :].rearrange("p (t r c) -> p t r c", t=NT, r=2), writes=[bsb])
            k.dma("sp", csb[:], L.s5c[:, :].rearrange("p (t r c) -> p t r c", t=NT, r=2), writes=[csb])
            k.op("dve", lambda e: e.tensor_scalar(out=sc[:, LR, :], in0=L.prm[:, ca:ca + NT], scalar1=-1e-4,
                                                  scalar2=None, op0=ALU.min), reads=[L.prm], writes=[sc])
            k.op("act", lambda e: e.activation(out=sc[:, DT, :], in_=L.prm[:, cl:cl + NT], func=AF.Exp),
                 reads=[L.prm], writes=[sc])
            k.op("dve", lambda e: e.tensor_tensor(out=sc[:, ML, :], in0=sc[:, DT, :], in1=sc[:, LR, :], op=ALU.mult),
                 reads=[sc], writes=[sc])
            k.op("dve", lambda e: e.tensor_tensor(out=sc[:, TH, :], in0=sc[:, DT, :], in1=AIM, op=ALU.mult),
                 reads=[sc, L.prm], writes=[sc])
            k.op("act", lambda e: e.activation(out=sc[:, MAG8, :], in_=sc[:, ML, :], func=AF.Exp, scale=8.0),
                 reads=[sc], writes=[sc])
            SH3 = [128, NT, 32]
            lm = k.sb("s5_lm", SH3, F32, st)
            an = k.sb("s5_an", SH3, F32, st)
            mg = k.sb("s5_mg", SH3, F32, st)
            sn = k.sb("s5_sn", SH3, F32, st)
            cs = k.sb("s5_cs", SH3, F32, st)
            zr = k.sb("s5_zr", SH3, F32, st)
            zi = k.sb("s5_zi", SH3, F32, st)
            tauB = c.cst[:, 8:40].unsqueeze(1).to_broadcast(SH3)
            k.op("dve", lambda e: e.tensor_tensor(out=lm[:], in0=sc[:, ML, :].unsqueeze(2).to_broadcast(SH3), in1=tauB,
                                                  op=ALU.mult), reads=[sc, c.cst], writes=[lm])
            k.op("dve", lambda e: e.tensor_tensor(out=an[:], in0=sc[:, TH, :].unsqueeze(2).to_broadcast(SH3), in1=tauB,
                                                  op=ALU.mult), reads=[sc, c.cst], writes=[an])
            k.op("act", lambda e: e.activation(out=mg[:], in_=lm[:], func=AF.Exp), reads=[lm], writes=[mg])
            sincos_tile(k, sincos_alloc(k, st, "s5sc0", SH3), an, sn, cs)
            k.op("dve", lambda e: e.tensor_tensor(out=zr[:], in0=mg[:], in1=cs[:], op=ALU.mult), reads=[mg, cs], writes=[zr])
            k.op("dve", lambda e: e.tensor_tensor(out=zi[:], in0=mg[:], in1=sn[:], op=ALU.mult), reads=[mg, sn], writes=[zi])
            if _os.environ.get("S5_STOP") == "a":
                k.barrier()
                return
            sm = k.sb("s5_sm", [128, 8, NT], F32, st)
            abr, abi = zr[:, :, Z_POS0 + 1], zi[:, :, Z_POS0 + 1]
            lr_ = sc[:, LR, :]

            def tt(out, a, b, op, rd, wr, E="dve"):
                k.op(E, lambda e: e.tensor_tensor(out=out, in0=a, in1=b, op=op), reads=rd, writes=wr)
            tt(sm[:, 0, :], lr_, lr_, ALU.mult, [sc], [sm])
            tt(sm[:, 1, :], AIM, AIM, ALU.mult, [L.prm], [sm])
            tt(sm[:, 0, :], sm[:, 0, :], sm[:, 1, :], ALU.add, [sm], [sm])
            k.op("dve", lambda e: e.reciprocal(out=sm[:, 0, :], in_=sm[:, 0, :]), reads=[sm], writes=[sm])
            k.op("dve", lambda e: e.tensor_scalar(out=sm[:, 1, :], in0=abr, scalar1=-1.0, scalar2=None, op0=ALU.add),
                 reads=[zr], writes=[sm])
            tt(sm[:, 2, :], sm[:, 1, :], lr_, ALU.mult, [sm, sc], [sm])
            tt(sm[:, 3, :], abi, AIM, ALU.mult, [zi, L.prm], [sm])
            tt(sm[:, 2, :], sm[:, 2, :], sm[:, 3, :], ALU.add, [sm], [sm])
            tt(sc[:, FRE, :], sm[:, 2, :], sm[:, 0, :], ALU.mult, [sm], [sc])
            tt(sm[:, 4, :], abi, lr_, ALU.mult, [zi, sc], [sm])
            tt(sm[:, 5, :], sm[:, 1, :], AIM, ALU.mult, [sm, L.prm], [sm])
            tt(sm[:, 4, :], sm[:, 4, :], sm[:, 5, :], ALU.subtract, [sm], [sm])
            tt(sc[:, FIM, :], sm[:, 4, :], sm[:, 0, :], ALU.mult, [sm], [sc])
            if _os.environ.get("S5_STOP") == "b":
                k.barrier()
                return
            SHB = [128, NT, 16]
            bbr = k.sb("s5_bbr", SHB, F32, st)
            bbi = k.sb("s5_bbi", SHB, F32, st)
            t1 = k.sb("s5_t1", SHB, F32, st)
            fre = sc[:, FRE, :].unsqueeze(2).to_broadcast(SHB)
            fim = sc[:, FIM, :].unsqueeze(2).to_broadcast(SHB)
            br_, bi_ = bsb[:, :, 0, :], bsb[:, :, 1, :]
            tt(bbr[:], fre, br_, ALU.mult, [sc, bsb], [bbr])
            tt(t1[:], fim, bi_, ALU.mult, [sc, bsb], [t1])
            tt(bbr[:], bbr[:], t1[:], ALU.subtract, [bbr, t1], [bbr])
            tt(bbi[:], fre, bi_, ALU.mult, [sc, bsb], [bbi])
            tt(t1[:], fim, br_, ALU.mult, [sc, bsb], [t1])
            tt(bbi[:], bbi[:], t1[:], ALU.add, [bbi, t1], [bbi])
            if _os.environ.get("S5_STOP") == "c":
                k.barrier()
                return
            BfR = k.sb("s5_BfR", SH4, BF16, st)
            BfnI = k.sb("s5_BfnI", SH4, BF16, st)
            CfR = k.sb("s5_CfR", SH4, BF16, st)
            CfI = k.sb("s5_CfI", SH4, BF16, st)
            GfR = k.sb("s5_GfR", SH4, BF16, st)
            GfI = k.sb("s5_GfI", SH4, BF16, st)
            u1 = [k.sb(f"s5_u1{i}", SH4, F32, st) for i in range(2)]
            u2 = [k.sb(f"s5_u2{i}", SH4, F32, st) for i in range(2)]

            def cmul(oR, oI, z0, xr, xi, xdeps, neg_im, i):
                E = "dve" if i % 2 == 0 else "pool"
                zR = zr[:, :, z0:z0 + 8].unsqueeze(3).to_broadcast(SH4)
                zI = zi[:, :, z0:z0 + 8].unsqueeze(3).to_broadcast(SH4)
                xR = xr.unsqueeze(2).to_broadcast(SH4)
                xI = xi.unsqueeze(2).to_broadcast(SH4)
                a, b_ = u1[i % 2], u2[i % 2]
                tt(a[:], zR, xR, ALU.mult, [zr, ] + xdeps, [a], E)
                tt(b_[:], zI, xI, ALU.mult, [zi, ] + xdeps, [b_], E)
                tt(oR[:], a[:], b_[:], ALU.subtract, [a, b_], [oR], E)
                tt(a[:], zR, xI, ALU.mult, [zr, ] + xdeps, [a], E)
                tt(b_[:], zI, xR, ALU.mult, [zi, ] + xdeps, [b_], E)
                if neg_im:
                    k.op("dve", lambda e: e.scalar_tensor_tensor(out=oI[:], in0=a[:], scalar=-1.0, in1=b_[:],
                                                                 op0=ALU.mult, op1=ALU.subtract),
                         reads=[a, b_], writes=[oI])
                else:
                    tt(oI[:], a[:], b_[:], ALU.add, [a, b_], [oI], E)
            cr_, ci2 = csb[:, :, 0, :], csb[:, :, 1, :]
            cmul(BfR, BfnI, Z_NEG0, bbr[:], bbi[:], [bbr, bbi], True, 0)
            cmul(CfR, CfI, Z_POS0, cr_, ci2, [csb], False, 1)
            cmul(GfR, GfI, Z_REV0, bbr[:], bbi[:], [bbr, bbi], False, 0)
            cmul(EfR, EfnI, Z_POS0 + 1, cr_, ci2, [csb], True, 1)
            if _os.environ.get("S5_STOP") == "d":
                k.barrier()
                return
            for t4 in range(4):
                for hh in range(2):
                    ps = c.next_pf()
                    hs = slice(hh * 64, (hh + 1) * 64)
                    for q in range(4):
                        t = t4 * 4 + q
                        k.op("pe", lambda e: e.matmul(ps[:, q * 128:(q + 1) * 128],
                                                      lhsT=BfR[hs, t, :, :].rearrange("p a b -> p (a b)"),
                                                      rhs=CfR[hs, t, :, :].rearrange("p a b -> p (a b)"), start=True, stop=False),
                             reads=[BfR, CfR], writes=[ps], inc=False)
                        k.op("pe", lambda e: e.matmul(ps[:, q * 128:(q + 1) * 128],
                                                      lhsT=BfnI[hs, t, :, :].rearrange("p a b -> p (a b)"),
                                                      rhs=CfI[hs, t, :, :].rearrange("p a b -> p (a b)"), start=False, stop=True),
                             reads=[BfnI, CfI], writes=[ps], inc=(q == 3))
                    for q in range(4):
                        g = 2 * (t4 * 4 + q) + hh
                        k.op("dve", lambda e: e.tensor_tensor(out=Tm[:, g, :], in0=ps[:, q * 128:(q + 1) * 128],
                                                              in1=c.tmask[:], op=ALU.mult),
                             reads=[ps, c.tmask], writes=[Tm])
            if _os.environ.get("S5_STOP") == "e":
                k.barrier()
                return
            for (src, dst) in ((GfR, GstR), (GfI, GstI)):
                for t8 in range(2):
                    pb = c.next_pb()
                    for q in range(8):
                        t = t8 * 8 + q
                        k.op("pe", lambda e: e.transpose(pb[:, q * 128:(q + 1) * 128],
                                                         src[:, t, :, :].rearrange("p a b -> p (a b)"), c.ident[:]),
                             reads=[src, c.ident], writes=[pb], inc=(q == 7))
                    k.op("act", lambda e: e.copy(out=dst[:, t8 * 8:(t8 + 1) * 8, :],
                                                 in_=pb[:].rearrange("p (q n) -> p q n", q=8)),
                         reads=[pb], writes=[dst])
            k.barrier()
        import os as _os
        if _os.environ.get("S5_STOP") == "1":
            return
        with ExitStack() as st:
            ug = [[k.sb(f"s5_u{i}{j}", [128, TCH], BF16, st) for j in range(2)] for i in range(2)]
            ang = k.sb("s5_ang", [128, TCH], F32, st)
            sn = k.sb("s5_snj", [128, TCH], F32, st)
            cs = k.sb("s5_csj", [128, TCH], F32, st)
            a_ = k.sb("s5_a", [128, TCH], F32, st)
            b_ = k.sb("s5_bq", [128, TCH], F32, st)
            mre = k.sb("s5_mre", [128, TCH], F32, st)
            mim = k.sb("s5_mim", [128, TCH], F32, st)
            hre = k.sb("s5_hre", [128, TCH], F32, st)
            him = k.sb("s5_him", [128, TCH], F32, st)
            Hp = [[k.sb(f"s5_Hp{i}{j}", [128, TCH + 1], BF16, st) for j in range(2)] for i in range(2)]
            yst = [k.sb(f"s5_y{i}", [128, TCH], F32, st) for i in range(3)]
            for i in range(2):
                for j in range(2):
                    k.op("pool", lambda e: e.memset(Hp[i][j][:, 0:1], 0.0), writes=[Hp[i][j]])
            sctmp = sincos_alloc(k, st, "s5scj", [128, TCH])
            jv = c.cst[:, 40:552]
            nyi = 0

            def tt(out, a, b, op, rd, wr, E="dve"):
                k.op(E, lambda e: e.tensor_tensor(out=out, in0=a, in1=b, op=op), reads=rd, writes=wr)
            if True:
                for t in range(NT):
                    pp = t % 2
                    for hh in range(2):
                        k.dma("sp", ug[pp][hh][:], c.us5[2 * t + hh, :, :], reads=[c.us5], writes=[ug[pp][hh]])
                    Xre, Xim = c.pf[0], c.pf[1]
                    for hh in range(2):
                        hs = slice(hh * 64, (hh + 1) * 64)
                        k.op("pe", lambda e: e.matmul(Xre[hs, :], lhsT=GstR[:, t, hs], rhs=ug[pp][hh][:], start=True, stop=True),
                             reads=[GstR, ug[pp][hh]], writes=[Xre], inc=(hh == 1))
                    for hh in range(2):
                        hs = slice(hh * 64, (hh + 1) * 64)
                        k.op("pe", lambda e: e.matmul(Xim[hs, :], lhsT=GstI[:, t, hs], rhs=ug[pp][hh][:], start=True, stop=True),
                             reads=[GstI, ug[pp][hh]], writes=[Xim], inc=(hh == 1))
                    k.op("dve", lambda e: e.tensor_scalar(out=ang[:], in0=jv, scalar1=sc[:, TH, t:t + 1], scalar2=None,
                                                           op0=ALU.mult), reads=[c.cst, sc], writes=[ang])
                    sincos_tile(k, sctmp, ang, sn, cs)
                    tt(a_[:], Xre[:, :], cs[:], ALU.mult, [Xre, cs], [a_])
                    tt(b_[:], Xim[:, :], sn[:], ALU.mult, [Xim, sn], [b_])
                    tt(mre[:], a_[:], b_[:], ALU.add, [a_, b_], [mre], "pool")
                    tt(a_[:], Xim[:, :], cs[:], ALU.mult, [Xim, cs], [a_])
                    tt(b_[:], Xre[:, :], sn[:], ALU.mult, [Xre, sn], [b_])
                    tt(mim[:], a_[:], b_[:], ALU.subtract, [a_, b_], [mim], "pool")
                    m8 = sc[:, MAG8, t:t + 1].to_broadcast([128, TCH])
                    k.op("dve", lambda e: e.tensor_tensor_scan(out=hre[:], data0=m8, data1=mre[:], initial=0.0,
                                                               op0=ALU.mult, op1=ALU.add), reads=[sc, mre], writes=[hre])
                    k.op("dve", lambda e: e.tensor_tensor_scan(out=him[:], data0=m8, data1=mim[:], initial=0.0,
                                                               op0=ALU.mult, op1=ALU.add), reads=[sc, mim], writes=[him])
                    hr, hi_ = Hp[pp][0], Hp[pp][1]
                    tt(a_[:], hre[:], cs[:], ALU.mult, [hre, cs], [a_])
                    tt(b_[:], him[:], sn[:], ALU.mult, [him, sn], [b_], "pool")
                    tt(hr[:, 1:TCH + 1], a_[:], b_[:], ALU.subtract, [a_, b_], [hr])
                    tt(mre[:], hre[:], sn[:], ALU.mult, [hre, sn], [mre])
                    tt(mim[:], him[:], cs[:], ALU.mult, [him, cs], [mim], "pool")
                    tt(hi_[:, 1:TCH + 1], mre[:], mim[:], ALU.add, [mre, mim], [hi_])
                    for hh in range(2):
                        g = 2 * t + hh
                        hs = slice(hh * 64, (hh + 1) * 64)
                        py = c.next_pf(2, 6)
                        k.op("pe", lambda e: e.matmul(py[:, :], lhsT=Tm[:, g, :], rhs=ug[pp][hh][:], start=True, stop=False),
                             reads=[Tm, ug[pp][hh]], writes=[py], inc=False)
                        k.op("pe", lambda e: e.matmul(py[:, :], lhsT=EfR[hs, t, :, :].rearrange("p a b -> p (a b)"),
                                                      rhs=hr[hs, 0:TCH], start=False, stop=False),
                             reads=[EfR, hr], writes=[py], inc=False)
                        k.op("pe", lambda e: e.matmul(py[:, :], lhsT=EfnI[hs, t, :, :].rearrange("p a b -> p (a b)"),
                                                      rhs=hi_[hs, 0:TCH], start=False, stop=True),
                             reads=[EfnI, hi_], writes=[py], inc=True)
                        ys = yst[nyi % 3]
                        nyi += 1
                        k.op("act", lambda e: e.copy(out=ys[:], in_=py[:, :]), reads=[py], writes=[ys])
                        k.dma(STQ, c.ys5[g, :, :], ys[:], reads=[ys], writes=[c.ys5], waw=False)
    k.barrier()
    if _os.environ.get("S5_STOP") == "2":
        return
    cd = L.col["s5_d"]
    with ExitStack() as st:
        Y = k.sb("s5p_Y", [128, 8, TCH], F32, st)
        U = k.sb("s5p_U", [128, S], F32, st)
        yy = k.sb("s5p_yy", [128, S], F32, st)
        q1 = k.sb("s5p_q1", [128, S], F32, st)
        gb = k.sb("s5p_gb", [128, S], BF16, st)
        for ct in range(4):
            for gl in range(8):
                k.dma("sp", Y[gl * 16:(gl + 1) * 16, :, :],
                      c.ys5[ct * 8 + gl, :, :].rearrange("(t c) j -> c t j", c=16), reads=[c.ys5], writes=[Y],
                      waw=(gl == 0))
            k.dma("sp", U[:], pT[R_U + ct * 128:R_U + (ct + 1) * 128, :], reads=[pT], writes=[U])
            k.op("dve", lambda e: e.scalar_tensor_tensor(out=yy[:].rearrange("p (j t) -> p j t", t=8),
                                                         in0=U[:].rearrange("p (j t) -> p j t", t=8),
                                                         scalar=L.prm[:, cd + ct:cd + ct + 1],
                                                         in1=Y[:].rearrange("p t j -> p j t"),
                                                         op0=ALU.mult, op1=ALU.add), reads=[U, Y, L.prm], writes=[yy])
            k.op("act", lambda e: e.activation(out=q1[:], in_=yy[:], func=AF.Square), reads=[yy], writes=[q1])
            k.op("dve", lambda e: e.tensor_scalar(out=q1[:], in0=q1[:], scalar1=0.044715, scalar2=1.0, op0=ALU.mult,
                                                  op1=ALU.add), reads=[q1], writes=[q1])
            k.op("pool", lambda e: e.tensor_tensor(out=q1[:], in0=q1[:], in1=yy[:], op=ALU.mult), reads=[q1, yy], writes=[q1])
            k.op("act", lambda e: e.activation(out=q1[:], in_=q1[:], func=AF.Sigmoid, scale=GELU_C), reads=[q1], writes=[q1])
            k.op("dve", lambda e: e.tensor_tensor(out=yy[:], in0=yy[:], in1=q1[:], op=ALU.mult), reads=[yy, q1], writes=[yy])
            k.op("act", lambda e: e.copy(out=gb[:], in_=yy[:]), reads=[yy], writes=[gb])
            k.dma(STQ, c.gT[ct * 128:(ct + 1) * 128, :], gb[:], reads=[gb], writes=[c.gT], waw=False)
            if ct < 2:
                k.dma(STQ, c.gF[ct * 128:(ct + 1) * 128, :], yy[:], reads=[yy], writes=[c.gF], waw=False)
    k.barrier()
    cb = L.col["s5_bg"]
    with ExitStack() as st:
        wbf, wd = load_w_bf16(k, st, lambda kc: L.wglu[kc * 128:(kc + 1) * 128, :], 4, 256, "wglu")
        gin = [k.sb(f"s5g_g{i}", [128, TCH], F32, st) for i in range(3)]
        sg = [k.sb(f"s5g_s{i}", [128, TCH], F32, st) for i in range(3)]
        n = [0]

        def epi(tc, ni, ps, ncol):
            i = n[0] % 3
            n[0] += 1
            tsl = slice(tc * TCH, (tc + 1) * TCH)
            k.dma("sp", gin[i][:], c.gF[ni * 128:(ni + 1) * 128, tsl], reads=[c.gF], writes=[gin[i]])
            k.op("act", lambda e: e.activation(out=sg[i][:], in_=ps[:, :], func=AF.Sigmoid,
                                               bias=L.prm[:, cb + ni:cb + ni + 1], scale=1.0),
                 reads=[ps, L.prm], writes=[sg[i]])
            k.op("dve", lambda e: e.tensor_tensor(out=sg[i][:], in0=sg[i][:], in1=gin[i][:], op=ALU.mult),
                 reads=[sg[i], gin[i]], writes=[sg[i]])
            k.dma(STQ, c.mixT[256 + ni * 128:256 + (ni + 1) * 128, tsl], sg[i][:], reads=[sg[i]], writes=[c.mixT],
                  waw=False)
        linear_fm(k, c, st, c.gT, 4, wbf, wd, [(0, 128), (128, 128)], epi, "s5g")
    k.barrier()


for _n, _w in (("rw_mur", 2), ("rw_muk", 2), ("rw_muv", 2), ("rw_mul", 1), ("rw_w0", 2), ("rw_a0", 2),
               ("rw_kk", 2), ("rw_ka", 2), ("rw_rk", 2), ("rw_lng", 2), ("rw_lnb", 2)):
    _reg(_n, _w)
R_RV = 3584
SEG = 1024
NCK = SEG // 64
RW_EPS = 64e-5
LDC = -0.6065306597126334


def stage_rwkv(k, c, L):
    pT = c.pT
    col = L.col
    with ExitStack() as stA:
        P = lambda nm, j: L.prm[:, col[nm] + j:col[nm] + j + 1]
        wl = k.sb("rw_wl", [128, 256], BF16, stA)
        omka = k.sb("rw_omka", [128, 2], F32, stA)
        rmask = k.sb("rw_rmask", [128, SEG], F32, stA)
        with ExitStack() as st0:
            wlf = k.sb("rw_wlf", [128, 256], F32, st0)
            k.dma("sp", wlf[:], L.w2a2[:, :], writes=[wlf])
            k.op("dve", lambda e: e.tensor_copy(out=wl[:], in_=wlf[:]), reads=[wlf], writes=[wl])
            k.op("dve", lambda e: e.tensor_scalar(out=omka[:], in0=L.prm[:, col["rw_ka"]:col["rw_ka"] + 2], scalar1=-1.0,
                                                  scalar2=1.0, op0=ALU.mult, op1=ALU.add), reads=[L.prm], writes=[omka])
            k.op("pool", lambda e: e.memset(rmask[:], 1.0), writes=[rmask])
            k.op("pool", lambda e: e.memset(rmask[:].rearrange("p (c i) -> p c i", i=64)[:, :, 0:1], 0.0), writes=[rmask])
            k.barrier()
        F = lambda nm: k.sb("rw_" + nm, [128, SEG], F32, stA)
        rz = k.sb("rw_rz", [128, SEG + 1], F32, stA)
        kz = k.sb("rw_kz", [128, SEG + 1], F32, stA)
        vz = k.sb("rw_vz", [128, SEG + 1], F32, stA)
        lz = k.sb("rw_lz", [128, SEG + 1], F32, stA)
        rm, km, vm, lm, t1, t2, sgw, av, cl, Epos, Eneg, Eex, Eh, kkr, kk, k2, bv = (F(n) for n in (
            "rm", "km", "vm", "lm", "t1", "t2", "sgw", "av", "cl", "Epos", "Eneg", "Eex", "Eh", "kkr", "kk", "k2", "bv"))
        lbf = k.sb("rw_lbf", [128, SEG], BF16, stA)
        sqb = k.sb("rw_sqb", [128, SEG], BF16, stA)
        BDn = ("AT", "BT", "KT", "RT", "bhT", "khT", "vT", "Bh", "Kh", "Vb", "Lst", "Mst", "LakT", "ArbT", "ArkT", "TT")
        BD = {n: k.sb("rw_bd_" + n, [128, NCK, 128], BF16, stA) for n in BDn}
        Ln = [k.sb(f"rw_Ln{i}", [128, 8, 128], BF16, stA) for i in range(2)]
        Mn = [k.sb(f"rw_Mn{i}", [128, 8, 128], BF16, stA) for i in range(2)]
        Pn = [k.sb(f"rw_Pn{i}", [128, 8, 128], BF16, stA) for i in range(2)]
        Sf = k.sb("rw_Sf", [128, 128], F32, stA)
        Sb = [k.sb(f"rw_Sb{i}", [128, 128], BF16, stA) for i in range(2)]
        RHSb = k.sb("rw_RHSb", [128, 128], BF16, stA)
        Ub = k.sb("rw_Ub", [128, 128], BF16, stA)
        OT = k.sb("rw_OT", [128, SEG], F32, stA)
        ob = k.sb("rw_ob", [128, SEG], BF16, stA)
        yo = [k.sb(f"rw_yo{i}", [128, SEG], F32, stA) for i in range(2)]
        for n in ("AT", "BT", "KT", "RT", "bhT", "khT", "vT"):
            k.op("pool", lambda e: e.memset(BD[n][:], 0.0), writes=[BD[n]])

        def tt(out, a, b, op, rd, wr, E="dve"):
            k.op(E, lambda e: e.tensor_tensor(out=out, in0=a, in1=b, op=op), reads=rd, writes=wr)

        def act(out, in_, func, rd, wr, **kw):
            k.op("act", lambda e: e.activation(out=out, in_=in_, func=func, **kw), reads=rd, writes=wr)

        def bdw(dst, a, b, rd, E="dve", scalar=None):
            for h in range(2):
                hs = slice(h * 64, (h + 1) * 64)
                o = dst[hs, :, h * 64:(h + 1) * 64]
                av_ = a[hs, :].rearrange("p (c i) -> p c i", i=64)
                if b is None:
                    k.op(E, lambda e: e.tensor_copy(out=o, in_=av_), reads=rd, writes=[dst])
                    continue
                bv_ = b[hs, :].rearrange("p (c i) -> p c i", i=64)
                if scalar is None:
                    k.op(E, lambda e: e.tensor_tensor(out=o, in0=av_, in1=bv_, op=ALU.mult), reads=rd, writes=[dst])
                else:
                    k.op("dve", lambda e: e.scalar_tensor_tensor(out=o, in0=av_, scalar=float(scalar), in1=bv_,
                                                                 op0=ALU.mult, op1=ALU.mult), reads=rd, writes=[dst])

        for hp in range(2):
            k.op("dve", lambda e: e.memset(Sf[:], 0.0), writes=[Sf])
            k.op("dve", lambda e: e.memset(Sb[0][:], 0.0), writes=[Sb[0]])
            sbi = 0
            for seg in range(S // SEG):
                t0 = seg * SEG
                for (zt, r0, mu, dst) in ((rz, R_R + hp * 128, P("rw_mur", hp), rm), (kz, R_K + hp * 128, P("rw_muk", hp), km),
                                          (vz, R_RV + hp * 128, P("rw_muv", hp), vm), (lz, R_LORA, P("rw_mul", 0), lm)):
                    if seg == 0:
                        k.op("pool", lambda e: e.memset(zt[:, 0:1], 0.0), writes=[zt])
                        k.dma("sp", zt[:, 1:SEG + 1], pT[r0:r0 + 128, 0:SEG], reads=[pT], writes=[zt])
                    else:
                        k.dma("sp", zt[:, 0:SEG + 1], pT[r0:r0 + 128, t0 - 1:t0 + SEG], reads=[pT], writes=[zt])
                    tt(t1[:], zt[:, 0:SEG], zt[:, 1:SEG + 1], ALU.subtract, [zt], [t1], "dve")
                    k.op("dve", lambda e: e.scalar_tensor_tensor(out=dst[:], in0=t1[:], scalar=mu, in1=zt[:, 1:SEG + 1],
                                                                 op0=ALU.mult, op1=ALU.add), reads=[t1, zt, L.prm], writes=[dst])
                act(lbf[0:64, :], lm[0:64, :], AF.Tanh, [lm], [lbf])
                k.op("dve", lambda e: e.tensor_copy(out=lbf[64:128, :], in_=lm[64:128, :]), reads=[lm], writes=[lbf])
                for hb in range(SEG // TCH):
                    cs_ = slice(hb * TCH, (hb + 1) * TCH)
                    pw, pa = c.pf[0], c.pf[1]
                    k.op("pe", lambda e: e.matmul(pw[:, :], lhsT=wl[0:64, hp * 128:(hp + 1) * 128], rhs=lbf[0:64, cs_],
                                                  start=True, stop=True), reads=[wl, lbf], writes=[pw])
                    k.op("pe", lambda e: e.matmul(pa[:, :], lhsT=wl[64:128, hp * 128:(hp + 1) * 128], rhs=lbf[64:128, cs_],
                                                  start=True, stop=True), reads=[wl, lbf], writes=[pa])
                    act(sgw[:, cs_], pw[:, :], AF.Sigmoid, [pw, L.prm], [sgw], bias=P("rw_w0", hp), scale=1.0)
                    act(av[:, cs_], pa[:, :], AF.Sigmoid, [pa, L.prm], [av], bias=P("rw_a0", hp), scale=1.0)
                k.op("dve", lambda e: e.tensor_tensor_scan(out=cl[:], data0=rmask[:], data1=sgw[:], initial=0.0,
                                                           op0=ALU.mult, op1=ALU.add), reads=[rmask, sgw], writes=[cl])
                act(Epos[:], cl[:], AF.Exp, [cl], [Epos], scale=LDC)
                act(Eneg[:], cl[:], AF.Exp, [cl], [Eneg], scale=-LDC)
                tt(t1[:], cl[:], sgw[:], ALU.subtract, [cl, sgw], [t1], "dve")
                act(Eex[:], t1[:], AF.Exp, [t1], [Eex], scale=LDC)
                clC = cl[:].rearrange("p (c i) -> p c i", i=64)[:, :, 63:64].to_broadcast([128, NCK, 64])
                tt(t2[:].rearrange("p (c i) -> p c i", i=64), clC, cl[:].rearrange("p (c i) -> p c i", i=64),
                   ALU.subtract, [cl], [t2])
                act(Eh[:], t2[:], AF.Exp, [t2], [Eh], scale=LDC)
                k.op("dve", lambda e: e.tensor_scalar(out=kkr[:], in0=km[:], scalar1=P("rw_kk", hp), scalar2=None,
                                                      op0=ALU.mult), reads=[km, L.prm], writes=[kkr])
                act(sqb[:], kkr[:], AF.Square, [kkr], [sqb])
                for hb in range(SEG // TCH):
                    cs_ = slice(hb * TCH, (hb + 1) * TCH)
                    pn = c.next_pf(2, 6)
                    k.op("pe", lambda e: e.matmul(pn[:, :], lhsT=c.blk1[:], rhs=sqb[:, cs_], start=True, stop=True),
                         reads=[c.blk1, sqb], writes=[pn])
                    k.op("dve", lambda e: e.tensor_scalar(out=t1[:, cs_], in0=pn[:, :], scalar1=1e-24, scalar2=None, op0=ALU.max),
                         reads=[pn], writes=[t1])
                act(t1[:], t1[:], AF.Ln, [t1], [t1])
                act(t1[:], t1[:], AF.Exp, [t1], [t1], scale=-0.5)
                tt(kk[:], kkr[:], t1[:], ALU.mult, [kkr, t1], [kk])
                k.op("dve", lambda e: e.tensor_scalar(out=t2[:], in0=av[:], scalar1=P("rw_ka", hp), scalar2=omka[:, hp:hp + 1],
                                                      op0=ALU.mult, op1=ALU.add), reads=[av, L.prm, omka], writes=[t2])
                tt(k2[:], km[:], t2[:], ALU.mult, [km, t2], [k2], "dve")
                tt(bv[:], kk[:], av[:], ALU.mult, [kk, av], [bv], "dve")
                bdw(BD["AT"], kk, Eex, [kk, Eex], scalar=-1.0)
                bdw(BD["BT"], bv, Eneg, [bv, Eneg], "dve")
                bdw(BD["KT"], k2, Eneg, [k2, Eneg], "dve")
                bdw(BD["RT"], rm, Epos, [rm, Epos], "dve")
                bdw(BD["bhT"], bv, Eh, [bv, Eh], "dve")
                bdw(BD["khT"], k2, Eh, [k2, Eh], "dve")
                bdw(BD["vT"], vm, None, [vm], "dve")
                for oc in range(NCK // 8):
                    c8 = slice(oc * 8, (oc + 1) * 8)
                    prods = (("AT", "BT", "Lst", c.mSL), ("BT", "AT", "Mst", c.mSU), ("KT", "AT", "LakT", c.mSU),
                             ("BT", "RT", "ArbT", c.mUI), ("KT", "RT", "ArkT", c.mUI))
                    for (la, rb, dn, mk) in prods:
                        for g4 in range(2):
                            ps = c.next_pf()
                            for q in range(4):
                                cc = oc * 8 + g4 * 4 + q
                                k.op("pe", lambda e: e.matmul(ps[:, q * 128:(q + 1) * 128], lhsT=BD[la][:, cc, :],
                                                              rhs=BD[rb][:, cc, :], start=True, stop=True),
                                     reads=[BD[la], BD[rb]], writes=[ps], inc=(q == 3))
                            c4 = slice(oc * 8 + g4 * 4, oc * 8 + g4 * 4 + 4)
                            tt(BD[dn][:, c4, :].rearrange("p a b -> p (a b)"), ps[:, :], mk[:], ALU.mult, [ps, mk], [BD[dn]])
                    for (src, dst) in (("bhT", "Bh"), ("khT", "Kh"), ("vT", "Vb")):
                        pb = c.next_pb()
                        for q in range(8):
                            cc = oc * 8 + q
                            k.op("pe", lambda e: e.transpose(pb[:, q * 128:(q + 1) * 128], BD[src][:, cc, :], c.ident[:]),
                                 reads=[BD[src], c.ident], writes=[pb], inc=(q == 7))
                        k.op("act", lambda e: e.copy(out=BD[dst][:, c8, :].rearrange("p a b -> p (a b)"), in_=pb[:]),
                             reads=[pb], writes=[BD[dst]])
                    Lc, Mc, Pc = BD["Lst"][:, c8, :], BD["Mst"][:, c8, :], None
                    Ld, Md = BD["Lst"], BD["Mst"]
                    k.op("dve", lambda e: e.tensor_tensor(out=Pn[0][:].rearrange("p a b -> p (a b)"),
                                                          in0=BD["Mst"][:, c8, :].rearrange("p a b -> p (a b)"),
                                                          in1=c.ident8[:], op=ALU.add), reads=[BD["Mst"], c.ident8], writes=[Pn[0]])
                    pcur = 0
                    for n in range(1, 6):
                        di = n % 2
                        psL = [c.pf[0], c.pf[1]]
                        psM = [c.pf[2], c.pf[3]]
                        for g4 in range(2):
                            for q in range(4):
                                qq = g4 * 4 + q
                                k.op("pe", lambda e: e.matmul(psL[g4][:, q * 128:(q + 1) * 128], lhsT=Mc[:, qq, :], rhs=Lc[:, qq, :],
                                                              start=True, stop=True), reads=[Md, Ld], writes=[psL[g4]], inc=(q == 3))
                            if n < 5:
                                for q in range(4):
                                    qq = g4 * 4 + q
                                    k.op("pe", lambda e: e.matmul(psM[g4][:, q * 128:(q + 1) * 128], lhsT=Lc[:, qq, :], rhs=Mc[:, qq, :],
                                                                  start=True, stop=True), reads=[Ld, Md], writes=[psM[g4]], inc=(q == 3))
                        for g4 in range(2):
                            k.op("act", lambda e: e.copy(out=Ln[di][:, g4 * 4:(g4 + 1) * 4, :].rearrange("p a b -> p (a b)"),
                                                         in_=psL[g4][:, :]), reads=[psL[g4]], writes=[Ln[di]])
                            if n < 5:
                                k.op("dve", lambda e: e.tensor_copy(out=Mn[di][:, g4 * 4:(g4 + 1) * 4, :].rearrange("p a b -> p (a b)"),
                                                                    in_=psM[g4][:, :]), reads=[psM[g4]], writes=[Mn[di]])
                        Lc, Mc, Ld, Md = Ln[di][:, :, :], Mn[di][:, :, :], Ln[di], Mn[di]
                        psP = [c.pf[4], c.pf[5]]
                        pnx = 1 - pcur
                        for g4 in range(2):
                            for q in range(4):
                                qq = g4 * 4 + q
                                k.op("pe", lambda e: e.matmul(psP[g4][:, q * 128:(q + 1) * 128], lhsT=Lc[:, qq, :], rhs=Pn[pcur][:, qq, :],
                                                              start=True, stop=True), reads=[Ld, Pn[pcur]], writes=[psP[g4]], inc=(q == 3))
                        for g4 in range(2):
                            g4s = slice(g4 * 4, (g4 + 1) * 4)
                            if n < 5:
                                o_ = Pn[pnx][:, g4s, :].rearrange("p a b -> p (a b)")
                                wr = Pn[pnx]
                            else:
                                o_ = BD["TT"][:, oc * 8 + g4 * 4:oc * 8 + g4 * 4 + 4, :].rearrange("p a b -> p (a b)")
                                wr = BD["TT"]
                            tt(o_, psP[g4][:, :], Pn[pcur][:, g4s, :].rearrange("p a b -> p (a b)"), ALU.add,
                               [psP[g4], Pn[pcur]], [wr])
                        pcur = pnx
                for cc in range(NCK):
                    So = Sb[sbi]
                    Sn = Sb[1 - sbi]
                    pR, pU, pS, pO = c.pf[0], c.pf[1], c.pf[2], c.pf[3]
                    k.op("pe", lambda e: e.matmul(pR[:, 0:128], lhsT=BD["LakT"][:, cc, :], rhs=BD["Vb"][:, cc, :], start=True, stop=False),
                         reads=[BD["LakT"], BD["Vb"]], writes=[pR], inc=False)
                    k.op("pe", lambda e: e.matmul(pR[:, 0:128], lhsT=BD["AT"][:, cc, :], rhs=So[:], start=False, stop=True),
                         reads=[BD["AT"], So], writes=[pR])
                    k.op("act", lambda e: e.copy(out=RHSb[:], in_=pR[:, 0:128]), reads=[pR], writes=[RHSb])
                    k.op("pe", lambda e: e.matmul(pU[:, 0:128], lhsT=BD["TT"][:, cc, :], rhs=RHSb[:], start=True, stop=True),
                         reads=[BD["TT"], RHSb], writes=[pU])
                    k.op("dve", lambda e: e.tensor_copy(out=Ub[:], in_=pU[:, 0:128]), reads=[pU], writes=[Ub])
                    k.op("pe", lambda e: e.matmul(pS[:, 0:128], lhsT=BD["Kh"][:, cc, :], rhs=BD["Vb"][:, cc, :], start=True, stop=False),
                         reads=[BD["Kh"], BD["Vb"]], writes=[pS], inc=False)
                    k.op("pe", lambda e: e.matmul(pS[:, 0:128], lhsT=BD["Bh"][:, cc, :], rhs=Ub[:], start=False, stop=True),
                         reads=[BD["Bh"], Ub], writes=[pS])
                    gC = Epos[:, cc * 64 + 63:cc * 64 + 64]
                    k.op("dve", lambda e: e.scalar_tensor_tensor(out=Sf[:], in0=Sf[:], scalar=gC, in1=pS[:, 0:128],
                                                                 op0=ALU.mult, op1=ALU.add), reads=[Sf, Epos, pS], writes=[Sf])
                    k.op("act", lambda e: e.copy(out=Sn[:], in_=Sf[:]), reads=[Sf], writes=[Sn])
                    k.op("pe", lambda e: e.matmul(pO[:, 0:128], lhsT=BD["Vb"][:, cc, :], rhs=BD["ArkT"][:, cc, :], start=True, stop=False),
                         reads=[BD["Vb"], BD["ArkT"]], writes=[pO], inc=False)
                    k.op("pe", lambda e: e.matmul(pO[:, 0:128], lhsT=So[:], rhs=BD["RT"][:, cc, :], start=False, stop=False),
                         reads=[So, BD["RT"]], writes=[pO], inc=False)
                    k.op("pe", lambda e: e.matmul(pO[:, 0:128], lhsT=Ub[:], rhs=BD["ArbT"][:, cc, :], start=False, stop=True),
                         reads=[Ub, BD["ArbT"]], writes=[pO])
                    for h in range(2):
                        hs = slice(h * 64, (h + 1) * 64)
                        k.op("pool" if False else "act", lambda e: e.copy(out=OT[hs, cc * 64:(cc + 1) * 64], in_=pO[hs, h * 64:(h + 1) * 64]),
                             reads=[pO], writes=[OT])
                    sbi = 1 - sbi
                y = yo[seg % 2]
                k.op("pool", lambda e: e.tensor_copy(out=ob[:], in_=OT[:]), reads=[OT], writes=[ob])
                for hb in range(SEG // TCH):
                    cs_ = slice(hb * TCH, (hb + 1) * TCH)
                    pm = c.next_pf(4, 6)
                    k.op("pe", lambda e: e.matmul(pm[:, :], lhsT=c.blk64[:], rhs=ob[:, cs_], start=True, stop=True),
                         reads=[c.blk64, ob], writes=[pm])
                    tt(t1[:, cs_], OT[:, cs_], pm[:, :], ALU.subtract, [OT, pm], [t1])
                act(sqb[:], t1[:], AF.Square, [t1], [sqb])
                for hb in range(SEG // TCH):
                    cs_ = slice(hb * TCH, (hb + 1) * TCH)
                    pm = c.next_pf(4, 6)
                    k.op("pe", lambda e: e.matmul(pm[:, :], lhsT=c.blk64[:], rhs=sqb[:, cs_], start=True, stop=True),
                         reads=[c.blk64, sqb], writes=[pm])
                    k.op("dve", lambda e: e.tensor_scalar(out=t2[:, cs_], in0=pm[:, :], scalar1=RW_EPS, scalar2=None, op0=ALU.add),
                         reads=[pm], writes=[t2])
                act(t2[:], t2[:], AF.Ln, [t2], [t2])
                act(t2[:], t2[:], AF.Exp, [t2], [t2], scale=-0.5)
                tt(t1[:], t1[:], t2[:], ALU.mult, [t1, t2], [t1])
                k.op("dve", lambda e: e.tensor_scalar(out=y[:], in0=t1[:], scalar1=P("rw_lng", hp), scalar2=P("rw_lnb", hp),
                                                      op0=ALU.mult, op1=ALU.add), reads=[t1, L.prm], writes=[y])
                k.op("dve", lambda e: e.scalar_tensor_tensor(out=sqb[:], in0=rm[:], scalar=P("rw_rk", hp), in1=k2[:],
                                                             op0=ALU.mult, op1=ALU.mult), reads=[rm, k2, L.prm], writes=[sqb])
                for hb in range(SEG // TCH):
                    cs_ = slice(hb * TCH, (hb + 1) * TCH)
                    pm = c.next_pf(4, 6)
                    k.op("pe", lambda e: e.matmul(pm[:, :], lhsT=c.blk1[:], rhs=sqb[:, cs_], start=True, stop=True),
                         reads=[c.blk1, sqb], writes=[pm])
                    tt(t2[:, cs_], pm[:, :], vm[:, cs_], ALU.mult, [pm, vm], [t2])
                tt(y[:], y[:], t2[:], ALU.add, [y, t2], [y], "dve")
                k.dma(STQ, c.mixT[512 + hp * 128:512 + (hp + 1) * 128, t0:t0 + SEG], y[:], reads=[y], writes=[c.mixT], waw=False)
        k.barrier()


def stage_outproj(k, c, L, xa, xb, y, xa_deps=None, cc=None):
    pT = c.pT
    with ExitStack() as st:
        wbf, wd = load_w_bf16(k, st, lambda kc: L.wout[kc * 128:(kc + 1) * 128, :], 8, D, "wout", nstg=2)
        mx = [k.sb(f"op_mx{i}", [128, 8, TCH], F32, st) for i in range(2)]
        gt = [k.sb(f"op_gt{i}", [128, 8, TCH], F32, st) for i in range(2)]
        sg = k.sb("op_sg", [128, 8, TCH], F32, st)
        mg = [k.sb(f"op_mg{i}", [128, 8, TCH], BF16, st) for i in range(2)]
        xt = [k.sb(f"op_xa{i}", [128, D], F32, st) for i in range(2)]
        xu = [k.sb(f"op_xb{i}", [128, D], F32, st) for i in range(2)]
        yo = [k.sb(f"op_y{i}", [128, D], F32, st) for i in range(2)]
        ydeps = [Dep() for _ in range(NTC)] if cc is not None else [y.dep] * NTC

        def emit_cc(i):
            xs_out, xs_deps, groups = cc
            rs = slice(i * TCH, (i + 1) * TCH)
            k.collective(y[rs, :], xs_out[rs, :], reads=[ydeps[i]], writes=[xs_deps[i]], groups=groups)
        for tc in range(NTC):
            b = tc % 2
            tsl = slice(tc * TCH, (tc + 1) * TCH)
            k.dma("sp", mx[b][:], c.mixT[:, tsl].rearrange("(kc p) t -> p kc t", p=128), reads=[c.mixT], writes=[mx[b]])
            k.dma("sp", gt[b][:], pT[R_GATE:R_GATE + 1024, tsl].rearrange("(kc p) t -> p kc t", p=128), reads=[pT],
                  writes=[gt[b]])
            k.op("act", lambda e: e.activation(out=sg[:], in_=gt[b][:], func=AF.Sigmoid), reads=[gt[b]], writes=[sg])
            k.op("pool", lambda e: e.tensor_tensor(out=gt[b][:], in0=gt[b][:], in1=mx[b][:], op=ALU.mult),
                 reads=[gt[b], mx[b]], writes=[gt[b]])
            k.op("dve", lambda e: e.tensor_tensor(out=mg[b][:], in0=gt[b][:], in1=sg[:], op=ALU.mult),
                 reads=[gt[b], sg], writes=[mg[b]])
            for sub in range(4):
                tt_ = tc * 4 + sub
                xb_i = tt_ % 2
                rows = slice(tt_ * 128, (tt_ + 1) * 128)
                k.dma("sp", xt[xb_i][:], xa[rows, :], reads=[xa_deps[tc] if xa_deps else xa], writes=[xt[xb_i]])
                if xb is not None:
                    k.dma("sp", xu[xb_i][:], xb[rows, :], reads=[xb], writes=[xu[xb_i]])
                    k.op("pool", lambda e: e.tensor_tensor(out=xt[xb_i][:], in0=xt[xb_i][:], in1=xu[xb_i][:], op=ALU.add),
                         reads=[xt[xb_i], xu[xb_i]], writes=[xt[xb_i]])
                yb = yo[xb_i]
                for n in range(4):
                    ps = c.next_pf()
                    for kc in range(8):
                        k.op("pe", lambda e: e.matmul(ps[:, :], lhsT=mg[b][:, kc, sub * 128:(sub + 1) * 128],
                                                      rhs=wbf[:, kc, n * 512:(n + 1) * 512], start=(kc == 0), stop=(kc == 7)),
                             reads=[mg[b], wd[kc]], writes=[ps], inc=(kc == 7))
                    k.op("dve", lambda e: e.scalar_tensor_tensor(out=yb[:, n * 512:(n + 1) * 512], in0=xt[xb_i][:, n * 512:(n + 1) * 512],
                                                                 scalar=0.5, in1=ps[:, :], op0=ALU.mult, op1=ALU.add),
                         reads=[xt[xb_i], ps], writes=[yb])
                k.dma(STQ, y[rows, :], yb[:], reads=[yb], writes=[ydeps[tc]], waw=False)
            if cc is not None and tc >= 1:
                emit_cc(tc - 1)
        if cc is not None:
            emit_cc(NTC - 1)
    k.barrier()


def stage_final(k, c, xa, xb, g_bc_d, out, xa_deps=None):
    with ExitStack() as st:
        gbc = k.sb("f_gbc", [128, D], F32, st)
        k.dma("sp", gbc[:], g_bc_d.partition_broadcast(128), writes=[gbc])
        xt = [k.sb(f"f_xt{i}", [128, D], F32, st) for i in range(2)]
        xu = [k.sb(f"f_xu{i}", [128, D], F32, st) for i in range(2)]
        junk = k.sb("f_junk", [128, D], BF16, st)
        yo = [k.sb(f"f_y{i}", [128, D], F32, st) for i in range(2)]
        ss = [k.sb(f"f_ss{i}", [128, 4], F32, st) for i in range(2)]
        for tt_ in range(S // 128):
            b = tt_ % 2
            rows = slice(tt_ * 128, (tt_ + 1) * 128)
            k.dma("sp", xt[b][:], xa[rows, :], reads=[xa_deps[tt_ // 4] if xa_deps else xa], writes=[xt[b]])
            if xb is not None:
                k.dma("sp", xu[b][:], xb[rows, :], reads=[xb], writes=[xu[b]])
                k.op("pool", lambda e: e.tensor_tensor(out=xt[b][:], in0=xt[b][:], in1=xu[b][:], op=ALU.add),
                     reads=[xt[b], xu[b]], writes=[xt[b]])
            k.op("act", lambda e: e.activation(out=junk[:], in_=xt[b][:], func=AF.Square, accum_out=ss[b][:, 0:1]),
                 reads=[xt[b]], writes=[junk, ss[b]])
            k.op("dve", lambda e: e.tensor_scalar(out=ss[b][:, 1:2], in0=ss[b][:, 0:1], scalar1=1.0 / D, scalar2=EPS,
                                                  op0=ALU.mult, op1=ALU.add), reads=[ss[b]], writes=[ss[b]])
            k.op("act", lambda e: e.activation(out=ss[b][:, 2:3], in_=ss[b][:, 1:2], func=AF.Sqrt), reads=[ss[b]], writes=[ss[b]])
            k.op("dve", lambda e: e.reciprocal(out=ss[b][:, 3:4], in_=ss[b][:, 2:3]), reads=[ss[b]], writes=[ss[b]])
            k.op("dve", lambda e: e.scalar_tensor_tensor(out=yo[b][:], in0=xt[b][:], scalar=ss[b][:, 3:4], in1=gbc[:],
                                                         op0=ALU.mult, op1=ALU.mult), reads=[xt[b], ss[b], gbc], writes=[yo[b]])
            k.dma(STQ, out[rows, :], yo[b][:], reads=[yo[b]], writes=[out], waw=False)
    k.barrier()


class LayerIO:
    pass


def declare_layer_inputs(k, l):
    EI = "ExternalInput"
    L = LayerIO()
    L.col = PRM_COLS
    L.norm_g = k.dram(f"norm_g{l}", [1, D], F32, kind=EI)
    L.w_in = k.dram(f"w_in{l}", [D, NF + NV], F32, kind=EI)
    L.wuq = k.dram(f"wuq{l}", [512, 384], F32, kind=EI)
    L.wukv = k.dram(f"wukv{l}", [256, 512], F32, kind=EI)
    L.prm_d = k.dram(f"prm{l}", [128, _pc], F32, kind=EI)
    L.s5b = k.dram(f"s5b{l}", [128, 512], F32, kind=EI)
    L.s5c = k.dram(f"s5c{l}", [128, 512], F32, kind=EI)
    L.wglu = k.dram(f"wglu{l}", [512, 256], F32, kind=EI)
    L.w2a2 = k.dram(f"w2a2{l}", [128, 256], F32, kind=EI)
    L.wout = k.dram(f"wout{l}", [1024, D], F32, kind=EI)
    return L


def layer_input_arrays(inp, l, half, suffix):
    a = core_layer_arrays(inp, l, half)
    out = {
        f"norm_g{suffix}": np.ascontiguousarray(inp["norm_g"][l][None, :]),
        f"w_in{suffix}": a["w_in"], f"wuq{suffix}": a["wuq"], f"wukv{suffix}": a["wukv"], f"prm{suffix}": a["prm"],
        f"s5b{suffix}": a["s5b"], f"s5c{suffix}": a["s5c"], f"wglu{suffix}": a["wglu"], f"w2a2{suffix}": a["w2a2"],
    }
    rows = np.concatenate([b * 512 + half * 256 + np.arange(256) for b in range(4)])
    out[f"wout{suffix}"] = np.ascontiguousarray(inp["w_out"][l][rows, :])
    return out


def declare_scratch(k, c):
    c.hT = k.dram("sc_hT", [D, S], BF16)
    c.pT = k.dram("sc_pT", [NF, S], F32)
    c.pV = k.dram("sc_pV", [S, NV], F32)
    c.us5 = k.dram("sc_us5", [32, 128, 512], BF16)
    c.ys5 = k.dram("sc_ys5", [32, 128, 512], F32)
    c.gT = k.dram("sc_gT", [512, S], BF16)
    c.gF = k.dram("sc_gF", [256, S], F32)
    c.mixT = k.dram("sc_mixT", [1024, S], F32)
    c.cqnT = k.dram("sc_cqnT", [512, S], BF16)
    c.ckvnT = k.dram("sc_ckvnT", [256, S], BF16)
    c.qT = k.dram("sc_qT", [384, S], BF16)
    c.knT = k.dram("sc_knT", [256, S], BF16)
    c.vA = k.dram("sc_vA", [S, 256], BF16)
    c.krT = k.dram("sc_krT", [128, S], BF16)
    c.qdT = k.dram("sc_qdT", [256, S], BF16)
    c.kdT = k.dram("sc_kdT", [256, S], BF16)
    c.ropeA_cos = k.dram("sc_rAc", [128, S], F32)
    c.ropeA_sin = k.dram("sc_rAs", [128, S], F32)
    c.ropeD_cos = k.dram("sc_rDc", [128, S], F32)
    c.ropeD_sin = k.dram("sc_rDs", [128, S], F32)


def emit_layer(k, c, L, xa, xb, y, xa_deps=None, cc=None):
    with ExitStack() as st:
        L.prm = k.sb("prm_sb", [128, _pc], F32, st)
        k.dma("sp", L.prm[:], L.prm_d[:, :], writes=[L.prm])
        import os as _os
        sel = _os.environ.get("LAYER_STAGES", "nimsrdo")
        if "n" in sel:
            stage_norm(k, c, xa, xb, L.norm_g[0:1, :], c.hT, xa_deps)
            k.barrier()
        if "i" in sel:
            stage_inproj(k, c, c.hT, L.w_in, NF, NV, c.pT, c.pV)
        if "m" in sel:
            stage_mla(k, c, L)
        if "s" in sel:
            stage_s5(k, c, L)
        if "r" in sel:
            stage_rwkv(k, c, L)
        if "d" in sel:
            stage_diff(k, c, L)
        if "o" in sel:
            stage_outproj(k, c, L, xa, xb, y, xa_deps, cc)
        k.barrier()


def build_layer_program():
    nc = bass.Bass("TRN2", target_bir_lowering=False)
    k = KB(nc)
    c = Ctx(k)
    EI = "ExternalInput"
    cmat = k.dram("cmat", [128, 768], F32, kind=EI)
    cmask = k.dram("cmask", [128, 2048], F32, kind=EI)
    cst = k.dram("cst", [128, NCST], F32, kind=EI)
    cm2 = k.dram("cm2", [128, 2560], F32, kind=EI)
    pos = k.dram("pos", [1, S], I32, kind=EI)
    xa = k.dram("xa", [S, D], F32, kind=EI)
    xb = k.dram("xb", [S, D], F32, kind=EI)
    y = k.dram("y", [S, D], F32, kind="ExternalOutput")
    L = declare_layer_inputs(k, "")
    declare_scratch(k, c)
    load_all_consts(k, c, cmat, cmask, cst, cm2)
    make_rope_tables(k, c, pos, c.cst, 0, 1, c.ropeA_cos, c.ropeA_sin, "rtA")
    make_rope_tables(k, c, pos, c.cst, 2, 3, c.ropeD_cos, c.ropeD_sin, "rtD")
    emit_layer(k, c, L, xa, xb, y)
    k.finish()
    return nc


def build_final_program():
    nc = bass.Bass("TRN2", target_bir_lowering=False)
    k = KB(nc)
    c = Ctx(k)
    xa = k.dram("xa", [S, D], F32, kind="ExternalInput")
    xb = k.dram("xb", [S, D], F32, kind="ExternalInput")
    g = k.dram("fg", [1, D], F32, kind="ExternalInput")
    out = k.dram("out", [S, D], F32, kind="ExternalOutput")
    stage_final(k, c, xa, xb, g[0:1, :], out)
    k.finish()
    return nc


def const_inputs():
    cmat, cmask, cst = const_mats()
    return {"cmat": cmat, "cmask": cmask, "cst": cst, "cm2": const_mats2()}


def kernel_unfused(**inp):
    inp = {k_: np.asarray(v) for k_, v in inp.items()}
    x = inp["x"]
    B = x.shape[0]
    consts = const_inputs()
    ncl = build_layer_program()
    cur_a = [np.ascontiguousarray(x[cid // 2]) for cid in range(8)]
    cur_b = [np.zeros((S, D), np.float32) for _ in range(8)]
    for l in range(4):
        in_maps = []
        for cid in range(8):
            b, half = divmod(cid, 2)
            m = dict(consts)
            m["pos"] = np.ascontiguousarray(inp["positions"][b:b + 1].astype(np.int32))
            m["xa"] = cur_a[cid]
            m["xb"] = cur_b[cid]
            m.update(layer_input_arrays(inp, l, half, ""))
            in_maps.append(m)
        res = run_bass_kernel_spmd(ncl, in_maps, core_ids=list(range(8)))
        ys = [np.asarray(r["y"]) for r in res.results]
        cur_a = [ys[cid] for cid in range(8)]
        cur_b = [ys[cid ^ 1] for cid in range(8)]
    ncf = build_final_program()
    fg = np.ascontiguousarray(inp["final_norm_g"][None, :])
    in_maps = [{"xa": cur_a[cid], "xb": cur_b[cid], "fg": fg} for cid in range(8)]
    res = run_bass_kernel_spmd(ncf, in_maps, core_ids=list(range(8)))
    out = np.stack([np.asarray(res.results[2 * b]["out"]) for b in range(B)], axis=0)
    return out.astype(np.float32)


from concourse.bass_utils import run_bass_kernel_spmd


PAIR_GROUPS = [[0, 1], [2, 3], [4, 5], [6, 7]]
DEPTH = 4


def build_fused_program(depth=DEPTH):
    nc = bass.Bass("TRN2", target_bir_lowering=False)
    k = KB(nc)
    c = Ctx(k)
    EI = "ExternalInput"
    cmat = k.dram("cmat", [128, 768], F32, kind=EI)
    cmask = k.dram("cmask", [128, 2048], F32, kind=EI)
    cst = k.dram("cst", [128, NCST], F32, kind=EI)
    cm2 = k.dram("cm2", [128, 2560], F32, kind=EI)
    pos = k.dram("pos", [1, S], I32, kind=EI)
    x_in = k.dram("xa", [S, D], F32, kind=EI)
    fg = k.dram("fg", [1, D], F32, kind=EI)
    out = k.dram("out", [S, D], F32, kind="ExternalOutput")
    Ls = [declare_layer_inputs(k, l) for l in range(depth)]
    declare_scratch(k, c)
    ybuf = k.dram("sc_y", [S, D], F32)
    xs = [k.dram(f"sc_xs{i}", [S, D], F32) for i in range(2)]
    xs_deps = [[Dep() for _ in range(NTC)] for _ in range(2)]
    load_all_consts(k, c, cmat, cmask, cst, cm2)
    make_rope_tables(k, c, pos, c.cst, 0, 1, c.ropeA_cos, c.ropeA_sin, "rtA")
    make_rope_tables(k, c, pos, c.cst, 2, 3, c.ropeD_cos, c.ropeD_sin, "rtD")
    cur, cur_deps = x_in, None
    for l in range(depth):
        o = l % 2
        emit_layer(k, c, Ls[l], cur, None, ybuf, cur_deps, (xs[o], xs_deps[o], PAIR_GROUPS))
        cur, cur_deps = xs[o], xs_deps[o]
    stage_final(k, c, cur, None, fg[0:1, :], out, cur_deps)
    k.finish()
    return nc


def kernel_fused(**inp):
    inp = {k_: np.asarray(v) for k_, v in inp.items()}
    x = inp["x"]
    B = x.shape[0]
    consts = const_inputs()
    nc = build_fused_program()
    fg = np.ascontiguousarray(inp["final_norm_g"][None, :])
    in_maps = []
    for cid in range(8):
        b, half = divmod(cid, 2)
        m = dict(consts)
        m["pos"] = np.ascontiguousarray(inp["positions"][b:b + 1].astype(np.int32))
        m["xa"] = np.ascontiguousarray(x[b])
        m["fg"] = fg
        for l in range(DEPTH):
            m.update(layer_input_arrays(inp, l, half, str(l)))
        in_maps.append(m)
    res = run_bass_kernel_spmd(nc, in_maps, core_ids=list(range(8)))
    out = np.stack([np.asarray(res.results[2 * b]["out"]) for b in range(B)], axis=0)
    return out.astype(np.float32)


def kernel(**inputs):
    return kernel_fused(**inputs)
```

```python
from contextlib import ExitStack
import math
import numpy as np
import concourse.bass as bass
import concourse.mybir as mybir

F32 = mybir.dt.float32
BF16 = mybir.dt.bfloat16
I32 = mybir.dt.int32
AF = mybir.ActivationFunctionType
ALU = mybir.AluOpType
AX = mybir.AxisListType

NDS = 44
NDS_HW = 24
DQ_POOLS = {"sp": (0, 16), "act": (16, 30), "pool": (30, 44)}
STQ = "act"


class Dep:
    __slots__ = ("w", "r", "excl")

    def __init__(self):
        self.excl = False
        self.w = {}
        self.r = {}


class T:
    def __init__(self, t, dep=None):
        self.t = t
        self.dep = dep or Dep()

    def __getitem__(self, k):
        return self.t[k]

    def ap(self):
        return self.t.ap() if hasattr(self.t, "ap") else self.t[:]


def _deps(x):
    return x.dep if isinstance(x, T) else x


class KB:
    def __init__(self, nc):
        self.nc = nc
        self.es = ExitStack()
        self.eng = {"pe": nc.tensor, "act": nc.scalar, "dve": nc.vector,
                    "pool": nc.gpsimd, "sp": nc.sync}
        self.sem = {}
        for e in ("pe", "act", "dve", "pool"):
            self.sem[e] = self.es.enter_context(nc.semaphore("s_" + e))
        self.cnt = {e: 0 for e in self.sem}
        self.dsem = [self.es.enter_context(nc.semaphore(f"sd{i}")) for i in range(NDS)]
        self.dcnt = [0] * NDS
        self.dq_next = {}
        self.seen = {e: {} for e in self.eng}
        self.ccsem = self.es.enter_context(nc.semaphore("s_cc"))
        self.cccnt = 0
        self.ninst = 0
        self.scopes = []

    def sb(self, name, shape, dtype, stack=None):
        self.uid = getattr(self, "uid", 0) + 1
        name = f"{name}_u{self.uid}"
        t = (stack or self.es).enter_context(self.nc.sbuf_tensor(name, list(shape), dtype))
        return T(t)

    def ps(self, name, shape, dtype, stack=None):
        t = (stack or self.es).enter_context(self.nc.psum_tensor(name, list(shape), dtype))
        r = T(t)
        r.dep.excl = True
        return r

    def dram(self, name, shape, dtype, kind="Internal"):
        t = self.nc.dram_tensor(name, list(shape), dtype, kind=kind)
        return T(t)

    def _wait(self, E, tok):
        if tok is None:
            return
        kind, key, val = tok
        if kind == "e" and key == E and E in ("pe", "sp"):
            return
        sk = (kind, key)
        if self.seen[E].get(sk, 0) >= val:
            return
        sem = self.sem[key] if kind == "e" else (self.dsem[key] if kind == "d" else self.ccsem)
        self.eng[E].wait_ge(sem, val)
        self.ninst += 1
        self.seen[E][sk] = val

    def _collect(self, E, reads, writes, waw=True):
        for d in reads:
            d = _deps(d)
            for (kd, ky), v in list(d.w.items()):
                self._wait(E, (kd, ky, v))
            if d.excl:
                for (kd, ky), v in list(d.r.items()):
                    if ky != E:
                        self._wait(E, (kd, ky, v))
        for d in writes:
            d = _deps(d)
            if waw:
                for (kd, ky), v in list(d.w.items()):
                    self._wait(E, (kd, ky, v))
            for (kd, ky), v in list(d.r.items()):
                self._wait(E, (kd, ky, v))

    def _update(self, tok, reads, writes, waw=True):
        kk = (tok[0], tok[1])
        for d in writes:
            d = _deps(d)
            if waw:
                d.w = {kk: tok[2]}
                d.r = {}
            else:
                d.w[kk] = max(d.w.get(kk, 0), tok[2])
        for d in reads:
            d = _deps(d)
            d.r[kk] = max(d.r.get(kk, 0), tok[2])

    def op(self, E, fn, reads=(), writes=(), inc=True):
        self._collect(E, reads, writes)
        inst = fn(self.eng[E])
        self.ninst += 1
        if inc:
            self.cnt[E] += 1
            inst.then_inc(self.sem[E], 1)
            tok = ("e", E, self.cnt[E])
        else:
            tok = ("e", E, self.cnt[E] + 1)
        self._update(tok, reads, writes)
        return inst

    def dma(self, Q, out, in_, reads=(), writes=(), waw=True, **kw):
        self._collect(Q, reads, writes, waw)
        lo, hi = DQ_POOLS[Q]
        i = lo + self.dq_next.get(Q, 0)
        self.dq_next[Q] = (self.dq_next.get(Q, 0) + 1) % (hi - lo)
        if self.dcnt[i] > 0:
            self._wait(Q, ("d", i, 16 * self.dcnt[i]))
        inst = self.eng[Q].dma_start(out=out, in_=in_, **kw)
        inst.then_inc(self.dsem[i], 16)
        self.ninst += 1
        self.dcnt[i] += 1
        tok = ("d", i, 16 * self.dcnt[i])
        self._update(tok, reads, writes, waw)
        return tok

    def collective(self, in_ap, out_ap, reads=(), writes=(), groups=None):
        self._collect("pool", reads, writes, True)
        inst = self.nc.gpsimd.collective_compute("AllReduce", ALU.add, replica_groups=groups,
                                                 ins=[in_ap], outs=[out_ap])
        self.cccnt += 1
        inst.then_inc(self.ccsem, 1)
        self.ninst += 1
        tok = ("c", 0, self.cccnt)
        self._update(tok, reads, writes, True)
        return tok

    def barrier(self, engines=("pe", "act", "dve", "pool", "sp")):
        for E in engines:
            if self.cccnt > 0:
                self._wait(E, ("c", 0, self.cccnt))
            for p in self.sem:
                if p != E and self.cnt[p] > 0:
                    self._wait(E, ("e", p, self.cnt[p]))
            for i in range(NDS):
                if self.dcnt[i] > 0:
                    self._wait(E, ("d", i, 16 * self.dcnt[i]))
        for E in ("act", "dve", "pool"):
            if E in engines and self.cnt[E] > 0:
                self._wait(E, ("e", E, self.cnt[E]))

    def finish(self):
        self.barrier(engines=("sp",))


S = 4096
D = 2048
TCH = 512
NTC = S // TCH
EPS = 1e-6


class Ctx:
    def __init__(self, k):
        self.k = k
        self.pf = [k.ps(f"pf{i}", [128, 512], F32) for i in range(6)]
        self.pb = [k.ps(f"pb{i}", [128, 1024], BF16) for i in range(2)]
        self.pfi = 0
        self.pbi = 0
        self.rr = 0
        self.us5 = None

    def next_pf(self, lo=0, hi=6):
        n = hi - lo
        p = self.pf[lo + (self.pfi % n)]
        self.pfi += 1
        return p

    def next_pb(self):
        p = self.pb[self.pbi % 2]
        self.pbi += 1
        return p

    def evac_eng(self):
        self.rr += 1
        return "act" if self.rr % 2 else "dve"


def copy_op(k, E, out, in_, reads, writes):
    if E == "act":
        k.op("act", lambda e: e.copy(out=out, in_=in_), reads=reads, writes=writes)
    else:
        k.op(E, lambda e: e.tensor_copy(out=out, in_=in_), reads=reads, writes=writes)


def load_consts(k, c, ident_d, stack):
    idf = k.sb("idf", [128, 128], F32, stack)
    c.ident = k.sb("c_ident", [128, 128], BF16)
    c.ones = k.sb("c_ones", [128, 128], BF16)
    k.dma("sp", idf[:], ident_d[:, :], writes=[idf])
    k.op("dve", lambda e: e.tensor_copy(out=c.ident[:], in_=idf[:]), reads=[idf], writes=[c.ident])
    k.op("dve", lambda e: e.memset(c.ones[:], 1.0), writes=[c.ones])


def stage_norm(k, c, xa, xb, g_bc_d, hT, xa_deps=None):
    with ExitStack() as st:
        gbc = k.sb("n_gbc", [128, D], F32, st)
        k.dma("sp", gbc[:], g_bc_d.partition_broadcast(128), writes=[gbc])
        xt = [k.sb(f"n_xt{i}", [128, D], F32, st) for i in range(2)]
        xt2 = [k.sb(f"n_xu{i}", [128, D], F32, st) for i in range(2)]
        junk = k.sb("n_junk", [128, D], BF16, st)
        xn = [k.sb(f"n_xn{i}", [128, D], BF16, st) for i in range(2)]
        ss = [k.sb(f"n_ss{i}", [128, 4], F32, st) for i in range(2)]
        hst = [k.sb(f"n_hst{i}", [128, 16, TCH], BF16, st) for i in range(2)]
        for tt in range(S // 128):
            b = tt % 2
            tcn, tl = divmod(tt, 4)
            hs = hst[tcn % 2]
            rows = slice(tt * 128, (tt + 1) * 128)
            k.dma("sp", xt[b][:], xa[rows, :], reads=[xa_deps[tt // 4] if xa_deps else xa], writes=[xt[b]])
            if xb is not None:
                k.dma("sp", xt2[b][:], xb[rows, :], reads=[xb], writes=[xt2[b]])
                k.op("pool", lambda e: e.tensor_tensor(out=xt[b][:], in0=xt[b][:], in1=xt2[b][:], op=ALU.add),
                     reads=[xt[b], xt2[b]], writes=[xt[b]])
            k.op("act", lambda e: e.activation(out=junk[:], in_=xt[b][:], func=AF.Square,
                                               accum_out=ss[b][:, 0:1]),
                 reads=[xt[b]], writes=[junk, ss[b]])
            k.op("dve", lambda e: e.tensor_scalar(out=ss[b][:, 1:2], in0=ss[b][:, 0:1], scalar1=1.0 / D,
                                                  scalar2=EPS, op0=ALU.mult, op1=ALU.add),
                 reads=[ss[b]], writes=[ss[b]])
            k.op("act", lambda e: e.activation(out=ss[b][:, 2:3], in_=ss[b][:, 1:2], func=AF.Sqrt),
                 reads=[ss[b]], writes=[ss[b]])
            k.op("dve", lambda e: e.reciprocal(out=ss[b][:, 3:4], in_=ss[b][:, 2:3]),
                 reads=[ss[b]], writes=[ss[b]])
            k.op("dve", lambda e: e.scalar_tensor_tensor(out=xn[b][:], in0=xt[b][:], scalar=ss[b][:, 3:4],
                                                         in1=gbc[:], op0=ALU.mult, op1=ALU.mult),
                 reads=[xt[b], ss[b], gbc], writes=[xn[b]])
            for half in range(2):
                pb = c.next_pb()
                for j in range(8):
                    kc = half * 8 + j
                    k.op("pe", lambda e: e.transpose(pb[:, j * 128:(j + 1) * 128],
                                                     xn[b][:, kc * 128:(kc + 1) * 128], c.ident[:]),
                         reads=[xn[b], c.ident], writes=[pb], inc=(j == 7))
                k.op("act", lambda e: e.copy(out=hs[:, half * 8:(half + 1) * 8, tl * 128:(tl + 1) * 128],
                                             in_=pb[:].rearrange("p (a b) -> p a b", a=8)),
                     reads=[pb], writes=[hs])
            if tl == 3:
                k.dma(STQ, hT[:, tcn * TCH:(tcn + 1) * TCH].rearrange("(kc p) t -> p kc t", p=128),
                      hs[:], reads=[hs], writes=[hT], waw=False)


def load_w_bf16(k, st, w_ap_fn, KC, N, name, nstg=3):
    wbf = k.sb(name, [128, KC, N], BF16, st)
    stg = [k.sb(f"{name}_s{i}", [128, N], F32, st) for i in range(nstg)]
    deps = [Dep() for _ in range(KC)]
    for kc in range(KC):
        s = stg[kc % nstg]
        k.dma("sp", s[:], w_ap_fn(kc), writes=[s])
        E = ("dve", "act", "dve", "pool")[kc % 4]
        copy_op(k, E, wbf[:, kc, :], s[:], [s], [deps[kc]])
    return wbf, deps


def linear_fm(k, c, st, inT, KC, wbf, wdeps, n_tiles, epilogue, name, in_cast=False):
    xin = [k.sb(f"{name}_x{i}", [128, KC, TCH], BF16, st) for i in range(2)]
    for tc in range(NTC):
        xi = xin[tc % 2]
        k.dma("sp", xi[:], inT[:, tc * TCH:(tc + 1) * TCH].rearrange("(kc p) t -> p kc t", p=128),
              reads=[inT], writes=[xi])
        for ni, (c0, ncol) in enumerate(n_tiles):
            ps = c.next_pf()
            for kc in range(KC):
                k.op("pe", lambda e: e.matmul(ps[:ncol, :], lhsT=wbf[:, kc, c0:c0 + ncol], rhs=xi[:, kc, :],
                                              start=(kc == 0), stop=(kc == KC - 1)),
                     reads=[wdeps[kc], xi], writes=[ps], inc=(kc == KC - 1))
            epilogue(tc, ni, ps, ncol)


def linear_tm(k, c, st, inT, KC, wbf, wdeps, c0, N, epilogue, name):
    xin = [k.sb(f"{name}_x{i}", [128, KC, TCH], BF16, st) for i in range(2)]
    for tc in range(NTC):
        xi = xin[tc % 2]
        k.dma("sp", xi[:], inT[:, tc * TCH:(tc + 1) * TCH].rearrange("(kc p) t -> p kc t", p=128),
              reads=[inT], writes=[xi])
        for sub in range(4):
            ps = c.next_pf()
            for kc in range(KC):
                k.op("pe", lambda e: e.matmul(ps[:, :N], lhsT=xi[:, kc, sub * 128:(sub + 1) * 128],
                                              rhs=wbf[:, kc, c0:c0 + N],
                                              start=(kc == 0), stop=(kc == KC - 1)),
                     reads=[wdeps[kc], xi], writes=[ps], inc=(kc == KC - 1))
            epilogue(tc * 4 + sub, ps)


class Stager:
    def __init__(self, k, st, name, shape, dtype, n=4):
        self.k = k
        self.bufs = [k.sb(f"{name}{i}", shape, dtype, st) for i in range(n)]
        self.i = 0

    def next(self):
        b = self.bufs[self.i % len(self.bufs)]
        self.i += 1
        return b


def epi_store_fm(k, c, stg, outT, row0_of):
    def epi(tc, ni, ps, ncol):
        s = stg.next()
        copy_op(k, c.evac_eng(), s[:ncol, :], ps[:ncol, :], [ps], [s])
        r0 = row0_of(ni)
        k.dma(STQ, outT[r0:r0 + ncol, tc * TCH:(tc + 1) * TCH], s[:ncol, :], reads=[s], writes=[outT], waw=False)
        return s
    return epi


def stage_inproj(k, c, hT, w_in_d, NF, NV, pT, pV):
    KC = D // 128
    tiles = []
    c0 = 0
    while c0 < NF:
        tiles.append((c0, min(128, NF - c0)))
        c0 += 128
    half = (len(tiles) + 1) // 2
    groups = [tiles[:half], tiles[half:]]
    for gi, grp in enumerate(groups):
        with ExitStack() as st:
            g0 = grp[0][0]
            gN = grp[-1][0] + grp[-1][1] - g0
            wbf, wd = load_w_bf16(k, st, lambda kc: w_in_d[kc * 128:(kc + 1) * 128, g0:g0 + gN], KC, gN, f"ip_w{gi}")
            stg = Stager(k, st, f"ip_o{gi}_", [128, TCH], F32, 4)
            rel = [(a - g0, b) for a, b in grp]
            base_epi = epi_store_fm(k, c, stg, pT, lambda ni: grp[ni][0])
            stu = Stager(k, st, f"ip_u{gi}_", [128, 8, 64], BF16, 3)

            def epi(tc, ni, ps, ncol, grp=grp, base_epi=base_epi, stu=stu):
                sst = base_epi(tc, ni, ps, ncol)
                r0 = grp[ni][0]
                import os as _os
                if R_U <= r0 < R_U + 512 and c.us5 is not None and _os.environ.get('NOHOOK') != '1':
                    su = stu.next()
                    k.op("pool", lambda e: e.tensor_copy(out=su[:],
                                                         in_=sst[:, :].rearrange("p (j t) -> p t j", t=8)),
                         reads=[sst], writes=[su])
                    g0 = (r0 - R_U) // 16
                    hq = _os.environ.get("HOOKDMA", STQ)
                    for gl in range(8 if hq != "none" else 0):
                        k.dma(hq, c.us5[g0 + gl, :, tc * 64:(tc + 1) * 64].rearrange("(t c) j -> c t j", c=16),
                              su[gl * 16:(gl + 1) * 16, :, :], reads=[su], writes=[c.us5], waw=False)
            linear_fm(k, c, st, hT, KC, wbf, wd, rel, epi, f"ip{gi}")
        k.barrier()
    with ExitStack() as st:
        wbf, wd = load_w_bf16(k, st, lambda kc: w_in_d[kc * 128:(kc + 1) * 128, NF:NF + NV], KC, NV, "ip_wv")
        stg = Stager(k, st, "ip_ov_", [128, NV], F32, 4)

        def epi(tt, ps):
            s = stg.next()
            copy_op(k, c.evac_eng(), s[:, :], ps[:, :NV], [ps], [s])
            k.dma(STQ, pV[tt * 128:(tt + 1) * 128, :], s[:, :], reads=[s], writes=[pV], waw=False)
        linear_tm(k, c, st, hT, KC, wbf, wd, 0, NV, epi, "ipv")
    k.barrier()


TWO_PI = 2.0 * np.pi
CW1 = 6.28125
CW2 = float(np.float32(TWO_PI - 6.28125))
CW3 = float(TWO_PI - 6.28125 - float(np.float32(TWO_PI - 6.28125)))


def make_rope_tables(k, c, pos_d, cst, col_inv, col_sgn, cosT, sinT, name):
    HS = S // 2
    with ExitStack() as st:
        posi = k.sb(name + "_pi", [128, HS], I32, st)
        ang = k.sb(name + "_ang", [128, HS], F32, st)
        a2 = k.sb(name + "_a2", [128, HS], F32, st)
        ni = k.sb(name + "_ni", [128, HS], I32, st)
        nf = k.sb(name + "_nf", [128, HS], F32, st)
        r = k.sb(name + "_r", [128, HS], F32, st)
        m = k.sb(name + "_m", [128, HS], F32, st)
        o = k.sb(name + "_o", [128, HS], F32, st)
        for hh in range(2):
            sl = slice(hh * HS, (hh + 1) * HS)
            k.dma("sp", posi[:], pos_d[0:1, sl].partition_broadcast(128), writes=[posi])
            k.op("dve", lambda e: e.tensor_copy(out=ang[:], in_=posi[:]), reads=[posi], writes=[ang])
            k.op("dve", lambda e: e.tensor_scalar(out=ang[:], in0=ang[:], scalar1=cst[:, col_inv:col_inv + 1],
                                                  scalar2=None, op0=ALU.mult), reads=[ang, cst], writes=[ang])
            for which, shift, dst in (("s", 0.0, sinT), ("c", np.pi / 2, cosT)):
                if which == "s":
                    k.op("dve", lambda e: e.tensor_scalar(out=ni[:], in0=ang[:], scalar1=float(1.0 / TWO_PI),
                                                          scalar2=None, op0=ALU.mult), reads=[ang], writes=[ni])
                    k.op("dve", lambda e: e.tensor_copy(out=nf[:], in_=ni[:]), reads=[ni], writes=[nf])
                    k.op("dve", lambda e: e.scalar_tensor_tensor(out=a2[:], in0=nf[:], scalar=-CW1, in1=ang[:],
                                                                 op0=ALU.mult, op1=ALU.add), reads=[nf, ang], writes=[a2])
                    k.op("dve", lambda e: e.scalar_tensor_tensor(out=a2[:], in0=nf[:], scalar=-CW2, in1=a2[:],
                                                                 op0=ALU.mult, op1=ALU.add), reads=[nf, a2], writes=[a2])
                    k.op("dve", lambda e: e.scalar_tensor_tensor(out=a2[:], in0=nf[:], scalar=-CW3, in1=a2[:],
                                                                 op0=ALU.mult, op1=ALU.add), reads=[nf, a2], writes=[a2])
                k.op("dve", lambda e: e.tensor_scalar(out=r[:], in0=a2[:], scalar1=float(shift), scalar2=None,
                                                      op0=ALU.add), reads=[a2], writes=[r])
                k.op("dve", lambda e: e.tensor_single_scalar(out=m[:], in_=r[:], scalar=float(np.pi), op=ALU.is_gt),
                     reads=[r], writes=[m])
                k.op("dve", lambda e: e.scalar_tensor_tensor(out=r[:], in0=m[:], scalar=-TWO_PI, in1=r[:],
                                                             op0=ALU.mult, op1=ALU.add), reads=[m, r], writes=[r])
                k.op("dve", lambda e: e.tensor_single_scalar(out=m[:], in_=r[:], scalar=float(-np.pi), op=ALU.is_lt),
                     reads=[r], writes=[m])
                k.op("dve", lambda e: e.scalar_tensor_tensor(out=r[:], in0=m[:], scalar=TWO_PI, in1=r[:],
                                                             op0=ALU.mult, op1=ALU.add), reads=[m, r], writes=[r])
                k.op("dve", lambda e: e.tensor_scalar(out=r[:], in0=r[:], scalar1=float(np.pi), scalar2=float(-np.pi),
                                                      op0=ALU.min, op1=ALU.max), reads=[r], writes=[r])
                k.op("act", lambda e: e.activation(out=o[:], in_=r[:], func=AF.Sin), reads=[r], writes=[o])
                if which == "s":
                    k.op("dve", lambda e: e.tensor_scalar(out=o[:], in0=o[:], scalar1=cst[:, col_sgn:col_sgn + 1],
                                                          scalar2=None, op0=ALU.mult), reads=[o, cst], writes=[o])
                k.dma("sp", dst[:, sl], o[:], reads=[o], writes=[dst], waw=False)
    k.barrier()


def rmsnorm_fm(k, c, srcT, r0, nt, g_sb, gcol0, dstT, eps, name):
    n = nt * 128
    with ExitStack() as st:
        xin = [k.sb(f"{name}_x{i}", [128, nt, TCH], F32, st) for i in range(2)]
        sq = [k.sb(f"{name}_q{i}", [128, nt, TCH], BF16, st) for i in range(2)]
        rs = [k.sb(f"{name}_r{i}", [128, TCH], F32, st) for i in range(2)]
        ob = [k.sb(f"{name}_o{i}", [128, nt, TCH], BF16, st) for i in range(2)]
        for tc in range(NTC):
            b = tc % 2
            tsl = slice(tc * TCH, (tc + 1) * TCH)
            k.dma("sp", xin[b][:], srcT[r0:r0 + n, tsl].rearrange("(t p) s -> p t s", p=128),
                  reads=[srcT], writes=[xin[b]])
            k.op("act", lambda e: e.activation(out=sq[b][:], in_=xin[b][:], func=AF.Square),
                 reads=[xin[b]], writes=[sq[b]])
            ps = c.next_pf()
            for t in range(nt):
                k.op("pe", lambda e: e.matmul(ps[:, :], lhsT=c.ones[:], rhs=sq[b][:, t, :],
                                              start=(t == 0), stop=(t == nt - 1)),
                     reads=[c.ones, sq[b]], writes=[ps], inc=(t == nt - 1))
            k.op("dve", lambda e: e.tensor_scalar(out=rs[b][:], in0=ps[:, :], scalar1=1.0 / n, scalar2=float(eps),
                                                  op0=ALU.mult, op1=ALU.add), reads=[ps], writes=[rs[b]])
            k.op("act", lambda e: e.activation(out=rs[b][:], in_=rs[b][:], func=AF.Ln),
                 reads=[rs[b]], writes=[rs[b]])
            k.op("act", lambda e: e.activation(out=rs[b][:], in_=rs[b][:], func=AF.Exp, scale=-0.5),
                 reads=[rs[b]], writes=[rs[b]])
            for t in range(nt):
                k.op("dve", lambda e: e.scalar_tensor_tensor(out=ob[b][:, t, :], in0=xin[b][:, t, :],
                                                             scalar=g_sb[:, gcol0 + t:gcol0 + t + 1], in1=rs[b][:],
                                                             op0=ALU.mult, op1=ALU.mult),
                     reads=[xin[b], g_sb, rs[b]], writes=[ob[b]])
            k.dma(STQ, dstT[0:n, tsl].rearrange("(t p) s -> p t s", p=128), ob[b][:],
                  reads=[ob[b]], writes=[dstT], waw=False)
    k.barrier()


def rope_tile(k, c, st_bufs, src_ap, src_dep, perm, cos_sb, sin_sb, out_ap, out_dep):
    xb, xf, t1 = st_bufs["xb"], st_bufs["xf"], st_bufs["t1"]
    k.op("act", lambda e: e.copy(out=xf[:], in_=src_ap), reads=[src_dep], writes=[xf])
    k.op("dve", lambda e: e.tensor_copy(out=xb[:], in_=xf[:]), reads=[xf], writes=[xb])
    ps = c.next_pf()
    k.op("pe", lambda e: e.matmul(ps[:, :], lhsT=perm[:], rhs=xb[:], start=True, stop=True),
         reads=[perm, xb], writes=[ps])
    k.op("dve", lambda e: e.tensor_tensor(out=t1[:], in0=ps[:, :], in1=sin_sb, op=ALU.mult),
         reads=[ps, st_bufs["tab"]], writes=[t1])
    k.op("pool", lambda e: e.tensor_tensor(out=xf[:], in0=xf[:], in1=cos_sb, op=ALU.mult),
         reads=[xf, st_bufs["tab"]], writes=[xf])
    k.op("dve", lambda e: e.tensor_tensor(out=out_ap, in0=xf[:], in1=t1[:], op=ALU.add),
         reads=[xf, t1], writes=[out_dep])


def attention_head(k, c, st, name, maps, Vsb, vdep, dv, scale, masks, post):
    raise NotImplementedError


def attn_qchunk(k, c, j, maps, Vfn, vdep, dv, scale, masks, ptbufs, acc_banks):
    nkt = 4 * j + 4
    items = [(mi, kt) for mi in range(len(maps)) for kt in range(nkt)]
    tri = masks[0]

    def c0_of(kt):
        return 128 * (kt - 4 * j) if kt >= 4 * j else 0

    def emit_qk(i):
        mi, kt = items[i]
        parts = maps[mi]
        ps = c.pf[i % 2]
        c0 = c0_of(kt)
        for pi, p in enumerate(parts):
            k.op("pe", lambda e: e.matmul(ps[:, c0:TCH], lhsT=p["K"](kt), rhs=p["Q"](c0),
                                          start=(pi == 0), stop=(pi == len(parts) - 1)),
                 reads=[p["kd"], p["qd"]], writes=[ps], inc=(pi == len(parts) - 1))

    emit_qk(0)
    for i, (mi, kt) in enumerate(items):
        if i + 1 < len(items):
            emit_qk(i + 1)
        oacc, sacc = acc_banks[mi]
        ps = c.pf[i % 2]
        c0 = c0_of(kt)
        pt = ptbufs[i % len(ptbufs)]
        k.op("act", lambda e: e.activation(out=pt[:, c0:TCH], in_=ps[:, c0:TCH], func=AF.Exp, scale=float(scale)),
             reads=[ps], writes=[pt])
        if kt >= 4 * j:
            k.op("dve", lambda e: e.tensor_tensor(out=pt[:, c0:c0 + 128], in0=pt[:, c0:c0 + 128], in1=tri[:, 0:128],
                                                  op=ALU.mult), reads=[pt, tri], writes=[pt])
        k.op("pe", lambda e: e.matmul(oacc[:dv, c0:TCH], lhsT=Vfn(kt), rhs=pt[:, c0:TCH],
                                      start=(kt == 0), stop=(kt == nkt - 1)),
             reads=[vdep, pt], writes=[oacc], inc=False)
        k.op("pe", lambda e: e.matmul(sacc[:, c0:TCH], lhsT=c.ones[:], rhs=pt[:, c0:TCH],
                                      start=(kt == 0), stop=(kt == nkt - 1)),
             reads=[c.ones, pt], writes=[sacc], inc=True)


R_CQ, R_CKV, R_U, R_R, R_K, R_QD, R_KD, R_GATE, R_KROPE, R_LORA = 0, 512, 768, 1280, 1536, 1792, 2048, 2304, 3328, 3456
NF = 3840
NV = 256


class RopeCtx:
    def __init__(self, k, c, st, name, cosT, sinT, perm):
        self.k, self.c = k, c
        self.cosT, self.sinT, self.perm = cosT, sinT, perm
        self.tab = [k.sb(f"{name}_tab{i}", [128, 2, TCH], F32, st) for i in range(2)]
        self.xb = [k.sb(f"{name}_xb{i}", [128, TCH], BF16, st) for i in range(2)]
        self.xf = [k.sb(f"{name}_xf{i}", [128, TCH], F32, st) for i in range(2)]
        self.t1 = [k.sb(f"{name}_t1{i}", [128, TCH], F32, st) for i in range(2)]
        self.cur = None
        self.n = 0

    def load(self, tc):
        k = self.k
        tb = self.tab[tc % 2]
        tsl = slice(tc * TCH, (tc + 1) * TCH)
        k.dma("sp", tb[:, 0, :], self.cosT[:, tsl], reads=[self.cosT], writes=[tb])
        k.dma("sp", tb[:, 1, :], self.sinT[:, tsl], reads=[self.cosT], writes=[tb], waw=False)
        self.cur = tb

    def apply(self, src_ap, src_dep, out_ap, out_dep):
        k, c = self.k, self.c
        i = self.n % 2
        self.n += 1
        xb, xf, t1, tb = self.xb[i], self.xf[i], self.t1[i], self.cur
        k.op("act", lambda e: e.copy(out=xf[:], in_=src_ap), reads=[src_dep], writes=[xf])
        k.op("dve", lambda e: e.tensor_copy(out=xb[:], in_=xf[:]), reads=[xf], writes=[xb])
        ps = c.next_pf(0, 2)
        k.op("pe", lambda e: e.matmul(ps[:, :], lhsT=self.perm[:], rhs=xb[:], start=True, stop=True),
             reads=[self.perm, xb], writes=[ps])
        k.op("dve", lambda e: e.tensor_tensor(out=t1[:], in0=ps[:, :], in1=tb[:, 1, :], op=ALU.mult),
             reads=[ps, tb], writes=[t1])
        k.op("pool", lambda e: e.tensor_tensor(out=xf[:], in0=xf[:], in1=tb[:, 0, :], op=ALU.mult),
             reads=[xf, tb], writes=[xf])
        k.op("dve", lambda e: e.tensor_tensor(out=out_ap, in0=xf[:], in1=t1[:], op=ALU.add),
             reads=[xf, t1], writes=[out_dep])


def stage_mla(k, c, L):
    pT = c.pT
    rmsnorm_fm(k, c, pT, R_CQ, 4, L.prm, L.col["gq"], c.cqnT, EPS, "nq")
    rmsnorm_fm(k, c, pT, R_CKV, 2, L.prm, L.col["gkv"], c.ckvnT, EPS, "nkv")
    with ExitStack() as st:
        wbf, wd = load_w_bf16(k, st, lambda kc: L.wuq[kc * 128:(kc + 1) * 128, :], 4, 384, "wuq")
        stg = Stager(k, st, "mq_o", [128, TCH], BF16, 4)
        rp = RopeCtx(k, c, st, "mqr", c.ropeA_cos, c.ropeA_sin, c.permA)

        def epi(tc, ni, ps, ncol):
            s = stg.next()
            if ni < 2:
                copy_op(k, c.evac_eng(), s[:, :], ps[:, :], [ps], [s])
            else:
                rp.load(tc)
                rp.apply(ps[:, :], ps, s[:, :], s)
            k.dma(STQ, c.qT[ni * 128:(ni + 1) * 128, tc * TCH:(tc + 1) * TCH], s[:, :], reads=[s],
                  writes=[c.qT], waw=False)
        linear_fm(k, c, st, c.cqnT, 4, wbf, wd, [(0, 128), (128, 128), (256, 128)], epi, "mq")
    k.barrier()
    with ExitStack() as st:
        wbf, wd = load_w_bf16(k, st, lambda kc: L.wukv[kc * 128:(kc + 1) * 128, :], 2, 512, "wukv")
        stg = Stager(k, st, "mk_o", [128, TCH], BF16, 4)
        epi = epi_store_fm(k, c, stg, c.knT, lambda ni: ni * 128)
        linear_fm(k, c, st, c.ckvnT, 2, wbf, wd, [(0, 128), (128, 128)], epi, "mk")
        stgv = Stager(k, st, "mv_o", [128, 256], BF16, 4)

        def epiv(tt, ps):
            s = stgv.next()
            copy_op(k, c.evac_eng(), s[:, :], ps[:, :256], [ps], [s])
            k.dma(STQ, c.vA[tt * 128:(tt + 1) * 128, :], s[:, :], reads=[s], writes=[c.vA], waw=False)
        linear_tm(k, c, st, c.ckvnT, 2, wbf, wd, 256, 256, epiv, "mv")
        rp = RopeCtx(k, c, st, "mkr", c.ropeA_cos, c.ropeA_sin, c.permA)
        kin = [k.sb(f"mkr_in{i}", [128, TCH], F32, st) for i in range(2)]
        for tc in range(NTC):
            tsl = slice(tc * TCH, (tc + 1) * TCH)
            ki = kin[tc % 2]
            k.dma("sp", ki[:], pT[R_KROPE:R_KROPE + 128, tsl], reads=[pT], writes=[ki])
            rp.load(tc)
            s = stg.next()
            rp.apply(ki[:], ki, s[:, :], s)
            k.dma(STQ, c.krT[:, tsl], s[:, :], reads=[s], writes=[c.krT], waw=False)
    k.barrier()
    scale = (128 + 64) ** -0.5
    for h in range(2):
        with ExitStack() as st:
            Kn = k.sb("ma_kn", [128, S], BF16, st)
            Kr = k.sb("ma_kr", [128, S], BF16, st)
            Vs = k.sb("ma_v", [128, S // 128, 128], BF16, st)
            k.dma("sp", Kn[:], c.knT[h * 128:(h + 1) * 128, :], reads=[c.knT], writes=[Kn])
            k.dma("sp", Kr[:], c.krT[:, :], reads=[c.krT], writes=[Kr])
            k.dma("sp", Vs[:], c.vA[:, h * 128:(h + 1) * 128].rearrange("(kt p) d -> p kt d", p=128),
                  reads=[c.vA], writes=[Vs])
            Qn = [k.sb(f"ma_qn{i}", [128, TCH], BF16, st) for i in range(2)]
            Qr = [k.sb(f"ma_qr{i}", [128, TCH], BF16, st) for i in range(2)]
            ptb = [k.sb(f"ma_pt{i}", [128, TCH], BF16, st) for i in range(3)]
            rec = [k.sb(f"ma_rec{i}", [128, TCH], F32, st) for i in range(2)]
            ost = [k.sb(f"ma_o{i}", [128, TCH], F32, st) for i in range(2)]
            hs = slice(h * 64, (h + 1) * 64)
            for j in range(NTC):
                b = j % 2
                tsl = slice(j * TCH, (j + 1) * TCH)
                k.dma("sp", Qn[b][:], c.qT[h * 128:(h + 1) * 128, tsl], reads=[c.qT], writes=[Qn[b]])
                k.dma("sp", Qr[b][:], c.qT[256:384, tsl], reads=[c.qT], writes=[Qr[b]])
                parts = [dict(K=lambda kt: Kn[:, kt * 128:(kt + 1) * 128], Q=lambda c0: Qn[b][:, c0:TCH], kd=Kn, qd=Qn[b]),
                         dict(K=lambda kt: Kr[hs, kt * 128:(kt + 1) * 128], Q=lambda c0: Qr[b][hs, c0:TCH], kd=Kr, qd=Qr[b])]
                oacc, sacc = c.pf[2 + 2 * b], c.pf[3 + 2 * b]
                attn_qchunk(k, c, j, [parts], lambda kt: Vs[:, kt, :], Vs, 128, scale, c.masks, ptb, [(oacc, sacc)])
                k.op("act", lambda e: e.activation(out=rec[b][:], in_=sacc[:, :], func=AF.Ln), reads=[sacc], writes=[rec[b]])
                k.op("act", lambda e: e.activation(out=rec[b][:], in_=rec[b][:], func=AF.Exp, scale=-1.0), reads=[rec[b]], writes=[rec[b]])
                k.op("dve", lambda e: e.tensor_tensor(out=ost[b][:], in0=oacc[:, :], in1=rec[b][:], op=ALU.mult),
                     reads=[oacc, rec[b]], writes=[ost[b]])
                k.dma(STQ, c.mixT[h * 128:(h + 1) * 128, tsl], ost[b][:], reads=[ost[b]], writes=[c.mixT],
                      waw=False)
        k.barrier()


PRM_COLS = {}
_pc = 0


def _reg(name, n):
    global _pc
    PRM_COLS[name] = _pc
    _pc += n


_reg("gq", 4)
_reg("gkv", 2)


def const_mats():
    ident = np.eye(128, dtype=np.float32)
    permA = np.zeros((128, 128), np.float32)
    for r in range(128):
        d = r % 64
        permA[r, r + 32 if d < 32 else r - 32] = 1.0
    permD = np.zeros((128, 128), np.float32)
    for r in range(128):
        d = r % 64
        if d < 8:
            permD[r, r + 8] = 1.0
        elif d < 16:
            permD[r, r - 8] = 1.0
    masks = np.zeros((128, 4, 512), np.float32)
    kk = np.arange(128)[:, None]
    qq = np.arange(512)[None, :]
    for r in range(4):
        masks[:, r, :] = (qq >= 128 * r + kk)
    tmask = np.zeros((128, 128), np.float32)
    ti = np.arange(128) // 16
    tmask[:, :] = (ti[None, :] >= ti[:, None])
    hidx = np.arange(128) // 64
    same = (hidx[:, None] == hidx[None, :]).astype(np.float32)
    blk1 = same.copy()
    blk64 = same / 64.0
    cmat = np.concatenate([ident, permA, permD, tmask, blk1, blk64], axis=1)
    cst = np.zeros((128, 8 + 32 + 512), np.float32)
    invA = (500000.0 ** (-np.arange(0, 64, 2, dtype=np.float32) / np.float32(64))).astype(np.float32)
    invD = (500000.0 ** (-np.arange(0, 16, 2, dtype=np.float32) / np.float32(16))).astype(np.float32)
    for r in range(128):
        d = r % 64
        cst[r, 0] = invA[d % 32]
        cst[r, 1] = -1.0 if d < 32 else 1.0
        cst[r, 2] = invD[d % 8] if d < 16 else 0.0
        cst[r, 3] = (-1.0 if d < 8 else 1.0) if d < 16 else 0.0
    tau = np.zeros(32, np.float32)
    tau[0:8] = -np.arange(8)
    tau[8:17] = np.arange(9)
    tau[17:25] = 7 - np.arange(8)
    cst[:, 8:40] = tau[None, :]
    cst[:, 40:552] = (8.0 * (np.arange(512) + 1))[None, :]
    return cmat, masks.reshape(128, 2048), cst


def const_mats2():
    hidx = np.arange(128) // 64
    idx = np.arange(128) % 64
    same = (hidx[:, None] == hidx[None, :])
    mSL = (same & (idx[:, None] > idx[None, :])).astype(np.float32)
    mSU = (same & (idx[:, None] < idx[None, :])).astype(np.float32)
    mUI = (same & (idx[:, None] <= idx[None, :])).astype(np.float32)
    ident = np.eye(128, dtype=np.float32)
    return np.concatenate([np.tile(mSL, (1, 4)), np.tile(mSU, (1, 4)), np.tile(mUI, (1, 4)), np.tile(ident, (1, 8))], axis=1)


NCST = 552


def load_all_consts(k, c, cmat_d, cmask_d, cst_d, cm2_d=None):
    c.ident = k.sb("c_ident", [128, 128], BF16)
    c.permA = k.sb("c_permA", [128, 128], BF16)
    c.permD = k.sb("c_permD", [128, 128], BF16)
    c.ones = k.sb("c_ones", [128, 128], BF16)
    c.cst = k.sb("c_cst", [128, NCST], F32)
    c.tmask = k.sb("c_tmask", [128, 128], F32)
    c.blk1 = k.sb("c_blk1", [128, 128], BF16)
    c.blk64 = k.sb("c_blk64", [128, 128], BF16)
    c.mSL = k.sb("c_mSL", [128, 512], BF16)
    c.mSU = k.sb("c_mSU", [128, 512], BF16)
    c.mUI = k.sb("c_mUI", [128, 512], BF16)
    c.ident8 = k.sb("c_ident8", [128, 1024], BF16)
    c.masks = [k.sb(f"c_mask{r}", [128, 512], BF16) for r in range(4)]
    with ExitStack() as st:
        f = k.sb("lc_f", [128, 768], F32, st)
        m2 = k.sb("lc_m2", [128, 2560], F32, st)
        m = k.sb("lc_m", [128, 2048], F32, st)
        k.dma("sp", f[:], cmat_d[:, :], writes=[f])
        k.dma("sp", m[:], cmask_d[:, :], writes=[m])
        k.dma("sp", c.cst[:], cst_d[:, :], writes=[c.cst])
        k.op("dve", lambda e: e.tensor_copy(out=c.ident[:], in_=f[:, 0:128]), reads=[f], writes=[c.ident])
        k.op("dve", lambda e: e.tensor_copy(out=c.permA[:], in_=f[:, 128:256]), reads=[f], writes=[c.permA])
        k.op("dve", lambda e: e.tensor_copy(out=c.permD[:], in_=f[:, 256:384]), reads=[f], writes=[c.permD])
        k.op("dve", lambda e: e.tensor_copy(out=c.tmask[:], in_=f[:, 384:512]), reads=[f], writes=[c.tmask])
        k.op("dve", lambda e: e.tensor_copy(out=c.blk1[:], in_=f[:, 512:640]), reads=[f], writes=[c.blk1])
        k.op("dve", lambda e: e.tensor_copy(out=c.blk64[:], in_=f[:, 640:768]), reads=[f], writes=[c.blk64])
        if cm2_d is not None:
            k.dma("sp", m2[:], cm2_d[:, :], writes=[m2])
            k.op("dve", lambda e: e.tensor_copy(out=c.mSL[:], in_=m2[:, 0:512]), reads=[m2], writes=[c.mSL])
            k.op("dve", lambda e: e.tensor_copy(out=c.mSU[:], in_=m2[:, 512:1024]), reads=[m2], writes=[c.mSU])
            k.op("dve", lambda e: e.tensor_copy(out=c.mUI[:], in_=m2[:, 1024:1536]), reads=[m2], writes=[c.mUI])
            k.op("dve", lambda e: e.tensor_copy(out=c.ident8[:], in_=m2[:, 1536:2560]), reads=[m2], writes=[c.ident8])
        k.op("dve", lambda e: e.memset(c.ones[:], 1.0), writes=[c.ones])
        for r in range(4):
            k.op("dve", lambda e: e.tensor_copy(out=c.masks[r][:], in_=m[:, r * 512:(r + 1) * 512]),
                 reads=[m], writes=[c.masks[r]])
        k.barrier()


O_CQ, O_CKV, O_KR, O_U, O_Z, O_QD, O_KD, O_VD, O_GATE = 0, 512, 768, 832, 1344, 3008, 3520, 4032, 4544


def s5_gperm(half):
    return np.concatenate([half * 16 + np.arange(16), (1 - half) * 16 + np.arange(16)])


def s5_chperm(half):
    return (s5_gperm(half)[:, None] * 16 + np.arange(16)[None, :]).reshape(-1)


def fm_cols(half):
    a = np.arange
    cols = [O_CQ + a(512), O_CKV + a(256), O_U + s5_chperm(half),
            O_Z + half * 256 + a(256), O_Z + 512 + half * 256 + a(256),
            O_QD + half * 256 + a(256), O_KD + half * 256 + a(256)]
    for b in range(4):
        cols.append(O_GATE + b * 512 + half * 256 + a(256))
    cols += [O_KR + a(64), O_KR + a(64), O_Z + 1536 + a(64), O_Z + 1600 + a(64), O_Z + 1024 + half * 256 + a(256)]
    return np.concatenate(cols)


def tm_cols(half):
    a = np.arange
    return O_VD + half * 256 + a(256)


def pt_layout(v, nt):
    return np.ascontiguousarray(np.asarray(v).reshape(nt, 128).T)


def core_layer_arrays(inp, l, half):
    out = {}
    w_in = inp["w_in"][l]
    out["w_in"] = np.ascontiguousarray(w_in[:, np.concatenate([fm_cols(half), tm_cols(half)])])
    hs = [2 * half, 2 * half + 1]
    wuq = inp["mla_w_uq"][l]
    out["wuq"] = np.ascontiguousarray(np.concatenate(
        [wuq[:, h * 192:h * 192 + 128] for h in hs] + [wuq[:, h * 192 + 128:(h + 1) * 192] for h in hs], axis=1))
    wukv = inp["mla_w_ukv"][l]
    out["wukv"] = np.ascontiguousarray(np.concatenate(
        [wukv[:, h * 256:h * 256 + 128] for h in hs] + [wukv[:, h * 256 + 128:(h + 1) * 256] for h in hs], axis=1))
    prm = np.zeros((128, _pc), np.float32)

    def put(name, arr):
        arr = np.asarray(arr, np.float32)
        if arr.ndim == 1:
            arr = arr[:, None]
        prm[:arr.shape[0], PRM_COLS[name]:PRM_COLS[name] + arr.shape[1]] = arr
    put("gq", pt_layout(inp["mla_q_norm_g"][l], 4))
    put("gkv", pt_layout(inp["mla_kv_norm_g"][l], 2))
    for nm in ("lq1", "lk1", "lq2", "lk2"):
        put(nm, np.broadcast_to(inp["diff_" + nm][l][None, :], (128, 64)))
    put("gsub", inp["diff_subln_g"][l])
    lam_init = 0.8 - 0.6 * math.exp(-0.3 * l)
    put("lam_init", np.full((128,), lam_init, np.float32))
    put("omlam", np.full((128,), 1.0 - lam_init, np.float32))
    fill_more(inp, l, half, put, out)
    out["prm"] = prm
    return out


def st_layout(a):
    a = np.asarray(a)
    rest = a.shape[2:]
    a = a.reshape((16, 128) + rest)
    return np.ascontiguousarray(np.moveaxis(a, 0, 1))


def fill_more(inp, l, half, put, out=None):
    gp = s5_gperm(half)
    chp = s5_chperm(half)
    put("s5_are", st_layout(inp["s5_a_re"][l][gp]))
    put("s5_aim", st_layout(inp["s5_a_im"][l][gp]))
    put("s5_ldt", st_layout(np.broadcast_to(inp["s5_log_dt"][l][gp][:, None], (32, 64))))
    put("s5_d", pt_layout(inp["s5_d"][l][chp], 4))
    put("s5_bg", pt_layout(inp["s5_b_glu"][l][half * 256:(half + 1) * 256], 2))
    mu = inp["rwkv_mu"][l]
    my = slice(half * 256, (half + 1) * 256)
    put("rw_mur", pt_layout(mu[0:512][my], 2))
    put("rw_muk", pt_layout(mu[512:1024][my], 2))
    put("rw_muv", pt_layout(mu[1024:1536][my], 2))
    put("rw_mul", mu[1536:1664])
    put("rw_w0", pt_layout(inp["rwkv_w0"][l][my], 2))
    put("rw_a0", pt_layout(inp["rwkv_a0"][l][my], 2))
    put("rw_kk", pt_layout(inp["rwkv_k_k"][l][my], 2))
    put("rw_ka", pt_layout(inp["rwkv_k_a"][l][my], 2))
    put("rw_rk", pt_layout(inp["rwkv_r_k"][l].reshape(512)[my], 2))
    put("rw_lng", pt_layout(inp["rwkv_ln_g"][l][my], 2))
    put("rw_lnb", pt_layout(inp["rwkv_ln_b"][l][my], 2))
    if out is not None:
        out["w2a2"] = np.ascontiguousarray(np.concatenate([inp["rwkv_w2"][l][:, my], inp["rwkv_a2"][l][:, my]], axis=0))
        b = np.stack([inp["s5_b_re"][l][gp], inp["s5_b_im"][l][gp]], axis=2)
        out["s5b"] = st_layout(b).reshape(128, 512).astype(np.float32)
        cc = np.stack([np.swapaxes(inp["s5_c_re"][l][gp], 1, 2), np.swapaxes(inp["s5_c_im"][l][gp], 1, 2)], axis=2)
        out["s5c"] = st_layout(cc).reshape(128, 512).astype(np.float32)
        out["wglu"] = np.ascontiguousarray(inp["s5_w_glu"][l][chp][:, half * 256:(half + 1) * 256])


_reg("lq1", 64)
_reg("lk1", 64)
_reg("lq2", 64)
_reg("lk2", 64)
_reg("gsub", 1)
_reg("lam_init", 1)
_reg("omlam", 1)
DIFF_EPS = 1e-5


def stage_diff(k, c, L):
    pT, pV = c.pT, c.pV
    with ExitStack() as st:
        rp = RopeCtx(k, c, st, "dr", c.ropeD_cos, c.ropeD_sin, c.permD)
        xin = [k.sb(f"dr_in{i}", [128, TCH], F32, st) for i in range(3)]
        stg = Stager(k, st, "dr_o", [128, TCH], BF16, 4)
        n = 0
        for tc in range(NTC):
            tsl = slice(tc * TCH, (tc + 1) * TCH)
            rp.load(tc)
            for (r0, dst) in ((R_QD, c.qdT), (R_KD, c.kdT)):
                for hd in range(2):
                    xi = xin[n % 3]
                    n += 1
                    k.dma("sp", xi[:], pT[r0 + hd * 128:r0 + (hd + 1) * 128, tsl], reads=[pT], writes=[xi])
                    s = stg.next()
                    rp.apply(xi[:], xi, s[:, :], s)
                    k.dma(STQ, dst[hd * 128:(hd + 1) * 128, tsl], s[:, :], reads=[s], writes=[dst], waw=False)
    k.barrier()
    with ExitStack() as st0:
        sm = k.sb("df_sm", [128, 8], F32, st0)
        tmp = k.sb("df_tmp", [128, 64], F32, st0)
        cq1, ck1, cq2, ck2, cg = (L.col[n_] for n_ in ("lq1", "lk1", "lq2", "lk2", "gsub"))
        for i, (a, b_) in enumerate(((cq1, ck1), (cq2, ck2))):
            k.op("dve", lambda e: e.tensor_tensor(out=tmp[:], in0=L.prm[:, a:a + 64], in1=L.prm[:, b_:b_ + 64],
                                                  op=ALU.mult), reads=[L.prm], writes=[tmp])
            k.op("dve", lambda e: e.reduce_sum(out=sm[:, i:i + 1], in_=tmp[:], axis=AX.X), reads=[tmp], writes=[sm])
            k.op("act", lambda e: e.activation(out=sm[:, 2 + i:3 + i], in_=sm[:, i:i + 1], func=AF.Exp),
                 reads=[sm], writes=[sm])
        k.op("dve", lambda e: e.tensor_tensor(out=sm[:, 4:5], in0=sm[:, 3:4], in1=sm[:, 2:3], op=ALU.subtract),
             reads=[sm], writes=[sm])
        cli, col_ = L.col["lam_init"], L.col["omlam"]
        k.op("dve", lambda e: e.tensor_tensor(out=sm[:, 5:6], in0=sm[:, 4:5], in1=L.prm[:, cli:cli + 1], op=ALU.subtract),
             reads=[sm, L.prm], writes=[sm])
        k.op("dve", lambda e: e.tensor_tensor(out=sm[:, 6:7], in0=L.prm[:, cg:cg + 1], in1=L.prm[:, col_:col_ + 1], op=ALU.mult),
             reads=[L.prm], writes=[sm])
        nlam = sm[:, 5:6]
        gs = sm[:, 6:7]
        scale = 64 ** -0.5
        for hd in range(2):
            with ExitStack() as st:
                Kd = k.sb("da_k", [128, S], BF16, st)
                Vf = k.sb("da_vf", [128, S // 128, 128], F32, st)
                Vs = k.sb("da_v", [128, S // 128, 128], BF16, st)
                k.dma("sp", Kd[:], c.kdT[hd * 128:(hd + 1) * 128, :], reads=[c.kdT], writes=[Kd])
                k.dma("sp", Vf[:], pV[:, hd * 128:(hd + 1) * 128].rearrange("(kt p) d -> p kt d", p=128),
                      reads=[pV], writes=[Vf])
                k.op("pool", lambda e: e.tensor_copy(out=Vs[:], in_=Vf[:]), reads=[Vf], writes=[Vs])
                Qd = [k.sb(f"da_q{i}", [128, TCH], BF16, st) for i in range(2)]
                ptb = [k.sb(f"da_pt{i}", [128, TCH], BF16, st) for i in range(3)]
                rec = k.sb("da_rec", [128, TCH], F32, st)
                o1 = k.sb("da_o1", [128, TCH], F32, st)
                o2 = k.sb("da_o2", [128, TCH], F32, st)
                sq = k.sb("da_sq", [128, TCH], BF16, st)
                rs = k.sb("da_rs", [128, TCH], F32, st)
                ost = [k.sb(f"da_o{i}", [128, TCH], F32, st) for i in range(2)]
                for j in range(NTC):
                    b = j % 2
                    tsl = slice(j * TCH, (j + 1) * TCH)
                    k.dma("sp", Qd[b][:], c.qdT[hd * 128:(hd + 1) * 128, tsl], reads=[c.qdT], writes=[Qd[b]])
                    maps = []
                    for m in range(2):
                        ms = slice(m * 64, (m + 1) * 64)
                        maps.append([dict(K=(lambda kt, ms=ms: Kd[ms, kt * 128:(kt + 1) * 128]),
                                          Q=(lambda c0, ms=ms: Qd[b][ms, c0:TCH]), kd=Kd, qd=Qd[b])])
                    accs = [(c.pf[2], c.pf[3]), (c.pf[4], c.pf[5])]
                    attn_qchunk(k, c, j, maps, lambda kt: Vs[:, kt, :], Vs, 128, scale, c.masks, ptb, accs)
                    (oa1, sa1), (oa2, sa2) = accs
                    k.op("act", lambda e: e.activation(out=rec[:], in_=sa1[:, :], func=AF.Ln), reads=[sa1], writes=[rec])
                    k.op("act", lambda e: e.activation(out=rec[:], in_=rec[:], func=AF.Exp, scale=-1.0), reads=[rec], writes=[rec])
                    k.op("dve", lambda e: e.tensor_tensor(out=o1[:], in0=oa1[:, :], in1=rec[:], op=ALU.mult),
                         reads=[oa1, rec], writes=[o1])
                    k.op("act", lambda e: e.activation(out=rec[:], in_=sa2[:, :], func=AF.Ln), reads=[sa2], writes=[rec])
                    k.op("act", lambda e: e.activation(out=rec[:], in_=rec[:], func=AF.Exp, scale=-1.0), reads=[rec], writes=[rec])
                    k.op("dve", lambda e: e.tensor_tensor(out=o2[:], in0=oa2[:, :], in1=rec[:], op=ALU.mult),
                         reads=[oa2, rec], writes=[o2])
                    k.op("dve", lambda e: e.scalar_tensor_tensor(out=o1[:], in0=o2[:], scalar=nlam, in1=o1[:],
                                                                 op0=ALU.mult, op1=ALU.add),
                         reads=[o2, o1, sm], writes=[o1])
                    k.op("pool", lambda e: e.tensor_tensor(out=sq[:], in0=o1[:], in1=o1[:], op=ALU.mult), reads=[o1], writes=[sq])
                    ps = c.next_pf(0, 2)
                    k.op("pe", lambda e: e.matmul(ps[:, :], lhsT=c.ones[:], rhs=sq[:], start=True, stop=True),
                         reads=[c.ones, sq], writes=[ps])
                    k.op("dve", lambda e: e.tensor_scalar(out=rs[:], in0=ps[:, :], scalar1=1.0 / 128, scalar2=DIFF_EPS,
                                                          op0=ALU.mult, op1=ALU.add), reads=[ps], writes=[rs])
                    k.op("act", lambda e: e.activation(out=rs[:], in_=rs[:], func=AF.Ln), reads=[rs], writes=[rs])
                    k.op("act", lambda e: e.activation(out=rs[:], in_=rs[:], func=AF.Exp, scale=-0.5), reads=[rs], writes=[rs])
                    k.op("dve", lambda e: e.scalar_tensor_tensor(out=ost[b][:], in0=o1[:], scalar=gs, in1=rs[:],
                                                                 op0=ALU.mult, op1=ALU.mult),
                         reads=[o1, rs, sm], writes=[ost[b]])
                    k.dma(STQ, c.mixT[768 + hd * 128:768 + (hd + 1) * 128, tsl], ost[b][:], reads=[ost[b]],
                          writes=[c.mixT], waw=False)
            k.barrier()


_reg("s5_are", 16)
_reg("s5_aim", 16)
_reg("s5_ldt", 16)
_reg("s5_d", 4)
_reg("s5_bg", 2)
Z_NEG0, Z_POS0, Z_REV0 = 0, 8, 17
GELU_C = 1.5957691216057308


def sincos_alloc(k, st, name, shape):
    return dict(ni=k.sb(name + "_ni", shape, I32, st), nf=k.sb(name + "_nf", shape, F32, st),
                a2=k.sb(name + "_a2", shape, F32, st), r=k.sb(name + "_r", shape, F32, st),
                m=k.sb(name + "_m", shape, F32, st))


def sincos_tile(k, tmp, ang, sin_out, cos_out):
    ni, nf, a2, r, m = tmp["ni"], tmp["nf"], tmp["a2"], tmp["r"], tmp["m"]
    k.op("dve", lambda e: e.tensor_scalar(out=ni[:], in0=ang[:], scalar1=float(1.0 / TWO_PI), scalar2=None,
                                          op0=ALU.mult), reads=[ang], writes=[ni])
    k.op("dve", lambda e: e.tensor_copy(out=nf[:], in_=ni[:]), reads=[ni], writes=[nf])
    k.op("dve", lambda e: e.scalar_tensor_tensor(out=a2[:], in0=nf[:], scalar=-CW1, in1=ang[:], op0=ALU.mult,
                                                 op1=ALU.add), reads=[nf, ang], writes=[a2])
    k.op("dve", lambda e: e.scalar_tensor_tensor(out=a2[:], in0=nf[:], scalar=-CW2, in1=a2[:], op0=ALU.mult,
                                                 op1=ALU.add), reads=[nf, a2], writes=[a2])
    k.op("dve", lambda e: e.scalar_tensor_tensor(out=a2[:], in0=nf[:], scalar=-CW3, in1=a2[:], op0=ALU.mult,
                                                 op1=ALU.add), reads=[nf, a2], writes=[a2])
    for shift, dst in ((0.0, sin_out), (np.pi / 2, cos_out)):
        k.op("dve", lambda e: e.tensor_scalar(out=r[:], in0=a2[:], scalar1=float(shift), scalar2=None, op0=ALU.add),
             reads=[a2], writes=[r])
        k.op("dve", lambda e: e.tensor_single_scalar(out=m[:], in_=r[:], scalar=float(np.pi), op=ALU.is_gt),
             reads=[r], writes=[m])
        k.op("dve", lambda e: e.scalar_tensor_tensor(out=r[:], in0=m[:], scalar=-TWO_PI, in1=r[:], op0=ALU.mult,
                                                     op1=ALU.add), reads=[m, r], writes=[r])
        k.op("dve", lambda e: e.tensor_single_scalar(out=m[:], in_=r[:], scalar=float(-np.pi), op=ALU.is_lt),
             reads=[r], writes=[m])
        k.op("dve", lambda e: e.scalar_tensor_tensor(out=r[:], in0=m[:], scalar=TWO_PI, in1=r[:], op0=ALU.mult,
                                                     op1=ALU.add), reads=[m, r], writes=[r])
        k.op("dve", lambda e: e.tensor_scalar(out=r[:], in0=r[:], scalar1=float(np.pi), scalar2=float(-np.pi),
                                              op0=ALU.min, op1=ALU.max), reads=[r], writes=[r])
        k.op("act", lambda e: e.activation(out=dst[:], in_=r[:], func=AF.Sin), reads=[r], writes=[dst])


def stage_s5(k, c, L):
    import os as _os
    NT = 16
    pT = c.pT
    SH4 = [128, NT, 8, 16]
    with ExitStack() as stA:
        Tm = k.sb("s5_Tm", [128, 32, 128], BF16, stA)
        GstR = k.sb("s5_GstR", [128, NT, 128], BF16, stA)
        GstI = k.sb("s5_GstI", [128, NT, 128], BF16, stA)
        EfR = k.sb("s5_EfR", SH4, BF16, stA)
        EfnI = k.sb("s5_EfnI", SH4, BF16, stA)
        sc = k.sb("s5_sc", [128, 8, NT], F32, stA)
        LR, DT, ML, TH, MAG8, FRE, FIM, TMP = range(8)
        ca, ci_, cl = L.col["s5_are"], L.col["s5_aim"], L.col["s5_ldt"]
        AIM = L.prm[:, ci_:ci_ + NT]
        with ExitStack() as st:
            bsb = k.sb("s5_b", [128, NT, 2, 16], F32, st)
            csb = k.sb("s5_c", [128, NT, 2, 16], F32, st)
            k.dma("sp", bsb[:], L.s5b[:, :].rearrange("p (t r c) -> p t r c", t=NT, r=2), writes=[bsb])
            k.dma("sp", csb[:], L.s5c[:, :].rearrange("p (t r c) -> p t r c", t=NT, r=2), writes=[csb])
            k.op("dve", lambda e: e.tensor_scalar(out=sc[:, LR, :], in0=L.prm[:, ca:ca + NT], scalar1=-1e-4,
                                                  scalar2=None, op0=ALU.min), reads=[L.prm], writes=[sc])
            k.op("act", lambda e: e.activation(out=sc[:, DT, :], in_=L.prm[:, cl:cl + NT], func=AF.Exp),
                 reads=[L.prm], writes=[sc])
            k.op("dve", lambda e: e.tensor_tensor(out=sc[:, ML, :], in0=sc[:, DT, :], in1=sc[:, LR, :], op=ALU.mult),
                 reads=[sc], writes=[sc])
            k.op("dve", lambda e: e.tensor_tensor(out=sc[:, TH, :], in0=sc[:, DT, :], in1=AIM, op=ALU.mult),
                 reads=[sc, L.prm], writes=[sc])
            k.op("act", lambda e: e.activation(out=sc[:, MAG8, :], in_=sc[:, ML, :], func=AF.Exp, scale=8.0),
                 reads=[sc], writes=[sc])
            SH3 = [128, NT, 32]
            lm = k.sb("s5_lm", SH3, F32, st)
            an = k.sb("s5_an", SH3, F32, st)
            mg = k.sb("s5_mg", SH3, F32, st)
            sn = k.sb("s5_sn", SH3, F32, st)
            cs = k.sb("s5_cs", SH3, F32, st)
            zr = k.sb("s5_zr", SH3, F32, st)
            zi = k.sb("s5_zi", SH3, F32, st)
            tauB = c.cst[:, 8:40].unsqueeze(1).to_broadcast(SH3)
            k.op("dve", lambda e: e.tensor_tensor(out=lm[:], in0=sc[:, ML, :].unsqueeze(2).to_broadcast(SH3), in1=tauB,
                                                  op=ALU.mult), reads=[sc, c.cst], writes=[lm])
            k.op("dve", lambda e: e.tensor_tensor(out=an[:], in0=sc[:, TH, :].unsqueeze(2).to_broadcast(SH3), in1=tauB,
                                                  op=ALU.mult), reads=[sc, c.cst], writes=[an])
            k.op("act", lambda e: e.activation(out=mg[:], in_=lm[:], func=AF.Exp), reads=[lm], writes=[mg])
            sincos_tile(k, sincos_alloc(k, st, "s5sc0", SH3), an, sn, cs)
            k.op("dve", lambda e: e.tensor_tensor(out=zr[:], in0=mg[:], in1=cs[:], op=ALU.mult), reads=[mg, cs], writes=[zr])
            k.op("dve", lambda e: e.tensor_tensor(out=zi[:], in0=mg[:], in1=sn[:], op=ALU.mult), reads=[mg, sn], writes=[zi])
            if _os.environ.get("S5_STOP") == "a":
                k.barrier()
                return
            sm = k.sb("s5_sm", [128, 8, NT], F32, st)
            abr, abi = zr[:, :, Z_POS0 + 1], zi[:, :, Z_POS0 + 1]
            lr_ = sc[:, LR, :]

            def tt(out, a, b, op, rd, wr, E="dve"):
                k.op(E, lambda e: e.tensor_tensor(out=out, in0=a, in1=b, op=op), reads=rd, writes=wr)
            tt(sm[:, 0, :], lr_, lr_, ALU.mult, [sc], [sm])
            tt(sm[:, 1, :], AIM, AIM, ALU.mult, [L.prm], [sm])
            tt(sm[:, 0, :], sm[:, 0, :], sm[:, 1, :], ALU.add, [sm], [sm])
            k.op("dve", lambda e: e.reciprocal(out=sm[:, 0, :], in_=sm[:, 0, :]), reads=[sm], writes=[sm])
            k.op("dve", lambda e: e.tensor_scalar(out=sm[:, 1, :], in0=abr, scalar1=-1.0, scalar2=None, op0=ALU.add),
                 reads=[zr], writes=[sm])
            tt(sm[:, 2, :], sm[:, 1, :], lr_, ALU.mult, [sm, sc], [sm])
            tt(sm[:, 3, :], abi, AIM, ALU.mult, [zi, L.prm], [sm])
            tt(sm[:, 2, :], sm[:, 2, :], sm[:, 3, :], ALU.add, [sm], [sm])
            tt(sc[:, FRE, :], sm[:, 2, :], sm[:, 0, :], ALU.mult, [sm], [sc])
            tt(sm[:, 4, :], abi, lr_, ALU.mult, [zi, sc], [sm])
            tt(sm[:, 5, :], sm[:, 1, :], AIM, ALU.mult, [sm, L.prm], [sm])
            tt(sm[:, 4, :], sm[:, 4, :], sm[:, 5, :], ALU.subtract, [sm], [sm])
            tt(sc[:, FIM, :], sm[:, 4, :], sm[:, 0, :], ALU.mult, [sm], [sc])
            if _os.environ.get("S5_STOP") == "b":
                k.barrier()
                return
            SHB = [128, NT, 16]
            bbr = k.sb("s5_bbr", SHB, F32, st)
            bbi = k.sb("s5_bbi", SHB, F32, st)
            t1 = k.sb("s5_t1", SHB, F32, st)
            fre = sc[:, FRE, :].unsqueeze(2).to_broadcast(SHB)
            fim = sc[:, FIM, :].unsqueeze(2).to_broadcast(SHB)
            br_, bi_ = bsb[:, :, 0, :], bsb[:, :, 1, :]
            tt(bbr[:], fre, br_, ALU.mult, [sc, bsb], [bbr])
            tt(t1[:], fim, bi_, ALU.mult, [sc, bsb], [t1])
            tt(bbr[:], bbr[:], t1[:], ALU.subtract, [bbr, t1], [bbr])
            tt(bbi[:], fre, bi_, ALU.mult, [sc, bsb], [bbi])
            tt(t1[:], fim, br_, ALU.mult, [sc, bsb], [t1])
            tt(bbi[:], bbi[:], t1[:], ALU.add, [bbi, t1], [bbi])
            if _os.environ.get("S5_STOP") == "c":
                k.barrier()
                return
            BfR = k.sb("s5_BfR", SH4, BF16, st)
            BfnI = k.sb("s5_BfnI", SH4, BF16, st)
            CfR = k.sb("s5_CfR", SH4, BF16, st)
            CfI = k.sb("s5_CfI", SH4, BF16, st)
            GfR = k.sb("s5_GfR", SH4, BF16, st)
            GfI = k.sb("s5_GfI", SH4, BF16, st)
            u1 = [k.sb(f"s5_u1{i}", SH4, F32, st) for i in range(2)]
            u2 = [k.sb(f"s5_u2{i}", SH4, F32, st) for i in range(2)]

            def cmul(oR, oI, z0, xr, xi, xdeps, neg_im, i):
                E = "dve" if i % 2 == 0 else "pool"
                zR = zr[:, :, z0:z0 + 8].unsqueeze(3).to_broadcast(SH4)
                zI = zi[:, :, z0:z0 + 8].unsqueeze(3).to_broadcast(SH4)
                xR = xr.unsqueeze(2).to_broadcast(SH4)
                xI = xi.unsqueeze(2).to_broadcast(SH4)
                a, b_ = u1[i % 2], u2[i % 2]
                tt(a[:], zR, xR, ALU.mult, [zr, ] + xdeps, [a], E)
                tt(b_[:], zI, xI, ALU.mult, [zi, ] + xdeps, [b_], E)
                tt(oR[:], a[:], b_[:], ALU.subtract, [a, b_], [oR], E)
                tt(a[:], zR, xI, ALU.mult, [zr, ] + xdeps, [a], E)
                tt(b_[:], zI, xR, ALU.mult, [zi, ] + xdeps, [b_], E)
                if neg_im:
                    k.op("dve", lambda e: e.scalar_tensor_tensor(out=oI[:], in0=a[:], scalar=-1.0, in1=b_[:],
                                                                 op0=ALU.mult, op1=ALU.subtract),
                         reads=[a, b_], writes=[oI])
                else:
                    tt(oI[:], a[:], b_[:], ALU.add, [a, b_], [oI], E)
            cr_, ci2 = csb[:, :, 0, :], csb[:, :, 1, :]
            cmul(BfR, BfnI, Z_NEG0, bbr[:], bbi[:], [bbr, bbi], True, 0)
            cmul(CfR, CfI, Z_POS0, cr_, ci2, [csb], False, 1)
            cmul(GfR, GfI, Z_REV0, bbr[:], bbi[:], [bbr, bbi], False, 0)
            cmul(EfR, EfnI, Z_POS0 + 1, cr_, ci2, [csb], True, 1)
            if _os.environ.get("S5_STOP") == "d":
                k.barrier()
                return
            for t4 in range(4):
                for hh in range(2):
                    ps = c.next_pf()
                    hs = slice(hh * 64, (hh + 1) * 64)
                    for q in range(4):
                        t = t4 * 4 + q
                        k.op("pe", lambda e: e.matmul(ps[:, q * 128:(q + 1) * 128],
                                                      lhsT=BfR[hs, t, :, :].rearrange("p a b -> p (a b)"),
                                                      rhs=CfR[hs, t, :, :].rearrange("p a b -> p (a b)"), start=True, stop=False),
                             reads=[BfR, CfR], writes=[ps], inc=False)
                        k.op("pe", lambda e: e.matmul(ps[:, q * 128:(q + 1) * 128],
                                                      lhsT=BfnI[hs, t, :, :].rearrange("p a b -> p (a b)"),
                                                      rhs=CfI[hs, t, :, :].rearrange("p a b -> p (a b)"), start=False, stop=True),
                             reads=[BfnI, CfI], writes=[ps], inc=(q == 3))
                    for q in range(4):
                        g = 2 * (t4 * 4 + q) + hh
                        k.op("dve", lambda e: e.tensor_tensor(out=Tm[:, g, :], in0=ps[:, q * 128:(q + 1) * 128],
                                                              in1=c.tmask[:], op=ALU.mult),
                             reads=[ps, c.tmask], writes=[Tm])
            if _os.environ.get("S5_STOP") == "e":
                k.barrier()
                return
            for (src, dst) in ((GfR, GstR), (GfI, GstI)):
                for t8 in range(2):
                    pb = c.next_pb()
                    for q in range(8):
                        t = t8 * 8 + q
                        k.op("pe", lambda e: e.transpose(pb[:, q * 128:(q + 1) * 128],
                                                         src[:, t, :, :].rearrange("p a b -> p (a b)"), c.ident[:]),
                             reads=[src, c.ident], writes=[pb], inc=(q == 7))
                    k.op("act", lambda e: e.copy(out=dst[:, t8 * 8:(t8 + 1) * 8, :],
                                                 in_=pb[:].rearrange("p (q n) -> p q n", q=8)),
                         reads=[pb], writes=[dst])
            k.barrier()
        import os as _os
        if _os.environ.get("S5_STOP") == "1":
            return
        with ExitStack() as st:
            ug = [[k.sb(f"s5_u{i}{j}", [128, TCH], BF16, st) for j in range(2)] for i in range(2)]
            ang = k.sb("s5_ang", [128, TCH], F32, st)
            sn = k.sb("s5_snj", [128, TCH], F32, st)
            cs = k.sb("s5_csj", [128, TCH], F32, st)
            a_ = k.sb("s5_a", [128, TCH], F32, st)
            b_ = k.sb("s5_bq", [128, TCH], F32, st)
            mre = k.sb("s5_mre", [128, TCH], F32, st)
            mim = k.sb("s5_mim", [128, TCH], F32, st)
            hre = k.sb("s5_hre", [128, TCH], F32, st)
            him = k.sb("s5_him", [128, TCH], F32, st)
            Hp = [[k.sb(f"s5_Hp{i}{j}", [128, TCH + 1], BF16, st) for j in range(2)] for i in range(2)]
            yst = [k.sb(f"s5_y{i}", [128, TCH], F32, st) for i in range(3)]
            for i in range(2):
                for j in range(2):
                    k.op("pool", lambda e: e.memset(Hp[i][j][:, 0:1], 0.0), writes=[Hp[i][j]])
            sctmp = sincos_alloc(k, st, "s5scj", [128, TCH])
            jv = c.cst[:, 40:552]
            nyi = 0

            def tt(out, a, b, op, rd, wr, E="dve"):
                k.op(E, lambda e: e.tensor_tensor(out=out, in0=a, in1=b, op=op), reads=rd, writes=wr)
            if True:
                for t in range(NT):
                    pp = t % 2
                    for hh in range(2):
                        k.dma("sp", ug[pp][hh][:], c.us5[2 * t + hh, :, :], reads=[c.us5], writes=[ug[pp][hh]])
                    Xre, Xim = c.pf[0], c.pf[1]
                    for hh in range(2):
                        hs = slice(hh * 64, (hh + 1) * 64)
                        k.op("pe", lambda e: e.matmul(Xre[hs, :], lhsT=GstR[:, t, hs], rhs=ug[pp][hh][:], start=True, stop=True),
                             reads=[GstR, ug[pp][hh]], writes=[Xre], inc=(hh == 1))
                    for hh in range(2):
                        hs = slice(hh * 64, (hh + 1) * 64)
                        k.op("pe", lambda e: e.matmul(Xim[hs, :], lhsT=GstI[:, t, hs], rhs=ug[pp][hh][:], start=True, stop=True),
                             reads=[GstI, ug[pp][hh]], writes=[Xim], inc=(hh == 1))
                    k.op("dve", lambda e: e.tensor_scalar(out=ang[:], in0=jv, scalar1=sc[:, TH, t:t + 1], scalar2=None,
                                                           op0=ALU.mult), reads=[c.cst, sc], writes=[ang])
                    sincos_tile(k, sctmp, ang, sn, cs)
                    tt(a_[:], Xre[:, :], cs[:], ALU.mult, [Xre, cs], [a_])
                    tt(b_[:], Xim[:, :], sn[:], ALU.mult, [Xim, sn], [b_])
                    tt(mre[:], a_[:], b_[:], ALU.add, [a_, b_], [mre], "pool")
                    tt(a_[:], Xim[:, :], cs[:], ALU.mult, [Xim, cs], [a_])
                    tt(b_[:], Xre[:, :], sn[:], ALU.mult, [Xre, sn], [b_])
                    tt(mim[:], a_[:], b_[:], ALU.subtract, [a_, b_], [mim], "pool")
                    m8 = sc[:, MAG8, t:t + 1].to_broadcast([128, TCH])
                    k.op("dve", lambda e: e.tensor_tensor_scan(out=hre[:], data0=m8, data1=mre[:], initial=0.0,
                                                               op0=ALU.mult, op1=ALU.add), reads=[sc, mre], writes=[hre])
                    k.op("dve", lambda e: e.tensor_tensor_scan(out=him[:], data0=m8, data1=mim[:], initial=0.0,
                                                               op0=ALU.mult, op1=ALU.add), reads=[sc, mim], writes=[him])
                    hr, hi_ = Hp[pp][0], Hp[pp][1]
                    tt(a_[:], hre[:], cs[:], ALU.mult, [hre, cs], [a_])
                    tt(b_[:], him[:], sn[:], ALU.mult, [him, sn], [b_], "pool")
                    tt(hr[:, 1:TCH + 1], a_[:], b_[:], ALU.subtract, [a_, b_], [hr])
                    tt(mre[:], hre[:], sn[:], ALU.mult, [hre, sn], [mre])
                    tt(mim[:], him[:], cs[:], ALU.mult, [him, cs], [mim], "pool")
                    tt(hi_[:, 1:TCH + 1], mre[:], mim[:], ALU.add, [mre, mim], [hi_])
                    for hh in range(2):
                        g = 2 * t + hh
                        hs = slice(hh * 64, (hh + 1) * 64)
                        py = c.next_pf(2, 6)
                        k.op("pe", lambda e: e.matmul(py[:, :], lhsT=Tm[:, g, :], rhs=ug[pp][hh][:], start=True, stop=False),
                             reads=[Tm, ug[pp][hh]], writes=[py], inc=False)
                        k.op("pe", lambda e: e.matmul(py[:, :], lhsT=EfR[hs, t, :, :].rearrange("p a b -> p (a b)"),
                                                      rhs=hr[hs, 0:TCH], start=False, stop=False),
                             reads=[EfR, hr], writes=[py], inc=False)
                        k.op("pe", lambda e: e.matmul(py[:, :], lhsT=EfnI[hs, t, :, :].rearrange("p a b -> p (a b)"),
                                                      rhs=hi_[hs, 0:TCH], start=False, stop=True),
                             reads=[EfnI, hi_], writes=[py], inc=True)
                        ys = yst[nyi % 3]
                        nyi += 1
                        k.op("act", lambda e: e.copy(out=ys[:], in_=py[:, :]), reads=[py], writes=[ys])
                        k.dma(STQ, c.ys5[g, :, :], ys[:], reads=[ys], writes=[c.ys5], waw=False)
    k.barrier()
    if _os.environ.get("S5_STOP") == "2":
        return
    cd = L.col["s5_d"]
    with ExitStack() as st:
        Y = k.sb("s5p_Y", [128, 8, TCH], F32, st)
        U = k.sb("s5p_U", [128, S], F32, st)
        yy = k.sb("s5p_yy", [128, S], F32, st)
        q1 = k.sb("s5p_q1", [128, S], F32, st)
        gb = k.sb("s5p_gb", [128, S], BF16, st)
        for ct in range(4):
            for gl in range(8):
                k.dma("sp", Y[gl * 16:(gl + 1) * 16, :, :],
                      c.ys5[ct * 8 + gl, :, :].rearrange("(t c) j -> c t j", c=16), reads=[c.ys5], writes=[Y],
                      waw=(gl == 0))
            k.dma("sp", U[:], pT[R_U + ct * 128:R_U + (ct + 1) * 128, :], reads=[pT], writes=[U])
            k.op("dve", lambda e: e.scalar_tensor_tensor(out=yy[:].rearrange("p (j t) -> p j t", t=8),
                                                         in0=U[:].rearrange("p (j t) -> p j t", t=8),
                                                         scalar=L.prm[:, cd + ct:cd + ct + 1],
                                                         in1=Y[:].rearrange("p t j -> p j t"),
                                                         op0=ALU.mult, op1=ALU.add), reads=[U, Y, L.prm], writes=[yy])
            k.op("act", lambda e: e.activation(out=q1[:], in_=yy[:], func=AF.Square), reads=[yy], writes=[q1])
            k.op("dve", lambda e: e.tensor_scalar(out=q1[:], in0=q1[:], scalar1=0.044715, scalar2=1.0, op0=ALU.mult,
                                                  op1=ALU.add), reads=[q1], writes=[q1])
            k.op("pool", lambda e: e.tensor_tensor(out=q1[:], in0=q1[:], in1=yy[:], op=ALU.mult), reads=[q1, yy], writes=[q1])
            k.op("act", lambda e: e.activation(out=q1[:], in_=q1[:], func=AF.Sigmoid, scale=GELU_C), reads=[q1], writes=[q1])
            k.op("dve", lambda e: e.tensor_tensor(out=yy[:], in0=yy[:], in1=q1[:], op=ALU.mult), reads=[yy, q1], writes=[yy])
            k.op("act", lambda e: e.copy(out=gb[:], in_=yy[:]), reads=[yy], writes=[gb])
            k.dma(STQ, c.gT[ct * 128:(ct + 1) * 128, :], gb[:], reads=[gb], writes=[c.gT], waw=False)
            if ct < 2:
                k.dma(STQ, c.gF[ct * 128:(ct + 1) * 128, :], yy[:], reads=[yy], writes=[c.gF], waw=False)
    k.barrier()
    cb = L.col["s5_bg"]
    with ExitStack() as st:
        wbf, wd = load_w_bf16(k, st, lambda kc: L.wglu[kc * 128:(kc + 1) * 128, :], 4, 256, "wglu")
        gin = [k.sb(f"s5g_g{i}", [128, TCH], F32, st) for i in range(3)]
        sg = [k.sb(f"s5g_s{i}", [128, TCH], F32, st) for i in range(3)]
        n = [0]

        def epi(tc, ni, ps, ncol):
            i = n[0] % 3
            n[0] += 1
            tsl = slice(tc * TCH, (tc + 1) * TCH)
            k.dma("sp", gin[i][:], c.gF[ni * 128:(ni + 1) * 128, tsl], reads=[c.gF], writes=[gin[i]])
            k.op("act", lambda e: e.activation(out=sg[i][:], in_=ps[:, :], func=AF.Sigmoid,
                                               bias=L.prm[:, cb + ni:cb + ni + 1], scale=1.0),
                 reads=[ps, L.prm], writes=[sg[i]])
            k.op("dve", lambda e: e.tensor_tensor(out=sg[i][:], in0=sg[i][:], in1=gin[i][:], op=ALU.mult),
                 reads=[sg[i], gin[i]], writes=[sg[i]])
            k.dma(STQ, c.mixT[256 + ni * 128:256 + (ni + 1) * 128, tsl], sg[i][:], reads=[sg[i]], writes=[c.mixT],
                  waw=False)
        linear_fm(k, c, st, c.gT, 4, wbf, wd, [(0, 128), (128, 128)], epi, "s5g")
    k.barrier()


for _n, _w in (("rw_mur", 2), ("rw_muk", 2), ("rw_muv", 2), ("rw_mul", 1), ("rw_w0", 2), ("rw_a0", 2),
               ("rw_kk", 2), ("rw_ka", 2), ("rw_rk", 2), ("rw_lng", 2), ("rw_lnb", 2)):
    _reg(_n, _w)
R_RV = 3584
SEG = 1024
NCK = SEG // 64
RW_EPS = 64e-5
LDC = -0.6065306597126334


def stage_rwkv(k, c, L):
    pT = c.pT
    col = L.col
    with ExitStack() as stA:
        P = lambda nm, j: L.prm[:, col[nm] + j:col[nm] + j + 1]
        wl = k.sb("rw_wl", [128, 256], BF16, stA)
        omka = k.sb("rw_omka", [128, 2], F32, stA)
        rmask = k.sb("rw_rmask", [128, SEG], F32, stA)
        with ExitStack() as st0:
            wlf = k.sb("rw_wlf", [128, 256], F32, st0)
            k.dma("sp", wlf[:], L.w2a2[:, :], writes=[wlf])
            k.op("dve", lambda e: e.tensor_copy(out=wl[:], in_=wlf[:]), reads=[wlf], writes=[wl])
            k.op("dve", lambda e: e.tensor_scalar(out=omka[:], in0=L.prm[:, col["rw_ka"]:col["rw_ka"] + 2], scalar1=-1.0,
                                                  scalar2=1.0, op0=ALU.mult, op1=ALU.add), reads=[L.prm], writes=[omka])
            k.op("pool", lambda e: e.memset(rmask[:], 1.0), writes=[rmask])
            k.op("pool", lambda e: e.memset(rmask[:].rearrange("p (c i) -> p c i", i=64)[:, :, 0:1], 0.0), writes=[rmask])
            k.barrier()
        F = lambda nm: k.sb("rw_" + nm, [128, SEG], F32, stA)
        rz = k.sb("rw_rz", [128, SEG + 1], F32, stA)
        kz = k.sb("rw_kz", [128, SEG + 1], F32, stA)
        vz = k.sb("rw_vz", [128, SEG + 1], F32, stA)
        lz = k.sb("rw_lz", [128, SEG + 1], F32, stA)
        rm, km, vm, lm, t1, t2, sgw, av, cl, Epos, Eneg, Eex, Eh, kkr, kk, k2, bv = (F(n) for n in (
            "rm", "km", "vm", "lm", "t1", "t2", "sgw", "av", "cl", "Epos", "Eneg", "Eex", "Eh", "kkr", "kk", "k2", "bv"))
        lbf = k.sb("rw_lbf", [128, SEG], BF16, stA)
        sqb = k.sb("rw_sqb", [128, SEG], BF16, stA)
        BDn = ("AT", "BT", "KT", "RT", "bhT", "khT", "vT", "Bh", "Kh", "Vb", "Lst", "Mst", "LakT", "ArbT", "ArkT", "TT")
        BD = {n: k.sb("rw_bd_" + n, [128, NCK, 128], BF16, stA) for n in BDn}
        Ln = [k.sb(f"rw_Ln{i}", [128, 8, 128], BF16, stA) for i in range(2)]
        Mn = [k.sb(f"rw_Mn{i}", [128, 8, 128], BF16, stA) for i in range(2)]
        Pn = [k.sb(f"rw_Pn{i}", [128, 8, 128], BF16, stA) for i in range(2)]
        Sf = k.sb("rw_Sf", [128, 128], F32, stA)
        Sb = [k.sb(f"rw_Sb{i}", [128, 128], BF16, stA) for i in range(2)]
        RHSb = k.sb("rw_RHSb", [128, 128], BF16, stA)
        Ub = k.sb("rw_Ub", [128, 128], BF16, stA)
        OT = k.sb("rw_OT", [128, SEG], F32, stA)
        ob = k.sb("rw_ob", [128, SEG], BF16, stA)
        yo = [k.sb(f"rw_yo{i}", [128, SEG], F32, stA) for i in range(2)]
        for n in ("AT", "BT", "KT", "RT", "bhT", "khT", "vT"):
            k.op("pool", lambda e: e.memset(BD[n][:], 0.0), writes=[BD[n]])

        def tt(out, a, b, op, rd, wr, E="dve"):
            k.op(E, lambda e: e.tensor_tensor(out=out, in0=a, in1=b, op=op), reads=rd, writes=wr)

        def act(out, in_, func, rd, wr, **kw):
            k.op("act", lambda e: e.activation(out=out, in_=in_, func=func, **kw), reads=rd, writes=wr)

        def bdw(dst, a, b, rd, E="dve", scalar=None):
            for h in range(2):
                hs = slice(h * 64, (h + 1) * 64)
                o = dst[hs, :, h * 64:(h + 1) * 64]
                av_ = a[hs, :].rearrange("p (c i) -> p c i", i=64)
                if b is None:
                    k.op(E, lambda e: e.tensor_copy(out=o, in_=av_), reads=rd, writes=[dst])
                    continue
                bv_ = b[hs, :].rearrange("p (c i) -> p c i", i=64)
                if scalar is None:
                    k.op(E, lambda e: e.tensor_tensor(out=o, in0=av_, in1=bv_, op=ALU.mult), reads=rd, writes=[dst])
                else:
                    k.op("dve", lambda e: e.scalar_tensor_tensor(out=o, in0=av_, scalar=float(scalar), in1=bv_,
                                                                 op0=ALU.mult, op1=ALU.mult), reads=rd, writes=[dst])

        for hp in range(2):
            k.op("dve", lambda e: e.memset(Sf[:], 0.0), writes=[Sf])
            k.op("dve", lambda e: e.memset(Sb[0][:], 0.0), writes=[Sb[0]])
            sbi = 0
            for seg in range(S // SEG):
                t0 = seg * SEG
                for (zt, r0, mu, dst) in ((rz, R_R + hp * 128, P("rw_mur", hp), rm), (kz, R_K + hp * 128, P("rw_muk", hp), km),
                                          (vz, R_RV + hp * 128, P("rw_muv", hp), vm), (lz, R_LORA, P("rw_mul", 0), lm)):
                    if seg == 0:
                        k.op("pool", lambda e: e.memset(zt[:, 0:1], 0.0), writes=[zt])
                        k.dma("sp", zt[:, 1:SEG + 1], pT[r0:r0 + 128, 0:SEG], reads=[pT], writes=[zt])
                    else:
                        k.dma("sp", zt[:, 0:SEG + 1], pT[r0:r0 + 128, t0 - 1:t0 + SEG], reads=[pT], writes=[zt])
                    tt(t1[:], zt[:, 0:SEG], zt[:, 1:SEG + 1], ALU.subtract, [zt], [t1], "dve")
                    k.op("dve", lambda e: e.scalar_tensor_tensor(out=dst[:], in0=t1[:], scalar=mu, in1=zt[:, 1:SEG + 1],
                                                                 op0=ALU.mult, op1=ALU.add), reads=[t1, zt, L.prm], writes=[dst])
                act(lbf[0:64, :], lm[0:64, :], AF.Tanh, [lm], [lbf])
                k.op("dve", lambda e: e.tensor_copy(out=lbf[64:128, :], in_=lm[64:128, :]), reads=[lm], writes=[lbf])
                for hb in range(SEG // TCH):
                    cs_ = slice(hb * TCH, (hb + 1) * TCH)
                    pw, pa = c.pf[0], c.pf[1]
                    k.op("pe", lambda e: e.matmul(pw[:, :], lhsT=wl[0:64, hp * 128:(hp + 1) * 128], rhs=lbf[0:64, cs_],
                                                  start=True, stop=True), reads=[wl, lbf], writes=[pw])
                    k.op("pe", lambda e: e.matmul(pa[:, :], lhsT=wl[64:128, hp * 128:(hp + 1) * 128], rhs=lbf[64:128, cs_],
                                                  start=True, stop=True), reads=[wl, lbf], writes=[pa])
                    act(sgw[:, cs_], pw[:, :], AF.Sigmoid, [pw, L.prm], [sgw], bias=P("rw_w0", hp), scale=1.0)
                    act(av[:, cs_], pa[:, :], AF.Sigmoid, [pa, L.prm], [av], bias=P("rw_a0", hp), scale=1.0)
                k.op("dve", lambda e: e.tensor_tensor_scan(out=cl[:], data0=rmask[:], data1=sgw[:], initial=0.0,
                                                           op0=ALU.mult, op1=ALU.add), reads=[rmask, sgw], writes=[cl])
                act(Epos[:], cl[:], AF.Exp, [cl], [Epos], scale=LDC)
                act(Eneg[:], cl[:], AF.Exp, [cl], [Eneg], scale=-LDC)
                tt(t1[:], cl[:], sgw[:], ALU.subtract, [cl, sgw], [t1], "dve")
                act(Eex[:], t1[:], AF.Exp, [t1], [Eex], scale=LDC)
                clC = cl[:].rearrange("p (c i) -> p c i", i=64)[:, :, 63:64].to_broadcast([128, NCK, 64])
                tt(t2[:].rearrange("p (c i) -> p c i", i=64), clC, cl[:].rearrange("p (c i) -> p c i", i=64),
                   ALU.subtract, [cl], [t2])
                act(Eh[:], t2[:], AF.Exp, [t2], [Eh], scale=LDC)
                k.op("dve", lambda e: e.tensor_scalar(out=kkr[:], in0=km[:], scalar1=P("rw_kk", hp), scalar2=None,
                                                      op0=ALU.mult), reads=[km, L.prm], writes=[kkr])
                act(sqb[:], kkr[:], AF.Square, [kkr], [sqb])
                for hb in range(SEG // TCH):
                    cs_ = slice(hb * TCH, (hb + 1) * TCH)
                    pn = c.next_pf(2, 6)
                    k.op("pe", lambda e: e.matmul(pn[:, :], lhsT=c.blk1[:], rhs=sqb[:, cs_], start=True, stop=True),
                         reads=[c.blk1, sqb], writes=[pn])
                    k.op("dve", lambda e: e.tensor_scalar(out=t1[:, cs_], in0=pn[:, :], scalar1=1e-24, scalar2=None, op0=ALU.max),
                         reads=[pn], writes=[t1])
                act(t1[:], t1[:], AF.Ln, [t1], [t1])
                act(t1[:], t1[:], AF.Exp, [t1], [t1], scale=-0.5)
                tt(kk[:], kkr[:], t1[:], ALU.mult, [kkr, t1], [kk])
                k.op("dve", lambda e: e.tensor_scalar(out=t2[:], in0=av[:], scalar1=P("rw_ka", hp), scalar2=omka[:, hp:hp + 1],
                                                      op0=ALU.mult, op1=ALU.add), reads=[av, L.prm, omka], writes=[t2])
                tt(k2[:], km[:], t2[:], ALU.mult, [km, t2], [k2], "dve")
                tt(bv[:], kk[:], av[:], ALU.mult, [kk, av], [bv], "dve")
                bdw(BD["AT"], kk, Eex, [kk, Eex], scalar=-1.0)
                bdw(BD["BT"], bv, Eneg, [bv, Eneg], "dve")
                bdw(BD["KT"], k2, Eneg, [k2, Eneg], "dve")
                bdw(BD["RT"], rm, Epos, [rm, Epos], "dve")
                bdw(BD["bhT"], bv, Eh, [bv, Eh], "dve")
                bdw(BD["khT"], k2, Eh, [k2, Eh], "dve")
                bdw(BD["vT"], vm, None, [vm], "dve")
                for oc in range(NCK // 8):
                    c8 = slice(oc * 8, (oc + 1) * 8)
                    prods = (("AT", "BT", "Lst", c.mSL), ("BT", "AT", "Mst", c.mSU), ("KT", "AT", "LakT", c.mSU),
                             ("BT", "RT", "ArbT", c.mUI), ("KT", "RT", "ArkT", c.mUI))
                    for (la, rb, dn, mk) in prods:
                        for g4 in range(2):
                            ps = c.next_pf()
                            for q in range(4):
                                cc = oc * 8 + g4 * 4 + q
                                k.op("pe", lambda e: e.matmul(ps[:, q * 128:(q + 1) * 128], lhsT=BD[la][:, cc, :],
                                                              rhs=BD[rb][:, cc, :], start=True, stop=True),
                                     reads=[BD[la], BD[rb]], writes=[ps], inc=(q == 3))
                            c4 = slice(oc * 8 + g4 * 4, oc * 8 + g4 * 4 + 4)
                            tt(BD[dn][:, c4, :].rearrange("p a b -> p (a b)"), ps[:, :], mk[:], ALU.mult, [ps, mk], [BD[dn]])
                    for (src, dst) in (("bhT", "Bh"), ("khT", "Kh"), ("vT", "Vb")):
                        pb = c.next_pb()
                        for q in range(8):
                            cc = oc * 8 + q
                            k.op("pe", lambda e: e.transpose(pb[:, q * 128:(q + 1) * 128], BD[src][:, cc, :], c.ident[:]),
                                 reads=[BD[src], c.ident], writes=[pb], inc=(q == 7))
                        k.op("act", lambda e: e.copy(out=BD[dst][:, c8, :].rearrange("p a b -> p (a b)"), in_=pb[:]),
                             reads=[pb], writes=[BD[dst]])
                    Lc, Mc, Pc = BD["Lst"][:, c8, :], BD["Mst"][:, c8, :], None
                    Ld, Md = BD["Lst"], BD["Mst"]
                    k.op("dve", lambda e: e.tensor_tensor(out=Pn[0][:].rearrange("p a b -> p (a b)"),
                                                          in0=BD["Mst"][:, c8, :].rearrange("p a b -> p (a b)"),
                                                          in1=c.ident8[:], op=ALU.add), reads=[BD["Mst"], c.ident8], writes=[Pn[0]])
                    pcur = 0
                    for n in range(1, 6):
                        di = n % 2
                        psL = [c.pf[0], c.pf[1]]
                        psM = [c.pf[2], c.pf[3]]
                        for g4 in range(2):
                            for q in range(4):
                                qq = g4 * 4 + q
                                k.op("pe", lambda e: e.matmul(psL[g4][:, q * 128:(q + 1) * 128], lhsT=Mc[:, qq, :], rhs=Lc[:, qq, :],
                                                              start=True, stop=True), reads=[Md, Ld], writes=[psL[g4]], inc=(q == 3))
                            if n < 5:
                                for q in range(4):
                                    qq = g4 * 4 + q
                                    k.op("pe", lambda e: e.matmul(psM[g4][:, q * 128:(q + 1) * 128], lhsT=Lc[:, qq, :], rhs=Mc[:, qq, :],
                                                                  start=True, stop=True), reads=[Ld, Md], writes=[psM[g4]], inc=(q == 3))
                        for g4 in range(2):
                            k.op("act", lambda e: e.copy(out=Ln[di][:, g4 * 4:(g4 + 1) * 4, :].rearrange("p a b -> p (a b)"),
                                                         in_=psL[g4][:, :]), reads=[psL[g4]], writes=[Ln[di]])
                            if n < 5:
                                k.op("dve", lambda e: e.tensor_copy(out=Mn[di][:, g4 * 4:(g4 + 1) * 4, :].rearrange("p a b -> p (a b)"),
                                                                    in_=psM[g4][:, :]), reads=[psM[g4]], writes=[Mn[di]])
                        Lc, Mc, Ld, Md = Ln[di][:, :, :], Mn[di][:, :, :], Ln[di], Mn[di]
                        psP = [c.pf[4], c.pf[5]]
                        pnx = 1 - pcur
                        for g4 in range(2):
                            for q in range(4):
                                qq = g4 * 4 + q
                                k.op("pe", lambda e: e.matmul(psP[g4][:, q * 128:(q + 1) * 128], lhsT=Lc[:, qq, :], rhs=Pn[pcur][:, qq, :],
                                                              start=True, stop=True), reads=[Ld, Pn[pcur]], writes=[psP[g4]], inc=(q == 3))
                        for g4 in range(2):
                            g4s = slice(g4 * 4, (g4 + 1) * 4)
                            if n < 5:
                                o_ = Pn[pnx][:, g4s, :].rearrange("p a b -> p (a b)")
                                wr = Pn[pnx]
                            else:
                                o_ = BD["TT"][:, oc * 8 + g4 * 4:oc * 8 + g4 * 4 + 4, :].rearrange("p a b -> p (a b)")
                                wr = BD["TT"]
                            tt(o_, psP[g4][:, :], Pn[pcur][:, g4s, :].rearrange("p a b -> p (a b)"), ALU.add,
                               [psP[g4], Pn[pcur]], [wr])
                        pcur = pnx
                for cc in range(NCK):
                    So = Sb[sbi]
                    Sn = Sb[1 - sbi]
                    pR, pU, pS, pO = c.pf[0], c.pf[1], c.pf[2], c.pf[3]
                    k.op("pe", lambda e: e.matmul(pR[:, 0:128], lhsT=BD["LakT"][:, cc, :], rhs=BD["Vb"][:, cc, :], start=True, stop=False),
                         reads=[BD["LakT"], BD["Vb"]], writes=[pR], inc=False)
                    k.op("pe", lambda e: e.matmul(pR[:, 0:128], lhsT=BD["AT"][:, cc, :], rhs=So[:], start=False, stop=True),
                         reads=[BD["AT"], So], writes=[pR])
                    k.op("act", lambda e: e.copy(out=RHSb[:], in_=pR[:, 0:128]), reads=[pR], writes=[RHSb])
                    k.op("pe", lambda e: e.matmul(pU[:, 0:128], lhsT=BD["TT"][:, cc, :], rhs=RHSb[:], start=True, stop=True),
                         reads=[BD["TT"], RHSb], writes=[pU])
                    k.op("dve", lambda e: e.tensor_copy(out=Ub[:], in_=pU[:, 0:128]), reads=[pU], writes=[Ub])
                    k.op("pe", lambda e: e.matmul(pS[:, 0:128], lhsT=BD["Kh"][:, cc, :], rhs=BD["Vb"][:, cc, :], start=True, stop=False),
                         reads=[BD["Kh"], BD["Vb"]], writes=[pS], inc=False)
                    k.op("pe", lambda e: e.matmul(pS[:, 0:128], lhsT=BD["Bh"][:, cc, :], rhs=Ub[:], start=False, stop=True),
                         reads=[BD["Bh"], Ub], writes=[pS])
                    gC = Epos[:, cc * 64 + 63:cc * 64 + 64]
                    k.op("dve", lambda e: e.scalar_tensor_tensor(out=Sf[:], in0=Sf[:], scalar=gC, in1=pS[:, 0:128],
                                                                 op0=ALU.mult, op1=ALU.add), reads=[Sf, Epos, pS], writes=[Sf])
                    k.op("act", lambda e: e.copy(out=Sn[:], in_=Sf[:]), reads=[Sf], writes=[Sn])
                    k.op("pe", lambda e: e.matmul(pO[:, 0:128], lhsT=BD["Vb"][:, cc, :], rhs=BD["ArkT"][:, cc, :], start=True, stop=False),
                         reads=[BD["Vb"], BD["ArkT"]], writes=[pO], inc=False)
                    k.op("pe", lambda e: e.matmul(pO[:, 0:128], lhsT=So[:], rhs=BD["RT"][:, cc, :], start=False, stop=False),
                         reads=[So, BD["RT"]], writes=[pO], inc=False)
                    k.op("pe", lambda e: e.matmul(pO[:, 0:128], lhsT=Ub[:], rhs=BD["ArbT"][:, cc, :], start=False, stop=True),
                         reads=[Ub, BD["ArbT"]], writes=[pO])
                    for h in range(2):
                        hs = slice(h * 64, (h + 1) * 64)
                        k.op("pool" if False else "act", lambda e: e.copy(out=OT[hs, cc * 64:(cc + 1) * 64], in_=pO[hs, h * 64:(h + 1) * 64]),
                             reads=[pO], writes=[OT])
                    sbi = 1 - sbi
                y = yo[seg % 2]
                k.op("pool", lambda e: e.tensor_copy(out=ob[:], in_=OT[:]), reads=[OT], writes=[ob])
                for hb in range(SEG // TCH):
                    cs_ = slice(hb * TCH, (hb + 1) * TCH)
                    pm = c.next_pf(4, 6)
                    k.op("pe", lambda e: e.matmul(pm[:, :], lhsT=c.blk64[:], rhs=ob[:, cs_], start=True, stop=True),
                         reads=[c.blk64, ob], writes=[pm])
                    tt(t1[:, cs_], OT[:, cs_], pm[:, :], ALU.subtract, [OT, pm], [t1])
                act(sqb[:], t1[:], AF.Square, [t1], [sqb])
                for hb in range(SEG // TCH):
                    cs_ = slice(hb * TCH, (hb + 1) * TCH)
                    pm = c.next_pf(4, 6)
                    k.op("pe", lambda e: e.matmul(pm[:, :], lhsT=c.blk64[:], rhs=sqb[:, cs_], start=True, stop=True),
                         reads=[c.blk64, sqb], writes=[pm])
                    k.op("dve", lambda e: e.tensor_scalar(out=t2[:, cs_], in0=pm[:, :], scalar1=RW_EPS, scalar2=None, op0=ALU.add),
                         reads=[pm], writes=[t2])
                act(t2[:], t2[:], AF.Ln, [t2], [t2])
                act(t2[:], t2[:], AF.Exp, [t2], [t2], scale=-0.5)
                tt(t1[:], t1[:], t2[:], ALU.mult, [t1, t2], [t1])
                k.op("dve", lambda e: e.tensor_scalar(out=y[:], in0=t1[:], scalar1=P("rw_lng", hp), scalar2=P("rw_lnb", hp),
                                                      op0=ALU.mult, op1=ALU.add), reads=[t1, L.prm], writes=[y])
                k.op("dve", lambda e: e.scalar_tensor_tensor(out=sqb[:], in0=rm[:], scalar=P("rw_rk", hp), in1=k2[:],
                                                             op0=ALU.mult, op1=ALU.mult), reads=[rm, k2, L.prm], writes=[sqb])
                for hb in range(SEG // TCH):
                    cs_ = slice(hb * TCH, (hb + 1) * TCH)
                    pm = c.next_pf(4, 6)
                    k.op("pe", lambda e: e.matmul(pm[:, :], lhsT=c.blk1[:], rhs=sqb[:, cs_], start=True, stop=True),
                         reads=[c.blk1, sqb], writes=[pm])
                    tt(t2[:, cs_], pm[:, :], vm[:, cs_], ALU.mult, [pm, vm], [t2])
                tt(y[:], y[:], t2[:], ALU.add, [y, t2], [y], "dve")
                k.dma(STQ, c.mixT[512 + hp * 128:512 + (hp + 1) * 128, t0:t0 + SEG], y[:], reads=[y], writes=[c.mixT], waw=False)
        k.barrier()


def stage_outproj(k, c, L, xa, xb, y, xa_deps=None, cc=None):
    pT = c.pT
    with ExitStack() as st:
        wbf, wd = load_w_bf16(k, st, lambda kc: L.wout[kc * 128:(kc + 1) * 128, :], 8, D, "wout", nstg=2)
        mx = [k.sb(f"op_mx{i}", [128, 8, TCH], F32, st) for i in range(2)]
        gt = [k.sb(f"op_gt{i}", [128, 8, TCH], F32, st) for i in range(2)]
        sg = k.sb("op_sg", [128, 8, TCH], F32, st)
        mg = [k.sb(f"op_mg{i}", [128, 8, TCH], BF16, st) for i in range(2)]
        xt = [k.sb(f"op_xa{i}", [128, D], F32, st) for i in range(2)]
        xu = [k.sb(f"op_xb{i}", [128, D], F32, st) for i in range(2)]
        yo = [k.sb(f"op_y{i}", [128, D], F32, st) for i in range(2)]
        ydeps = [Dep() for _ in range(NTC)] if cc is not None else [y.dep] * NTC

        def emit_cc(i):
            xs_out, xs_deps, groups = cc
            rs = slice(i * TCH, (i + 1) * TCH)
            k.collective(y[rs, :], xs_out[rs, :], reads=[ydeps[i]], writes=[xs_deps[i]], groups=groups)
        for tc in range(NTC):
            b = tc % 2
            tsl = slice(tc * TCH, (tc + 1) * TCH)
            k.dma("sp", mx[b][:], c.mixT[:, tsl].rearrange("(kc p) t -> p kc t", p=128), reads=[c.mixT], writes=[mx[b]])
            k.dma("sp", gt[b][:], pT[R_GATE:R_GATE + 1024, tsl].rearrange("(kc p) t -> p kc t", p=128), reads=[pT],
                  writes=[gt[b]])
            k.op("act", lambda e: e.activation(out=sg[:], in_=gt[b][:], func=AF.Sigmoid), reads=[gt[b]], writes=[sg])
            k.op("pool", lambda e: e.tensor_tensor(out=gt[b][:], in0=gt[b][:], in1=mx[b][:], op=ALU.mult),
                 reads=[gt[b], mx[b]], writes=[gt[b]])
            k.op("dve", lambda e: e.tensor_tensor(out=mg[b][:], in0=gt[b][:], in1=sg[:], op=ALU.mult),
                 reads=[gt[b], sg], writes=[mg[b]])
            for sub in range(4):
                tt_ = tc * 4 + sub
                xb_i = tt_ % 2
                rows = slice(tt_ * 128, (tt_ + 1) * 128)
                k.dma("sp", xt[xb_i][:], xa[rows, :], reads=[xa_deps[tc] if xa_deps else xa], writes=[xt[xb_i]])
                if xb is not None:
                    k.dma("sp", xu[xb_i][:], xb[rows, :], reads=[xb], writes=[xu[xb_i]])
                    k.op("pool", lambda e: e.tensor_tensor(out=xt[xb_i][:], in0=xt[xb_i][:], in1=xu[xb_i][:], op=ALU.add),
                         reads=[xt[xb_i], xu[xb_i]], writes=[xt[xb_i]])
                yb = yo[xb_i]
                for n in range(4):
                    ps = c.next_pf()
                    for kc in range(8):
                        k.op("pe", lambda e: e.matmul(ps[:, :], lhsT=mg[b][:, kc, sub * 128:(sub + 1) * 128],
                                                      rhs=wbf[:, kc, n * 512:(n + 1) * 512], start=(kc == 0), stop=(kc == 7)),
                             reads=[mg[b], wd[kc]], writes=[ps], inc=(kc == 7))
                    k.op("dve", lambda e: e.scalar_tensor_tensor(out=yb[:, n * 512:(n + 1) * 512], in0=xt[xb_i][:, n * 512:(n + 1) * 512],
                                                                 scalar=0.5, in1=ps[:, :], op0=ALU.mult, op1=ALU.add),
                         reads=[xt[xb_i], ps], writes=[yb])
                k.dma(STQ, y[rows, :], yb[:], reads=[yb], writes=[ydeps[tc]], waw=False)
            if cc is not None and tc >= 1:
                emit_cc(tc - 1)
        if cc is not None:
            emit_cc(NTC - 1)
    k.barrier()


def stage_final(k, c, xa, xb, g_bc_d, out, xa_deps=None):
    with ExitStack() as st:
        gbc = k.sb("f_gbc", [128, D], F32, st)
        k.dma("sp", gbc[:], g_bc_d.partition_broadcast(128), writes=[gbc])
        xt = [k.sb(f"f_xt{i}", [128, D], F32, st) for i in range(2)]
        xu = [k.sb(f"f_xu{i}", [128, D], F32, st) for i in range(2)]
        junk = k.sb("f_junk", [128, D], BF16, st)
        yo = [k.sb(f"f_y{i}", [128, D], F32, st) for i in range(2)]
        ss = [k.sb(f"f_ss{i}", [128, 4], F32, st) for i in range(2)]
        for tt_ in range(S // 128):
            b = tt_ % 2
            rows = slice(tt_ * 128, (tt_ + 1) * 128)
            k.dma("sp", xt[b][:], xa[rows, :], reads=[xa_deps[tt_ // 4] if xa_deps else xa], writes=[xt[b]])
            if xb is not None:
                k.dma("sp", xu[b][:], xb[rows, :], reads=[xb], writes=[xu[b]])
                k.op("pool", lambda e: e.tensor_tensor(out=xt[b][:], in0=xt[b][:], in1=xu[b][:], op=ALU.add),
                     reads=[xt[b], xu[b]], writes=[xt[b]])
            k.op("act", lambda e: e.activation(out=junk[:], in_=xt[b][:], func=AF.Square, accum_out=ss[b][:, 0:1]),
                 reads=[xt[b]], writes=[junk, ss[b]])
            k.op("dve", lambda e: e.tensor_scalar(out=ss[b][:, 1:2], in0=ss[b][:, 0:1], scalar1=1.0 / D, scalar2=EPS,
                                                  op0=ALU.mult, op1=ALU.add), reads=[ss[b]], writes=[ss[b]])
            k.op("act", lambda e: e.activation(out=ss[b][:, 2:3], in_=ss[b][:, 1:2], func=AF.Sqrt), reads=[ss[b]], writes=[ss[b]])
            k.op("dve", lambda e: e.reciprocal(out=ss[b][:, 3:4], in_=ss[b][:, 2:3]), reads=[ss[b]], writes=[ss[b]])
            k.op("dve", lambda e: e.scalar_tensor_tensor(out=yo[b][:], in0=xt[b][:], scalar=ss[b][:, 3:4], in1=gbc[:],
                                                         op0=ALU.mult, op1=ALU.mult), reads=[xt[b], ss[b], gbc], writes=[yo[b]])
            k.dma(STQ, out[rows, :], yo[b][:], reads=[yo[b]], writes=[out], waw=False)
    k.barrier()


class LayerIO:
    pass


def declare_layer_inputs(k, l):
    EI = "ExternalInput"
    L = LayerIO()
    L.col = PRM_COLS
    L.norm_g = k.dram(f"norm_g{l}", [1, D], F32, kind=EI)
    L.w_in = k.dram(f"w_in{l}", [D, NF + NV], F32, kind=EI)
    L.wuq = k.dram(f"wuq{l}", [512, 384], F32, kind=EI)
    L.wukv = k.dram(f"wukv{l}", [256, 512], F32, kind=EI)
    L.prm_d = k.dram(f"prm{l}", [128, _pc], F32, kind=EI)
    L.s5b = k.dram(f"s5b{l}", [128, 512], F32, kind=EI)
    L.s5c = k.dram(f"s5c{l}", [128, 512], F32, kind=EI)
    L.wglu = k.dram(f"wglu{l}", [512, 256], F32, kind=EI)
    L.w2a2 = k.dram(f"w2a2{l}", [128, 256], F32, kind=EI)
    L.wout = k.dram(f"wout{l}", [1024, D], F32, kind=EI)
    return L


def layer_input_arrays(inp, l, half, suffix):
    a = core_layer_arrays(inp, l, half)
    out = {
        f"norm_g{suffix}": np.ascontiguousarray(inp["norm_g"][l][None, :]),
        f"w_in{suffix}": a["w_in"], f"wuq{suffix}": a["wuq"], f"wukv{suffix}": a["wukv"], f"prm{suffix}": a["prm"],
        f"s5b{suffix}": a["s5b"], f"s5c{suffix}": a["s5c"], f"wglu{suffix}": a["wglu"], f"w2a2{suffix}": a["w2a2"],
    }
    rows = np.concatenate([b * 512 + half * 256 + np.arange(256) for b in range(4)])
    out[f"wout{suffix}"] = np.ascontiguousarray(inp["w_out"][l][rows, :])
    return out


def declare_scratch(k, c):
    c.hT = k.dram("sc_hT", [D, S], BF16)
    c.pT = k.dram("sc_pT", [NF, S], F32)
    c.pV = k.dram("sc_pV", [S, NV], F32)
    c.us5 = k.dram("sc_us5", [32, 128, 512], BF16)
    c.ys5 = k.dram("sc_ys5", [32, 128, 512], F32)
    c.gT = k.dram("sc_gT", [512, S], BF16)
    c.gF = k.dram("sc_gF", [256, S], F32)
    c.mixT = k.dram("sc_mixT", [1024, S], F32)
    c.cqnT = k.dram("sc_cqnT", [512, S], BF16)
    c.ckvnT = k.dram("sc_ckvnT", [256, S], BF16)
    c.qT = k.dram("sc_qT", [384, S], BF16)
    c.knT = k.dram("sc_knT", [256, S], BF16)
    c.vA = k.dram("sc_vA", [S, 256], BF16)
    c.krT = k.dram("sc_krT", [128, S], BF16)
    c.qdT = k.dram("sc_qdT", [256, S], BF16)
    c.kdT = k.dram("sc_kdT", [256, S], BF16)
    c.ropeA_cos = k.dram("sc_rAc", [128, S], F32)
    c.ropeA_sin = k.dram("sc_rAs", [128, S], F32)
    c.ropeD_cos = k.dram("sc_rDc", [128, S], F32)
    c.ropeD_sin = k.dram("sc_rDs", [128, S], F32)


def emit_layer(k, c, L, xa, xb, y, xa_deps=None, cc=None):
    with ExitStack() as st:
        L.prm = k.sb("prm_sb", [128, _pc], F32, st)
        k.dma("sp", L.prm[:], L.prm_d[:, :], writes=[L.prm])
        import os as _os
        sel = _os.environ.get("LAYER_STAGES", "nimsrdo")
        if "n" in sel:
            stage_norm(k, c, xa, xb, L.norm_g[0:1, :], c.hT, xa_deps)
            k.barrier()
        if "i" in sel:
            stage_inproj(k, c, c.hT, L.w_in, NF, NV, c.pT, c.pV)
        if "m" in sel:
            stage_mla(k, c, L)
        if "s" in sel:
            stage_s5(k, c, L)
        if "r" in sel:
            stage_rwkv(k, c, L)
        if "d" in sel:
            stage_diff(k, c, L)
        if "o" in sel:
            stage_outproj(k, c, L, xa, xb, y, xa_deps, cc)
        k.barrier()


def build_layer_program():
    nc = bass.Bass("TRN2", target_bir_lowering=False)
    k = KB(nc)
    c = Ctx(k)
    EI = "ExternalInput"
    cmat = k.dram("cmat", [128, 768], F32, kind=EI)
    cmask = k.dram("cmask", [128, 2048], F32, kind=EI)
    cst = k.dram("cst", [128, NCST], F32, kind=EI)
    cm2 = k.dram("cm2", [128, 2560], F32, kind=EI)
    pos = k.dram("pos", [1, S], I32, kind=EI)
    xa = k.dram("xa", [S, D], F32, kind=EI)
    xb = k.dram("xb", [S, D], F32, kind=EI)
    y = k.dram("y", [S, D], F32, kind="ExternalOutput")
    L = declare_layer_inputs(k, "")
    declare_scratch(k, c)
    load_all_consts(k, c, cmat, cmask, cst, cm2)
    make_rope_tables(k, c, pos, c.cst, 0, 1, c.ropeA_cos, c.ropeA_sin, "rtA")
    make_rope_tables(k, c, pos, c.cst, 2, 3, c.ropeD_cos, c.ropeD_sin, "rtD")
    emit_layer(k, c, L, xa, xb, y)
    k.finish()
    return nc


def build_final_program():
    nc = bass.Bass("TRN2", target_bir_lowering=False)
    k = KB(nc)
    c = Ctx(k)
    xa = k.dram("xa", [S, D], F32, kind="ExternalInput")
    xb = k.dram("xb", [S, D], F32, kind="ExternalInput")
    g = k.dram("fg", [1, D], F32, kind="ExternalInput")
    out = k.dram("out", [S, D], F32, kind="ExternalOutput")
    stage_final(k, c, xa, xb, g[0:1, :], out)
    k.finish()
    return nc


def const_inputs():
    cmat, cmask, cst = const_mats()
    return {"cmat": cmat, "cmask": cmask, "cst": cst, "cm2": const_mats2()}


def kernel_unfused(**inp):
    inp = {k_: np.asarray(v) for k_, v in inp.items()}
    x = inp["x"]
    B = x.shape[0]
    consts = const_inputs()
    ncl = build_layer_program()
    cur_a = [np.ascontiguousarray(x[cid // 2]) for cid in range(8)]
    cur_b = [np.zeros((S, D), np.float32) for _ in range(8)]
    for l in range(4):
        in_maps = []
        for cid in range(8):
            b, half = divmod(cid, 2)
            m = dict(consts)
            m["pos"] = np.ascontiguousarray(inp["positions"][b:b + 1].astype(np.int32))
            m["xa"] = cur_a[cid]
            m["xb"] = cur_b[cid]
            m.update(layer_input_arrays(inp, l, half, ""))
            in_maps.append(m)
        res = run_bass_kernel_spmd(ncl, in_maps, core_ids=list(range(8)))
        ys = [np.asarray(r["y"]) for r in res.results]
        cur_a = [ys[cid] for cid in range(8)]
        cur_b = [ys[cid ^ 1] for cid in range(8)]
    ncf = build_final_program()
    fg = np.ascontiguousarray(inp["final_norm_g"][None, :])
    in_maps = [{"xa": cur_a[cid], "xb": cur_b[cid], "fg": fg} for cid in range(8)]
    res = run_bass_kernel_spmd(ncf, in_maps, core_ids=list(range(8)))
    out = np.stack([np.asarray(res.results[2 * b]["out"]) for b in range(B)], axis=0)
    return out.astype(np.float32)


from concourse.bass_utils import run_bass_kernel_spmd


PAIR_GROUPS = [[0, 1], [2, 3], [4, 5], [6, 7]]
DEPTH = 4


def build_fused_program(depth=DEPTH):
    nc = bass.Bass("TRN2", target_bir_lowering=False)
    k = KB(nc)
    c = Ctx(k)
    EI = "ExternalInput"
    cmat = k.dram("cmat", [128, 768], F32, kind=EI)
    cmask = k.dram("cmask", [128, 2048], F32, kind=EI)
    cst = k.dram("cst", [128, NCST], F32, kind=EI)
    cm2 = k.dram("cm2", [128, 2560], F32, kind=EI)
    pos = k.dram("pos", [1, S], I32, kind=EI)
    x_in = k.dram("xa", [S, D], F32, kind=EI)
    fg = k.dram("fg", [1, D], F32, kind=EI)
    out = k.dram("out", [S, D], F32, kind="ExternalOutput")
    Ls = [declare_layer_inputs(k, l) for l in range(depth)]
    declare_scratch(k, c)
    ybuf = k.dram("sc_y", [S, D], F32)
    xs = [k.dram(f"sc_xs{i}", [S, D], F32) for i in range(2)]
    xs_deps = [[Dep() for _ in range(NTC)] for _ in range(2)]
    load_all_consts(k, c, cmat, cmask, cst, cm2)
    make_rope_tables(k, c, pos, c.cst, 0, 1, c.ropeA_cos, c.ropeA_sin, "rtA")
    make_rope_tables(k, c, pos, c.cst, 2, 3, c.ropeD_cos, c.ropeD_sin, "rtD")
    cur, cur_deps = x_in, None
    for l in range(depth):
        o = l % 2
        emit_layer(k, c, Ls[l], cur, None, ybuf, cur_deps, (xs[o], xs_deps[o], PAIR_GROUPS))
        cur, cur_deps = xs[o], xs_deps[o]
    stage_final(k, c, cur, None, fg[0:1, :], out, cur_deps)
    k.finish()
    return nc


def kernel_fused(**inp):
    inp = {k_: np.asarray(v) for k_, v in inp.items()}
    x = inp["x"]
    B = x.shape[0]
    consts = const_inputs()
    nc = build_fused_program()
    fg = np.ascontiguousarray(inp["final_norm_g"][None, :])
    in_maps = []
    for cid in range(8):
        b, half = divmod(cid, 2)
        m = dict(consts)
        m["pos"] = np.ascontiguousarray(inp["positions"][b:b + 1].astype(np.int32))
        m["xa"] = np.ascontiguousarray(x[b])
        m["fg"] = fg
        for l in range(DEPTH):
            m.update(layer_input_arrays(inp, l, half, str(l)))
        in_maps.append(m)
    res = run_bass_kernel_spmd(nc, in_maps, core_ids=list(range(8)))
    out = np.stack([np.asarray(res.results[2 * b]["out"]) for b in range(B)], axis=0)
    return out.astype(np.float32)


def kernel(**inputs):
    return kernel_fused(**inputs)
```
